# Optimizing a Trainium2 kernel written in Bass

```python
import jax
import jax.numpy as jnp
from jax import lax
import numpy as np

D_MODEL = 1024
BATCH = 8
SEQ = 2048
DEPTH = 4

GRID_W = 64
CTX_LEN = 256
CONV_WIDTH = 512
CONV_TAPS = 3
HG_WIDTH = 512
HG_HEADS = 4
HG_HEAD_DIM = HG_WIDTH // HG_HEADS
MIX_WIDTH = CONV_WIDTH + HG_WIDTH
PROJ_WIDTH = 3 * CONV_WIDTH + 5 * HG_WIDTH
PROJ_SPLITS = (CONV_WIDTH, 2 * CONV_WIDTH, 3 * CONV_WIDTH, 3 * CONV_WIDTH + HG_WIDTH, 3 * CONV_WIDTH + 2 * HG_WIDTH, 3 * CONV_WIDTH + 3 * HG_WIDTH, 3 * CONV_WIDTH + 4 * HG_WIDTH)
CTX_STATE_LO = 3 * CONV_WIDTH + HG_WIDTH
CTX_STATE_HI = 3 * CONV_WIDTH + 4 * HG_WIDTH
CHUNK = 16
N_GROUPS = 4
EXPERTS_PER_GROUP = 8
N_EXPERTS = N_GROUPS * EXPERTS_PER_GROUP
TOP_K = 2
D_EXPERT = 512
ROUTE_BLOCK = 128
N_MOD = 6
NORM_EPS = 1e-6

kernel_name = 'hybrid_conv_hgrn2_hmoe_dit'


def rms_norm(x, w):
    xf = x.astype(jnp.float32)
    y = xf * lax.rsqrt(jnp.mean(xf * xf, axis=-1, keepdims=True) + NORM_EPS)
    return (y * w.astype(jnp.float32)).astype(x.dtype)


def modulate(x, shift, scale):
    return x * (1 + scale) + shift


def heads(a):
    return a.reshape(a.shape[0], a.shape[1], HG_HEADS, HG_HEAD_DIM)


def conv3_seq(u, w):
    up = jnp.pad(u, ((0, 0), (1, 1), (0, 0)))
    return w[0] * up[:, :-2] + w[1] * up[:, 1:-1] + w[2] * up[:, 2:]


def conv3_grid(u, w, rows):
    bsz, t_len, ch = u.shape
    half = ch // 2
    g = u.reshape(bsz, rows, GRID_W, ch)
    gh = jnp.pad(g[..., :half], ((0, 0), (0, 0), (1, 1), (0, 0)))
    wh = w[:, :half]
    yh = wh[0] * gh[:, :, :-2] + wh[1] * gh[:, :, 1:-1] + wh[2] * gh[:, :, 2:]
    gv = jnp.pad(g[..., half:], ((0, 0), (1, 1), (0, 0), (0, 0)))
    wv = w[:, half:]
    yv = wv[0] * gv[:, :-2] + wv[1] * gv[:, 1:-1] + wv[2] * gv[:, 2:]
    return jnp.concatenate([yh, yv], axis=-1).reshape(bsz, t_len, ch)


def hgrn2_lower_bounds(lb_raw):
    p = jax.nn.softmax(lb_raw.astype(jnp.float32), axis=1)
    cs = jnp.cumsum(p, axis=1)
    return cs - cs[:, :1]


def hgrn2_forget(zf, lb):
    zf = zf.astype(jnp.float32)
    logf = jnp.logaddexp(jnp.log(lb), jnp.log1p(-lb) + jax.nn.log_sigmoid(zf))
    return heads(logf), heads(-jnp.expm1(logf))


def gla_chunked(q, k, v, logf, s0):
    bsz, t_len, n_h, d_k = q.shape
    d_v = v.shape[-1]
    n_c = t_len // CHUNK
    q, k, logf = [a.astype(jnp.float32).reshape(bsz, n_c, CHUNK, n_h, d_k) for a in (q, k, logf)]
    v = v.astype(jnp.float32).reshape(bsz, n_c, CHUNK, n_h, d_v)
    b = jnp.cumsum(logf, axis=2)
    order = jnp.tril(jnp.ones((CHUNK, CHUNK), dtype=bool))[None, None, :, :, None, None]
    diff = b[:, :, :, None] - b[:, :, None, :]
    decay = jnp.exp(jnp.where(order, diff, -jnp.inf))
    scores = jnp.einsum('bnthk,bnshk,bntshk->bnhts', q, k, decay)
    o_intra = jnp.einsum('bnhts,bnshv->bnthv', scores, v)
    b_last = b[:, :, -1:]
    q_in = q * jnp.exp(b)
    k_out = k * jnp.exp(b_last - b)
    d_chunk = jnp.exp(b_last[:, :, 0])

    def step(state, inp):
        q_c, k_c, v_c, d_c = inp
        o_c = jnp.einsum('bthk,bhkv->bthv', q_c, state)
        state = d_c[..., None] * state + jnp.einsum('bthk,bthv->bhkv', k_c, v_c)
        return state, o_c

    xs = tuple(jnp.moveaxis(a, 1, 0) for a in (q_in, k_out, v, d_chunk))
    s_fin, o_inter = lax.scan(step, s0.astype(jnp.float32), xs)
    o = o_intra + jnp.moveaxis(o_inter, 0, 1)
    return o.reshape(bsz, t_len, n_h, d_v), s_fin


def gla_final_state(k, logf, v):
    b = jnp.cumsum(logf, axis=1)
    return jnp.einsum('bthk,bthv->bhkv', k * jnp.exp(b[:, -1:] - b), v.astype(jnp.float32))


def hgrn2_mixer(q_raw, zf_f, zf_b, i_raw, g_raw, lb_f, lb_b, norm_w, s0_f, s0_b):
    q = heads(jax.nn.silu(q_raw.astype(jnp.float32)))
    v = heads(i_raw.astype(jnp.float32))
    logf_f, k_f = hgrn2_forget(zf_f, lb_f)
    logf_b, k_b = hgrn2_forget(zf_b, lb_b)
    o_f, s_f = gla_chunked(q, k_f, v, logf_f, s0_f)
    rev = lambda a: jnp.flip(a, axis=1)
    o_b, s_b = gla_chunked(rev(q), rev(k_b), rev(v), rev(logf_b), s0_b)
    o = rms_norm(o_f + rev(o_b), norm_w) * jax.nn.silu(heads(g_raw.astype(jnp.float32)))
    return o.reshape(q_raw.shape[0], q_raw.shape[1], HG_WIDTH).astype(q_raw.dtype), s_f, s_b


def hgrn2_context_states(zf_f, zf_b, i_raw, lb_f, lb_b):
    v = heads(i_raw.astype(jnp.float32))
    logf_f, k_f = hgrn2_forget(zf_f, lb_f)
    logf_b, k_b = hgrn2_forget(zf_b, lb_b)
    rev = lambda a: jnp.flip(a, axis=1)
    return gla_final_state(k_f, logf_f, v), gla_final_state(rev(k_b), rev(logf_b), rev(v))


def token_mixer(h, w_in_l, conv_w_l, conv_norm_w_l, lb_f, lb_b, hg_norm_w_l, s0_f, s0_b, conv_fn):
    p = h @ w_in_l
    bg, cg, hv, q, zf_f, zf_b, iv, g = jnp.split(p, PROJ_SPLITS, axis=-1)
    y_conv = rms_norm(bg * conv_fn(cg * hv, conv_w_l), conv_norm_w_l)
    y_rec, s_f, s_b = hgrn2_mixer(q, zf_f, zf_b, iv, g, lb_f, lb_b, hg_norm_w_l, s0_f, s0_b)
    return jnp.concatenate([y_conv, y_rec], axis=-1), s_f, s_b


def hier_moe(t, w_rg, b_rg, w_re, b_re, w_gu, w_down):
    n_tok, d = t.shape
    tf = t.astype(jnp.float32)
    g_logits = tf @ w_rg.astype(jnp.float32) + b_rg.astype(jnp.float32)
    g_sel = jnp.argmax(g_logits, axis=-1)
    p_group = jnp.take_along_axis(jax.nn.softmax(g_logits, axis=-1), g_sel[:, None], axis=-1)
    e_logits = (tf @ w_re.astype(jnp.float32) + b_re.astype(jnp.float32)).reshape(n_tok, N_GROUPS, EXPERTS_PER_GROUP)
    e_logits = jnp.take_along_axis(e_logits, g_sel[:, None, None], axis=1)[:, 0]
    top_p, top_i = lax.top_k(jax.nn.softmax(e_logits, axis=-1), TOP_K)
    gate = p_group * top_p / jnp.sum(top_p, axis=-1, keepdims=True)
    expert_id = g_sel[:, None].astype(jnp.int32) * EXPERTS_PER_GROUP + top_i.astype(jnp.int32)
    n_assign = n_tok * TOP_K
    flat_e = expert_id.reshape(-1)
    flat_tok = jnp.repeat(jnp.arange(n_tok, dtype=jnp.int32), TOP_K)
    flat_gate = gate.reshape(-1)
    order = jnp.argsort(flat_e)
    sorted_e = flat_e[order]
    counts = jnp.bincount(flat_e, length=N_EXPERTS)
    starts = jnp.cumsum(counts) - counts
    padded = (counts + ROUTE_BLOCK - 1) // ROUTE_BLOCK * ROUTE_BLOCK
    pad_ends = jnp.cumsum(padded)
    pad_starts = pad_ends - padded
    dest = pad_starts[sorted_e] + jnp.arange(n_assign, dtype=jnp.int32) - starts[sorted_e]
    n_blocks = -(-(n_assign + N_EXPERTS * (ROUTE_BLOCK - 1)) // ROUTE_BLOCK)
    n_rows = n_blocks * ROUTE_BLOCK
    row_tok = jnp.full((n_rows,), n_tok, dtype=jnp.int32).at[dest].set(flat_tok[order])
    row_gate = jnp.zeros((n_rows,), jnp.float32).at[dest].set(flat_gate[order])
    block_e = jnp.minimum(jnp.searchsorted(pad_ends, jnp.arange(n_blocks) * ROUTE_BLOCK, side='right'), N_EXPERTS - 1)
    t_pad = jnp.concatenate([t, jnp.zeros((1, d), t.dtype)], axis=0)
    xb = t_pad[row_tok].reshape(n_blocks, ROUTE_BLOCK, d)

    def expert_block(args):
        x_blk, e = args
        a, u = jnp.split(x_blk @ w_gu[e], 2, axis=-1)
        return (jax.nn.silu(a) * u) @ w_down[e]

    yb = lax.map(expert_block, (xb, block_e)).reshape(n_rows, d)
    out = jnp.zeros((n_tok + 1, d), t.dtype).at[row_tok].add((yb * row_gate[:, None]).astype(t.dtype))
    return out[:n_tok]


def setup_inputs(seed: int = 0) -> dict:
    key = jax.random.key(seed)
    ks = jax.random.split(key, 20)
    f32 = jnp.float32
    nrm = lambda k, shape, s: jax.random.normal(k, shape, f32) * s
    d = D_MODEL
    return {
        'x': nrm(ks[0], (BATCH, SEQ, d), 1.0),
        'c': nrm(ks[1], (BATCH, d), 1.0),
        'ctx': nrm(ks[2], (BATCH, CTX_LEN, d), 1.0),
        'c_ctx': nrm(ks[3], (d,), 1.0),
        'norm_w': 1.0 + nrm(ks[4], (DEPTH, 2, d), 0.02),
        'w_ada': nrm(ks[5], (DEPTH, d, N_MOD * d), 0.5 * d ** -0.5),
        'b_ada': nrm(ks[6], (DEPTH, N_MOD * d), 0.02),
        'w_in': nrm(ks[7], (DEPTH, d, PROJ_WIDTH), d ** -0.5),
        'conv_w': nrm(ks[8], (DEPTH, CONV_TAPS, CONV_WIDTH), CONV_TAPS ** -0.5),
        'conv_norm_w': 1.0 + nrm(ks[9], (DEPTH, CONV_WIDTH), 0.02),
        'hg_lb': nrm(ks[10], (2, DEPTH, HG_WIDTH), 1.0),
        'hg_norm_w': 1.0 + nrm(ks[11], (DEPTH, HG_HEAD_DIM), 0.02),
        'w_out': nrm(ks[12], (DEPTH, MIX_WIDTH, d), MIX_WIDTH ** -0.5),
        'w_rg': nrm(ks[13], (DEPTH, d, N_GROUPS), d ** -0.5),
        'b_rg': nrm(ks[14], (DEPTH, N_GROUPS), 0.01),
        'w_re': nrm(ks[15], (DEPTH, d, N_EXPERTS), d ** -0.5),
        'b_re': nrm(ks[16], (DEPTH, N_EXPERTS), 0.01),
        'w_e_gu': nrm(ks[17], (DEPTH, N_EXPERTS, d, 2 * D_EXPERT), d ** -0.5),
        'w_e_down': nrm(ks[18], (DEPTH, N_EXPERTS, D_EXPERT, d), D_EXPERT ** -0.5),
        'final_norm_w': 1.0 + nrm(ks[19], (d,), 0.02),
    }


def reference(x, c, ctx, c_ctx, norm_w, w_ada, b_ada, w_in, conv_w, conv_norm_w, hg_lb, hg_norm_w, w_out, w_rg, b_rg, w_re, b_re, w_e_gu, w_e_down, final_norm_w):
    bsz, t_len, d = x.shape
    rows = t_len // GRID_W
    lb = hgrn2_lower_bounds(hg_lb)
    sc = jax.nn.silu(c)
    scc = jax.nn.silu(c_ctx)
    latent_conv = lambda u, w: conv3_grid(u, w, rows)
    zero_state = jnp.zeros((bsz, HG_HEADS, HG_HEAD_DIM, HG_HEAD_DIM), jnp.float32)
    for l in range(DEPTH):
        last = l == DEPTH - 1
        mod = (sc @ w_ada[l] + b_ada[l]).reshape(bsz, N_MOD, 1, d)
        modc = (scc @ w_ada[l] + b_ada[l]).reshape(N_MOD, d)
        h = modulate(rms_norm(x, norm_w[l, 0]), mod[:, 0], mod[:, 1])
        hc = modulate(rms_norm(ctx, norm_w[l, 0]), modc[0], modc[1])
        if last:
            zf_f, zf_b, iv = jnp.split(hc @ w_in[l][:, CTX_STATE_LO:CTX_STATE_HI], 3, axis=-1)
            s_f, s_b = hgrn2_context_states(zf_f, zf_b, iv, lb[0, l], lb[1, l])
        else:
            yc, s_f, s_b = token_mixer(hc, w_in[l], conv_w[l], conv_norm_w[l], lb[0, l], lb[1, l], hg_norm_w[l], zero_state, zero_state, conv3_seq)
            ctx = ctx + modc[2] * (yc @ w_out[l])
        y, _, _ = token_mixer(h, w_in[l], conv_w[l], conv_norm_w[l], lb[0, l], lb[1, l], hg_norm_w[l], s_f, s_b, latent_conv)
        x = x + mod[:, 2] * (y @ w_out[l])
        h2 = modulate(rms_norm(x, norm_w[l, 1]), mod[:, 3], mod[:, 4])
        moe_w = (w_rg[l], b_rg[l], w_re[l], b_re[l], w_e_gu[l], w_e_down[l])
        if last:
            f = hier_moe(h2.reshape(bsz * t_len, d), *moe_w).reshape(bsz, t_len, d)
        else:
            hc2 = modulate(rms_norm(ctx, norm_w[l, 1]), modc[3], modc[4])
            n_lat = bsz * t_len
            f_all = hier_moe(jnp.concatenate([h2.reshape(n_lat, d), hc2.reshape(-1, d)], axis=0), *moe_w)
            f = f_all[:n_lat].reshape(bsz, t_len, d)
            ctx = ctx + modc[5] * f_all[n_lat:].reshape(ctx.shape)
        x = x + mod[:, 5] * f
    return rms_norm(x, final_norm_w)
```

```python
import contextlib
import numpy as np
import concourse.bass as bass
import concourse.mybir as mybir
from concourse.bass_utils import run_bass_kernel_spmd

F32 = mybir.dt.float32
BF16 = mybir.dt.bfloat16
AF = mybir.ActivationFunctionType
ALU = mybir.AluOpType
AX = mybir.AxisListType

ENGS = ['pe', 'act', 'dve', 'pool', 'sp']
DMA_RING = 8


class Op:
    __slots__ = ('eng', 'fn', 'deps', 'idx', 'signal', 'semkey', 'semval', 'dma', 'waits')

    def __init__(self, eng, fn, dma):
        self.eng = eng
        self.fn = fn
        self.dma = dma
        self.deps = []
        self.signal = False
        self.semkey = None
        self.semval = 0
        self.waits = []


class Sched:
    def __init__(self):
        self.ops = {e: [] for e in ENGS}
        self.bufs = {}
        self.ndma = {e: 0 for e in ENGS}
        self.dma_ops = {e: [] for e in ENGS}

    @staticmethod
    def _key(x):
        if isinstance(x, (tuple, str)):
            return x
        return x.name

    def op(self, eng, fn, r=(), w=(), dma=False, extra_deps=()):
        o = Op(eng, fn, dma)
        deps = list(extra_deps)
        rk = [self._key(x) for x in r]
        wk = [self._key(x) for x in w]
        for k in rk:
            st = self.bufs.get(k)
            if st is not None and st[0] is not None:
                deps.append(st[0])
        for k in wk:
            st = self.bufs.get(k)
            if st is not None:
                if st[0] is not None:
                    deps.append(st[0])
                deps.extend(st[1].values())
        if dma:
            i = self.ndma[eng]
            self.ndma[eng] += 1
            o.semkey = ('dma', eng, i % DMA_RING)
            o.semval = 16 * (i // DMA_RING + 1)
            if i >= DMA_RING:
                deps.append(self.dma_ops[eng][i - DMA_RING])
            self.dma_ops[eng].append(o)
        seen = set()
        for d in deps:
            if id(d) in seen or d is o:
                continue
            seen.add(id(d))
            o.deps.append(d)
        o.idx = len(self.ops[eng])
        self.ops[eng].append(o)
        for k in rk:
            st = self.bufs.setdefault(k, [None, {}])
            st[1][id(o) if dma else eng] = o
        for k in wk:
            self.bufs[k] = [o, {}]
        return o

    def barrier(self):
        last = []
        for e in ENGS:
            if self.ops[e]:
                last.append(self.ops[e][-1])
            last.extend(self.dma_ops[e][-DMA_RING:])
        for e in ENGS:
            self.op(e, lambda g: g.nop(), extra_deps=last)
        self.bufs = {}

    def finalize(self):
        for e in ENGS:
            for o in self.ops[e]:
                for d in o.deps:
                    if d.dma:
                        continue
                    if d.eng == 'pe' and o.eng == 'pe' and not o.dma:
                        continue
                    d.signal = True
        for e in ENGS:
            c = 0
            for o in self.ops[e]:
                if o.dma:
                    continue
                if o.signal:
                    c += 1
                    o.semkey = ('eng', e)
                    o.semval = c
        for e in ENGS:
            waited = {}
            for o in self.ops[e]:
                need = {}
                for d in o.deps:
                    if (not d.dma) and d.eng == 'pe' and o.eng == 'pe' and not o.dma:
                        continue
                    if d.semval > need.get(d.semkey, 0):
                        need[d.semkey] = d.semval
                for k, v in need.items():
                    if waited.get(k, 0) < v:
                        waited[k] = v
                        o.waits.append((k, v))

    def run(self, nc):
        self.finalize()
        keys = set()
        for e in ENGS:
            for o in self.ops[e]:
                if o.semkey is not None and (o.signal or o.dma):
                    keys.add(o.semkey)
        keys = sorted(keys, key=str)
        self.stats = {e: (len(self.ops[e]), max([o.semval for o in self.ops[e] if not o.dma] + [0])) for e in ENGS}
        with contextlib.ExitStack() as st:
            sems = {}
            for i, k in enumerate(keys):
                sems[k] = st.enter_context(nc.semaphore('s%d' % i))
            block = st.enter_context(nc.Block())

            def replay(eng_name):
                def body(e):
                    for o in self.ops[eng_name]:
                        for (k, v) in o.waits:
                            e.wait_ge(sems[k], v)
                        inst = o.fn(e)
                        if o.dma:
                            inst.then_inc(sems[o.semkey], 16)
                        elif o.signal:
                            inst.then_inc(sems[o.semkey], 1)
                return body

            block.tensor(replay('pe'))
            block.scalar(replay('act'))
            block.vector(replay('dve'))
            block.gpsimd(replay('pool'))
            block.sync(replay('sp'))


class Cfg:
    def __init__(self, depth=4, t_lat=2048, layers=None, first=True, final=True):
        self.D = 1024
        self.DC = 8
        self.TC = 256
        self.TL = t_lat
        self.T = self.TC + self.TL
        self.DEPTH = depth
        self.layers = list(range(depth)) if layers is None else layers
        self.first = first
        self.final = final
        self.NE = 32
        self.skip = set()
        self.EPS = 1e-6
        self.groups = [(0, 256, 1, 0)]
        for k in range(self.TL // 512):
            self.groups.append((256 + 512 * k, 512, 0, k + 1))
        self.NT = self.T // 128
        self.NB = -(-(2 * self.T + 32 * 127) // 128)
        d = depth
        self.V_C = 0
        self.V_CC = 8
        self.V_FNW = 16
        self.V_LB = 24
        self.V_L0 = 24 + 2 * d * 4
        self.V_LN = 81
        self.NV = self.V_L0 + d * self.V_LN


def build_program(cfg):
    nc = bass.Bass("TRN2", target_bir_lowering=False)
    T, TC, TL, DC, DEPTH = cfg.T, cfg.TC, cfg.TL, cfg.DC, cfg.DEPTH
    xT_d = nc.dram_tensor("xT", [128, DC * T], F32, kind="ExternalInput").ap()
    vecs_d = nc.dram_tensor("vecs", [128, cfg.NV], F32, kind="ExternalInput").ap()
    wr_d = nc.dram_tensor("wr", [128, DEPTH * DC * 36], F32, kind="ExternalInput").ap()
    br_d = nc.dram_tensor("br", [1, DEPTH * 36], F32, kind="ExternalInput").ap()
    wada_d = nc.dram_tensor("w_ada", [DEPTH, 1024, 6144], F32, kind="ExternalInput").ap()
    win_d = nc.dram_tensor("w_in", [DEPTH, 1024, 4096], F32, kind="ExternalInput").ap()
    wout_d = nc.dram_tensor("w_out", [DEPTH, 1024, 1024], F32, kind="ExternalInput").ap()
    wgu_d = nc.dram_tensor("w_gu", [DEPTH, cfg.NE, 1024, 1024], F32, kind="ExternalInput").ap()
    wdn_d = nc.dram_tensor("w_dn", [DEPTH, cfg.NE, 512, 1024], F32, kind="ExternalInput").ap()
    outT_d = nc.dram_tensor("outT", [128, DC * TL], F32, kind="ExternalOutput").ap()
    xs_d = nc.dram_tensor("xs_scr", [cfg.NB * 128, 1024], F32, kind="Internal").ap()
    ys_d = nc.dram_tensor("ys_scr", [cfg.NB * 128, 1024], F32, kind="Internal").ap()
    xT_v = xT_d.rearrange("p (c t) -> p c t", c=DC)
    outT_v = outT_d.rearrange("p (c t) -> p c t", c=DC)

    S = Sched()
    uid = [0]

    with contextlib.ExitStack() as top:
        def sb(stack, name, shape, dt=F32):
            uid[0] += 1
            return stack.enter_context(nc.sbuf_tensor("%s_%d" % (name, uid[0]), shape, dt))

        XT = sb(top, "XT", [128, DC, T])
        hT = sb(top, "hT", [128, DC, T], BF16)
        G = sb(top, "G", [128, cfg.NT, 32])
        vecs = sb(top, "vecs", [128, cfg.NV])
        wr = sb(top, "wr", [128, DEPTH, DC, 36])
        br = sb(top, "br", [1, DEPTH * 36])
        ident = sb(top, "ident", [128, 128])
        ones = sb(top, "ones", [128, 128])
        ones1 = sb(top, "ones1", [1, 128])
        rmask = sb(top, "rmask", [128, 512], BF16)
        maskF = sb(top, "maskF", [32, 4, 32])
        maskB = sb(top, "maskB", [32, 4, 32])
        scT = sb(top, "scT", [128, DC, 2])
        modT = sb(top, "modT", [128, DEPTH, 48, 2])
        A1 = sb(top, "A1", [128, DEPTH, DC, 2])
        A2 = sb(top, "A2", [128, DEPTH, DC, 2])
        lbT = sb(top, "lbT", [128, 2 * DEPTH * 4])
        omlT = sb(top, "omlT", [128, 2 * DEPTH * 4])
        I32 = mybir.dt.int32
        NB = cfg.NB
        SEL = sb(top, "SEL", [128, cfg.NT, 32])
        RK = sb(top, "RK", [128, cfg.NT, 32])
        IDX = sb(top, "IDX", [128, cfg.NT, 2], I32)
        GHL = sb(top, "GHL", [128, cfg.NT, 2])
        IDXW = sb(top, "IDXW", [128, NB], I32)
        PIDXi = sb(top, "PIDXi", [128, 1], I32)
        PIDX = sb(top, "PIDX", [128, 1])
        Lst = sb(top, "Lst", [128, 128])
        THR = sb(top, "THR", [128, 18])
        JR = sb(top, "JR", [128, NB])
        c128 = sb(top, "c128", [128, NB])
        pb = [top.enter_context(nc.psum_tensor("pb%d" % i, [128, 512], F32)) for i in range(8)]

        tile_gi = {}
        for (t0_, n_, sidx_, gi_) in cfg.groups:
            for ti_ in range(n_ // 128):
                tile_gi[(t0_ + ti_ * 128) // 128] = (gi_, sidx_)
        tile_gi = {k_: v_ for k_, v_ in tile_gi.items()}

        def XK(gi):
            return ('XT', gi)

        def HK(gi):
            return ('hT', gi)

        def mm(out, lhsT, rhs, start, stop, r, w):
            return S.op('pe', lambda e: e.matmul(out, lhsT, rhs, start=start, stop=stop), r=r, w=w)

        def tr(out, in_, r, w):
            return S.op('pe', lambda e: e.transpose(out, in_, ident[:in_.shape[0], :in_.shape[0]]), r=list(r) + [ident], w=w)

        def act(out, in_, func, r, w, bias=None, scale=None, accum=None):
            kw = {}
            if bias is not None:
                kw['bias'] = bias
            if scale is not None:
                kw['scale'] = scale
            if accum is not None:
                kw['accum_out'] = accum
            return S.op('act', lambda e: e.activation(out=out, in_=in_, func=func, **kw), r=r, w=w)

        def tt(eng, out, in0, in1, op, r, w):
            return S.op(eng, lambda e: e.tensor_tensor(out=out, in0=in0, in1=in1, op=op), r=r, w=w)

        def tsc(eng, out, in0, s1, s2, op0, op1, r, w):
            if op1 is None:
                return S.op(eng, lambda e: e.tensor_scalar(out=out, in0=in0, scalar1=s1, scalar2=None, op0=op0), r=r, w=w)
            return S.op(eng, lambda e: e.tensor_scalar(out=out, in0=in0, scalar1=s1, scalar2=s2, op0=op0, op1=op1), r=r, w=w)

        def stt(out, in0, scalar, in1, op0, op1, r, w):
            return S.op('dve', lambda e: e.scalar_tensor_tensor(out=out, in0=in0, scalar=scalar, in1=in1, op0=op0, op1=op1), r=r, w=w)

        def cp(eng, out, in_, r, w):
            return S.op(eng, lambda e: e.tensor_copy(out=out, in_=in_), r=r, w=w)

        def recip(out, in_, r, w):
            return S.op('dve', lambda e: e.reciprocal(out=out, in_=in_), r=r, w=w)

        def memset(eng, ap, val, w):
            return S.op(eng, lambda e: e.memset(ap, val), w=w)

        def dma(eng, out, in_, r, w):
            return S.op(eng, lambda e: e.dma_start(out=out, in_=in_), r=r, w=w, dma=True)

        def V(col, n=1):
            return vecs[:, col:col + n]

        memset('pool', ident[:], 0.0, [ident])
        S.op('pool', lambda e: e.affine_select(out=ident[:], in_=ident[:], compare_op=ALU.not_equal, fill=1.0,
                                               base=0, pattern=[[-1, 128]], channel_multiplier=1), r=[ident], w=[ident])
        memset('pool', ones[:], 1.0, [ones])
        memset('pool', ones1[:], 1.0, [ones1])
        memset('pool', rmask[:], 1.0, [rmask])
        S.op('pool', lambda e: e.memset(rmask[:].rearrange("p (a b) -> p a b", b=32)[:, :, 0:1], 0.0), r=[rmask], w=[rmask])
        memset('pool', maskF[:], 1.0, [maskF])
        memset('pool', maskB[:], 1.0, [maskB])
        S.op('pool', lambda e: e.affine_select(out=maskF[:], in_=maskF[:], compare_op=ALU.is_ge, fill=0.0,
                                               base=0, pattern=[[0, 4], [1, 32]], channel_multiplier=-1), r=[maskF], w=[maskF])
        S.op('pool', lambda e: e.affine_select(out=maskB[:], in_=maskB[:], compare_op=ALU.is_ge, fill=0.0,
                                               base=0, pattern=[[0, 4], [-1, 32]], channel_multiplier=1), r=[maskB], w=[maskB])

        S.op('pool', lambda e: e.iota(PIDXi[:], pattern=[[0, 1]], base=0, channel_multiplier=1), w=[PIDXi])
        cp('dve', PIDX[:], PIDXi[:], [PIDXi], [PIDX])
        memset('pool', Lst[:], 1.0, [Lst])
        S.op('pool', lambda e: e.affine_select(out=Lst[:], in_=Lst[:], compare_op=ALU.is_ge, fill=0.0,
                                               base=-1, pattern=[[1, 128]], channel_multiplier=-1), r=[Lst], w=[Lst])
        memset('pool', c128[:], 128.0, [c128])
        S.op('dve', lambda e: e.tensor_tensor_scan(out=JR[:], data0=ones[:, :NB], data1=c128[:], initial=-128.0,
                                                   op0=ALU.mult, op1=ALU.add), r=[ones, c128], w=[JR])
        cp('dve', THR[:], JR[:, 0:18], [JR], [THR])

        dma('sp', vecs[:], vecs_d, [], [vecs])
        dma('sp', wr[:].rearrange("p l c n -> p (l c n)"), wr_d, [], [wr])
        dma('sp', br[:], br_d, [], [br])
        for (t0, n, sidx, gi) in cfg.groups:
            dma('sp', XT[:, :, t0:t0 + n], xT_v[:, :, t0:t0 + n], [], [XK(gi)])

        act(scT[:, :, 0], V(cfg.V_C, 8), AF.Silu, [vecs], [scT])
        act(scT[:, :, 1], V(cfg.V_CC, 8), AF.Silu, [vecs], [scT])
        with contextlib.ExitStack() as ph:
            nlb = 2 * DEPTH * 4
            E = sb(ph, "lbE", [128, nlb])
            sE = sb(ph, "lbS", [128, 8])
            rE = sb(ph, "lbR", [128, 8])
            act(E[:], V(cfg.V_LB, nlb), AF.Exp, [vecs], [E])
            E3 = E[:].rearrange("p (d l h) -> p d l h", d=2, l=DEPTH)
            sE2 = sE[:].rearrange("p (d h) -> p d h", d=2)
            cp('dve', sE2, E3[:, :, 0, :], [E], [sE])
            for l in range(1, DEPTH):
                tt('dve', sE2, sE2, E3[:, :, l, :], ALU.add, [sE, E], [sE])
            recip(rE[:], sE[:], [sE], [rE])
            rE2 = rE[:].rearrange("p (d h) -> p d h", d=2)
            lb3 = lbT[:].rearrange("p (d l h) -> p d l h", d=2, l=DEPTH)
            memset('dve', lbT[:], 0.0, [lbT])
            for l in range(1, DEPTH):
                tt('dve', E3[:, :, l, :], E3[:, :, l, :], rE2, ALU.mult, [E, rE], [E])
                tt('dve', lb3[:, :, l, :], lb3[:, :, l - 1, :], E3[:, :, l, :], ALU.add, [lbT, E], [lbT])
            tsc('dve', omlT[:], lbT[:], -1.0, 1.0, ALU.mult, ALU.add, [lbT], [omlT])

            wa = [sb(ph, "wada%d" % i, [128, DC, 512]) for i in range(2)]
            k = 0
            for l in cfg.layers:
                voff = cfg.V_L0 + l * cfg.V_LN
                for jb in range(12):
                    wt = wa[k % 2]
                    k += 1
                    dma('sp', wt[:], wada_d[l, :, jb * 512:(jb + 1) * 512].rearrange("(c p) n -> p c n", p=128), [], [wt])
                    pm = pb[jb % 2]
                    for jj in range(4):
                        for c in range(DC):
                            mm(pm[:, jj * 2:jj * 2 + 2], wt[:, c, jj * 128:(jj + 1) * 128], scT[:, c, :], c == 0, c == DC - 1,
                               [wt, scT], [pm])
                    for s in range(2):
                        tt('dve', modT[:, l, jb * 4:jb * 4 + 4, s], pm[:, 0:8].rearrange("p (j s) -> p j s", s=2)[:, :, s],
                           V(voff + 16 + jb * 4, 4), ALU.add, [pm, vecs], [modT])
                for s in range(2):
                    stt(A1[:, l, :, s], modT[:, l, 8:16, s], 1.0, V(voff + 0, 8), ALU.add, ALU.mult, [modT, vecs], [A1])
                    stt(A2[:, l, :, s], modT[:, l, 32:40, s], 1.0, V(voff + 8, 8), ALU.add, ALU.mult, [modT, vecs], [A2])
        S.barrier()

        def norm_modulate(l, which, router):
            A = A1 if which == 0 else A2
            sh0 = 0 if which == 0 else 24
            with contextlib.ExitStack() as ph:
                sq = [sb(ph, "nsq%d" % i, [128, 512]) for i in range(2)]
                rs = [sb(ph, "nrs%d" % i, [128, 512]) for i in range(2)]
                tmp = [sb(ph, "ntmp%d" % i, [128, 512]) for i in range(2)]
                if router:
                    h2f = [sb(ph, "h2f%d" % i, [128, DC, 512]) for i in range(2)]
                    rt = {nm: [sb(ph, "rt_%s%d" % (nm, i), shp) for i in range(2)] for nm, shp in
                          [("lg", [128, 36]), ("gm", [128, 1]), ("ngm", [128, 1]), ("gmask", [128, 4]), ("ge", [128, 4]),
                           ("gs", [128, 1]), ("pen", [128, 4]), ("el", [128, 32]), ("t8", [128, 8]), ("nm1", [128, 1]),
                           ("sel", [128, 32]), ("ex", [128, 32]), ("gx", [128, 32]), ("den", [128, 1]), ("pr", [128, 1]),
                           ("rp", [128, 1])]}
                kk = 0
                tl = 0
                cpar = [0]
                if router:
                    cums = [sb(ph, "cums%d" % i, [128, 32]) for i in range(2)]
                    memset('dve', cums[0][:], 0.0, [cums[0]])
                for (t0, n, sidx, gi) in cfg.groups:
                    pss = pb[gi % 2]
                    for c in range(DC):
                        q = sq[kk % 2]
                        kk += 1
                        act(q[:, :n], XT[:, c, t0:t0 + n], AF.Square, [XK(gi)], [q])
                        mm(pss[:, :n], ones[:], q[:, :n], c == 0, c == DC - 1, [ones, q], [pss])
                    r_ = rs[gi % 2]
                    act(r_[:, :n], pss[:, :n], AF.Sqrt, [pss], [r_], bias=cfg.EPS, scale=1.0 / 1024)
                    recip(r_[:, :n], r_[:, :n], [r_], [r_])
                    for c in range(DC):
                        tm = tmp[kk % 2]
                        kk += 1
                        tt('dve', tm[:, :n], XT[:, c, t0:t0 + n], r_[:, :n], ALU.mult, [XK(gi), r_], [tm])
                        if router:
                            hf = h2f[gi % 2]
                            act(hf[:, c, :n], tm[:, :n], AF.Identity, [tm, A, modT], [hf],
                                bias=modT[:, l, sh0 + c, sidx:sidx + 1], scale=A[:, l, c, sidx:sidx + 1])
                            cp('pool', hT[:, c, t0:t0 + n], hf[:, c, :n], [hf], [HK(gi)])
                        else:
                            act(hT[:, c, t0:t0 + n], tm[:, :n], AF.Identity, [tm, A, modT], [HK(gi)],
                                bias=modT[:, l, sh0 + c, sidx:sidx + 1], scale=A[:, l, c, sidx:sidx + 1])
                    if router:
                        hf = h2f[gi % 2]
                        for ti in range(n // 128):
                            tg = (t0 + ti * 128) // 128
                            R = {nm: v[tl % 2] for nm, v in rt.items()}
                            pl = pb[2 + tl % 2]
                            tl += 1
                            for c in range(DC):
                                mm(pl[:, 0:36], hf[:, c, ti * 128:(ti + 1) * 128], wr[:, l, c, :], c == 0, False, [hf, wr], [pl])
                            mm(pl[:, 0:36], ones1[:], br[:, l * 36:(l + 1) * 36], False, True, [ones1, br], [pl])
                            lg = R["lg"]
                            cp('dve', lg[:], pl[:, 0:36], [pl], [lg])
                            S.op('dve', lambda e, o=R["gm"], i=lg: e.tensor_reduce(out=o[:], in_=i[:, 0:4], axis=AX.X, op=ALU.max),
                                 r=[lg], w=[R["gm"]])
                            tsc('dve', R["ngm"][:], R["gm"][:], -1.0, None, ALU.mult, None, [R["gm"]], [R["ngm"]])
                            tsc('dve', R["gmask"][:], lg[:, 0:4], R["gm"][:], None, ALU.is_equal, None, [lg, R["gm"]], [R["gmask"]])
                            act(R["ge"][:], lg[:, 0:4], AF.Exp, [lg, R["ngm"]], [R["ge"], R["gs"]], bias=R["ngm"][:], scale=1.0,
                                accum=R["gs"][:])
                            tsc('dve', R["pen"][:], R["gmask"][:], 1e30, -1e30, ALU.mult, ALU.add, [R["gmask"]], [R["pen"]])
                            tt('dve', R["el"][:].rearrange("p (g e) -> p g e", g=4), lg[:, 4:36].rearrange("p (g e) -> p g e", g=4),
                               R["pen"][:].unsqueeze(2).to_broadcast([128, 4, 8]), ALU.add, [lg, R["pen"]], [R["el"]])
                            S.op('dve', lambda e, o=R["t8"], i=R["el"]: e.max(out=o[:], in_=i[:]), r=[R["el"]], w=[R["t8"]])
                            tsc('dve', R["nm1"][:], R["t8"][:, 0:1], -1.0, None, ALU.mult, None, [R["t8"]], [R["nm1"]])
                            tsc('dve', R["sel"][:], R["el"][:], R["t8"][:, 1:2], None, ALU.is_ge, None, [R["el"], R["t8"]], [R["sel"]])
                            act(R["ex"][:], R["el"][:], AF.Exp, [R["el"], R["nm1"]], [R["ex"]], bias=R["nm1"][:], scale=1.0)
                            tt('dve', R["gx"][:], R["sel"][:], R["ex"][:], ALU.mult, [R["sel"], R["ex"]], [R["gx"]])
                            S.op('dve', lambda e, o=R["den"], i=R["gx"]: e.tensor_reduce(out=o[:], in_=i[:], axis=AX.X, op=ALU.add),
                                 r=[R["gx"]], w=[R["den"]])
                            tt('dve', R["pr"][:], R["den"][:], R["gs"][:], ALU.mult, [R["den"], R["gs"]], [R["pr"]])
                            recip(R["rp"][:], R["pr"][:], [R["pr"]], [R["rp"]])
                            tsc('dve', G[:, tg, :], R["gx"][:], R["rp"][:], None, ALU.mult, None, [R["gx"], R["rp"]], [('G', gi)])
                            cp('dve', SEL[:, tg, :], R["sel"][:], [R["sel"]], [('SEL', tg)])
                            prk = pb[4 + tl % 2]
                            mm(prk[:, 0:32], Lst[:], R["sel"][:], True, False, [Lst, R["sel"]], [prk])
                            mm(prk[:, 0:32], ones[:], cums[cpar[0]][:], False, True, [ones, cums[cpar[0]]], [prk])
                            cp('dve', RK[:, tg, :], prk[:, 0:32], [prk], [('RK', tg)])
                            tt('dve', cums[1 - cpar[0]][:], cums[cpar[0]][:], R["sel"][:], ALU.add, [cums[cpar[0]], R["sel"]], [cums[1 - cpar[0]]])
                            cpar[0] = 1 - cpar[0]
                if router:
                    CNT = sb(ph, "CNT", [128, 32])
                    cmp18 = sb(ph, "cmp18", [128, 32, 18])
                    NBLK = sb(ph, "NBLK", [128, 32])
                    PADD = sb(ph, "PADD", [128, 32])
                    PEND = sb(ph, "PEND", [128, 32])
                    PST = sb(ph, "PST", [128, 32])
                    cmpB = sb(ph, "cmpB", [128, NB, 32])
                    BEf = sb(ph, "BEf", [128, NB])
                    pc = pb[6]
                    mm(pc[:, 0:32], ones[:], cums[cpar[0]][:], True, True, [ones, cums[cpar[0]]], [pc])
                    cp('dve', CNT[:], pc[:, 0:32], [pc], [CNT])
                    tt('dve', cmp18[:], CNT[:].unsqueeze(2).to_broadcast([128, 32, 18]), THR[:].unsqueeze(1).to_broadcast([128, 32, 18]),
                       ALU.is_gt, [CNT, THR], [cmp18])
                    S.op('dve', lambda e: e.tensor_reduce(out=NBLK[:], in_=cmp18[:], axis=AX.X, op=ALU.add), r=[cmp18], w=[NBLK])
                    tsc('dve', PADD[:], NBLK[:], 128.0, None, ALU.mult, None, [NBLK], [PADD])
                    S.op('dve', lambda e: e.tensor_tensor_scan(out=PEND[:], data0=ones[:, 0:32], data1=PADD[:], initial=0.0,
                                                               op0=ALU.mult, op1=ALU.add), r=[ones, PADD], w=[PEND])
                    tt('dve', PST[:], PEND[:], PADD[:], ALU.subtract, [PEND, PADD], [PST])
                    tt('dve', cmpB[:], PEND[:].unsqueeze(1).to_broadcast([128, NB, 32]), JR[:].unsqueeze(2).to_broadcast([128, NB, 32]),
                       ALU.is_le, [PEND, JR], [cmpB])
                    S.op('dve', lambda e: e.tensor_reduce(out=BEf[:], in_=cmpB[:], axis=AX.X, op=ALU.add), r=[cmpB], w=[BEf])
                    tsc('dve', BEf[:], BEf[:], 31.0, float(32 * l), ALU.min, ALU.add, [BEf], [BEf])
                    tsc('dve', IDXW[:], BEf[:], 128.0, PIDX[:], ALU.mult, ALU.add, [BEf, PIDX], [IDXW])
                    pp = {nm: [sb(ph, "pp_%s%d" % (nm, i), shp) for i in range(2)] for nm, shp in
                          [("pos", [128, 32]), ("t8", [128, 8]), ("eq", [128, 32]), ("pr", [128, 32])]}
                    for tg in range(cfg.NT):
                        Q = {nm: v[tg % 2] for nm, v in pp.items()}
                        gi_t = tile_gi[tg][0]
                        tt('dve', Q["pos"][:], RK[:, tg, :], PST[:], ALU.add, [('RK', tg), PST], [Q["pos"]])
                        stt(Q["pos"][:], Q["pos"][:], 1.0, SEL[:, tg, :], ALU.add, ALU.mult, [Q["pos"], ('SEL', tg)], [Q["pos"]])
                        S.op('dve', lambda e, o=Q["t8"], i=Q["pos"]: e.max(out=o[:], in_=i[:]), r=[Q["pos"]], w=[Q["t8"]])
                        tsc('dve', IDX[:, tg, :], Q["t8"][:, 0:2], -1.0, None, ALU.add, None, [Q["t8"]], [('IDX', tg)])
                        for k in range(2):
                            tsc('dve', Q["eq"][:], Q["pos"][:], Q["t8"][:, k:k + 1], None, ALU.is_equal, None, [Q["pos"], Q["t8"]], [Q["eq"]])
                            tt('dve', Q["pr"][:], Q["eq"][:], G[:, tg, :], ALU.mult, [Q["eq"], ('G', gi_t)], [Q["pr"]])
                            S.op('dve', lambda e, o=GHL[:, tg, k:k + 1], i=Q["pr"]: e.tensor_reduce(out=o, in_=i[:], axis=AX.X, op=ALU.add),
                                 r=[Q["pr"]], w=[('GHL', tg)])
            S.barrier()

        def out_proj(l, Y, nk, k0, ph):
            Wo = sb(ph, "Wo", [128, nk, 1024], BF16)
            dma('pool', Wo[:], wout_d[l, k0 * 128:(k0 + nk) * 128, :].rearrange("(c p) n -> p c n", p=128), [], [Wo])
            kk = 0
            for (t0, n, sidx, gi) in cfg.groups:
                for j in range(DC):
                    po = pb[4 + kk % 4]
                    kk += 1
                    for c in range(nk):
                        rhs = Y[:, c, t0:t0 + n] if nk > 1 else Y[:, t0:t0 + n]
                        mm(po[:, :n], Wo[:, c, j * 128:(j + 1) * 128], rhs, c == 0, c == nk - 1, [Wo, Y], [po])
                    stt(XT[:, j, t0:t0 + n], po[:, :n], modT[:, l, 16 + j, sidx:sidx + 1], XT[:, j, t0:t0 + n],
                        ALU.mult, ALU.add, [po, modT, XK(gi)], [XK(gi)])

        def load_win(ph, name, l, col):
            W = sb(ph, name, [128, DC, 128], BF16)
            dma('pool', W[:], win_d[l, :, col:col + 128].rearrange("(c p) n -> p c n", p=128), [], [W])
            return W

        def proj(W, ps, t0, n, gi):
            for c in range(DC):
                mm(ps[:, :n], W[:, c, :], hT[:, c, t0:t0 + n], c == 0, c == DC - 1, [W, HK(gi)], [ps])

        def conv_phase(l):
            voff = cfg.V_L0 + l * cfg.V_LN
            R_ = TL // 64
            with contextlib.ExitStack() as ph:
                Z = sb(ph, "Z", [128, 4, T], BF16)
                SS = sb(ph, "SS", [128, T])
                u = sb(ph, "u", [128, T])
                Bs = sb(ph, "Bs", [128, T])
                y = sb(ph, "y", [128, T])
                hv = [sb(ph, "hv%d" % i, [128, 512]) for i in range(2)]
                zs = [sb(ph, "zs%d" % i, [128, 512]) for i in range(2)]
                for cc in range(4):
                    with contextlib.ExitStack() as ph2:
                        WB = load_win(ph2, "WB", l, cc * 128)
                        WC = load_win(ph2, "WC", l, 512 + cc * 128)
                        WH = load_win(ph2, "WH", l, 1024 + cc * 128)
                        for (t0, n, sidx, gi) in cfg.groups:
                            pB, pC, pH = pb[0 + 4 * (gi % 2)], pb[1 + 4 * (gi % 2)], pb[2 + 4 * (gi % 2)]
                            proj(WB, pB, t0, n, gi)
                            proj(WC, pC, t0, n, gi)
                            proj(WH, pH, t0, n, gi)
                            h_ = hv[gi % 2]
                            act(h_[:, :n], pH[:, :n], AF.Copy, [pH], [h_])
                            act(Bs[:, t0:t0 + n], pB[:, :n], AF.Copy, [pB], [Bs])
                            tt('dve', u[:, t0:t0 + n], pC[:, :n], h_[:, :n], ALU.mult, [pC, h_], [u])
                        w0, w1, w2 = V(voff + 64 + 0 * 4 + cc), V(voff + 64 + 1 * 4 + cc), V(voff + 64 + 2 * 4 + cc)
                        act(y[:], u[:], AF.Identity, [u, vecs], [y], scale=w1)
                        stt(y[:, 1:TC], u[:, 0:TC - 1], w0, y[:, 1:TC], ALU.mult, ALU.add, [u, vecs, y], [y])
                        stt(y[:, 0:TC - 1], u[:, 1:TC], w2, y[:, 0:TC - 1], ALU.mult, ALU.add, [u, vecs, y], [y])
                        if cc < 2:
                            ul = u[:, TC:T].rearrange("p (r w) -> p r w", w=64)
                            yl = y[:, TC:T].rearrange("p (r w) -> p r w", w=64)
                            stt(yl[:, :, 1:64], ul[:, :, 0:63], w0, yl[:, :, 1:64], ALU.mult, ALU.add, [u, vecs, y], [y])
                            stt(yl[:, :, 0:63], ul[:, :, 1:64], w2, yl[:, :, 0:63], ALU.mult, ALU.add, [u, vecs, y], [y])
                        else:
                            stt(y[:, TC + 64:T], u[:, TC:T - 64], w0, y[:, TC + 64:T], ALU.mult, ALU.add, [u, vecs, y], [y])
                            stt(y[:, TC:T - 64], u[:, TC + 64:T], w2, y[:, TC:T - 64], ALU.mult, ALU.add, [u, vecs, y], [y])
                        tt('dve', y[:], y[:], Bs[:], ALU.mult, [y, Bs], [y])
                        act(Z[:, cc, :], y[:], AF.Copy, [y], [Z])
                        for (t0, n, sidx, gi) in cfg.groups:
                            z_ = zs[gi % 2]
                            pz = pb[3 + 4 * (gi % 2)]
                            act(z_[:, :n], y[:, t0:t0 + n], AF.Square, [y], [z_])
                            mm(pz[:, :n], ones[:], z_[:, :n], True, True, [ones, z_], [pz])
                            if cc == 0:
                                cp('dve', SS[:, t0:t0 + n], pz[:, :n], [pz], [SS])
                            else:
                                tt('dve', SS[:, t0:t0 + n], pz[:, :n], SS[:, t0:t0 + n], ALU.add, [pz, SS], [SS])
                    S.barrier()
                act(SS[:], SS[:], AF.Sqrt, [SS], [SS], bias=cfg.EPS, scale=1.0 / 512)
                recip(SS[:], SS[:], [SS], [SS])
                for cc in range(4):
                    stt(Z[:, cc, :], Z[:, cc, :], V(voff + 76 + cc), SS[:], ALU.mult, ALU.mult, [Z, vecs, SS], [Z])
                out_proj(l, Z, 4, 0, ph)
            S.barrier()

        def heads_phase(l):
            voff = cfg.V_L0 + l * cfg.V_LN
            hnw = V(voff + 80)
            lat = cfg.groups[1:]
            order = [cfg.groups, [cfg.groups[0]] + lat[::-1]]
            for hh in range(4):
                with contextlib.ExitStack() as ph:
                    Wq = load_win(ph, "Wq", l, 1536 + hh * 128)
                    Wf = [load_win(ph, "Wzf", l, 2048 + hh * 128), load_win(ph, "Wzb", l, 2560 + hh * 128)]
                    Wi = load_win(ph, "Wi", l, 3072 + hh * 128)
                    Wg = load_win(ph, "Wg", l, 3584 + hh * 128)
                    Of = sb(ph, "Of", [128, T])
                    Yh = sb(ph, "Yh", [128, T], BF16)
                    NS = 4
                    St = [sb(ph, "St%d" % i, [128, 128]) for i in range(NS)]
                    names = ["qs", "sg", "kk", "vs", "b", "eb", "enb", "ko", "gs"]
                    tmps = [{nm: sb(ph, "g%s%d" % (nm, i), [128, 512]) for nm in names} for i in range(2)]
                    dch = [sb(ph, "dch%d" % i, [128, 16]) for i in range(2)]
                    totc = [sb(ph, "totc%d" % i, [128, 16]) for i in range(2)]
                    koT = [sb(ph, "koT%d" % i, [32, 4, 128]) for i in range(2)]
                    vT = [sb(ph, "vT%d" % i, [32, 4, 128]) for i in range(2)]
                    PT = [sb(ph, "PT%d" % i, [32, 4, 32]) for i in range(2)]
                    osq = sb(ph, "osq", [128, 512])
                    ors = sb(ph, "ors", [128, 512])
                    gpar = 0
                    tpar = 0
                    for dirn in range(2):
                        lbc = (dirn * DEPTH + l) * 4 + hh
                        lb_ap, oml_ap = lbT[:, lbc:lbc + 1], omlT[:, lbc:lbc + 1]
                        cur = 0
                        memset('dve', St[0][:], 0.0, [St[0]])
                        msk = maskF if dirn == 0 else maskB
                        for (t0, n, sidx, gi) in order[dirn]:
                            tp = tmps[gpar % 2]
                            dc_ = dch[gpar % 2]
                            gpar += 1
                            nch = n // 32
                            pq, pz_, pi_, pg = pb[0], pb[1], pb[2], pb[3]
                            proj(Wq, pq, t0, n, gi)
                            proj(Wf[dirn], pz_, t0, n, gi)
                            proj(Wi, pi_, t0, n, gi)
                            act(tp["qs"][:, :n], pq[:, :n], AF.Silu, [pq], [tp["qs"]])
                            act(tp["sg"][:, :n], pz_[:, :n], AF.Sigmoid, [pz_], [tp["sg"]])
                            act(tp["vs"][:, :n], pi_[:, :n], AF.Copy, [pi_], [tp["vs"]])
                            if dirn == 1:
                                proj(Wg, pg, t0, n, gi)
                                act(tp["gs"][:, :n], pg[:, :n], AF.Silu, [pg], [tp["gs"]])
                            tc_ = totc[(gpar - 1) % 2]
                            tsc('dve', tp["sg"][:, :n], tp["sg"][:, :n], oml_ap, lb_ap, ALU.mult, ALU.add, [tp["sg"], omlT, lbT], [tp["sg"]])
                            tsc('dve', tp["kk"][:, :n], tp["sg"][:, :n], -1.0, 1.0, ALU.mult, ALU.add, [tp["sg"]], [tp["kk"]])
                            act(tp["sg"][:, :n], tp["sg"][:, :n], AF.Ln, [tp["sg"]], [tp["sg"]])
                            S.op('dve', lambda e, o=tp["b"], m=rmask, d1=tp["sg"], n=n: e.tensor_tensor_scan(
                                out=o[:, :n], data0=m[:, :n], data1=d1[:, :n], initial=0.0, op0=ALU.mult, op1=ALU.add),
                                r=[rmask, tp["sg"]], w=[tp["b"]])
                            b3 = tp["b"][:, :n].rearrange("p (a c) -> p a c", c=32)
                            cp('dve', tc_[:, :nch], b3[:, :, 31], [tp["b"]], [tc_])
                            act(dc_[:, :nch], tc_[:, :nch], AF.Exp, [tc_], [dc_])
                            bb = tp["b"]
                            if dirn == 1:
                                tt('dve', b3, b3, tc_[:, :nch].unsqueeze(2).to_broadcast([128, nch, 32]), ALU.subtract, [tp["b"], tc_], [tp["b"]])
                                tt('dve', bb[:, :n], tp["sg"][:, :n], bb[:, :n], ALU.subtract, [tp["sg"], bb], [bb])
                            act(tp["eb"][:, :n], bb[:, :n], AF.Exp, [bb], [tp["eb"]])
                            act(tp["enb"][:, :n], bb[:, :n], AF.Exp, [bb], [tp["enb"]], scale=-1.0)
                            tt('dve', tp["qs"][:, :n], tp["qs"][:, :n], tp["eb"][:, :n], ALU.mult, [tp["qs"], tp["eb"]], [tp["qs"]])
                            tt('dve', tp["kk"][:, :n], tp["kk"][:, :n], tp["enb"][:, :n], ALU.mult, [tp["kk"], tp["enb"]], [tp["kk"]])
                            tt('dve', tp["ko"][:, :n].rearrange("p (a c) -> p a c", c=32),
                               tp["kk"][:, :n].rearrange("p (a c) -> p a c", c=32),
                               dc_[:, :nch].unsqueeze(2).to_broadcast([128, nch, 32]), ALU.mult, [tp["kk"], dc_], [tp["ko"]])
                            tiles = list(range(n // 128))
                            chunks = [0, 1, 2, 3]
                            if dirn == 1:
                                tiles = tiles[::-1]
                                chunks = chunks[::-1]
                            for ti in tiles:
                                c0 = ti * 128
                                kT_, vT_, PT_ = koT[tpar % 2], vT[tpar % 2], PT[tpar % 2]
                                tpar += 1
                                pk, pv = pb[4], pb[5]
                                pk3 = pk[0:32, :].rearrange("p (j k) -> p j k", j=4)
                                pv3 = pv[0:32, :].rearrange("p (j k) -> p j k", j=4)
                                for j in range(4):
                                    tr(pk3[:, j, :], tp["ko"][:, c0 + 32 * j:c0 + 32 * j + 32], [tp["ko"]], [pk])
                                for j in range(4):
                                    tr(pv3[:, j, :], tp["vs"][:, c0 + 32 * j:c0 + 32 * j + 32], [tp["vs"]], [pv])
                                act(kT_[:], pk3, AF.Copy, [pk], [kT_])
                                cp('dve', vT_[:], pv3, [pv], [vT_])
                                par = tpar % 2
                                psc = pb[3][0:32, 256:384].rearrange("p (j k) -> p j k", j=4)
                                for j in range(4):
                                    cs = c0 + 32 * j
                                    mm(psc[:, j, :], tp["kk"][:, cs:cs + 32], tp["qs"][:, cs:cs + 32], True, True,
                                       [tp["kk"], tp["qs"]], [pb[3]])
                                tt('dve', PT_[:], psc, msk[:], ALU.mult, [pb[3], msk], [PT_])
                                po = pb[7][:, 256 * par:256 * par + 128]
                                for j in chunks:
                                    cs = c0 + 32 * j
                                    jg = cs // 32
                                    mm(pb[6][:, 128 * j:128 * j + 128], kT_[:, j, :], vT_[:, j, :], True, True, [kT_, vT_], [('pb6', j)])
                                    mm(po[:, 32 * j:32 * j + 32], St[cur][:], tp["qs"][:, cs:cs + 32], True, False, [St[cur], tp["qs"]], [('pb7', 'o', par)])
                                    mm(po[:, 32 * j:32 * j + 32], vT_[:, j, :], PT_[:, j, :], False, True, [vT_, PT_], [('pb7', 'o', par)])
                                    nxt = (cur + 1) % NS
                                    stt(St[nxt][:], St[cur][:], dc_[:, jg:jg + 1], pb[6][:, 128 * j:128 * j + 128], ALU.mult, ALU.add,
                                        [St[cur], dc_, ('pb6', j)], [St[nxt]])
                                    cur = nxt
                                if dirn == 0:
                                    act(Of[:, t0 + c0:t0 + c0 + 128], po[:, 0:128], AF.Copy, [('pb7', 'o', par)], [Of])
                                else:
                                    tt('dve', Of[:, t0 + c0:t0 + c0 + 128], po[:, 0:128], Of[:, t0 + c0:t0 + c0 + 128], ALU.add, [('pb7', 'o', par), Of], [Of])
                            if dirn == 1:
                                pn = pb[3]
                                act(osq[:, :n], Of[:, t0:t0 + n], AF.Square, [Of], [osq])
                                mm(pn[:, :n], ones[:], osq[:, :n], True, True, [ones, osq], [pn])
                                act(ors[:, :n], pn[:, :n], AF.Sqrt, [pn], [ors], bias=cfg.EPS, scale=1.0 / 128)
                                recip(ors[:, :n], ors[:, :n], [ors], [ors])
                                stt(osq[:, :n], Of[:, t0:t0 + n], hnw, ors[:, :n], ALU.mult, ALU.mult, [Of, vecs, ors], [osq])
                                tt('dve', Yh[:, t0:t0 + n], osq[:, :n], tp["gs"][:, :n], ALU.mult, [osq, tp["gs"]], [Yh])
                    out_proj(l, Yh, 1, 4 + hh, ph)
                S.barrier()

        def moe_phase_dense(l):
            with contextlib.ExitStack() as ph:
                WA = [sb(ph, "WA%d" % i, [128, DC, 512], BF16) for i in range(2)]
                WU = [sb(ph, "WU%d" % i, [128, DC, 512], BF16) for i in range(2)]
                WD = [sb(ph, "WD%d" % i, [128, 4, 1024], BF16) for i in range(2)]
                gbc = [sb(ph, "gbc%d" % i, [128, 512], BF16) for i in range(2)]
                sa = [sb(ph, "sa%d" % i, [128, 512]) for i in range(2)]
                t1 = [sb(ph, "t1%d" % i, [128, 512], BF16) for i in range(2)]
                hm = [sb(ph, "hm%d" % i, [128, 4, 512], BF16) for i in range(2)]

                def load(e):
                    s = e % 2
                    dma('pool', WA[s][:], wgu_d[l, e, :, 0:512].rearrange("(c p) n -> p c n", p=128), [], [WA[s]])
                    dma('pool', WU[s][:], wgu_d[l, e, :, 512:1024].rearrange("(c p) n -> p c n", p=128), [], [WU[s]])
                    dma('pool', WD[s][:], wdn_d[l, e, :, :].rearrange("(c p) n -> p c n", p=128), [], [WD[s]])

                load(0)
                kk = 0
                k2 = 0
                for e in range(cfg.NE):
                    if e + 1 < cfg.NE:
                        load(e + 1)
                    s = e % 2
                    for (t0, n, sidx, gi) in cfg.groups:
                        pg = pb[0]
                        for ti in range(n // 128):
                            tg = (t0 + ti * 128) // 128
                            mm(pg[:, ti * 128:(ti + 1) * 128], G[:, tg, e:e + 1].to_broadcast([128, 128]), ident[:], True, True,
                               [('G', gi), ident], [pg])
                        gb = gbc[kk % 2]
                        hm_ = hm[kk % 2]
                        kk += 1
                        act(gb[:, :n], pg[:, :n], AF.Copy, [pg], [gb])
                        for hc in range(4):
                            pa, pu = pb[1 + 2 * (k2 % 2)], pb[2 + 2 * (k2 % 2)]
                            sa_, t1_ = sa[k2 % 2], t1[k2 % 2]
                            k2 += 1
                            for c in range(DC):
                                mm(pa[:, :n], WA[s][:, c, hc * 128:(hc + 1) * 128], hT[:, c, t0:t0 + n], c == 0, c == DC - 1,
                                   [WA[s], HK(gi)], [pa])
                            for c in range(DC):
                                mm(pu[:, :n], WU[s][:, c, hc * 128:(hc + 1) * 128], hT[:, c, t0:t0 + n], c == 0, c == DC - 1,
                                   [WU[s], HK(gi)], [pu])
                            act(sa_[:, :n], pa[:, :n], AF.Silu, [pa], [sa_])
                            tt('dve', t1_[:, :n], sa_[:, :n], pu[:, :n], ALU.mult, [sa_, pu], [t1_])
                            tt('pool', hm_[:, hc, :n], t1_[:, :n], gb[:, :n], ALU.mult, [t1_, gb], [hm_])
                        for j in range(DC):
                            py = pb[5 + j % 3]
                            for hc in range(4):
                                mm(py[:, :n], WD[s][:, hc, j * 128:(j + 1) * 128], hm_[:, hc, :n], hc == 0, hc == 3, [WD[s], hm_], [py])
                            stt(XT[:, j, t0:t0 + n], py[:, :n], modT[:, l, 40 + j, sidx:sidx + 1], XT[:, j, t0:t0 + n],
                                ALU.mult, ALU.add, [py, modT, XK(gi)], [XK(gi)])
            S.barrier()

        def moe_phase(l):
            IOA = bass.IndirectOffsetOnAxis
            with contextlib.ExitStack() as ph:
                WAU = [sb(ph, "WAU%d" % i, [128, DC, 1024], BF16) for i in range(2)]
                WD = [sb(ph, "WD%d" % i, [128, 4, 1024], BF16) for i in range(2)]
                h32 = sb(ph, "h32", [128, DC, 128])
                xb = [sb(ph, "xb%d" % i, [128, 1024]) for i in range(2)]
                yb = [sb(ph, "yb%d" % i, [128, 1024]) for i in range(2)]
                xbT = [sb(ph, "xbT%d" % i, [128, DC, 128], BF16) for i in range(2)]
                sa = [sb(ph, "sa0", [128, 512])] * 2
                hm = [sb(ph, "hm%d" % i, [128, 4, 128], BF16) for i in range(2)]
                pT = [pb[0], pb[1]]

                wgu2 = wgu_d.rearrange("l e (p c) n -> (l e p) (c n)", c=8)
                wdn2 = wdn_d.rearrange("l e (p c) n -> (l e p) (c n)", c=4)

                def load(j):
                    s_ = j % 2
                    for q in range(4):
                        S.op('pool', lambda e, j=j, q=q, s_=s_: e.indirect_dma_start(
                            out=WAU[s_][:, 2 * q:2 * q + 2, :].rearrange("p a n -> p (a n)"), out_offset=None, in_=wgu2,
                            in_offset=IOA(ap=IDXW[:, j:j + 1], axis=0), element_offset=q * 2048), r=[IDXW], w=[('WAU', s_, q)], dma=True)
                    for q in range(2):
                        S.op('pool', lambda e, j=j, q=q, s_=s_: e.indirect_dma_start(
                            out=WD[s_][:, 2 * q:2 * q + 2, :].rearrange("p a n -> p (a n)"), out_offset=None, in_=wdn2,
                            in_offset=IOA(ap=IDXW[:, j:j + 1], axis=0), element_offset=q * 2048), r=[IDXW], w=[('WD', s_, q)], dma=True)

                load(0)
                scat = []
                for tg in range(cfg.NT):
                    gi, sidx = tile_gi[tg]
                    act(h32[:], hT[:, :, tg * 128:(tg + 1) * 128], AF.Copy, [HK(gi)], [h32])
                    for c in range(DC):
                        tr(pT[c // 4][:, (c % 4) * 128:(c % 4 + 1) * 128], h32[:, c, :], [h32], [pT[c // 4]])
                    x_ = xb[tg % 2]
                    act(x_[:, 0:512], pT[0][:, :], AF.Copy, [pT[0]], [x_])
                    cp('dve', x_[:, 512:1024], pT[1][:, :], [pT[1]], [x_])
                    for k in range(2):
                        scat.append(S.op('pool', lambda e, x_=x_, tg=tg, k=k: e.indirect_dma_start(
                            out=xs_d[:, :], out_offset=IOA(ap=IDX[:, tg, k:k + 1], axis=0), in_=x_[:, :], in_offset=None),
                            r=[x_, ('IDX', tg)], w=[('xs', tg, k)], dma=True))
                stores = []
                for j in range(NB):
                    if j + 1 < NB:
                        load(j + 1)
                    s_ = j % 2
                    S.op('sp', lambda e, j=j, s_=s_: e.dma_start(out=xb[s_][:], in_=xs_d[j * 128:(j + 1) * 128, :]), r=[], w=[xb[s_]],
                         dma=True, extra_deps=scat)
                    for c in range(DC):
                        tr(pT[c // 4][:, (c % 4) * 128:(c % 4 + 1) * 128], xb[s_][:, :].rearrange("r (p c) -> r c p", c=8)[:, c, :], [xb[s_]], [pT[c // 4]])
                    act(xbT[s_][:, 0:4, :], pT[0][:, :].rearrange("p (c r) -> p c r", c=4), AF.Copy, [pT[0]], [xbT[s_]])
                    cp('dve', xbT[s_][:, 4:8, :], pT[1][:, :].rearrange("p (c r) -> p c r", c=4), [pT[1]], [xbT[s_]])
                    pa, pu = pb[2 + 2 * (j % 2)], pb[3 + 2 * (j % 2)]
                    for hc in range(4):
                        for c in range(DC):
                            mm(pa[:, hc * 128:(hc + 1) * 128], WAU[s_][:, c, 0:512].rearrange("p (m h) -> p h m", h=4)[:, hc, :],
                               xbT[s_][:, c, :], c == 0, c == DC - 1, [('WAU', s_, c // 2), xbT[s_]], [pa])
                    for hc in range(4):
                        for c in range(DC):
                            mm(pu[:, hc * 128:(hc + 1) * 128], WAU[s_][:, c, 512:1024].rearrange("p (m h) -> p h m", h=4)[:, hc, :],
                               xbT[s_][:, c, :], c == 0, c == DC - 1, [('WAU', s_, c // 2), xbT[s_]], [pu])
                    act(sa[s_][:], pa[:, :], AF.Silu, [pa], [sa[s_]])
                    tt('dve', hm[s_][:].rearrange("p c r -> p (c r)"), sa[s_][:], pu[:, :], ALU.mult, [sa[s_], pu], [hm[s_]])
                    py = [pb[6], pb[7]]
                    for half in range(2):
                        for hc in range(4):
                            mm(py[half][:, :], hm[s_][:, hc, :], WD[s_][:, hc, half * 512:(half + 1) * 512], hc == 0, hc == 3,
                               [hm[s_], ('WD', s_, hc // 2)], [py[half]])
                    act(yb[s_][:, 0:512], py[0][:, :], AF.Copy, [py[0]], [yb[s_]])
                    cp('dve', yb[s_][:, 512:1024], py[1][:, :], [py[1]], [yb[s_]])
                    stores.append(S.op('sp', lambda e, j=j, s_=s_: e.dma_start(out=ys_d[j * 128:(j + 1) * 128, :], in_=yb[s_][:]),
                                       r=[yb[s_]], w=[('yd', j)], dma=True))
                for tg in range(cfg.NT):
                    gi, sidx = tile_gi[tg]
                    yh_, yl_ = xb[tg % 2], yb[tg % 2]
                    S.op('pool', lambda e, yh_=yh_, tg=tg: e.indirect_dma_start(
                        out=yh_[:, :], out_offset=None, in_=ys_d[:, :], in_offset=IOA(ap=IDX[:, tg, 0:1], axis=0)),
                        r=[('IDX', tg)], w=[yh_], dma=True, extra_deps=stores)
                    S.op('pool', lambda e, yl_=yl_, tg=tg: e.indirect_dma_start(
                        out=yl_[:, :], out_offset=None, in_=ys_d[:, :], in_offset=IOA(ap=IDX[:, tg, 1:2], axis=0)),
                        r=[('IDX', tg)], w=[yl_], dma=True, extra_deps=stores)
                    tsc('dve', yh_[:], yh_[:], GHL[:, tg, 0:1], None, ALU.mult, None, [yh_, ('GHL', tg)], [yh_])
                    stt(yh_[:], yl_[:], GHL[:, tg, 1:2], yh_[:], ALU.mult, ALU.add, [yl_, ('GHL', tg), yh_], [yh_])
                    for c in range(DC):
                        tr(pT[c // 4][:, (c % 4) * 128:(c % 4 + 1) * 128], yh_[:, c * 128:(c + 1) * 128], [yh_], [pT[c // 4]])
                    for c in range(DC):
                        stt(XT[:, c, tg * 128:(tg + 1) * 128], pT[c // 4][:, (c % 4) * 128:(c % 4 + 1) * 128], modT[:, l, 40 + c, sidx:sidx + 1],
                            XT[:, c, tg * 128:(tg + 1) * 128], ALU.mult, ALU.add, [pT[c // 4], modT, XK(gi)], [XK(gi)])
            S.barrier()

        for l in cfg.layers:
            norm_modulate(l, 0, False)
            if 'conv' not in cfg.skip:
                conv_phase(l)
            if 'heads' not in cfg.skip:
                heads_phase(l)
            norm_modulate(l, 1, True)
            if 'moe' not in cfg.skip:
                moe_phase(l)

        fin = []
        with contextlib.ExitStack() as ph:
            sq = [sb(ph, "fsq%d" % i, [128, 512]) for i in range(2)]
            rs = [sb(ph, "frs%d" % i, [128, 512]) for i in range(2)]
            ob = [sb(ph, "fob%d" % i, [128, 512]) for i in range(4)]
            kk = 0
            for (t0, n, sidx, gi) in cfg.groups[1:]:
                pss = pb[gi % 2]
                for c in range(DC):
                    q = sq[kk % 2]
                    kk += 1
                    act(q[:, :n], XT[:, c, t0:t0 + n], AF.Square, [XK(gi)], [q])
                    mm(pss[:, :n], ones[:], q[:, :n], c == 0, c == DC - 1, [ones, q], [pss])
                r_ = rs[gi % 2]
                act(r_[:, :n], pss[:, :n], AF.Sqrt, [pss], [r_], bias=cfg.EPS, scale=1.0 / 1024)
                recip(r_[:, :n], r_[:, :n], [r_], [r_])
                for c in range(DC):
                    o_ = ob[kk % 4]
                    kk += 1
                    if cfg.final:
                        stt(o_[:, :n], XT[:, c, t0:t0 + n], V(cfg.V_FNW + c), r_[:, :n], ALU.mult, ALU.mult, [XK(gi), vecs, r_], [o_])
                    else:
                        cp('dve', o_[:, :n], XT[:, c, t0:t0 + n], [XK(gi)], [o_])
                    fin.append(dma('sp', outT_v[:, c, t0 - TC:t0 - TC + n], o_[:, :n], [o_], []))
        S.op('sp', lambda e: e.nop(), extra_deps=fin)
        S.run(nc)
        nc._sched_stats = S.stats
    return nc


def pack_inputs(cfg, b, x, c, ctx, c_ctx, norm_w, w_ada, b_ada, w_in, conv_w, conv_norm_w, hg_lb, hg_norm_w, w_out,
                w_rg, b_rg, w_re, b_re, w_e_gu, w_e_down, final_norm_w):
    d = cfg.DEPTH
    tok = np.concatenate([ctx[b], x[b]], axis=0)
    xT = np.ascontiguousarray(tok.T.reshape(cfg.DC, 128, cfg.T).transpose(1, 0, 2)).reshape(128, cfg.DC * cfg.T)

    def fm(v):
        return np.asarray(v).reshape(-1, 128).T

    vecs = np.zeros((128, cfg.NV), np.float32)
    vecs[:, cfg.V_C:cfg.V_C + 8] = fm(c[b])
    vecs[:, cfg.V_CC:cfg.V_CC + 8] = fm(c_ctx)
    vecs[:, cfg.V_FNW:cfg.V_FNW + 8] = fm(final_norm_w)
    for dirn in range(2):
        for l in range(d):
            o = cfg.V_LB + (dirn * d + l) * 4
            vecs[:, o:o + 4] = fm(hg_lb[dirn, l])
    for l in range(d):
        o = cfg.V_L0 + l * cfg.V_LN
        vecs[:, o:o + 8] = fm(norm_w[l, 0])
        vecs[:, o + 8:o + 16] = fm(norm_w[l, 1])
        vecs[:, o + 16:o + 64] = fm(b_ada[l])
        for tap in range(3):
            vecs[:, o + 64 + tap * 4:o + 64 + tap * 4 + 4] = fm(conv_w[l, tap])
        vecs[:, o + 76:o + 80] = fm(conv_norm_w[l])
        vecs[:, o + 80:o + 81] = fm(hg_norm_w[l])
    wr = np.concatenate([w_rg, w_re], axis=2)
    wr = np.ascontiguousarray(wr.reshape(d, cfg.DC, 128, 36).transpose(2, 0, 1, 3)).reshape(128, d * cfg.DC * 36)
    br = np.ascontiguousarray(np.concatenate([b_rg, b_re], axis=1)).reshape(1, d * 36)
    return {"xT": xT.astype(np.float32), "vecs": vecs, "wr": wr.astype(np.float32), "br": br.astype(np.float32)}


def run(cfg, inputs, n_cores):
    inputs = {k: np.asarray(v) for k, v in inputs.items()}
    nc = build_program(cfg)
    shared = {"w_ada": np.ascontiguousarray(inputs["w_ada"]), "w_in": np.ascontiguousarray(inputs["w_in"]),
              "w_out": np.ascontiguousarray(inputs["w_out"]), "w_gu": np.ascontiguousarray(inputs["w_e_gu"]),
              "w_dn": np.ascontiguousarray(inputs["w_e_down"])}
    in_maps = []
    for b in range(n_cores):
        m = pack_inputs(cfg, b, **inputs)
        m.update(shared)
        in_maps.append(m)
    res = run_bass_kernel_spmd(nc, in_maps, core_ids=list(range(n_cores)))
    outs = []
    for b in range(n_cores):
        oT = np.asarray(res.results[b]["outT"]).reshape(128, cfg.DC, cfg.TL)
        outs.append(oT.transpose(2, 1, 0).reshape(cfg.TL, cfg.D))
    return np.stack(outs, axis=0).astype(np.float32)


def kernel(**inputs):
    cfg = Cfg(depth=4, t_lat=2048)
    return run(cfg, inputs, 8)
```

```python
import contextlib
import numpy as np
import concourse.bass as bass
import concourse.mybir as mybir
from concourse.bass_utils import run_bass_kernel_spmd

F32 = mybir.dt.float32
BF16 = mybir.dt.bfloat16
AF = mybir.ActivationFunctionType
ALU = mybir.AluOpType
AX = mybir.AxisListType

ENGS = ['pe', 'act', 'dve', 'pool', 'sp']
DMA_RING = 8


class Op:
    __slots__ = ('eng', 'fn', 'deps', 'idx', 'signal', 'semkey', 'semval', 'dma', 'waits')

    def __init__(self, eng, fn, dma):
        self.eng = eng
        self.fn = fn
        self.dma = dma
        self.deps = []
        self.signal = False
        self.semkey = None
        self.semval = 0
        self.waits = []


class Sched:
    def __init__(self):
        self.ops = {e: [] for e in ENGS}
        self.bufs = {}
        self.ndma = {e: 0 for e in ENGS}
        self.dma_ops = {e: [] for e in ENGS}

    @staticmethod
    def _key(x):
        if isinstance(x, (tuple, str)):
            return x
        return x.name

    def op(self, eng, fn, r=(), w=(), dma=False, extra_deps=()):
        o = Op(eng, fn, dma)
        deps = list(extra_deps)
        rk = [self._key(x) for x in r]
        wk = [self._key(x) for x in w]
        for k in rk:
            st = self.bufs.get(k)
            if st is not None and st[0] is not None:
                deps.append(st[0])
        for k in wk:
            st = self.bufs.get(k)
            if st is not None:
                if st[0] is not None:
                    deps.append(st[0])
                deps.extend(st[1].values())
        if dma:
            i = self.ndma[eng]
            self.ndma[eng] += 1
            o.semkey = ('dma', eng, i % DMA_RING)
            o.semval = 16 * (i // DMA_RING + 1)
            if i >= DMA_RING:
                deps.append(self.dma_ops[eng][i - DMA_RING])
            self.dma_ops[eng].append(o)
        seen = set()
        for d in deps:
            if id(d) in seen or d is o:
                continue
            seen.add(id(d))
            o.deps.append(d)
        o.idx = len(self.ops[eng])
        self.ops[eng].append(o)
        for k in rk:
            st = self.bufs.setdefault(k, [None, {}])
            st[1][id(o) if dma else eng] = o
        for k in wk:
            self.bufs[k] = [o, {}]
        return o

    def barrier(self):
        last = []
        for e in ENGS:
            if self.ops[e]:
                last.append(self.ops[e][-1])
            last.extend(self.dma_ops[e][-DMA_RING:])
        for e in ENGS:
            self.op(e, lambda g: g.nop(), extra_deps=last)
        self.bufs = {}

    def finalize(self):
        for e in ENGS:
            for o in self.ops[e]:
                for d in o.deps:
                    if d.dma:
                        continue
                    if d.eng == 'pe' and o.eng == 'pe' and not o.dma:
                        continue
                    d.signal = True
        for e in ENGS:
            c = 0
            for o in self.ops[e]:
                if o.dma:
                    continue
                if o.signal:
                    c += 1
                    o.semkey = ('eng', e)
                    o.semval = c
        for e in ENGS:
            waited = {}
            for o in self.ops[e]:
                need = {}
                for d in o.deps:
                    if (not d.dma) and d.eng == 'pe' and o.eng == 'pe' and not o.dma:
                        continue
                    if d.semval > need.get(d.semkey, 0):
                        need[d.semkey] = d.semval
                for k, v in need.items():
                    if waited.get(k, 0) < v:
                        waited[k] = v
                        o.waits.append((k, v))

    def run(self, nc):
        self.finalize()
        keys = set()
        for e in ENGS:
            for o in self.ops[e]:
                if o.semkey is not None and (o.signal or o.dma):
                    keys.add(o.semkey)
        keys = sorted(keys, key=str)
        self.stats = {e: (len(self.ops[e]), max([o.semval for o in self.ops[e] if not o.dma] + [0])) for e in ENGS}
        with contextlib.ExitStack() as st:
            sems = {}
            for i, k in enumerate(keys):
                sems[k] = st.enter_context(nc.semaphore('s%d' % i))
            block = st.enter_context(nc.Block())

            def replay(eng_name):
                def body(e):
                    for o in self.ops[eng_name]:
                        for (k, v) in o.waits:
                            e.wait_ge(sems[k], v)
                        inst = o.fn(e)
                        if o.dma:
                            inst.then_inc(sems[o.semkey], 16)
                        elif o.signal:
                            inst.then_inc(sems[o.semkey], 1)
                return body

            block.tensor(replay('pe'))
            block.scalar(replay('act'))
            block.vector(replay('dve'))
            block.gpsimd(replay('pool'))
            block.sync(replay('sp'))


class Cfg:
    def __init__(self, depth=4, t_lat=2048, layers=None, first=True, final=True):
        self.D = 1024
        self.DC = 8
        self.TC = 256
        self.TL = t_lat
        self.T = self.TC + self.TL
        self.DEPTH = depth
        self.layers = list(range(depth)) if layers is None else layers
        self.first = first
        self.final = final
        self.NE = 32
        self.skip = set()
        self.EPS = 1e-6
        self.groups = [(0, 256, 1, 0)]
        for k in range(self.TL // 512):
            self.groups.append((256 + 512 * k, 512, 0, k + 1))
        self.NT = self.T // 128
        self.BLK = 256
        self.NB = -(-(2 * self.T + 32 * (self.BLK - 1)) // self.BLK)
        d = depth
        self.V_C = 0
        self.V_CC = 8
        self.V_FNW = 16
        self.V_LB = 24
        self.V_L0 = 24 + 2 * d * 4
        self.V_LN = 81
        self.NV = self.V_L0 + d * self.V_LN


def build_program(cfg):
    nc = bass.Bass("TRN2", target_bir_lowering=False)
    T, TC, TL, DC, DEPTH = cfg.T, cfg.TC, cfg.TL, cfg.DC, cfg.DEPTH
    xT_d = nc.dram_tensor("xT", [128, DC * T], F32, kind="ExternalInput").ap()
    vecs_d = nc.dram_tensor("vecs", [128, cfg.NV], F32, kind="ExternalInput").ap()
    wr_d = nc.dram_tensor("wr", [128, DEPTH * DC * 36], F32, kind="ExternalInput").ap()
    br_d = nc.dram_tensor("br", [1, DEPTH * 36], F32, kind="ExternalInput").ap()
    wada_d = nc.dram_tensor("w_ada", [DEPTH, 1024, 6144], F32, kind="ExternalInput").ap()
    win_d = nc.dram_tensor("w_in", [DEPTH, 1024, 4096], F32, kind="ExternalInput").ap()
    wout_d = nc.dram_tensor("w_out", [DEPTH, 1024, 1024], F32, kind="ExternalInput").ap()
    wgu_d = nc.dram_tensor("w_gu", [DEPTH, cfg.NE, 1024, 1024], F32, kind="ExternalInput").ap()
    wdn_d = nc.dram_tensor("w_dn", [DEPTH, cfg.NE, 512, 1024], F32, kind="ExternalInput").ap()
    outT_d = nc.dram_tensor("outT", [128, DC * TL], F32, kind="ExternalOutput").ap()
    xs_d = nc.dram_tensor("xs_scr", [cfg.NB * cfg.BLK, 1024], F32, kind="Internal").ap()
    ys_d = nc.dram_tensor("ys_scr", [cfg.NB * cfg.BLK, 1024], F32, kind="Internal").ap()
    xT_v = xT_d.rearrange("p (c t) -> p c t", c=DC)
    outT_v = outT_d.rearrange("p (c t) -> p c t", c=DC)

    S = Sched()
    uid = [0]

    with contextlib.ExitStack() as top:
        def sb(stack, name, shape, dt=F32):
            uid[0] += 1
            return stack.enter_context(nc.sbuf_tensor("%s_%d" % (name, uid[0]), shape, dt))

        XT = sb(top, "XT", [128, DC, T])
        hT = sb(top, "hT", [128, DC, T], BF16)
        G = sb(top, "G", [128, cfg.NT, 32])
        vecs = sb(top, "vecs", [128, cfg.NV])
        wr = sb(top, "wr", [128, DEPTH, DC, 36])
        br = sb(top, "br", [1, DEPTH * 36])
        ident = sb(top, "ident", [128, 128])
        ones = sb(top, "ones", [128, 128])
        ones1 = sb(top, "ones1", [1, 128])
        rmask = sb(top, "rmask", [128, 512], BF16)
        maskF = sb(top, "maskF", [32, 4, 32])
        maskB = sb(top, "maskB", [32, 4, 32])
        scT = sb(top, "scT", [128, DC, 2])
        modT = sb(top, "modT", [128, DEPTH, 48, 2])
        A1 = sb(top, "A1", [128, DEPTH, DC, 2])
        A2 = sb(top, "A2", [128, DEPTH, DC, 2])
        lbT = sb(top, "lbT", [128, 2 * DEPTH * 4])
        omlT = sb(top, "omlT", [128, 2 * DEPTH * 4])
        I32 = mybir.dt.int32
        NB = cfg.NB
        SEL = sb(top, "SEL", [128, cfg.NT, 32])
        RK = sb(top, "RK", [128, cfg.NT, 32])
        IDX = sb(top, "IDX", [128, cfg.NT, 2], I32)
        GHL = sb(top, "GHL", [128, cfg.NT, 2])
        IDXW = sb(top, "IDXW", [128, NB], I32)
        PIDXi = sb(top, "PIDXi", [128, 1], I32)
        PIDX = sb(top, "PIDX", [128, 1])
        Lst = sb(top, "Lst", [128, 128])
        THR = sb(top, "THR", [128, 18])
        JR = sb(top, "JR", [128, NB])
        c128 = sb(top, "c128", [128, NB])
        pb = [top.enter_context(nc.psum_tensor("pb%d" % i, [128, 512], F32)) for i in range(8)]

        tile_gi = {}
        for (t0_, n_, sidx_, gi_) in cfg.groups:
            for ti_ in range(n_ // 128):
                tile_gi[(t0_ + ti_ * 128) // 128] = (gi_, sidx_)
        tile_gi = {k_: v_ for k_, v_ in tile_gi.items()}

        def XK(gi):
            return ('XT', gi)

        def HK(gi):
            return ('hT', gi)

        def mm(out, lhsT, rhs, start, stop, r, w):
            return S.op('pe', lambda e: e.matmul(out, lhsT, rhs, start=start, stop=stop), r=r, w=w)

        def tr(out, in_, r, w):
            return S.op('pe', lambda e: e.transpose(out, in_, ident[:in_.shape[0], :in_.shape[0]]), r=list(r) + [ident], w=w)

        def act(out, in_, func, r, w, bias=None, scale=None, accum=None):
            kw = {}
            if bias is not None:
                kw['bias'] = bias
            if scale is not None:
                kw['scale'] = scale
            if accum is not None:
                kw['accum_out'] = accum
            return S.op('act', lambda e: e.activation(out=out, in_=in_, func=func, **kw), r=r, w=w)

        def tt(eng, out, in0, in1, op, r, w):
            return S.op(eng, lambda e: e.tensor_tensor(out=out, in0=in0, in1=in1, op=op), r=r, w=w)

        def tsc(eng, out, in0, s1, s2, op0, op1, r, w):
            if op1 is None:
                return S.op(eng, lambda e: e.tensor_scalar(out=out, in0=in0, scalar1=s1, scalar2=None, op0=op0), r=r, w=w)
            return S.op(eng, lambda e: e.tensor_scalar(out=out, in0=in0, scalar1=s1, scalar2=s2, op0=op0, op1=op1), r=r, w=w)

        def stt(out, in0, scalar, in1, op0, op1, r, w):
            return S.op('dve', lambda e: e.scalar_tensor_tensor(out=out, in0=in0, scalar=scalar, in1=in1, op0=op0, op1=op1), r=r, w=w)

        def cp(eng, out, in_, r, w):
            return S.op(eng, lambda e: e.tensor_copy(out=out, in_=in_), r=r, w=w)

        def recip(out, in_, r, w):
            return S.op('dve', lambda e: e.reciprocal(out=out, in_=in_), r=r, w=w)

        def memset(eng, ap, val, w):
            return S.op(eng, lambda e: e.memset(ap, val), w=w)

        def dma(eng, out, in_, r, w):
            return S.op(eng, lambda e: e.dma_start(out=out, in_=in_), r=r, w=w, dma=True)

        def V(col, n=1):
            return vecs[:, col:col + n]

        memset('pool', ident[:], 0.0, [ident])
        S.op('pool', lambda e: e.affine_select(out=ident[:], in_=ident[:], compare_op=ALU.not_equal, fill=1.0,
                                               base=0, pattern=[[-1, 128]], channel_multiplier=1), r=[ident], w=[ident])
        memset('pool', ones[:], 1.0, [ones])
        memset('pool', ones1[:], 1.0, [ones1])
        memset('pool', rmask[:], 1.0, [rmask])
        S.op('pool', lambda e: e.memset(rmask[:].rearrange("p (a b) -> p a b", b=32)[:, :, 0:1], 0.0), r=[rmask], w=[rmask])
        memset('pool', maskF[:], 1.0, [maskF])
        memset('pool', maskB[:], 1.0, [maskB])
        S.op('pool', lambda e: e.affine_select(out=maskF[:], in_=maskF[:], compare_op=ALU.is_ge, fill=0.0,
                                               base=0, pattern=[[0, 4], [1, 32]], channel_multiplier=-1), r=[maskF], w=[maskF])
        S.op('pool', lambda e: e.affine_select(out=maskB[:], in_=maskB[:], compare_op=ALU.is_ge, fill=0.0,
                                               base=0, pattern=[[0, 4], [-1, 32]], channel_multiplier=1), r=[maskB], w=[maskB])

        S.op('pool', lambda e: e.iota(PIDXi[:], pattern=[[0, 1]], base=0, channel_multiplier=1), w=[PIDXi])
        cp('dve', PIDX[:], PIDXi[:], [PIDXi], [PIDX])
        memset('pool', Lst[:], 1.0, [Lst])
        S.op('pool', lambda e: e.affine_select(out=Lst[:], in_=Lst[:], compare_op=ALU.is_ge, fill=0.0,
                                               base=-1, pattern=[[1, 128]], channel_multiplier=-1), r=[Lst], w=[Lst])
        memset('pool', c128[:], float(cfg.BLK), [c128])
        S.op('dve', lambda e: e.tensor_tensor_scan(out=JR[:], data0=ones[:, :NB], data1=c128[:], initial=-float(cfg.BLK),
                                                   op0=ALU.mult, op1=ALU.add), r=[ones, c128], w=[JR])
        cp('dve', THR[:], JR[:, 0:18], [JR], [THR])

        dma('sp', vecs[:], vecs_d, [], [vecs])
        dma('sp', wr[:].rearrange("p l c n -> p (l c n)"), wr_d, [], [wr])
        dma('sp', br[:], br_d, [], [br])
        for (t0, n, sidx, gi) in cfg.groups:
            dma('sp', XT[:, :, t0:t0 + n], xT_v[:, :, t0:t0 + n], [], [XK(gi)])

        act(scT[:, :, 0], V(cfg.V_C, 8), AF.Silu, [vecs], [scT])
        act(scT[:, :, 1], V(cfg.V_CC, 8), AF.Silu, [vecs], [scT])
        with contextlib.ExitStack() as ph:
            nlb = 2 * DEPTH * 4
            E = sb(ph, "lbE", [128, nlb])
            sE = sb(ph, "lbS", [128, 8])
            rE = sb(ph, "lbR", [128, 8])
            act(E[:], V(cfg.V_LB, nlb), AF.Exp, [vecs], [E])
            E3 = E[:].rearrange("p (d l h) -> p d l h", d=2, l=DEPTH)
            sE2 = sE[:].rearrange("p (d h) -> p d h", d=2)
            cp('dve', sE2, E3[:, :, 0, :], [E], [sE])
            for l in range(1, DEPTH):
                tt('dve', sE2, sE2, E3[:, :, l, :], ALU.add, [sE, E], [sE])
            recip(rE[:], sE[:], [sE], [rE])
            rE2 = rE[:].rearrange("p (d h) -> p d h", d=2)
            lb3 = lbT[:].rearrange("p (d l h) -> p d l h", d=2, l=DEPTH)
            memset('dve', lbT[:], 0.0, [lbT])
            for l in range(1, DEPTH):
                tt('dve', E3[:, :, l, :], E3[:, :, l, :], rE2, ALU.mult, [E, rE], [E])
                tt('dve', lb3[:, :, l, :], lb3[:, :, l - 1, :], E3[:, :, l, :], ALU.add, [lbT, E], [lbT])
            tsc('dve', omlT[:], lbT[:], -1.0, 1.0, ALU.mult, ALU.add, [lbT], [omlT])

            wa = [sb(ph, "wada%d" % i, [128, DC, 512]) for i in range(2)]
            k = 0
            for l in cfg.layers:
                voff = cfg.V_L0 + l * cfg.V_LN
                for jb in range(12):
                    wt = wa[k % 2]
                    k += 1
                    dma('sp', wt[:], wada_d[l, :, jb * 512:(jb + 1) * 512].rearrange("(c p) n -> p c n", p=128), [], [wt])
                    pm = pb[jb % 2]
                    for jj in range(4):
                        for c in range(DC):
                            mm(pm[:, jj * 2:jj * 2 + 2], wt[:, c, jj * 128:(jj + 1) * 128], scT[:, c, :], c == 0, c == DC - 1,
                               [wt, scT], [pm])
                    for s in range(2):
                        tt('dve', modT[:, l, jb * 4:jb * 4 + 4, s], pm[:, 0:8].rearrange("p (j s) -> p j s", s=2)[:, :, s],
                           V(voff + 16 + jb * 4, 4), ALU.add, [pm, vecs], [modT])
                for s in range(2):
                    stt(A1[:, l, :, s], modT[:, l, 8:16, s], 1.0, V(voff + 0, 8), ALU.add, ALU.mult, [modT, vecs], [A1])
                    stt(A2[:, l, :, s], modT[:, l, 32:40, s], 1.0, V(voff + 8, 8), ALU.add, ALU.mult, [modT, vecs], [A2])
        S.barrier()

        def norm_modulate(l, which, router):
            A = A1 if which == 0 else A2
            sh0 = 0 if which == 0 else 24
            with contextlib.ExitStack() as ph:
                sq = [sb(ph, "nsq%d" % i, [128, 512]) for i in range(2)]
                rs = [sb(ph, "nrs%d" % i, [128, 512]) for i in range(2)]
                tmp = [sb(ph, "ntmp%d" % i, [128, 512]) for i in range(2)]
                if router:
                    h2f = [sb(ph, "h2f%d" % i, [128, DC, 512]) for i in range(2)]
                    rt = {nm: [sb(ph, "rt_%s%d" % (nm, i), shp) for i in range(2)] for nm, shp in
                          [("lg", [128, 36]), ("gm", [128, 1]), ("ngm", [128, 1]), ("gmask", [128, 4]), ("ge", [128, 4]),
                           ("gs", [128, 1]), ("pen", [128, 4]), ("el", [128, 32]), ("t8", [128, 8]), ("nm1", [128, 1]),
                           ("sel", [128, 32]), ("ex", [128, 32]), ("gx", [128, 32]), ("den", [128, 1]), ("pr", [128, 1]),
                           ("rp", [128, 1])]}
                kk = 0
                tl = 0
                cpar = [0]
                if router:
                    cums = [sb(ph, "cums%d" % i, [128, 32]) for i in range(2)]
                    memset('dve', cums[0][:], 0.0, [cums[0]])
                for (t0, n, sidx, gi) in cfg.groups:
                    pss = pb[gi % 2]
                    for c in range(DC):
                        q = sq[kk % 2]
                        kk += 1
                        act(q[:, :n], XT[:, c, t0:t0 + n], AF.Square, [XK(gi)], [q])
                        mm(pss[:, :n], ones[:], q[:, :n], c == 0, c == DC - 1, [ones, q], [pss])
                    r_ = rs[gi % 2]
                    act(r_[:, :n], pss[:, :n], AF.Sqrt, [pss], [r_], bias=cfg.EPS, scale=1.0 / 1024)
                    recip(r_[:, :n], r_[:, :n], [r_], [r_])
                    for c in range(DC):
                        tm = tmp[kk % 2]
                        kk += 1
                        tt('dve', tm[:, :n], XT[:, c, t0:t0 + n], r_[:, :n], ALU.mult, [XK(gi), r_], [tm])
                        if router:
                            hf = h2f[gi % 2]
                            act(hf[:, c, :n], tm[:, :n], AF.Identity, [tm, A, modT], [hf],
                                bias=modT[:, l, sh0 + c, sidx:sidx + 1], scale=A[:, l, c, sidx:sidx + 1])
                            cp('pool', hT[:, c, t0:t0 + n], hf[:, c, :n], [hf], [HK(gi)])
                        else:
                            act(hT[:, c, t0:t0 + n], tm[:, :n], AF.Identity, [tm, A, modT], [HK(gi)],
                                bias=modT[:, l, sh0 + c, sidx:sidx + 1], scale=A[:, l, c, sidx:sidx + 1])
                    if router:
                        hf = h2f[gi % 2]
                        for ti in range(n // 128):
                            tg = (t0 + ti * 128) // 128
                            R = {nm: v[tl % 2] for nm, v in rt.items()}
                            pl = pb[2 + tl % 2]
                            tl += 1
                            for c in range(DC):
                                mm(pl[:, 0:36], hf[:, c, ti * 128:(ti + 1) * 128], wr[:, l, c, :], c == 0, False, [hf, wr], [pl])
                            mm(pl[:, 0:36], ones1[:], br[:, l * 36:(l + 1) * 36], False, True, [ones1, br], [pl])
                            lg = R["lg"]
                            cp('dve', lg[:], pl[:, 0:36], [pl], [lg])
                            S.op('dve', lambda e, o=R["gm"], i=lg: e.tensor_reduce(out=o[:], in_=i[:, 0:4], axis=AX.X, op=ALU.max),
                                 r=[lg], w=[R["gm"]])
                            tsc('dve', R["ngm"][:], R["gm"][:], -1.0, None, ALU.mult, None, [R["gm"]], [R["ngm"]])
                            tsc('dve', R["gmask"][:], lg[:, 0:4], R["gm"][:], None, ALU.is_equal, None, [lg, R["gm"]], [R["gmask"]])
                            act(R["ge"][:], lg[:, 0:4], AF.Exp, [lg, R["ngm"]], [R["ge"], R["gs"]], bias=R["ngm"][:], scale=1.0,
                                accum=R["gs"][:])
                            tsc('dve', R["pen"][:], R["gmask"][:], 1e30, -1e30, ALU.mult, ALU.add, [R["gmask"]], [R["pen"]])
                            tt('dve', R["el"][:].rearrange("p (g e) -> p g e", g=4), lg[:, 4:36].rearrange("p (g e) -> p g e", g=4),
                               R["pen"][:].unsqueeze(2).to_broadcast([128, 4, 8]), ALU.add, [lg, R["pen"]], [R["el"]])
                            S.op('dve', lambda e, o=R["t8"], i=R["el"]: e.max(out=o[:], in_=i[:]), r=[R["el"]], w=[R["t8"]])
                            tsc('dve', R["nm1"][:], R["t8"][:, 0:1], -1.0, None, ALU.mult, None, [R["t8"]], [R["nm1"]])
                            tsc('dve', R["sel"][:], R["el"][:], R["t8"][:, 1:2], None, ALU.is_ge, None, [R["el"], R["t8"]], [R["sel"]])
                            act(R["ex"][:], R["el"][:], AF.Exp, [R["el"], R["nm1"]], [R["ex"]], bias=R["nm1"][:], scale=1.0)
                            tt('dve', R["gx"][:], R["sel"][:], R["ex"][:], ALU.mult, [R["sel"], R["ex"]], [R["gx"]])
                            S.op('dve', lambda e, o=R["den"], i=R["gx"]: e.tensor_reduce(out=o[:], in_=i[:], axis=AX.X, op=ALU.add),
                                 r=[R["gx"]], w=[R["den"]])
                            tt('dve', R["pr"][:], R["den"][:], R["gs"][:], ALU.mult, [R["den"], R["gs"]], [R["pr"]])
                            recip(R["rp"][:], R["pr"][:], [R["pr"]], [R["rp"]])
                            tsc('dve', G[:, tg, :], R["gx"][:], R["rp"][:], None, ALU.mult, None, [R["gx"], R["rp"]], [('G', gi)])
                            cp('dve', SEL[:, tg, :], R["sel"][:], [R["sel"]], [('SEL', tg)])
                            prk = pb[4 + tl % 2]
                            mm(prk[:, 0:32], Lst[:], R["sel"][:], True, False, [Lst, R["sel"]], [prk])
                            mm(prk[:, 0:32], ones[:], cums[cpar[0]][:], False, True, [ones, cums[cpar[0]]], [prk])
                            cp('dve', RK[:, tg, :], prk[:, 0:32], [prk], [('RK', tg)])
                            tt('dve', cums[1 - cpar[0]][:], cums[cpar[0]][:], R["sel"][:], ALU.add, [cums[cpar[0]], R["sel"]], [cums[1 - cpar[0]]])
                            cpar[0] = 1 - cpar[0]
                if router:
                    CNT = sb(ph, "CNT", [128, 32])
                    cmp18 = sb(ph, "cmp18", [128, 32, 18])
                    NBLK = sb(ph, "NBLK", [128, 32])
                    PADD = sb(ph, "PADD", [128, 32])
                    PEND = sb(ph, "PEND", [128, 32])
                    PST = sb(ph, "PST", [128, 32])
                    cmpB = sb(ph, "cmpB", [128, NB, 32])
                    BEf = sb(ph, "BEf", [128, NB])
                    pc = pb[6]
                    mm(pc[:, 0:32], ones[:], cums[cpar[0]][:], True, True, [ones, cums[cpar[0]]], [pc])
                    cp('dve', CNT[:], pc[:, 0:32], [pc], [CNT])
                    tt('dve', cmp18[:], CNT[:].unsqueeze(2).to_broadcast([128, 32, 18]), THR[:].unsqueeze(1).to_broadcast([128, 32, 18]),
                       ALU.is_gt, [CNT, THR], [cmp18])
                    S.op('dve', lambda e: e.tensor_reduce(out=NBLK[:], in_=cmp18[:], axis=AX.X, op=ALU.add), r=[cmp18], w=[NBLK])
                    tsc('dve', PADD[:], NBLK[:], float(cfg.BLK), None, ALU.mult, None, [NBLK], [PADD])
                    S.op('dve', lambda e: e.tensor_tensor_scan(out=PEND[:], data0=ones[:, 0:32], data1=PADD[:], initial=0.0,
                                                               op0=ALU.mult, op1=ALU.add), r=[ones, PADD], w=[PEND])
                    tt('dve', PST[:], PEND[:], PADD[:], ALU.subtract, [PEND, PADD], [PST])
                    tt('dve', cmpB[:], PEND[:].unsqueeze(1).to_broadcast([128, NB, 32]), JR[:].unsqueeze(2).to_broadcast([128, NB, 32]),
                       ALU.is_le, [PEND, JR], [cmpB])
                    S.op('dve', lambda e: e.tensor_reduce(out=BEf[:], in_=cmpB[:], axis=AX.X, op=ALU.add), r=[cmpB], w=[BEf])
                    tsc('dve', BEf[:], BEf[:], 31.0, float(32 * l), ALU.min, ALU.add, [BEf], [BEf])
                    tsc('dve', IDXW[:], BEf[:], 128.0, PIDX[:], ALU.mult, ALU.add, [BEf, PIDX], [IDXW])
                    pp = {nm: [sb(ph, "pp_%s%d" % (nm, i), shp) for i in range(2)] for nm, shp in
                          [("pos", [128, 32]), ("t8", [128, 8]), ("eq", [128, 32]), ("pr", [128, 32])]}
                    for tg in range(cfg.NT):
                        Q = {nm: v[tg % 2] for nm, v in pp.items()}
                        gi_t = tile_gi[tg][0]
                        tt('dve', Q["pos"][:], RK[:, tg, :], PST[:], ALU.add, [('RK', tg), PST], [Q["pos"]])
                        stt(Q["pos"][:], Q["pos"][:], 1.0, SEL[:, tg, :], ALU.add, ALU.mult, [Q["pos"], ('SEL', tg)], [Q["pos"]])
                        S.op('dve', lambda e, o=Q["t8"], i=Q["pos"]: e.max(out=o[:], in_=i[:]), r=[Q["pos"]], w=[Q["t8"]])
                        tsc('dve', IDX[:, tg, :], Q["t8"][:, 0:2], -1.0, None, ALU.add, None, [Q["t8"]], [('IDX', tg)])
                        for k in range(2):
                            tsc('dve', Q["eq"][:], Q["pos"][:], Q["t8"][:, k:k + 1], None, ALU.is_equal, None, [Q["pos"], Q["t8"]], [Q["eq"]])
                            tt('dve', Q["pr"][:], Q["eq"][:], G[:, tg, :], ALU.mult, [Q["eq"], ('G', gi_t)], [Q["pr"]])
                            S.op('dve', lambda e, o=GHL[:, tg, k:k + 1], i=Q["pr"]: e.tensor_reduce(out=o, in_=i[:], axis=AX.X, op=ALU.add),
                                 r=[Q["pr"]], w=[('GHL', tg)])
            S.barrier()

        def out_proj(l, Y, nk, k0, ph):
            Wo = sb(ph, "Wo", [128, nk, 1024], BF16)
            dma('pool', Wo[:], wout_d[l, k0 * 128:(k0 + nk) * 128, :].rearrange("(c p) n -> p c n", p=128), [], [Wo])
            kk = 0
            for (t0, n, sidx, gi) in cfg.groups:
                for j in range(DC):
                    po = pb[4 + kk % 4]
                    kk += 1
                    for c in range(nk):
                        rhs = Y[:, c, t0:t0 + n] if nk > 1 else Y[:, t0:t0 + n]
                        mm(po[:, :n], Wo[:, c, j * 128:(j + 1) * 128], rhs, c == 0, c == nk - 1, [Wo, Y], [po])
                    stt(XT[:, j, t0:t0 + n], po[:, :n], modT[:, l, 16 + j, sidx:sidx + 1], XT[:, j, t0:t0 + n],
                        ALU.mult, ALU.add, [po, modT, XK(gi)], [XK(gi)])

        def load_win(ph, name, l, col):
            W = sb(ph, name, [128, DC, 128], BF16)
            dma('pool', W[:], win_d[l, :, col:col + 128].rearrange("(c p) n -> p c n", p=128), [], [W])
            return W

        def proj(W, ps, t0, n, gi):
            for c in range(DC):
                mm(ps[:, :n], W[:, c, :], hT[:, c, t0:t0 + n], c == 0, c == DC - 1, [W, HK(gi)], [ps])

        def conv_phase(l):
            voff = cfg.V_L0 + l * cfg.V_LN
            R_ = TL // 64
            with contextlib.ExitStack() as ph:
                Z = sb(ph, "Z", [128, 4, T], BF16)
                SS = sb(ph, "SS", [128, T])
                u = sb(ph, "u", [128, T])
                Bs = sb(ph, "Bs", [128, T])
                y = sb(ph, "y", [128, T])
                hv = [sb(ph, "hv%d" % i, [128, 512]) for i in range(2)]
                zs = [sb(ph, "zs%d" % i, [128, 512]) for i in range(2)]
                for cc in range(4):
                    with contextlib.ExitStack() as ph2:
                        WB = load_win(ph2, "WB", l, cc * 128)
                        WC = load_win(ph2, "WC", l, 512 + cc * 128)
                        WH = load_win(ph2, "WH", l, 1024 + cc * 128)
                        for (t0, n, sidx, gi) in cfg.groups:
                            pB, pC, pH = pb[0 + 4 * (gi % 2)], pb[1 + 4 * (gi % 2)], pb[2 + 4 * (gi % 2)]
                            proj(WB, pB, t0, n, gi)
                            proj(WC, pC, t0, n, gi)
                            proj(WH, pH, t0, n, gi)
                            h_ = hv[gi % 2]
                            act(h_[:, :n], pH[:, :n], AF.Copy, [pH], [h_])
                            act(Bs[:, t0:t0 + n], pB[:, :n], AF.Copy, [pB], [Bs])
                            tt('dve', u[:, t0:t0 + n], pC[:, :n], h_[:, :n], ALU.mult, [pC, h_], [u])
                        w0, w1, w2 = V(voff + 64 + 0 * 4 + cc), V(voff + 64 + 1 * 4 + cc), V(voff + 64 + 2 * 4 + cc)
                        act(y[:], u[:], AF.Identity, [u, vecs], [y], scale=w1)
                        stt(y[:, 1:TC], u[:, 0:TC - 1], w0, y[:, 1:TC], ALU.mult, ALU.add, [u, vecs, y], [y])
                        stt(y[:, 0:TC - 1], u[:, 1:TC], w2, y[:, 0:TC - 1], ALU.mult, ALU.add, [u, vecs, y], [y])
                        if cc < 2:
                            ul = u[:, TC:T].rearrange("p (r w) -> p r w", w=64)
                            yl = y[:, TC:T].rearrange("p (r w) -> p r w", w=64)
                            stt(yl[:, :, 1:64], ul[:, :, 0:63], w0, yl[:, :, 1:64], ALU.mult, ALU.add, [u, vecs, y], [y])
                            stt(yl[:, :, 0:63], ul[:, :, 1:64], w2, yl[:, :, 0:63], ALU.mult, ALU.add, [u, vecs, y], [y])
                        else:
                            stt(y[:, TC + 64:T], u[:, TC:T - 64], w0, y[:, TC + 64:T], ALU.mult, ALU.add, [u, vecs, y], [y])
                            stt(y[:, TC:T - 64], u[:, TC + 64:T], w2, y[:, TC:T - 64], ALU.mult, ALU.add, [u, vecs, y], [y])
                        tt('dve', y[:], y[:], Bs[:], ALU.mult, [y, Bs], [y])
                        act(Z[:, cc, :], y[:], AF.Copy, [y], [Z])
                        for (t0, n, sidx, gi) in cfg.groups:
                            z_ = zs[gi % 2]
                            pz = pb[3 + 4 * (gi % 2)]
                            act(z_[:, :n], y[:, t0:t0 + n], AF.Square, [y], [z_])
                            mm(pz[:, :n], ones[:], z_[:, :n], True, True, [ones, z_], [pz])
                            if cc == 0:
                                cp('dve', SS[:, t0:t0 + n], pz[:, :n], [pz], [SS])
                            else:
                                tt('dve', SS[:, t0:t0 + n], pz[:, :n], SS[:, t0:t0 + n], ALU.add, [pz, SS], [SS])
                    S.barrier()
                act(SS[:], SS[:], AF.Sqrt, [SS], [SS], bias=cfg.EPS, scale=1.0 / 512)
                recip(SS[:], SS[:], [SS], [SS])
                for cc in range(4):
                    stt(Z[:, cc, :], Z[:, cc, :], V(voff + 76 + cc), SS[:], ALU.mult, ALU.mult, [Z, vecs, SS], [Z])
                out_proj(l, Z, 4, 0, ph)
            S.barrier()

        def heads_phase(l):
            voff = cfg.V_L0 + l * cfg.V_LN
            hnw = V(voff + 80)
            lat = cfg.groups[1:]
            order = [cfg.groups, [cfg.groups[0]] + lat[::-1]]
            for hh in range(4):
                with contextlib.ExitStack() as ph:
                    Wq = load_win(ph, "Wq", l, 1536 + hh * 128)
                    Wf = [load_win(ph, "Wzf", l, 2048 + hh * 128), load_win(ph, "Wzb", l, 2560 + hh * 128)]
                    Wi = load_win(ph, "Wi", l, 3072 + hh * 128)
                    Wg = load_win(ph, "Wg", l, 3584 + hh * 128)
                    Of = sb(ph, "Of", [128, T])
                    Yh = sb(ph, "Yh", [128, T], BF16)
                    NS = 4
                    St = [sb(ph, "St%d" % i, [128, 128]) for i in range(NS)]
                    names = ["qs", "sg", "kk", "vs", "b", "eb", "enb", "ko", "gs"]
                    tmps = [{nm: sb(ph, "g%s%d" % (nm, i), [128, 512]) for nm in names} for i in range(2)]
                    dch = [sb(ph, "dch%d" % i, [128, 16]) for i in range(2)]
                    totc = [sb(ph, "totc%d" % i, [128, 16]) for i in range(2)]
                    koT = [sb(ph, "koT%d" % i, [32, 4, 128]) for i in range(2)]
                    vT = [sb(ph, "vT%d" % i, [32, 4, 128]) for i in range(2)]
                    PT = [sb(ph, "PT%d" % i, [32, 4, 32]) for i in range(2)]
                    osq = sb(ph, "osq", [128, 512])
                    ors = sb(ph, "ors", [128, 512])
                    gpar = 0
                    tpar = 0
                    for dirn in range(2):
                        lbc = (dirn * DEPTH + l) * 4 + hh
                        lb_ap, oml_ap = lbT[:, lbc:lbc + 1], omlT[:, lbc:lbc + 1]
                        cur = 0
                        memset('dve', St[0][:], 0.0, [St[0]])
                        msk = maskF if dirn == 0 else maskB
                        for (t0, n, sidx, gi) in order[dirn]:
                            tp = tmps[gpar % 2]
                            dc_ = dch[gpar % 2]
                            gpar += 1
                            nch = n // 32
                            pq, pz_, pi_, pg = pb[0], pb[1], pb[2], pb[3]
                            proj(Wq, pq, t0, n, gi)
                            proj(Wf[dirn], pz_, t0, n, gi)
                            proj(Wi, pi_, t0, n, gi)
                            act(tp["qs"][:, :n], pq[:, :n], AF.Silu, [pq], [tp["qs"]])
                            act(tp["sg"][:, :n], pz_[:, :n], AF.Sigmoid, [pz_], [tp["sg"]])
                            act(tp["vs"][:, :n], pi_[:, :n], AF.Copy, [pi_], [tp["vs"]])
                            if dirn == 1:
                                proj(Wg, pg, t0, n, gi)
                                act(tp["gs"][:, :n], pg[:, :n], AF.Silu, [pg], [tp["gs"]])
                            tc_ = totc[(gpar - 1) % 2]
                            tsc('dve', tp["sg"][:, :n], tp["sg"][:, :n], oml_ap, lb_ap, ALU.mult, ALU.add, [tp["sg"], omlT, lbT], [tp["sg"]])
                            tsc('dve', tp["kk"][:, :n], tp["sg"][:, :n], -1.0, 1.0, ALU.mult, ALU.add, [tp["sg"]], [tp["kk"]])
                            act(tp["sg"][:, :n], tp["sg"][:, :n], AF.Ln, [tp["sg"]], [tp["sg"]])
                            S.op('dve', lambda e, o=tp["b"], m=rmask, d1=tp["sg"], n=n: e.tensor_tensor_scan(
                                out=o[:, :n], data0=m[:, :n], data1=d1[:, :n], initial=0.0, op0=ALU.mult, op1=ALU.add),
                                r=[rmask, tp["sg"]], w=[tp["b"]])
                            b3 = tp["b"][:, :n].rearrange("p (a c) -> p a c", c=32)
                            cp('dve', tc_[:, :nch], b3[:, :, 31], [tp["b"]], [tc_])
                            act(dc_[:, :nch], tc_[:, :nch], AF.Exp, [tc_], [dc_])
                            bb = tp["b"]
                            if dirn == 1:
                                tt('dve', b3, b3, tc_[:, :nch].unsqueeze(2).to_broadcast([128, nch, 32]), ALU.subtract, [tp["b"], tc_], [tp["b"]])
                                tt('dve', bb[:, :n], tp["sg"][:, :n], bb[:, :n], ALU.subtract, [tp["sg"], bb], [bb])
                            act(tp["eb"][:, :n], bb[:, :n], AF.Exp, [bb], [tp["eb"]])
                            act(tp["enb"][:, :n], bb[:, :n], AF.Exp, [bb], [tp["enb"]], scale=-1.0)
                            tt('dve', tp["qs"][:, :n], tp["qs"][:, :n], tp["eb"][:, :n], ALU.mult, [tp["qs"], tp["eb"]], [tp["qs"]])
                            tt('dve', tp["kk"][:, :n], tp["kk"][:, :n], tp["enb"][:, :n], ALU.mult, [tp["kk"], tp["enb"]], [tp["kk"]])
                            tt('dve', tp["ko"][:, :n].rearrange("p (a c) -> p a c", c=32),
                               tp["kk"][:, :n].rearrange("p (a c) -> p a c", c=32),
                               dc_[:, :nch].unsqueeze(2).to_broadcast([128, nch, 32]), ALU.mult, [tp["kk"], dc_], [tp["ko"]])
                            tiles = list(range(n // 128))
                            chunks = [0, 1, 2, 3]
                            if dirn == 1:
                                tiles = tiles[::-1]
                                chunks = chunks[::-1]
                            for ti in tiles:
                                c0 = ti * 128
                                kT_, vT_, PT_ = koT[tpar % 2], vT[tpar % 2], PT[tpar % 2]
                                tpar += 1
                                pk, pv = pb[4], pb[5]
                                pk3 = pk[0:32, :].rearrange("p (j k) -> p j k", j=4)
                                pv3 = pv[0:32, :].rearrange("p (j k) -> p j k", j=4)
                                for j in range(4):
                                    tr(pk3[:, j, :], tp["ko"][:, c0 + 32 * j:c0 + 32 * j + 32], [tp["ko"]], [pk])
                                for j in range(4):
                                    tr(pv3[:, j, :], tp["vs"][:, c0 + 32 * j:c0 + 32 * j + 32], [tp["vs"]], [pv])
                                act(kT_[:], pk3, AF.Copy, [pk], [kT_])
                                cp('dve', vT_[:], pv3, [pv], [vT_])
                                par = tpar % 2
                                psc = pb[3][0:32, 256:384].rearrange("p (j k) -> p j k", j=4)
                                for j in range(4):
                                    cs = c0 + 32 * j
                                    mm(psc[:, j, :], tp["kk"][:, cs:cs + 32], tp["qs"][:, cs:cs + 32], True, True,
                                       [tp["kk"], tp["qs"]], [pb[3]])
                                tt('dve', PT_[:], psc, msk[:], ALU.mult, [pb[3], msk], [PT_])
                                po = pb[7][:, 256 * par:256 * par + 128]
                                for j in chunks:
                                    cs = c0 + 32 * j
                                    jg = cs // 32
                                    mm(pb[6][:, 128 * j:128 * j + 128], kT_[:, j, :], vT_[:, j, :], True, True, [kT_, vT_], [('pb6', j)])
                                    mm(po[:, 32 * j:32 * j + 32], St[cur][:], tp["qs"][:, cs:cs + 32], True, False, [St[cur], tp["qs"]], [('pb7', 'o', par)])
                                    mm(po[:, 32 * j:32 * j + 32], vT_[:, j, :], PT_[:, j, :], False, True, [vT_, PT_], [('pb7', 'o', par)])
                                    nxt = (cur + 1) % NS
                                    stt(St[nxt][:], St[cur][:], dc_[:, jg:jg + 1], pb[6][:, 128 * j:128 * j + 128], ALU.mult, ALU.add,
                                        [St[cur], dc_, ('pb6', j)], [St[nxt]])
                                    cur = nxt
                                if dirn == 0:
                                    act(Of[:, t0 + c0:t0 + c0 + 128], po[:, 0:128], AF.Copy, [('pb7', 'o', par)], [Of])
                                else:
                                    tt('dve', Of[:, t0 + c0:t0 + c0 + 128], po[:, 0:128], Of[:, t0 + c0:t0 + c0 + 128], ALU.add, [('pb7', 'o', par), Of], [Of])
                            if dirn == 1:
                                pn = pb[3]
                                act(osq[:, :n], Of[:, t0:t0 + n], AF.Square, [Of], [osq])
                                mm(pn[:, :n], ones[:], osq[:, :n], True, True, [ones, osq], [pn])
                                act(ors[:, :n], pn[:, :n], AF.Sqrt, [pn], [ors], bias=cfg.EPS, scale=1.0 / 128)
                                recip(ors[:, :n], ors[:, :n], [ors], [ors])
                                stt(osq[:, :n], Of[:, t0:t0 + n], hnw, ors[:, :n], ALU.mult, ALU.mult, [Of, vecs, ors], [osq])
                                tt('dve', Yh[:, t0:t0 + n], osq[:, :n], tp["gs"][:, :n], ALU.mult, [osq, tp["gs"]], [Yh])
                    out_proj(l, Yh, 1, 4 + hh, ph)
                S.barrier()

        def moe_phase_dense(l):
            with contextlib.ExitStack() as ph:
                WA = [sb(ph, "WA%d" % i, [128, DC, 512], BF16) for i in range(2)]
                WU = [sb(ph, "WU%d" % i, [128, DC, 512], BF16) for i in range(2)]
                WD = [sb(ph, "WD%d" % i, [128, 4, 1024], BF16) for i in range(2)]
                gbc = [sb(ph, "gbc%d" % i, [128, 512], BF16) for i in range(2)]
                sa = [sb(ph, "sa%d" % i, [128, 512]) for i in range(2)]
                t1 = [sb(ph, "t1%d" % i, [128, 512], BF16) for i in range(2)]
                hm = [sb(ph, "hm%d" % i, [128, 4, 512], BF16) for i in range(2)]

                def load(e):
                    s = e % 2
                    dma('pool', WA[s][:], wgu_d[l, e, :, 0:512].rearrange("(c p) n -> p c n", p=128), [], [WA[s]])
                    dma('pool', WU[s][:], wgu_d[l, e, :, 512:1024].rearrange("(c p) n -> p c n", p=128), [], [WU[s]])
                    dma('pool', WD[s][:], wdn_d[l, e, :, :].rearrange("(c p) n -> p c n", p=128), [], [WD[s]])

                load(0)
                kk = 0
                k2 = 0
                for e in range(cfg.NE):
                    if e + 1 < cfg.NE:
                        load(e + 1)
                    s = e % 2
                    for (t0, n, sidx, gi) in cfg.groups:
                        pg = pb[0]
                        for ti in range(n // 128):
                            tg = (t0 + ti * 128) // 128
                            mm(pg[:, ti * 128:(ti + 1) * 128], G[:, tg, e:e + 1].to_broadcast([128, 128]), ident[:], True, True,
                               [('G', gi), ident], [pg])
                        gb = gbc[kk % 2]
                        hm_ = hm[kk % 2]
                        kk += 1
                        act(gb[:, :n], pg[:, :n], AF.Copy, [pg], [gb])
                        for hc in range(4):
                            pa, pu = pb[1 + 2 * (k2 % 2)], pb[2 + 2 * (k2 % 2)]
                            sa_, t1_ = sa[k2 % 2], t1[k2 % 2]
                            k2 += 1
                            for c in range(DC):
                                mm(pa[:, :n], WA[s][:, c, hc * 128:(hc + 1) * 128], hT[:, c, t0:t0 + n], c == 0, c == DC - 1,
                                   [WA[s], HK(gi)], [pa])
                            for c in range(DC):
                                mm(pu[:, :n], WU[s][:, c, hc * 128:(hc + 1) * 128], hT[:, c, t0:t0 + n], c == 0, c == DC - 1,
                                   [WU[s], HK(gi)], [pu])
                            act(sa_[:, :n], pa[:, :n], AF.Silu, [pa], [sa_])
                            tt('dve', t1_[:, :n], sa_[:, :n], pu[:, :n], ALU.mult, [sa_, pu], [t1_])
                            tt('pool', hm_[:, hc, :n], t1_[:, :n], gb[:, :n], ALU.mult, [t1_, gb], [hm_])
                        for j in range(DC):
                            py = pb[5 + j % 3]
                            for hc in range(4):
                                mm(py[:, :n], WD[s][:, hc, j * 128:(j + 1) * 128], hm_[:, hc, :n], hc == 0, hc == 3, [WD[s], hm_], [py])
                            stt(XT[:, j, t0:t0 + n], py[:, :n], modT[:, l, 40 + j, sidx:sidx + 1], XT[:, j, t0:t0 + n],
                                ALU.mult, ALU.add, [py, modT, XK(gi)], [XK(gi)])
            S.barrier()

        def moe_phase(l):
            IOA = bass.IndirectOffsetOnAxis
            with contextlib.ExitStack() as ph:
                WAU = [sb(ph, "WAU%d" % i, [128, DC, 1024], BF16) for i in range(2)]
                WD = [sb(ph, "WD%d" % i, [128, 4, 1024], BF16) for i in range(2)]
                h32 = sb(ph, "h32", [128, DC, 128])
                xb = [sb(ph, "xb%d" % i, [128, 1024]) for i in range(2)]
                yb = [sb(ph, "yb%d" % i, [128, 1024]) for i in range(2)]
                xbT = [sb(ph, "xbT%d" % i, [128, DC, 128], BF16) for i in range(2)]
                sa = [sb(ph, "sa0", [128, 512])] * 2
                hm = [sb(ph, "hm%d" % i, [128, 4, 128], BF16) for i in range(2)]
                pT = [pb[0], pb[1]]

                wgu2 = wgu_d.rearrange("l e (p c) n -> (l e p) (c n)", c=8)
                wdn2 = wdn_d.rearrange("l e (p c) n -> (l e p) (c n)", c=4)

                def load(j):
                    s_ = j % 2
                    for q in range(4):
                        S.op('pool', lambda e, j=j, q=q, s_=s_: e.indirect_dma_start(
                            out=WAU[s_][:, 2 * q:2 * q + 2, :].rearrange("p a n -> p (a n)"), out_offset=None, in_=wgu2,
                            in_offset=IOA(ap=IDXW[:, j:j + 1], axis=0), element_offset=q * 2048), r=[IDXW], w=[('WAU', s_, q)], dma=True)
                    for q in range(2):
                        S.op('pool', lambda e, j=j, q=q, s_=s_: e.indirect_dma_start(
                            out=WD[s_][:, 2 * q:2 * q + 2, :].rearrange("p a n -> p (a n)"), out_offset=None, in_=wdn2,
                            in_offset=IOA(ap=IDXW[:, j:j + 1], axis=0), element_offset=q * 2048), r=[IDXW], w=[('WD', s_, q)], dma=True)

                load(0)
                scat = []
                for tg in range(cfg.NT):
                    gi, sidx = tile_gi[tg]
                    act(h32[:], hT[:, :, tg * 128:(tg + 1) * 128], AF.Copy, [HK(gi)], [h32])
                    for c in range(DC):
                        tr(pT[c // 4][:, (c % 4) * 128:(c % 4 + 1) * 128], h32[:, c, :], [h32], [pT[c // 4]])
                    x_ = xb[tg % 2]
                    act(x_[:, 0:512], pT[0][:, :], AF.Copy, [pT[0]], [x_])
                    cp('dve', x_[:, 512:1024], pT[1][:, :], [pT[1]], [x_])
                    for k in range(2):
                        scat.append(S.op('pool', lambda e, x_=x_, tg=tg, k=k: e.indirect_dma_start(
                            out=xs_d[:, :], out_offset=IOA(ap=IDX[:, tg, k:k + 1], axis=0), in_=x_[:, :], in_offset=None),
                            r=[x_, ('IDX', tg)], w=[('xs', tg, k)], dma=True))
                stores = []
                RT = cfg.BLK // 128

                def prefetch(rt):
                    s_ = rt % 2
                    S.op('sp', lambda e: e.dma_start(out=xb[s_][:], in_=xs_d[rt * 128:(rt + 1) * 128, :]), r=[], w=[xb[s_]],
                         dma=True, extra_deps=scat)

                def stage_a(rt):
                    w_, s_ = (rt // RT) % 2, rt % 2
                    for c in range(DC):
                        tr(pT[c // 4][:, (c % 4) * 128:(c % 4 + 1) * 128], xb[s_][:, :].rearrange("r (p c) -> r c p", c=8)[:, c, :], [xb[s_]], [pT[c // 4]])
                    act(xbT[s_][:, 0:4, :], pT[0][:, :].rearrange("p (c r) -> p c r", c=4), AF.Copy, [pT[0]], [xbT[s_]])
                    cp('dve', xbT[s_][:, 4:8, :], pT[1][:, :].rearrange("p (c r) -> p c r", c=4), [pT[1]], [xbT[s_]])
                    pa, pu = pb[2 + 2 * (rt % 2)], pb[3 + 2 * (rt % 2)]
                    for hc in range(4):
                        for c in range(DC):
                            mm(pa[:, hc * 128:(hc + 1) * 128], WAU[w_][:, c, 0:512].rearrange("p (m h) -> p h m", h=4)[:, hc, :],
                               xbT[s_][:, c, :], c == 0, c == DC - 1, [('WAU', w_, c // 2), xbT[s_]], [pa])
                    for hc in range(4):
                        for c in range(DC):
                            mm(pu[:, hc * 128:(hc + 1) * 128], WAU[w_][:, c, 512:1024].rearrange("p (m h) -> p h m", h=4)[:, hc, :],
                               xbT[s_][:, c, :], c == 0, c == DC - 1, [('WAU', w_, c // 2), xbT[s_]], [pu])
                    act(sa[s_][:], pa[:, :], AF.Silu, [pa], [sa[s_]])
                    tt('dve', hm[s_][:].rearrange("p c r -> p (c r)"), sa[s_][:], pu[:, :], ALU.mult, [sa[s_], pu], [hm[s_]])

                def stage_b(rt):
                    w_, s_ = (rt // RT) % 2, rt % 2
                    py = [pb[6], pb[7]]
                    for half in range(2):
                        for hc in range(4):
                            mm(py[half][:, :], hm[s_][:, hc, :], WD[w_][:, hc, half * 512:(half + 1) * 512], hc == 0, hc == 3,
                               [hm[s_], ('WD', w_, hc // 2)], [py[half]])
                    act(yb[s_][:, 0:512], py[0][:, :], AF.Copy, [py[0]], [yb[s_]])
                    cp('dve', yb[s_][:, 512:1024], py[1][:, :], [py[1]], [yb[s_]])
                    stores.append(S.op('act', lambda e: e.dma_start(out=ys_d[rt * 128:(rt + 1) * 128, :], in_=yb[s_][:]),
                                       r=[yb[s_]], w=[('yd', rt)], dma=True))

                prefetch(0)
                prefetch(1)
                for rt in range(NB * RT):
                    stage_a(rt)
                    if rt + 2 < NB * RT:
                        prefetch(rt + 2)
                    if rt > 0:
                        stage_b(rt - 1)
                    if rt % RT == 0 and rt // RT + 1 < NB:
                        load(rt // RT + 1)
                stage_b(NB * RT - 1)
                for tg in range(cfg.NT):
                    gi, sidx = tile_gi[tg]
                    yh_, yl_ = xb[tg % 2], yb[tg % 2]
                    S.op('pool', lambda e, yh_=yh_, tg=tg: e.indirect_dma_start(
                        out=yh_[:, :], out_offset=None, in_=ys_d[:, :], in_offset=IOA(ap=IDX[:, tg, 0:1], axis=0)),
                        r=[('IDX', tg)], w=[yh_], dma=True, extra_deps=stores)
                    S.op('pool', lambda e, yl_=yl_, tg=tg: e.indirect_dma_start(
                        out=yl_[:, :], out_offset=None, in_=ys_d[:, :], in_offset=IOA(ap=IDX[:, tg, 1:2], axis=0)),
                        r=[('IDX', tg)], w=[yl_], dma=True, extra_deps=stores)
                    tsc('dve', yh_[:], yh_[:], GHL[:, tg, 0:1], None, ALU.mult, None, [yh_, ('GHL', tg)], [yh_])
                    stt(yh_[:], yl_[:], GHL[:, tg, 1:2], yh_[:], ALU.mult, ALU.add, [yl_, ('GHL', tg), yh_], [yh_])
                    for c in range(DC):
                        tr(pT[c // 4][:, (c % 4) * 128:(c % 4 + 1) * 128], yh_[:, c * 128:(c + 1) * 128], [yh_], [pT[c // 4]])
                    for c in range(DC):
                        stt(XT[:, c, tg * 128:(tg + 1) * 128], pT[c // 4][:, (c % 4) * 128:(c % 4 + 1) * 128], modT[:, l, 40 + c, sidx:sidx + 1],
                            XT[:, c, tg * 128:(tg + 1) * 128], ALU.mult, ALU.add, [pT[c // 4], modT, XK(gi)], [XK(gi)])
            S.barrier()

        for l in cfg.layers:
            norm_modulate(l, 0, False)
            if 'conv' not in cfg.skip:
                conv_phase(l)
            if 'heads' not in cfg.skip:
                heads_phase(l)
            norm_modulate(l, 1, True)
            if 'moe' not in cfg.skip:
                moe_phase(l)

        fin = []
        with contextlib.ExitStack() as ph:
            sq = [sb(ph, "fsq%d" % i, [128, 512]) for i in range(2)]
            rs = [sb(ph, "frs%d" % i, [128, 512]) for i in range(2)]
            ob = [sb(ph, "fob%d" % i, [128, 512]) for i in range(4)]
            kk = 0
            for (t0, n, sidx, gi) in cfg.groups[1:]:
                pss = pb[gi % 2]
                for c in range(DC):
                    q = sq[kk % 2]
                    kk += 1
                    act(q[:, :n], XT[:, c, t0:t0 + n], AF.Square, [XK(gi)], [q])
                    mm(pss[:, :n], ones[:], q[:, :n], c == 0, c == DC - 1, [ones, q], [pss])
                r_ = rs[gi % 2]
                act(r_[:, :n], pss[:, :n], AF.Sqrt, [pss], [r_], bias=cfg.EPS, scale=1.0 / 1024)
                recip(r_[:, :n], r_[:, :n], [r_], [r_])
                for c in range(DC):
                    o_ = ob[kk % 4]
                    kk += 1
                    if cfg.final:
                        stt(o_[:, :n], XT[:, c, t0:t0 + n], V(cfg.V_FNW + c), r_[:, :n], ALU.mult, ALU.mult, [XK(gi), vecs, r_], [o_])
                    else:
                        cp('dve', o_[:, :n], XT[:, c, t0:t0 + n], [XK(gi)], [o_])
                    fin.append(dma('sp', outT_v[:, c, t0 - TC:t0 - TC + n], o_[:, :n], [o_], []))
        S.op('sp', lambda e: e.nop(), extra_deps=fin)
        S.run(nc)
        nc._sched_stats = S.stats
    return nc


def pack_inputs(cfg, b, x, c, ctx, c_ctx, norm_w, w_ada, b_ada, w_in, conv_w, conv_norm_w, hg_lb, hg_norm_w, w_out,
                w_rg, b_rg, w_re, b_re, w_e_gu, w_e_down, final_norm_w):
    d = cfg.DEPTH
    tok = np.concatenate([ctx[b], x[b]], axis=0)
    xT = np.ascontiguousarray(tok.T.reshape(cfg.DC, 128, cfg.T).transpose(1, 0, 2)).reshape(128, cfg.DC * cfg.T)

    def fm(v):
        return np.asarray(v).reshape(-1, 128).T

    vecs = np.zeros((128, cfg.NV), np.float32)
    vecs[:, cfg.V_C:cfg.V_C + 8] = fm(c[b])
    vecs[:, cfg.V_CC:cfg.V_CC + 8] = fm(c_ctx)
    vecs[:, cfg.V_FNW:cfg.V_FNW + 8] = fm(final_norm_w)
    for dirn in range(2):
        for l in range(d):
            o = cfg.V_LB + (dirn * d + l) * 4
            vecs[:, o:o + 4] = fm(hg_lb[dirn, l])
    for l in range(d):
        o = cfg.V_L0 + l * cfg.V_LN
        vecs[:, o:o + 8] = fm(norm_w[l, 0])
        vecs[:, o + 8:o + 16] = fm(norm_w[l, 1])
        vecs[:, o + 16:o + 64] = fm(b_ada[l])
        for tap in range(3):
            vecs[:, o + 64 + tap * 4:o + 64 + tap * 4 + 4] = fm(conv_w[l, tap])
        vecs[:, o + 76:o + 80] = fm(conv_norm_w[l])
        vecs[:, o + 80:o + 81] = fm(hg_norm_w[l])
    wr = np.concatenate([w_rg, w_re], axis=2)
    wr = np.ascontiguousarray(wr.reshape(d, cfg.DC, 128, 36).transpose(2, 0, 1, 3)).reshape(128, d * cfg.DC * 36)
    br = np.ascontiguousarray(np.concatenate([b_rg, b_re], axis=1)).reshape(1, d * 36)
    return {"xT": xT.astype(np.float32), "vecs": vecs, "wr": wr.astype(np.float32), "br": br.astype(np.float32)}


def run(cfg, inputs, n_cores):
    inputs = {k: np.asarray(v) for k, v in inputs.items()}
    nc = build_program(cfg)
    shared = {"w_ada": np.ascontiguousarray(inputs["w_ada"]), "w_in": np.ascontiguousarray(inputs["w_in"]),
              "w_out": np.ascontiguousarray(inputs["w_out"]), "w_gu": np.ascontiguousarray(inputs["w_e_gu"]),
              "w_dn": np.ascontiguousarray(inputs["w_e_down"])}
    in_maps = []
    for b in range(n_cores):
        m = pack_inputs(cfg, b, **inputs)
        m.update(shared)
        in_maps.append(m)
    res = run_bass_kernel_spmd(nc, in_maps, core_ids=list(range(n_cores)))
    outs = []
    for b in range(n_cores):
        oT = np.asarray(res.results[b]["outT"]).reshape(128, cfg.DC, cfg.TL)
        outs.append(oT.transpose(2, 1, 0).reshape(cfg.TL, cfg.D))
    return np.stack(outs, axis=0).astype(np.float32)


def kernel(**inputs):
    cfg = Cfg(depth=4, t_lat=2048)
    return run(cfg, inputs, 8)
```

```python
import contextlib
import numpy as np
import concourse.bass as bass
import concourse.mybir as mybir
from concourse.bass_utils import run_bass_kernel_spmd

F32 = mybir.dt.float32
BF16 = mybir.dt.bfloat16
AF = mybir.ActivationFunctionType
ALU = mybir.AluOpType
AX = mybir.AxisListType

ENGS = ['pe', 'act', 'dve', 'pool', 'sp']
DMA_RING = 8


class Op:
    __slots__ = ('eng', 'fn', 'deps', 'idx', 'signal', 'semkey', 'semval', 'dma', 'waits')

    def __init__(self, eng, fn, dma):
        self.eng = eng
        self.fn = fn
        self.dma = dma
        self.deps = []
        self.signal = False
        self.semkey = None
        self.semval = 0
        self.waits = []


class Sched:
    def __init__(self):
        self.ops = {e: [] for e in ENGS}
        self.bufs = {}
        self.ndma = {e: 0 for e in ENGS}
        self.dma_ops = {e: [] for e in ENGS}

    @staticmethod
    def _key(x):
        if isinstance(x, (tuple, str)):
            return x
        return x.name

    def op(self, eng, fn, r=(), w=(), dma=False, extra_deps=()):
        o = Op(eng, fn, dma)
        deps = list(extra_deps)
        rk = [self._key(x) for x in r]
        wk = [self._key(x) for x in w]
        for k in rk:
            st = self.bufs.get(k)
            if st is not None and st[0] is not None:
                deps.append(st[0])
        for k in wk:
            st = self.bufs.get(k)
            if st is not None:
                if st[0] is not None:
                    deps.append(st[0])
                deps.extend(st[1].values())
        if dma:
            i = self.ndma[eng]
            self.ndma[eng] += 1
            o.semkey = ('dma', eng, i % DMA_RING)
            o.semval = 16 * (i // DMA_RING + 1)
            if i >= DMA_RING:
                deps.append(self.dma_ops[eng][i - DMA_RING])
            self.dma_ops[eng].append(o)
        seen = set()
        for d in deps:
            if id(d) in seen or d is o:
                continue
            seen.add(id(d))
            o.deps.append(d)
        o.idx = len(self.ops[eng])
        self.ops[eng].append(o)
        for k in rk:
            st = self.bufs.setdefault(k, [None, {}])
            st[1][id(o) if dma else eng] = o
        for k in wk:
            self.bufs[k] = [o, {}]
        return o

    def barrier(self):
        last = []
        for e in ENGS:
            if self.ops[e]:
                last.append(self.ops[e][-1])
            last.extend(self.dma_ops[e][-DMA_RING:])
        for e in ENGS:
            self.op(e, lambda g: g.nop(), extra_deps=last)
        self.bufs = {}

    def finalize(self):
        for e in ENGS:
            for o in self.ops[e]:
                for d in o.deps:
                    if d.dma:
                        continue
                    if d.eng == 'pe' and o.eng == 'pe' and not o.dma:
                        continue
                    d.signal = True
        for e in ENGS:
            c = 0
            for o in self.ops[e]:
                if o.dma:
                    continue
                if o.signal:
                    c += 1
                    o.semkey = ('eng', e)
                    o.semval = c
        for e in ENGS:
            waited = {}
            for o in self.ops[e]:
                need = {}
                for d in o.deps:
                    if (not d.dma) and d.eng == 'pe' and o.eng == 'pe' and not o.dma:
                        continue
                    if d.semval > need.get(d.semkey, 0):
                        need[d.semkey] = d.semval
                for k, v in need.items():
                    if waited.get(k, 0) < v:
                        waited[k] = v
                        o.waits.append((k, v))

    def run(self, nc):
        self.finalize()
        keys = set()
        for e in ENGS:
            for o in self.ops[e]:
                if o.semkey is not None and (o.signal or o.dma):
                    keys.add(o.semkey)
        keys = sorted(keys, key=str)
        self.stats = {e: (len(self.ops[e]), max([o.semval for o in self.ops[e] if not o.dma] + [0])) for e in ENGS}
        with contextlib.ExitStack() as st:
            sems = {}
            for i, k in enumerate(keys):
                sems[k] = st.enter_context(nc.semaphore('s%d' % i))
            block = st.enter_context(nc.Block())

            def replay(eng_name):
                def body(e):
                    for o in self.ops[eng_name]:
                        for (k, v) in o.waits:
                            e.wait_ge(sems[k], v)
                        inst = o.fn(e)
                        if o.dma:
                            inst.then_inc(sems[o.semkey], 16)
                        elif o.signal:
                            inst.then_inc(sems[o.semkey], 1)
                return body

            block.tensor(replay('pe'))
            block.scalar(replay('act'))
            block.vector(replay('dve'))
            block.gpsimd(replay('pool'))
            block.sync(replay('sp'))


class Cfg:
    def __init__(self, depth=4, t_lat=2048, layers=None, first=True, final=True):
        self.D = 1024
        self.DC = 8
        self.TC = 256
        self.TL = t_lat
        self.T = self.TC + self.TL
        self.DEPTH = depth
        self.layers = list(range(depth)) if layers is None else layers
        self.first = first
        self.final = final
        self.NE = 32
        self.skip = set()
        self.EPS = 1e-6
        self.groups = [(0, 256, 1, 0)]
        for k in range(self.TL // 512):
            self.groups.append((256 + 512 * k, 512, 0, k + 1))
        self.NT = self.T // 128
        self.BLK = 256
        self.NB = -(-(2 * self.T + 32 * (self.BLK - 1)) // self.BLK)
        d = depth
        self.V_C = 0
        self.V_CC = 8
        self.V_FNW = 16
        self.V_LB = 24
        self.V_L0 = 24 + 2 * d * 4
        self.V_LN = 81
        self.NV = self.V_L0 + d * self.V_LN


def build_program(cfg):
    nc = bass.Bass("TRN2", target_bir_lowering=False)
    T, TC, TL, DC, DEPTH = cfg.T, cfg.TC, cfg.TL, cfg.DC, cfg.DEPTH
    xT_d = nc.dram_tensor("xT", [128, DC * T], F32, kind="ExternalInput").ap()
    vecs_d = nc.dram_tensor("vecs", [128, cfg.NV], F32, kind="ExternalInput").ap()
    wr_d = nc.dram_tensor("wr", [128, DEPTH * DC * 36], F32, kind="ExternalInput").ap()
    br_d = nc.dram_tensor("br", [1, DEPTH * 36], F32, kind="ExternalInput").ap()
    wada_d = nc.dram_tensor("w_ada", [DEPTH, 1024, 6144], F32, kind="ExternalInput").ap()
    win_d = nc.dram_tensor("w_in", [DEPTH, 1024, 4096], F32, kind="ExternalInput").ap()
    wout_d = nc.dram_tensor("w_out", [DEPTH, 1024, 1024], F32, kind="ExternalInput").ap()
    wgu_d = nc.dram_tensor("w_gu", [DEPTH, cfg.NE, 1024, 1024], F32, kind="ExternalInput").ap()
    wdn_d = nc.dram_tensor("w_dn", [DEPTH, cfg.NE, 512, 1024], F32, kind="ExternalInput").ap()
    outT_d = nc.dram_tensor("outT", [128, DC * TL], F32, kind="ExternalOutput").ap()
    xs_d = nc.dram_tensor("xs_scr", [cfg.NB * cfg.BLK, 1024], F32, kind="Internal").ap()
    ys_d = nc.dram_tensor("ys_scr", [cfg.NB * cfg.BLK, 1024], F32, kind="Internal").ap()
    xT_v = xT_d.rearrange("p (c t) -> p c t", c=DC)
    outT_v = outT_d.rearrange("p (c t) -> p c t", c=DC)

    S = Sched()
    uid = [0]

    with contextlib.ExitStack() as top:
        def sb(stack, name, shape, dt=F32):
            uid[0] += 1
            return stack.enter_context(nc.sbuf_tensor("%s_%d" % (name, uid[0]), shape, dt))

        XT = sb(top, "XT", [128, DC, T])
        hT = sb(top, "hT", [128, DC, T], BF16)
        G = sb(top, "G", [128, cfg.NT, 32])
        vecs = sb(top, "vecs", [128, cfg.NV])
        wr = sb(top, "wr", [128, DEPTH, DC, 36])
        br = sb(top, "br", [1, DEPTH * 36])
        ident = sb(top, "ident", [128, 128])
        ones = sb(top, "ones", [128, 128])
        ones1 = sb(top, "ones1", [1, 128])
        rmask = sb(top, "rmask", [128, 512], BF16)
        CH = 64
        NCHT = 128 // CH
        maskF = sb(top, "maskF", [CH, NCHT, CH])
        maskB = sb(top, "maskB", [CH, NCHT, CH])
        scT = sb(top, "scT", [128, DC, 2])
        modT = sb(top, "modT", [128, DEPTH, 48, 2])
        A1 = sb(top, "A1", [128, DEPTH, DC, 2])
        A2 = sb(top, "A2", [128, DEPTH, DC, 2])
        lbT = sb(top, "lbT", [128, 2 * DEPTH * 4])
        omlT = sb(top, "omlT", [128, 2 * DEPTH * 4])
        I32 = mybir.dt.int32
        NB = cfg.NB
        SEL = sb(top, "SEL", [128, cfg.NT, 32])
        RK = sb(top, "RK", [128, cfg.NT, 32])
        IDX = sb(top, "IDX", [128, cfg.NT, 2], I32)
        GHL = sb(top, "GHL", [128, cfg.NT, 2])
        IDXW = sb(top, "IDXW", [128, NB], I32)
        PIDXi = sb(top, "PIDXi", [128, 1], I32)
        PIDX = sb(top, "PIDX", [128, 1])
        Lst = sb(top, "Lst", [128, 128])
        THR = sb(top, "THR", [128, 18])
        JR = sb(top, "JR", [128, NB])
        c128 = sb(top, "c128", [128, NB])
        pb = [top.enter_context(nc.psum_tensor("pb%d" % i, [128, 512], F32)) for i in range(8)]

        tile_gi = {}
        for (t0_, n_, sidx_, gi_) in cfg.groups:
            for ti_ in range(n_ // 128):
                tile_gi[(t0_ + ti_ * 128) // 128] = (gi_, sidx_)
        tile_gi = {k_: v_ for k_, v_ in tile_gi.items()}

        def XK(gi):
            return ('XT', gi)

        def HK(gi):
            return ('hT', gi)

        def mm(out, lhsT, rhs, start, stop, r, w):
            return S.op('pe', lambda e: e.matmul(out, lhsT, rhs, start=start, stop=stop), r=r, w=w)

        def tr(out, in_, r, w):
            return S.op('pe', lambda e: e.transpose(out, in_, ident[:in_.shape[0], :in_.shape[0]]), r=list(r) + [ident], w=w)

        def act(out, in_, func, r, w, bias=None, scale=None, accum=None):
            kw = {}
            if bias is not None:
                kw['bias'] = bias
            if scale is not None:
                kw['scale'] = scale
            if accum is not None:
                kw['accum_out'] = accum
            return S.op('act', lambda e: e.activation(out=out, in_=in_, func=func, **kw), r=r, w=w)

        def tt(eng, out, in0, in1, op, r, w):
            return S.op(eng, lambda e: e.tensor_tensor(out=out, in0=in0, in1=in1, op=op), r=r, w=w)

        def tsc(eng, out, in0, s1, s2, op0, op1, r, w):
            if op1 is None:
                return S.op(eng, lambda e: e.tensor_scalar(out=out, in0=in0, scalar1=s1, scalar2=None, op0=op0), r=r, w=w)
            return S.op(eng, lambda e: e.tensor_scalar(out=out, in0=in0, scalar1=s1, scalar2=s2, op0=op0, op1=op1), r=r, w=w)

        def stt(out, in0, scalar, in1, op0, op1, r, w):
            return S.op('dve', lambda e: e.scalar_tensor_tensor(out=out, in0=in0, scalar=scalar, in1=in1, op0=op0, op1=op1), r=r, w=w)

        def cp(eng, out, in_, r, w):
            return S.op(eng, lambda e: e.tensor_copy(out=out, in_=in_), r=r, w=w)

        def recip(out, in_, r, w):
            return S.op('dve', lambda e: e.reciprocal(out=out, in_=in_), r=r, w=w)

        def memset(eng, ap, val, w):
            return S.op(eng, lambda e: e.memset(ap, val), w=w)

        def dma(eng, out, in_, r, w):
            return S.op(eng, lambda e: e.dma_start(out=out, in_=in_), r=r, w=w, dma=True)

        def V(col, n=1):
            return vecs[:, col:col + n]

        memset('pool', ident[:], 0.0, [ident])
        S.op('pool', lambda e: e.affine_select(out=ident[:], in_=ident[:], compare_op=ALU.not_equal, fill=1.0,
                                               base=0, pattern=[[-1, 128]], channel_multiplier=1), r=[ident], w=[ident])
        memset('pool', ones[:], 1.0, [ones])
        memset('pool', ones1[:], 1.0, [ones1])
        memset('pool', rmask[:], 1.0, [rmask])
        S.op('pool', lambda e: e.memset(rmask[:].rearrange("p (a b) -> p a b", b=CH)[:, :, 0:1], 0.0), r=[rmask], w=[rmask])
        memset('pool', maskF[:], 1.0, [maskF])
        memset('pool', maskB[:], 1.0, [maskB])
        S.op('pool', lambda e: e.affine_select(out=maskF[:], in_=maskF[:], compare_op=ALU.is_ge, fill=0.0,
                                               base=0, pattern=[[0, NCHT], [1, CH]], channel_multiplier=-1), r=[maskF], w=[maskF])
        S.op('pool', lambda e: e.affine_select(out=maskB[:], in_=maskB[:], compare_op=ALU.is_ge, fill=0.0,
                                               base=0, pattern=[[0, NCHT], [-1, CH]], channel_multiplier=1), r=[maskB], w=[maskB])

        S.op('pool', lambda e: e.iota(PIDXi[:], pattern=[[0, 1]], base=0, channel_multiplier=1), w=[PIDXi])
        cp('dve', PIDX[:], PIDXi[:], [PIDXi], [PIDX])
        memset('pool', Lst[:], 1.0, [Lst])
        S.op('pool', lambda e: e.affine_select(out=Lst[:], in_=Lst[:], compare_op=ALU.is_ge, fill=0.0,
                                               base=-1, pattern=[[1, 128]], channel_multiplier=-1), r=[Lst], w=[Lst])
        memset('pool', c128[:], float(cfg.BLK), [c128])
        S.op('dve', lambda e: e.tensor_tensor_scan(out=JR[:], data0=ones[:, :NB], data1=c128[:], initial=-float(cfg.BLK),
                                                   op0=ALU.mult, op1=ALU.add), r=[ones, c128], w=[JR])
        cp('dve', THR[:], JR[:, 0:18], [JR], [THR])

        dma('sp', vecs[:], vecs_d, [], [vecs])
        dma('sp', wr[:].rearrange("p l c n -> p (l c n)"), wr_d, [], [wr])
        dma('sp', br[:], br_d, [], [br])
        for (t0, n, sidx, gi) in cfg.groups:
            dma('sp', XT[:, :, t0:t0 + n], xT_v[:, :, t0:t0 + n], [], [XK(gi)])

        act(scT[:, :, 0], V(cfg.V_C, 8), AF.Silu, [vecs], [scT])
        act(scT[:, :, 1], V(cfg.V_CC, 8), AF.Silu, [vecs], [scT])
        with contextlib.ExitStack() as ph:
            nlb = 2 * DEPTH * 4
            E = sb(ph, "lbE", [128, nlb])
            sE = sb(ph, "lbS", [128, 8])
            rE = sb(ph, "lbR", [128, 8])
            act(E[:], V(cfg.V_LB, nlb), AF.Exp, [vecs], [E])
            E3 = E[:].rearrange("p (d l h) -> p d l h", d=2, l=DEPTH)
            sE2 = sE[:].rearrange("p (d h) -> p d h", d=2)
            cp('dve', sE2, E3[:, :, 0, :], [E], [sE])
            for l in range(1, DEPTH):
                tt('dve', sE2, sE2, E3[:, :, l, :], ALU.add, [sE, E], [sE])
            recip(rE[:], sE[:], [sE], [rE])
            rE2 = rE[:].rearrange("p (d h) -> p d h", d=2)
            lb3 = lbT[:].rearrange("p (d l h) -> p d l h", d=2, l=DEPTH)
            memset('dve', lbT[:], 0.0, [lbT])
            for l in range(1, DEPTH):
                tt('dve', E3[:, :, l, :], E3[:, :, l, :], rE2, ALU.mult, [E, rE], [E])
                tt('dve', lb3[:, :, l, :], lb3[:, :, l - 1, :], E3[:, :, l, :], ALU.add, [lbT, E], [lbT])
            tsc('dve', omlT[:], lbT[:], -1.0, 1.0, ALU.mult, ALU.add, [lbT], [omlT])

            wa = [sb(ph, "wada%d" % i, [128, DC, 512]) for i in range(2)]
            k = 0
            for l in cfg.layers:
                voff = cfg.V_L0 + l * cfg.V_LN
                for jb in range(12):
                    wt = wa[k % 2]
                    k += 1
                    dma('sp', wt[:], wada_d[l, :, jb * 512:(jb + 1) * 512].rearrange("(c p) n -> p c n", p=128), [], [wt])
                    pm = pb[jb % 2]
                    for jj in range(4):
                        for c in range(DC):
                            mm(pm[:, jj * 2:jj * 2 + 2], wt[:, c, jj * 128:(jj + 1) * 128], scT[:, c, :], c == 0, c == DC - 1,
                               [wt, scT], [pm])
                    for s in range(2):
                        tt('dve', modT[:, l, jb * 4:jb * 4 + 4, s], pm[:, 0:8].rearrange("p (j s) -> p j s", s=2)[:, :, s],
                           V(voff + 16 + jb * 4, 4), ALU.add, [pm, vecs], [modT])
                for s in range(2):
                    stt(A1[:, l, :, s], modT[:, l, 8:16, s], 1.0, V(voff + 0, 8), ALU.add, ALU.mult, [modT, vecs], [A1])
                    stt(A2[:, l, :, s], modT[:, l, 32:40, s], 1.0, V(voff + 8, 8), ALU.add, ALU.mult, [modT, vecs], [A2])
        S.barrier()

        def norm_modulate(l, which, router):
            A = A1 if which == 0 else A2
            sh0 = 0 if which == 0 else 24
            with contextlib.ExitStack() as ph:
                sq = [sb(ph, "nsq%d" % i, [128, 512]) for i in range(2)]
                rs = [sb(ph, "nrs%d" % i, [128, 512]) for i in range(2)]
                tmp = [sb(ph, "ntmp%d" % i, [128, 512]) for i in range(2)]
                if router:
                    h2f = [sb(ph, "h2f%d" % i, [128, DC, 512]) for i in range(2)]
                    rt = {nm: [sb(ph, "rt_%s%d" % (nm, i), shp) for i in range(2)] for nm, shp in
                          [("lg", [128, 36]), ("gm", [128, 1]), ("ngm", [128, 1]), ("gmask", [128, 4]), ("ge", [128, 4]),
                           ("gs", [128, 1]), ("pen", [128, 4]), ("el", [128, 32]), ("t8", [128, 8]), ("nm1", [128, 1]),
                           ("sel", [128, 32]), ("ex", [128, 32]), ("gx", [128, 32]), ("den", [128, 1]), ("pr", [128, 1]),
                           ("rp", [128, 1])]}
                kk = 0
                tl = 0
                cpar = [0]
                if router:
                    cums = [sb(ph, "cums%d" % i, [128, 32]) for i in range(2)]
                    memset('dve', cums[0][:], 0.0, [cums[0]])
                for (t0, n, sidx, gi) in cfg.groups:
                    pss = pb[gi % 2]
                    for c in range(DC):
                        q = sq[kk % 2]
                        kk += 1
                        act(q[:, :n], XT[:, c, t0:t0 + n], AF.Square, [XK(gi)], [q])
                        mm(pss[:, :n], ones[:], q[:, :n], c == 0, c == DC - 1, [ones, q], [pss])
                    r_ = rs[gi % 2]
                    act(r_[:, :n], pss[:, :n], AF.Sqrt, [pss], [r_], bias=cfg.EPS, scale=1.0 / 1024)
                    recip(r_[:, :n], r_[:, :n], [r_], [r_])
                    for c in range(DC):
                        tm = tmp[kk % 2]
                        kk += 1
                        tt('dve', tm[:, :n], XT[:, c, t0:t0 + n], r_[:, :n], ALU.mult, [XK(gi), r_], [tm])
                        if router:
                            hf = h2f[gi % 2]
                            act(hf[:, c, :n], tm[:, :n], AF.Identity, [tm, A, modT], [hf],
                                bias=modT[:, l, sh0 + c, sidx:sidx + 1], scale=A[:, l, c, sidx:sidx + 1])
                            cp('pool', hT[:, c, t0:t0 + n], hf[:, c, :n], [hf], [HK(gi)])
                        else:
                            act(hT[:, c, t0:t0 + n], tm[:, :n], AF.Identity, [tm, A, modT], [HK(gi)],
                                bias=modT[:, l, sh0 + c, sidx:sidx + 1], scale=A[:, l, c, sidx:sidx + 1])
                    if router:
                        hf = h2f[gi % 2]
                        for ti in range(n // 128):
                            tg = (t0 + ti * 128) // 128
                            R = {nm: v[tl % 2] for nm, v in rt.items()}
                            pl = pb[2 + tl % 2]
                            tl += 1
                            for c in range(DC):
                                mm(pl[:, 0:36], hf[:, c, ti * 128:(ti + 1) * 128], wr[:, l, c, :], c == 0, False, [hf, wr], [pl])
                            mm(pl[:, 0:36], ones1[:], br[:, l * 36:(l + 1) * 36], False, True, [ones1, br], [pl])
                            lg = R["lg"]
                            cp('dve', lg[:], pl[:, 0:36], [pl], [lg])
                            S.op('dve', lambda e, o=R["gm"], i=lg: e.tensor_reduce(out=o[:], in_=i[:, 0:4], axis=AX.X, op=ALU.max),
                                 r=[lg], w=[R["gm"]])
                            tsc('dve', R["ngm"][:], R["gm"][:], -1.0, None, ALU.mult, None, [R["gm"]], [R["ngm"]])
                            tsc('dve', R["gmask"][:], lg[:, 0:4], R["gm"][:], None, ALU.is_equal, None, [lg, R["gm"]], [R["gmask"]])
                            act(R["ge"][:], lg[:, 0:4], AF.Exp, [lg, R["ngm"]], [R["ge"], R["gs"]], bias=R["ngm"][:], scale=1.0,
                                accum=R["gs"][:])
                            tsc('dve', R["pen"][:], R["gmask"][:], 1e30, -1e30, ALU.mult, ALU.add, [R["gmask"]], [R["pen"]])
                            tt('dve', R["el"][:].rearrange("p (g e) -> p g e", g=4), lg[:, 4:36].rearrange("p (g e) -> p g e", g=4),
                               R["pen"][:].unsqueeze(2).to_broadcast([128, 4, 8]), ALU.add, [lg, R["pen"]], [R["el"]])
                            S.op('dve', lambda e, o=R["t8"], i=R["el"]: e.max(out=o[:], in_=i[:]), r=[R["el"]], w=[R["t8"]])
                            tsc('dve', R["nm1"][:], R["t8"][:, 0:1], -1.0, None, ALU.mult, None, [R["t8"]], [R["nm1"]])
                            tsc('dve', R["sel"][:], R["el"][:], R["t8"][:, 1:2], None, ALU.is_ge, None, [R["el"], R["t8"]], [R["sel"]])
                            act(R["ex"][:], R["el"][:], AF.Exp, [R["el"], R["nm1"]], [R["ex"]], bias=R["nm1"][:], scale=1.0)
                            tt('dve', R["gx"][:], R["sel"][:], R["ex"][:], ALU.mult, [R["sel"], R["ex"]], [R["gx"]])
                            S.op('dve', lambda e, o=R["den"], i=R["gx"]: e.tensor_reduce(out=o[:], in_=i[:], axis=AX.X, op=ALU.add),
                                 r=[R["gx"]], w=[R["den"]])
                            tt('dve', R["pr"][:], R["den"][:], R["gs"][:], ALU.mult, [R["den"], R["gs"]], [R["pr"]])
                            recip(R["rp"][:], R["pr"][:], [R["pr"]], [R["rp"]])
                            tsc('dve', G[:, tg, :], R["gx"][:], R["rp"][:], None, ALU.mult, None, [R["gx"], R["rp"]], [('G', gi)])
                            cp('dve', SEL[:, tg, :], R["sel"][:], [R["sel"]], [('SEL', tg)])
                            prk = pb[4 + tl % 2]
                            mm(prk[:, 0:32], Lst[:], R["sel"][:], True, False, [Lst, R["sel"]], [prk])
                            mm(prk[:, 0:32], ones[:], cums[cpar[0]][:], False, True, [ones, cums[cpar[0]]], [prk])
                            cp('dve', RK[:, tg, :], prk[:, 0:32], [prk], [('RK', tg)])
                            tt('dve', cums[1 - cpar[0]][:], cums[cpar[0]][:], R["sel"][:], ALU.add, [cums[cpar[0]], R["sel"]], [cums[1 - cpar[0]]])
                            cpar[0] = 1 - cpar[0]
                if router:
                    CNT = sb(ph, "CNT", [128, 32])
                    cmp18 = sb(ph, "cmp18", [128, 32, 18])
                    NBLK = sb(ph, "NBLK", [128, 32])
                    PADD = sb(ph, "PADD", [128, 32])
                    PEND = sb(ph, "PEND", [128, 32])
                    PST = sb(ph, "PST", [128, 32])
                    cmpB = sb(ph, "cmpB", [128, NB, 32])
                    BEf = sb(ph, "BEf", [128, NB])
                    pc = pb[6]
                    mm(pc[:, 0:32], ones[:], cums[cpar[0]][:], True, True, [ones, cums[cpar[0]]], [pc])
                    cp('dve', CNT[:], pc[:, 0:32], [pc], [CNT])
                    tt('dve', cmp18[:], CNT[:].unsqueeze(2).to_broadcast([128, 32, 18]), THR[:].unsqueeze(1).to_broadcast([128, 32, 18]),
                       ALU.is_gt, [CNT, THR], [cmp18])
                    S.op('dve', lambda e: e.tensor_reduce(out=NBLK[:], in_=cmp18[:], axis=AX.X, op=ALU.add), r=[cmp18], w=[NBLK])
                    tsc('dve', PADD[:], NBLK[:], float(cfg.BLK), None, ALU.mult, None, [NBLK], [PADD])
                    S.op('dve', lambda e: e.tensor_tensor_scan(out=PEND[:], data0=ones[:, 0:32], data1=PADD[:], initial=0.0,
                                                               op0=ALU.mult, op1=ALU.add), r=[ones, PADD], w=[PEND])
                    tt('dve', PST[:], PEND[:], PADD[:], ALU.subtract, [PEND, PADD], [PST])
                    tt('dve', cmpB[:], PEND[:].unsqueeze(1).to_broadcast([128, NB, 32]), JR[:].unsqueeze(2).to_broadcast([128, NB, 32]),
                       ALU.is_le, [PEND, JR], [cmpB])
                    S.op('dve', lambda e: e.tensor_reduce(out=BEf[:], in_=cmpB[:], axis=AX.X, op=ALU.add), r=[cmpB], w=[BEf])
                    tsc('dve', BEf[:], BEf[:], 31.0, float(32 * l), ALU.min, ALU.add, [BEf], [BEf])
                    tsc('dve', IDXW[:], BEf[:], 128.0, PIDX[:], ALU.mult, ALU.add, [BEf, PIDX], [IDXW])
                    pp = {nm: [sb(ph, "pp_%s%d" % (nm, i), shp) for i in range(2)] for nm, shp in
                          [("pos", [128, 32]), ("t8", [128, 8]), ("eq", [128, 32]), ("pr", [128, 32])]}
                    for tg in range(cfg.NT):
                        Q = {nm: v[tg % 2] for nm, v in pp.items()}
                        gi_t = tile_gi[tg][0]
                        tt('dve', Q["pos"][:], RK[:, tg, :], PST[:], ALU.add, [('RK', tg), PST], [Q["pos"]])
                        stt(Q["pos"][:], Q["pos"][:], 1.0, SEL[:, tg, :], ALU.add, ALU.mult, [Q["pos"], ('SEL', tg)], [Q["pos"]])
                        S.op('dve', lambda e, o=Q["t8"], i=Q["pos"]: e.max(out=o[:], in_=i[:]), r=[Q["pos"]], w=[Q["t8"]])
                        tsc('dve', IDX[:, tg, :], Q["t8"][:, 0:2], -1.0, None, ALU.add, None, [Q["t8"]], [('IDX', tg)])
                        for k in range(2):
                            tsc('dve', Q["eq"][:], Q["pos"][:], Q["t8"][:, k:k + 1], None, ALU.is_equal, None, [Q["pos"], Q["t8"]], [Q["eq"]])
                            tt('dve', Q["pr"][:], Q["eq"][:], G[:, tg, :], ALU.mult, [Q["eq"], ('G', gi_t)], [Q["pr"]])
                            S.op('dve', lambda e, o=GHL[:, tg, k:k + 1], i=Q["pr"]: e.tensor_reduce(out=o, in_=i[:], axis=AX.X, op=ALU.add),
                                 r=[Q["pr"]], w=[('GHL', tg)])
            S.barrier()

        def out_proj(l, Y, nk, k0, ph):
            Wo = sb(ph, "Wo", [128, nk, 1024], BF16)
            dma('pool', Wo[:], wout_d[l, k0 * 128:(k0 + nk) * 128, :].rearrange("(c p) n -> p c n", p=128), [], [Wo])
            kk = 0
            for (t0, n, sidx, gi) in cfg.groups:
                for j in range(DC):
                    po = pb[4 + kk % 4]
                    kk += 1
                    for c in range(nk):
                        rhs = Y[:, c, t0:t0 + n] if nk > 1 else Y[:, t0:t0 + n]
                        mm(po[:, :n], Wo[:, c, j * 128:(j + 1) * 128], rhs, c == 0, c == nk - 1, [Wo, Y], [po])
                    stt(XT[:, j, t0:t0 + n], po[:, :n], modT[:, l, 16 + j, sidx:sidx + 1], XT[:, j, t0:t0 + n],
                        ALU.mult, ALU.add, [po, modT, XK(gi)], [XK(gi)])

        def load_win(ph, name, l, col):
            W = sb(ph, name, [128, DC, 128], BF16)
            dma('pool', W[:], win_d[l, :, col:col + 128].rearrange("(c p) n -> p c n", p=128), [], [W])
            return W

        def proj(W, ps, t0, n, gi):
            for c in range(DC):
                mm(ps[:, :n], W[:, c, :], hT[:, c, t0:t0 + n], c == 0, c == DC - 1, [W, HK(gi)], [ps])

        def conv_phase(l):
            voff = cfg.V_L0 + l * cfg.V_LN
            R_ = TL // 64
            with contextlib.ExitStack() as ph:
                Z = sb(ph, "Z", [128, 4, T], BF16)
                SS = sb(ph, "SS", [128, T])
                u = sb(ph, "u", [128, T])
                Bs = sb(ph, "Bs", [128, T])
                y = sb(ph, "y", [128, T])
                hv = [sb(ph, "hv%d" % i, [128, 512]) for i in range(2)]
                zs = [sb(ph, "zs%d" % i, [128, 512]) for i in range(2)]
                for cc in range(4):
                    with contextlib.ExitStack() as ph2:
                        WB = load_win(ph2, "WB", l, cc * 128)
                        WC = load_win(ph2, "WC", l, 512 + cc * 128)
                        WH = load_win(ph2, "WH", l, 1024 + cc * 128)
                        for (t0, n, sidx, gi) in cfg.groups:
                            pB, pC, pH = pb[0 + 4 * (gi % 2)], pb[1 + 4 * (gi % 2)], pb[2 + 4 * (gi % 2)]
                            proj(WB, pB, t0, n, gi)
                            proj(WC, pC, t0, n, gi)
                            proj(WH, pH, t0, n, gi)
                            h_ = hv[gi % 2]
                            act(h_[:, :n], pH[:, :n], AF.Copy, [pH], [h_])
                            act(Bs[:, t0:t0 + n], pB[:, :n], AF.Copy, [pB], [Bs])
                            tt('dve', u[:, t0:t0 + n], pC[:, :n], h_[:, :n], ALU.mult, [pC, h_], [u])
                        w0, w1, w2 = V(voff + 64 + 0 * 4 + cc), V(voff + 64 + 1 * 4 + cc), V(voff + 64 + 2 * 4 + cc)
                        act(y[:], u[:], AF.Identity, [u, vecs], [y], scale=w1)
                        stt(y[:, 1:TC], u[:, 0:TC - 1], w0, y[:, 1:TC], ALU.mult, ALU.add, [u, vecs, y], [y])
                        stt(y[:, 0:TC - 1], u[:, 1:TC], w2, y[:, 0:TC - 1], ALU.mult, ALU.add, [u, vecs, y], [y])
                        if cc < 2:
                            ul = u[:, TC:T].rearrange("p (r w) -> p r w", w=64)
                            yl = y[:, TC:T].rearrange("p (r w) -> p r w", w=64)
                            stt(yl[:, :, 1:64], ul[:, :, 0:63], w0, yl[:, :, 1:64], ALU.mult, ALU.add, [u, vecs, y], [y])
                            stt(yl[:, :, 0:63], ul[:, :, 1:64], w2, yl[:, :, 0:63], ALU.mult, ALU.add, [u, vecs, y], [y])
                        else:
                            stt(y[:, TC + 64:T], u[:, TC:T - 64], w0, y[:, TC + 64:T], ALU.mult, ALU.add, [u, vecs, y], [y])
                            stt(y[:, TC:T - 64], u[:, TC + 64:T], w2, y[:, TC:T - 64], ALU.mult, ALU.add, [u, vecs, y], [y])
                        tt('dve', y[:], y[:], Bs[:], ALU.mult, [y, Bs], [y])
                        act(Z[:, cc, :], y[:], AF.Copy, [y], [Z])
                        for (t0, n, sidx, gi) in cfg.groups:
                            z_ = zs[gi % 2]
                            pz = pb[3 + 4 * (gi % 2)]
                            act(z_[:, :n], y[:, t0:t0 + n], AF.Square, [y], [z_])
                            mm(pz[:, :n], ones[:], z_[:, :n], True, True, [ones, z_], [pz])
                            if cc == 0:
                                cp('dve', SS[:, t0:t0 + n], pz[:, :n], [pz], [SS])
                            else:
                                tt('dve', SS[:, t0:t0 + n], pz[:, :n], SS[:, t0:t0 + n], ALU.add, [pz, SS], [SS])
                    S.barrier()
                act(SS[:], SS[:], AF.Sqrt, [SS], [SS], bias=cfg.EPS, scale=1.0 / 512)
                recip(SS[:], SS[:], [SS], [SS])
                for cc in range(4):
                    stt(Z[:, cc, :], Z[:, cc, :], V(voff + 76 + cc), SS[:], ALU.mult, ALU.mult, [Z, vecs, SS], [Z])
                out_proj(l, Z, 4, 0, ph)
            S.barrier()

        def heads_phase(l):
            voff = cfg.V_L0 + l * cfg.V_LN
            hnw = V(voff + 80)
            lat = cfg.groups[1:]
            order = [cfg.groups, [cfg.groups[0]] + lat[::-1]]
            for hh in range(4):
                with contextlib.ExitStack() as ph:
                    Wq = load_win(ph, "Wq", l, 1536 + hh * 128)
                    Wf = [load_win(ph, "Wzf", l, 2048 + hh * 128), load_win(ph, "Wzb", l, 2560 + hh * 128)]
                    Wi = load_win(ph, "Wi", l, 3072 + hh * 128)
                    Wg = load_win(ph, "Wg", l, 3584 + hh * 128)
                    Of = sb(ph, "Of", [128, T])
                    Yh = sb(ph, "Yh", [128, T], BF16)
                    NS = 4
                    St = [sb(ph, "St%d" % i, [128, 128]) for i in range(NS)]
                    names = ["qs", "sg", "kk", "vs", "b", "eb", "enb", "ko", "gs"]
                    tmps = [{nm: sb(ph, "g%s%d" % (nm, i), [128, 512]) for nm in names} for i in range(2)]
                    dch = [sb(ph, "dch%d" % i, [128, 16]) for i in range(2)]
                    totc = [sb(ph, "totc%d" % i, [128, 16]) for i in range(2)]
                    koT = [sb(ph, "koT%d" % i, [CH, NCHT, 128]) for i in range(2)]
                    vT = [sb(ph, "vT%d" % i, [CH, NCHT, 128]) for i in range(2)]
                    PT = [sb(ph, "PT%d" % i, [CH, NCHT, CH]) for i in range(2)]
                    midc = [sb(ph, "midc%d" % i, [128, 16]) for i in range(2)]
                    emid = [sb(ph, "emid%d" % i, [128, 16]) for i in range(2)]
                    etm = [sb(ph, "etm%d" % i, [128, 16]) for i in range(2)]
                    osq = sb(ph, "osq", [128, 512])
                    ors = sb(ph, "ors", [128, 512])
                    gpar = 0
                    tpar = 0
                    for dirn in range(2):
                        lbc = (dirn * DEPTH + l) * 4 + hh
                        lb_ap, oml_ap = lbT[:, lbc:lbc + 1], omlT[:, lbc:lbc + 1]
                        cur = 0
                        memset('dve', St[0][:], 0.0, [St[0]])
                        msk = maskF if dirn == 0 else maskB
                        for (t0, n, sidx, gi) in order[dirn]:
                            tp = tmps[gpar % 2]
                            dc_ = dch[gpar % 2]
                            gpar += 1
                            nch = n // CH
                            pq, pz_, pi_, pg = pb[0], pb[1], pb[2], pb[3]
                            proj(Wq, pq, t0, n, gi)
                            proj(Wf[dirn], pz_, t0, n, gi)
                            proj(Wi, pi_, t0, n, gi)
                            act(tp["qs"][:, :n], pq[:, :n], AF.Silu, [pq], [tp["qs"]])
                            act(tp["sg"][:, :n], pz_[:, :n], AF.Sigmoid, [pz_], [tp["sg"]])
                            act(tp["vs"][:, :n], pi_[:, :n], AF.Copy, [pi_], [tp["vs"]])
                            if dirn == 1:
                                proj(Wg, pg, t0, n, gi)
                                act(tp["gs"][:, :n], pg[:, :n], AF.Silu, [pg], [tp["gs"]])
                            tc_ = totc[(gpar - 1) % 2]
                            tsc('dve', tp["sg"][:, :n], tp["sg"][:, :n], oml_ap, lb_ap, ALU.mult, ALU.add, [tp["sg"], omlT, lbT], [tp["sg"]])
                            tsc('dve', tp["kk"][:, :n], tp["sg"][:, :n], -1.0, 1.0, ALU.mult, ALU.add, [tp["sg"]], [tp["kk"]])
                            act(tp["sg"][:, :n], tp["sg"][:, :n], AF.Ln, [tp["sg"]], [tp["sg"]])
                            S.op('dve', lambda e, o=tp["b"], m=rmask, d1=tp["sg"], n=n: e.tensor_tensor_scan(
                                out=o[:, :n], data0=m[:, :n], data1=d1[:, :n], initial=0.0, op0=ALU.mult, op1=ALU.add),
                                r=[rmask, tp["sg"]], w=[tp["b"]])
                            b3 = tp["b"][:, :n].rearrange("p (a c) -> p a c", c=CH)
                            cp('dve', tc_[:, :nch], b3[:, :, CH - 1], [tp["b"]], [tc_])
                            act(dc_[:, :nch], tc_[:, :nch], AF.Exp, [tc_], [dc_])
                            bb = tp["b"]
                            if dirn == 1:
                                tt('dve', b3, b3, tc_[:, :nch].unsqueeze(2).to_broadcast([128, nch, CH]), ALU.subtract, [tp["b"], tc_], [tp["b"]])
                                tt('dve', bb[:, :n], tp["sg"][:, :n], bb[:, :n], ALU.subtract, [tp["sg"], bb], [bb])
                            md_, em_, et_ = midc[(gpar - 1) % 2], emid[(gpar - 1) % 2], etm[(gpar - 1) % 2]
                            midcol = CH // 2 - 1 if dirn == 0 else CH // 2
                            cp('dve', md_[:, :nch], b3[:, :, midcol], [tp["b"]], [md_])
                            tt('dve', b3, b3, md_[:, :nch].unsqueeze(2).to_broadcast([128, nch, CH]), ALU.subtract, [tp["b"], md_], [tp["b"]])
                            act(tp["eb"][:, :n], bb[:, :n], AF.Exp, [bb], [tp["eb"]])
                            act(tp["enb"][:, :n], bb[:, :n], AF.Exp, [bb], [tp["enb"]], scale=-1.0)
                            act(em_[:, :nch], md_[:, :nch], AF.Exp, [md_], [em_])
                            tt('dve', et_[:, :nch], tc_[:, :nch], md_[:, :nch], ALU.subtract, [tc_, md_], [et_])
                            act(et_[:, :nch], et_[:, :nch], AF.Exp, [et_], [et_])
                            tt('dve', tp["qs"][:, :n], tp["qs"][:, :n], tp["eb"][:, :n], ALU.mult, [tp["qs"], tp["eb"]], [tp["qs"]])
                            tt('dve', tp["kk"][:, :n], tp["kk"][:, :n], tp["enb"][:, :n], ALU.mult, [tp["kk"], tp["enb"]], [tp["kk"]])
                            tt('dve', tp["eb"][:, :n].rearrange("p (a c) -> p a c", c=CH),
                               tp["qs"][:, :n].rearrange("p (a c) -> p a c", c=CH),
                               em_[:, :nch].unsqueeze(2).to_broadcast([128, nch, CH]), ALU.mult, [tp["qs"], em_], [tp["eb"]])
                            tt('dve', tp["ko"][:, :n].rearrange("p (a c) -> p a c", c=CH),
                               tp["kk"][:, :n].rearrange("p (a c) -> p a c", c=CH),
                               et_[:, :nch].unsqueeze(2).to_broadcast([128, nch, CH]), ALU.mult, [tp["kk"], et_], [tp["ko"]])
                            tiles = list(range(n // 128))
                            chunks = list(range(NCHT))
                            if dirn == 1:
                                tiles = tiles[::-1]
                                chunks = chunks[::-1]
                            for ti in tiles:
                                c0 = ti * 128
                                kT_, vT_, PT_ = koT[tpar % 2], vT[tpar % 2], PT[tpar % 2]
                                tpar += 1
                                pk, pv = pb[4], pb[5]
                                pk3 = pk[0:CH, 0:NCHT * 128].rearrange("p (j k) -> p j k", j=NCHT)
                                pv3 = pv[0:CH, 0:NCHT * 128].rearrange("p (j k) -> p j k", j=NCHT)
                                for j in range(NCHT):
                                    tr(pk3[:, j, :], tp["ko"][:, c0 + CH * j:c0 + CH * j + CH], [tp["ko"]], [pk])
                                for j in range(NCHT):
                                    tr(pv3[:, j, :], tp["vs"][:, c0 + CH * j:c0 + CH * j + CH], [tp["vs"]], [pv])
                                act(kT_[:], pk3, AF.Copy, [pk], [kT_])
                                cp('dve', vT_[:], pv3, [pv], [vT_])
                                par = tpar % 2
                                psc = pb[3][0:CH, 256:256 + NCHT * CH].rearrange("p (j k) -> p j k", j=NCHT)
                                for j in range(NCHT):
                                    cs = c0 + CH * j
                                    mm(psc[:, j, :], tp["kk"][:, cs:cs + CH], tp["qs"][:, cs:cs + CH], True, True,
                                       [tp["kk"], tp["qs"]], [pb[3]])
                                tsc('dve', PT_[:], psc, -1e30, 1e30, ALU.max, ALU.min, [pb[3]], [PT_])
                                tt('dve', PT_[:], PT_[:], msk[:], ALU.mult, [PT_, msk], [PT_])
                                po = pb[7][:, 256 * par:256 * par + 128]
                                for j in chunks:
                                    cs = c0 + CH * j
                                    jg = cs // CH
                                    mm(pb[6][:, 128 * j:128 * j + 128], kT_[:, j, :], vT_[:, j, :], True, True, [kT_, vT_], [('pb6', j)])
                                    mm(po[:, CH * j:CH * j + CH], St[cur][:], tp["eb"][:, cs:cs + CH], True, False, [St[cur], tp["eb"]], [('pb7', 'o', par)])
                                    mm(po[:, CH * j:CH * j + CH], vT_[:, j, :], PT_[:, j, :], False, True, [vT_, PT_], [('pb7', 'o', par)])
                                    nxt = (cur + 1) % NS
                                    stt(St[nxt][:], St[cur][:], dc_[:, jg:jg + 1], pb[6][:, 128 * j:128 * j + 128], ALU.mult, ALU.add,
                                        [St[cur], dc_, ('pb6', j)], [St[nxt]])
                                    cur = nxt
                                if dirn == 0:
                                    act(Of[:, t0 + c0:t0 + c0 + 128], po[:, 0:128], AF.Copy, [('pb7', 'o', par)], [Of])
                                else:
                                    tt('dve', Of[:, t0 + c0:t0 + c0 + 128], po[:, 0:128], Of[:, t0 + c0:t0 + c0 + 128], ALU.add, [('pb7', 'o', par), Of], [Of])
                            if dirn == 1:
                                pn = pb[3]
                                act(osq[:, :n], Of[:, t0:t0 + n], AF.Square, [Of], [osq])
                                mm(pn[:, :n], ones[:], osq[:, :n], True, True, [ones, osq], [pn])
                                act(ors[:, :n], pn[:, :n], AF.Sqrt, [pn], [ors], bias=cfg.EPS, scale=1.0 / 128)
                                recip(ors[:, :n], ors[:, :n], [ors], [ors])
                                stt(osq[:, :n], Of[:, t0:t0 + n], hnw, ors[:, :n], ALU.mult, ALU.mult, [Of, vecs, ors], [osq])
                                tt('dve', Yh[:, t0:t0 + n], osq[:, :n], tp["gs"][:, :n], ALU.mult, [osq, tp["gs"]], [Yh])
                    out_proj(l, Yh, 1, 4 + hh, ph)
                S.barrier()

        def moe_phase_dense(l):
            with contextlib.ExitStack() as ph:
                WA = [sb(ph, "WA%d" % i, [128, DC, 512], BF16) for i in range(2)]
                WU = [sb(ph, "WU%d" % i, [128, DC, 512], BF16) for i in range(2)]
                WD = [sb(ph, "WD%d" % i, [128, 4, 1024], BF16) for i in range(2)]
                gbc = [sb(ph, "gbc%d" % i, [128, 512], BF16) for i in range(2)]
                sa = [sb(ph, "sa%d" % i, [128, 512]) for i in range(2)]
                t1 = [sb(ph, "t1%d" % i, [128, 512], BF16) for i in range(2)]
                hm = [sb(ph, "hm%d" % i, [128, 4, 512], BF16) for i in range(2)]

                def load(e):
                    s = e % 2
                    dma('pool', WA[s][:], wgu_d[l, e, :, 0:512].rearrange("(c p) n -> p c n", p=128), [], [WA[s]])
                    dma('pool', WU[s][:], wgu_d[l, e, :, 512:1024].rearrange("(c p) n -> p c n", p=128), [], [WU[s]])
                    dma('pool', WD[s][:], wdn_d[l, e, :, :].rearrange("(c p) n -> p c n", p=128), [], [WD[s]])

                load(0)
                kk = 0
                k2 = 0
                for e in range(cfg.NE):
                    if e + 1 < cfg.NE:
                        load(e + 1)
                    s = e % 2
                    for (t0, n, sidx, gi) in cfg.groups:
                        pg = pb[0]
                        for ti in range(n // 128):
                            tg = (t0 + ti * 128) // 128
                            mm(pg[:, ti * 128:(ti + 1) * 128], G[:, tg, e:e + 1].to_broadcast([128, 128]), ident[:], True, True,
                               [('G', gi), ident], [pg])
                        gb = gbc[kk % 2]
                        hm_ = hm[kk % 2]
                        kk += 1
                        act(gb[:, :n], pg[:, :n], AF.Copy, [pg], [gb])
                        for hc in range(4):
                            pa, pu = pb[1 + 2 * (k2 % 2)], pb[2 + 2 * (k2 % 2)]
                            sa_, t1_ = sa[k2 % 2], t1[k2 % 2]
                            k2 += 1
                            for c in range(DC):
                                mm(pa[:, :n], WA[s][:, c, hc * 128:(hc + 1) * 128], hT[:, c, t0:t0 + n], c == 0, c == DC - 1,
                                   [WA[s], HK(gi)], [pa])
                            for c in range(DC):
                                mm(pu[:, :n], WU[s][:, c, hc * 128:(hc + 1) * 128], hT[:, c, t0:t0 + n], c == 0, c == DC - 1,
                                   [WU[s], HK(gi)], [pu])
                            act(sa_[:, :n], pa[:, :n], AF.Silu, [pa], [sa_])
                            tt('dve', t1_[:, :n], sa_[:, :n], pu[:, :n], ALU.mult, [sa_, pu], [t1_])
                            tt('pool', hm_[:, hc, :n], t1_[:, :n], gb[:, :n], ALU.mult, [t1_, gb], [hm_])
                        for j in range(DC):
                            py = pb[5 + j % 3]
                            for hc in range(4):
                                mm(py[:, :n], WD[s][:, hc, j * 128:(j + 1) * 128], hm_[:, hc, :n], hc == 0, hc == 3, [WD[s], hm_], [py])
                            stt(XT[:, j, t0:t0 + n], py[:, :n], modT[:, l, 40 + j, sidx:sidx + 1], XT[:, j, t0:t0 + n],
                                ALU.mult, ALU.add, [py, modT, XK(gi)], [XK(gi)])
            S.barrier()

        def moe_phase(l):
            IOA = bass.IndirectOffsetOnAxis
            with contextlib.ExitStack() as ph:
                WAU = [sb(ph, "WAU%d" % i, [128, DC, 1024], BF16) for i in range(2)]
                WD = [sb(ph, "WD%d" % i, [128, 4, 1024], BF16) for i in range(2)]
                h32 = sb(ph, "h32", [128, DC, 128])
                xb = [sb(ph, "xb%d" % i, [128, 1024]) for i in range(2)]
                yb = [sb(ph, "yb%d" % i, [128, 1024]) for i in range(2)]
                xbT = [sb(ph, "xbT%d" % i, [128, DC, 128], BF16) for i in range(2)]
                sa = [sb(ph, "sa0", [128, 512])] * 2
                hm = [sb(ph, "hm%d" % i, [128, 4, 128], BF16) for i in range(2)]
                pT = [pb[0], pb[1]]

                wgu2 = wgu_d.rearrange("l e (p c) n -> (l e p) (c n)", c=8)
                wdn2 = wdn_d.rearrange("l e (p c) n -> (l e p) (c n)", c=4)

                def load(j):
                    s_ = j % 2
                    for q in range(4):
                        S.op('pool', lambda e, j=j, q=q, s_=s_: e.indirect_dma_start(
                            out=WAU[s_][:, 2 * q:2 * q + 2, :].rearrange("p a n -> p (a n)"), out_offset=None, in_=wgu2,
                            in_offset=IOA(ap=IDXW[:, j:j + 1], axis=0), element_offset=q * 2048), r=[IDXW], w=[('WAU', s_, q)], dma=True)
                    for q in range(2):
                        S.op('pool', lambda e, j=j, q=q, s_=s_: e.indirect_dma_start(
                            out=WD[s_][:, 2 * q:2 * q + 2, :].rearrange("p a n -> p (a n)"), out_offset=None, in_=wdn2,
                            in_offset=IOA(ap=IDXW[:, j:j + 1], axis=0), element_offset=q * 2048), r=[IDXW], w=[('WD', s_, q)], dma=True)

                load(0)
                scat = []
                for tg in range(cfg.NT):
                    gi, sidx = tile_gi[tg]
                    act(h32[:], hT[:, :, tg * 128:(tg + 1) * 128], AF.Copy, [HK(gi)], [h32])
                    for c in range(DC):
                        tr(pT[c // 4][:, (c % 4) * 128:(c % 4 + 1) * 128], h32[:, c, :], [h32], [pT[c // 4]])
                    x_ = xb[tg % 2]
                    act(x_[:, 0:512], pT[0][:, :], AF.Copy, [pT[0]], [x_])
                    cp('dve', x_[:, 512:1024], pT[1][:, :], [pT[1]], [x_])
                    for k in range(2):
                        scat.append(S.op('pool', lambda e, x_=x_, tg=tg, k=k: e.indirect_dma_start(
                            out=xs_d[:, :], out_offset=IOA(ap=IDX[:, tg, k:k + 1], axis=0), in_=x_[:, :], in_offset=None),
                            r=[x_, ('IDX', tg)], w=[('xs', tg, k)], dma=True))
                stores = []
                RT = cfg.BLK // 128

                def prefetch(rt):
                    s_ = rt % 2
                    S.op('sp', lambda e: e.dma_start(out=xb[s_][:], in_=xs_d[rt * 128:(rt + 1) * 128, :]), r=[], w=[xb[s_]],
                         dma=True, extra_deps=scat)

                def stage_a(rt):
                    w_, s_ = (rt // RT) % 2, rt % 2
                    for c in range(DC):
                        tr(pT[c // 4][:, (c % 4) * 128:(c % 4 + 1) * 128], xb[s_][:, :].rearrange("r (p c) -> r c p", c=8)[:, c, :], [xb[s_]], [pT[c // 4]])
                    act(xbT[s_][:, 0:4, :], pT[0][:, :].rearrange("p (c r) -> p c r", c=4), AF.Copy, [pT[0]], [xbT[s_]])
                    cp('dve', xbT[s_][:, 4:8, :], pT[1][:, :].rearrange("p (c r) -> p c r", c=4), [pT[1]], [xbT[s_]])
                    pa, pu = pb[2 + 2 * (rt % 2)], pb[3 + 2 * (rt % 2)]
                    for hc in range(4):
                        for c in range(DC):
                            mm(pa[:, hc * 128:(hc + 1) * 128], WAU[w_][:, c, 0:512].rearrange("p (m h) -> p h m", h=4)[:, hc, :],
                               xbT[s_][:, c, :], c == 0, c == DC - 1, [('WAU', w_, c // 2), xbT[s_]], [pa])
                    for hc in range(4):
                        for c in range(DC):
                            mm(pu[:, hc * 128:(hc + 1) * 128], WAU[w_][:, c, 512:1024].rearrange("p (m h) -> p h m", h=4)[:, hc, :],
                               xbT[s_][:, c, :], c == 0, c == DC - 1, [('WAU', w_, c // 2), xbT[s_]], [pu])
                    act(sa[s_][:], pa[:, :], AF.Silu, [pa], [sa[s_]])
                    tt('dve', hm[s_][:].rearrange("p c r -> p (c r)"), sa[s_][:], pu[:, :], ALU.mult, [sa[s_], pu], [hm[s_]])

                def stage_b(rt):
                    w_, s_ = (rt // RT) % 2, rt % 2
                    py = [pb[6], pb[7]]
                    for half in range(2):
                        for hc in range(4):
                            mm(py[half][:, :], hm[s_][:, hc, :], WD[w_][:, hc, half * 512:(half + 1) * 512], hc == 0, hc == 3,
                               [hm[s_], ('WD', w_, hc // 2)], [py[half]])
                    act(yb[s_][:, 0:512], py[0][:, :], AF.Copy, [py[0]], [yb[s_]])
                    cp('dve', yb[s_][:, 512:1024], py[1][:, :], [py[1]], [yb[s_]])
                    stores.append(S.op('act', lambda e: e.dma_start(out=ys_d[rt * 128:(rt + 1) * 128, :], in_=yb[s_][:]),
                                       r=[yb[s_]], w=[('yd', rt)], dma=True))

                prefetch(0)
                prefetch(1)
                for rt in range(NB * RT):
                    stage_a(rt)
                    if rt + 2 < NB * RT:
                        prefetch(rt + 2)
                    if rt > 0:
                        stage_b(rt - 1)
                    if rt % RT == 0 and rt // RT + 1 < NB:
                        load(rt // RT + 1)
                stage_b(NB * RT - 1)
                for tg in range(cfg.NT):
                    gi, sidx = tile_gi[tg]
                    yh_, yl_ = xb[tg % 2], yb[tg % 2]
                    S.op('pool', lambda e, yh_=yh_, tg=tg: e.indirect_dma_start(
                        out=yh_[:, :], out_offset=None, in_=ys_d[:, :], in_offset=IOA(ap=IDX[:, tg, 0:1], axis=0)),
                        r=[('IDX', tg)], w=[yh_], dma=True, extra_deps=stores)
                    S.op('pool', lambda e, yl_=yl_, tg=tg: e.indirect_dma_start(
                        out=yl_[:, :], out_offset=None, in_=ys_d[:, :], in_offset=IOA(ap=IDX[:, tg, 1:2], axis=0)),
                        r=[('IDX', tg)], w=[yl_], dma=True, extra_deps=stores)
                    tsc('dve', yh_[:], yh_[:], GHL[:, tg, 0:1], None, ALU.mult, None, [yh_, ('GHL', tg)], [yh_])
                    stt(yh_[:], yl_[:], GHL[:, tg, 1:2], yh_[:], ALU.mult, ALU.add, [yl_, ('GHL', tg), yh_], [yh_])
                    for c in range(DC):
                        tr(pT[c // 4][:, (c % 4) * 128:(c % 4 + 1) * 128], yh_[:, c * 128:(c + 1) * 128], [yh_], [pT[c // 4]])
                    for c in range(DC):
                        stt(XT[:, c, tg * 128:(tg + 1) * 128], pT[c // 4][:, (c % 4) * 128:(c % 4 + 1) * 128], modT[:, l, 40 + c, sidx:sidx + 1],
                            XT[:, c, tg * 128:(tg + 1) * 128], ALU.mult, ALU.add, [pT[c // 4], modT, XK(gi)], [XK(gi)])
            S.barrier()

        for l in cfg.layers:
            norm_modulate(l, 0, False)
            if 'conv' not in cfg.skip:
                conv_phase(l)
            if 'heads' not in cfg.skip:
                heads_phase(l)
            norm_modulate(l, 1, True)
            if 'moe' not in cfg.skip:
                moe_phase(l)

        fin = []
        with contextlib.ExitStack() as ph:
            sq = [sb(ph, "fsq%d" % i, [128, 512]) for i in range(2)]
            rs = [sb(ph, "frs%d" % i, [128, 512]) for i in range(2)]
            ob = [sb(ph, "fob%d" % i, [128, 512]) for i in range(4)]
            kk = 0
            for (t0, n, sidx, gi) in cfg.groups[1:]:
                pss = pb[gi % 2]
                for c in range(DC):
                    q = sq[kk % 2]
                    kk += 1
                    act(q[:, :n], XT[:, c, t0:t0 + n], AF.Square, [XK(gi)], [q])
                    mm(pss[:, :n], ones[:], q[:, :n], c == 0, c == DC - 1, [ones, q], [pss])
                r_ = rs[gi % 2]
                act(r_[:, :n], pss[:, :n], AF.Sqrt, [pss], [r_], bias=cfg.EPS, scale=1.0 / 1024)
                recip(r_[:, :n], r_[:, :n], [r_], [r_])
                for c in range(DC):
                    o_ = ob[kk % 4]
                    kk += 1
                    if cfg.final:
                        stt(o_[:, :n], XT[:, c, t0:t0 + n], V(cfg.V_FNW + c), r_[:, :n], ALU.mult, ALU.mult, [XK(gi), vecs, r_], [o_])
                    else:
                        cp('dve', o_[:, :n], XT[:, c, t0:t0 + n], [XK(gi)], [o_])
                    fin.append(dma('sp', outT_v[:, c, t0 - TC:t0 - TC + n], o_[:, :n], [o_], []))
        S.op('sp', lambda e: e.nop(), extra_deps=fin)
        S.run(nc)
        nc._sched_stats = S.stats
    return nc


def pack_inputs(cfg, b, x, c, ctx, c_ctx, norm_w, w_ada, b_ada, w_in, conv_w, conv_norm_w, hg_lb, hg_norm_w, w_out,
                w_rg, b_rg, w_re, b_re, w_e_gu, w_e_down, final_norm_w):
    d = cfg.DEPTH
    tok = np.concatenate([ctx[b], x[b]], axis=0)
    xT = np.ascontiguousarray(tok.T.reshape(cfg.DC, 128, cfg.T).transpose(1, 0, 2)).reshape(128, cfg.DC * cfg.T)

    def fm(v):
        return np.asarray(v).reshape(-1, 128).T

    vecs = np.zeros((128, cfg.NV), np.float32)
    vecs[:, cfg.V_C:cfg.V_C + 8] = fm(c[b])
    vecs[:, cfg.V_CC:cfg.V_CC + 8] = fm(c_ctx)
    vecs[:, cfg.V_FNW:cfg.V_FNW + 8] = fm(final_norm_w)
    for dirn in range(2):
        for l in range(d):
            o = cfg.V_LB + (dirn * d + l) * 4
            vecs[:, o:o + 4] = fm(hg_lb[dirn, l])
    for l in range(d):
        o = cfg.V_L0 + l * cfg.V_LN
        vecs[:, o:o + 8] = fm(norm_w[l, 0])
        vecs[:, o + 8:o + 16] = fm(norm_w[l, 1])
        vecs[:, o + 16:o + 64] = fm(b_ada[l])
        for tap in range(3):
            vecs[:, o + 64 + tap * 4:o + 64 + tap * 4 + 4] = fm(conv_w[l, tap])
        vecs[:, o + 76:o + 80] = fm(conv_norm_w[l])
        vecs[:, o + 80:o + 81] = fm(hg_norm_w[l])
    wr = np.concatenate([w_rg, w_re], axis=2)
    wr = np.ascontiguousarray(wr.reshape(d, cfg.DC, 128, 36).transpose(2, 0, 1, 3)).reshape(128, d * cfg.DC * 36)
    br = np.ascontiguousarray(np.concatenate([b_rg, b_re], axis=1)).reshape(1, d * 36)
    return {"xT": xT.astype(np.float32), "vecs": vecs, "wr": wr.astype(np.float32), "br": br.astype(np.float32)}


def run(cfg, inputs, n_cores):
    inputs = {k: np.asarray(v) for k, v in inputs.items()}
    nc = build_program(cfg)
    shared = {"w_ada": np.ascontiguousarray(inputs["w_ada"]), "w_in": np.ascontiguousarray(inputs["w_in"]),
              "w_out": np.ascontiguousarray(inputs["w_out"]), "w_gu": np.ascontiguousarray(inputs["w_e_gu"]),
              "w_dn": np.ascontiguousarray(inputs["w_e_down"])}
    in_maps = []
    for b in range(n_cores):
        m = pack_inputs(cfg, b, **inputs)
        m.update(shared)
        in_maps.append(m)
    res = run_bass_kernel_spmd(nc, in_maps, core_ids=list(range(n_cores)))
    outs = []
    for b in range(n_cores):
        oT = np.asarray(res.results[b]["outT"]).reshape(128, cfg.DC, cfg.TL)
        outs.append(oT.transpose(2, 1, 0).reshape(cfg.TL, cfg.D))
    return np.stack(outs, axis=0).astype(np.float32)


def kernel(**inputs):
    cfg = Cfg(depth=4, t_lat=2048)
    return run(cfg, inputs, 8)
```

```python
import contextlib
import numpy as np
import concourse.bass as bass
import concourse.mybir as mybir
from concourse.bass_utils import run_bass_kernel_spmd

F32 = mybir.dt.float32
BF16 = mybir.dt.bfloat16
AF = mybir.ActivationFunctionType
ALU = mybir.AluOpType
AX = mybir.AxisListType

ENGS = ['pe', 'act', 'dve', 'pool', 'sp']
DMA_RING = 8


class Op:
    __slots__ = ('eng', 'fn', 'deps', 'idx', 'signal', 'semkey', 'semval', 'dma', 'waits')

    def __init__(self, eng, fn, dma):
        self.eng = eng
        self.fn = fn
        self.dma = dma
        self.deps = []
        self.signal = False
        self.semkey = None
        self.semval = 0
        self.waits = []


class Sched:
    def __init__(self):
        self.ops = {e: [] for e in ENGS}
        self.bufs = {}
        self.ndma = {e: 0 for e in ENGS}
        self.dma_ops = {e: [] for e in ENGS}

    @staticmethod
    def _key(x):
        if isinstance(x, (tuple, str)):
            return x
        return x.name

    def op(self, eng, fn, r=(), w=(), dma=False, extra_deps=()):
        o = Op(eng, fn, dma)
        deps = list(extra_deps)
        rk = [self._key(x) for x in r]
        wk = [self._key(x) for x in w]
        for k in rk:
            st = self.bufs.get(k)
            if st is not None and st[0] is not None:
                deps.append(st[0])
        for k in wk:
            st = self.bufs.get(k)
            if st is not None:
                if st[0] is not None:
                    deps.append(st[0])
                deps.extend(st[1].values())
        if dma:
            i = self.ndma[eng]
            self.ndma[eng] += 1
            o.semkey = ('dma', eng, i % DMA_RING)
            o.semval = 16 * (i // DMA_RING + 1)
            if i >= DMA_RING:
                deps.append(self.dma_ops[eng][i - DMA_RING])
            self.dma_ops[eng].append(o)
        seen = set()
        for d in deps:
            if id(d) in seen or d is o:
                continue
            seen.add(id(d))
            o.deps.append(d)
        o.idx = len(self.ops[eng])
        self.ops[eng].append(o)
        for k in rk:
            st = self.bufs.setdefault(k, [None, {}])
            st[1][id(o) if dma else eng] = o
        for k in wk:
            self.bufs[k] = [o, {}]
        return o

    def barrier(self):
        last = []
        for e in ENGS:
            if self.ops[e]:
                last.append(self.ops[e][-1])
            last.extend(self.dma_ops[e][-DMA_RING:])
        for e in ENGS:
            self.op(e, lambda g: g.nop(), extra_deps=last)
        self.bufs = {}

    def finalize(self):
        for e in ENGS:
            for o in self.ops[e]:
                for d in o.deps:
                    if d.dma:
                        continue
                    if d.eng == 'pe' and o.eng == 'pe' and not o.dma:
                        continue
                    d.signal = True
        for e in ENGS:
            c = 0
            for o in self.ops[e]:
                if o.dma:
                    continue
                if o.signal:
                    c += 1
                    o.semkey = ('eng', e)
                    o.semval = c
        for e in ENGS:
            waited = {}
            for o in self.ops[e]:
                need = {}
                for d in o.deps:
                    if (not d.dma) and d.eng == 'pe' and o.eng == 'pe' and not o.dma:
                        continue
                    if d.semval > need.get(d.semkey, 0):
                        need[d.semkey] = d.semval
                for k, v in need.items():
                    if waited.get(k, 0) < v:
                        waited[k] = v
                        o.waits.append((k, v))

    def run(self, nc):
        self.finalize()
        keys = set()
        for e in ENGS:
            for o in self.ops[e]:
                if o.semkey is not None and (o.signal or o.dma):
                    keys.add(o.semkey)
        keys = sorted(keys, key=str)
        self.stats = {e: (len(self.ops[e]), max([o.semval for o in self.ops[e] if not o.dma] + [0])) for e in ENGS}
        with contextlib.ExitStack() as st:
            sems = {}
            for i, k in enumerate(keys):
                sems[k] = st.enter_context(nc.semaphore('s%d' % i))
            block = st.enter_context(nc.Block())

            def replay(eng_name):
                def body(e):
                    for o in self.ops[eng_name]:
                        for (k, v) in o.waits:
                            e.wait_ge(sems[k], v)
                        inst = o.fn(e)
                        if o.dma:
                            inst.then_inc(sems[o.semkey], 16)
                        elif o.signal:
                            inst.then_inc(sems[o.semkey], 1)
                return body

            block.tensor(replay('pe'))
            block.scalar(replay('act'))
            block.vector(replay('dve'))
            block.gpsimd(replay('pool'))
            block.sync(replay('sp'))


class Cfg:
    def __init__(self, depth=4, t_lat=2048, layers=None, first=True, final=True):
        self.D = 1024
        self.DC = 8
        self.TC = 256
        self.TL = t_lat
        self.T = self.TC + self.TL
        self.DEPTH = depth
        self.layers = list(range(depth)) if layers is None else layers
        self.first = first
        self.final = final
        self.NE = 32
        self.skip = set()
        self.EPS = 1e-6
        self.groups = [(0, 256, 1, 0)]
        for k in range(self.TL // 512):
            self.groups.append((256 + 512 * k, 512, 0, k + 1))
        self.NT = self.T // 128
        self.BLK = 256
        self.NB = -(-(2 * self.T + 32 * (self.BLK - 1)) // self.BLK)
        d = depth
        self.V_C = 0
        self.V_CC = 8
        self.V_FNW = 16
        self.V_LB = 24
        self.V_L0 = 24 + 2 * d * 4
        self.V_LN = 81
        self.NV = self.V_L0 + d * self.V_LN


def build_program(cfg):
    nc = bass.Bass("TRN2", target_bir_lowering=False)
    T, TC, TL, DC, DEPTH = cfg.T, cfg.TC, cfg.TL, cfg.DC, cfg.DEPTH
    xT_d = nc.dram_tensor("xT", [128, DC * T], F32, kind="ExternalInput").ap()
    vecs_d = nc.dram_tensor("vecs", [128, cfg.NV], F32, kind="ExternalInput").ap()
    wr_d = nc.dram_tensor("wr", [128, DEPTH * DC * 36], F32, kind="ExternalInput").ap()
    br_d = nc.dram_tensor("br", [1, DEPTH * 36], F32, kind="ExternalInput").ap()
    wada_d = nc.dram_tensor("w_ada", [DEPTH, 1024, 6144], F32, kind="ExternalInput").ap()
    win_d = nc.dram_tensor("w_in", [DEPTH, 1024, 4096], F32, kind="ExternalInput").ap()
    wout_d = nc.dram_tensor("w_out", [DEPTH, 1024, 1024], F32, kind="ExternalInput").ap()
    wgu_d = nc.dram_tensor("w_gu", [DEPTH, cfg.NE, 1024, 1024], F32, kind="ExternalInput").ap()
    wdn_d = nc.dram_tensor("w_dn", [DEPTH, cfg.NE, 512, 1024], F32, kind="ExternalInput").ap()
    outT_d = nc.dram_tensor("outT", [128, DC * TL], F32, kind="ExternalOutput").ap()
    xs_d = nc.dram_tensor("xs_scr", [cfg.NB * cfg.BLK, 1024], F32, kind="Internal").ap()
    ys_d = nc.dram_tensor("ys_scr", [cfg.NB * cfg.BLK, 1024], F32, kind="Internal").ap()
    xT_v = xT_d.rearrange("p (c t) -> p c t", c=DC)
    outT_v = outT_d.rearrange("p (c t) -> p c t", c=DC)

    S = Sched()
    uid = [0]

    with contextlib.ExitStack() as top:
        def sb(stack, name, shape, dt=F32):
            uid[0] += 1
            return stack.enter_context(nc.sbuf_tensor("%s_%d" % (name, uid[0]), shape, dt))

        XT = sb(top, "XT", [128, DC, T])
        hT = sb(top, "hT", [128, DC, T], BF16)
        G = sb(top, "G", [128, cfg.NT, 32])
        vecs = sb(top, "vecs", [128, cfg.NV])
        wr = sb(top, "wr", [128, DEPTH, DC, 36])
        br = sb(top, "br", [1, DEPTH * 36])
        ident = sb(top, "ident", [128, 128])
        ones = sb(top, "ones", [128, 128])
        ones1 = sb(top, "ones1", [1, 128])
        rmask = sb(top, "rmask", [128, 512], BF16)
        CH = 64
        NCHT = 128 // CH
        maskF = sb(top, "maskF", [CH, NCHT, CH])
        maskB = sb(top, "maskB", [CH, NCHT, CH])
        scT = sb(top, "scT", [128, DC, 2])
        modT = sb(top, "modT", [128, DEPTH, 48, 2])
        A1 = sb(top, "A1", [128, DEPTH, DC, 2])
        A2 = sb(top, "A2", [128, DEPTH, DC, 2])
        lbT = sb(top, "lbT", [128, 2 * DEPTH * 4])
        omlT = sb(top, "omlT", [128, 2 * DEPTH * 4])
        I32 = mybir.dt.int32
        NB = cfg.NB
        SEL = sb(top, "SEL", [128, cfg.NT, 32])
        RK = sb(top, "RK", [128, cfg.NT, 32])
        IDX = sb(top, "IDX", [128, cfg.NT, 2], I32)
        GHL = sb(top, "GHL", [128, cfg.NT, 2])
        IDXW = sb(top, "IDXW", [128, NB], I32)
        PIDXi = sb(top, "PIDXi", [128, 1], I32)
        PIDX = sb(top, "PIDX", [128, 1])
        Lst = sb(top, "Lst", [128, 128])
        THR = sb(top, "THR", [128, 18])
        JR = sb(top, "JR", [128, NB])
        c128 = sb(top, "c128", [128, NB])
        pb = [top.enter_context(nc.psum_tensor("pb%d" % i, [128, 512], F32)) for i in range(8)]

        tile_gi = {}
        for (t0_, n_, sidx_, gi_) in cfg.groups:
            for ti_ in range(n_ // 128):
                tile_gi[(t0_ + ti_ * 128) // 128] = (gi_, sidx_)
        tile_gi = {k_: v_ for k_, v_ in tile_gi.items()}

        bc_cache = {}
        order_box = []

        def order_of(bpos):
            return order_box[0][bpos]

        def XK(gi):
            return ('XT', gi)

        def HK(gi):
            return ('hT', gi)

        def mm(out, lhsT, rhs, start, stop, r, w):
            return S.op('pe', lambda e: e.matmul(out, lhsT, rhs, start=start, stop=stop), r=r, w=w)

        def tr(out, in_, r, w):
            return S.op('pe', lambda e: e.transpose(out, in_, ident[:in_.shape[0], :in_.shape[0]]), r=list(r) + [ident], w=w)

        def act(out, in_, func, r, w, bias=None, scale=None, accum=None):
            kw = {}
            if bias is not None:
                kw['bias'] = bias
            if scale is not None:
                kw['scale'] = scale
            if accum is not None:
                kw['accum_out'] = accum
            return S.op('act', lambda e: e.activation(out=out, in_=in_, func=func, **kw), r=r, w=w)

        def tt(eng, out, in0, in1, op, r, w):
            return S.op(eng, lambda e: e.tensor_tensor(out=out, in0=in0, in1=in1, op=op), r=r, w=w)

        def tsc(eng, out, in0, s1, s2, op0, op1, r, w):
            if op1 is None:
                return S.op(eng, lambda e: e.tensor_scalar(out=out, in0=in0, scalar1=s1, scalar2=None, op0=op0), r=r, w=w)
            return S.op(eng, lambda e: e.tensor_scalar(out=out, in0=in0, scalar1=s1, scalar2=s2, op0=op0, op1=op1), r=r, w=w)

        def stt(out, in0, scalar, in1, op0, op1, r, w):
            return S.op('dve', lambda e: e.scalar_tensor_tensor(out=out, in0=in0, scalar=scalar, in1=in1, op0=op0, op1=op1), r=r, w=w)

        def cp(eng, out, in_, r, w):
            return S.op(eng, lambda e: e.tensor_copy(out=out, in_=in_), r=r, w=w)

        def recip(out, in_, r, w):
            return S.op('dve', lambda e: e.reciprocal(out=out, in_=in_), r=r, w=w)

        def memset(eng, ap, val, w):
            return S.op(eng, lambda e: e.memset(ap, val), w=w)

        def dma(eng, out, in_, r, w):
            return S.op(eng, lambda e: e.dma_start(out=out, in_=in_), r=r, w=w, dma=True)

        def V(col, n=1):
            return vecs[:, col:col + n]

        memset('pool', ident[:], 0.0, [ident])
        S.op('pool', lambda e: e.affine_select(out=ident[:], in_=ident[:], compare_op=ALU.not_equal, fill=1.0,
                                               base=0, pattern=[[-1, 128]], channel_multiplier=1), r=[ident], w=[ident])
        memset('pool', ones[:], 1.0, [ones])
        memset('pool', ones1[:], 1.0, [ones1])
        memset('pool', rmask[:], 1.0, [rmask])
        S.op('pool', lambda e: e.memset(rmask[:].rearrange("p (a b) -> p a b", b=CH)[:, :, 0:1], 0.0), r=[rmask], w=[rmask])
        memset('pool', maskF[:], 1.0, [maskF])
        memset('pool', maskB[:], 1.0, [maskB])
        S.op('pool', lambda e: e.affine_select(out=maskF[:], in_=maskF[:], compare_op=ALU.is_ge, fill=0.0,
                                               base=0, pattern=[[0, NCHT], [1, CH]], channel_multiplier=-1), r=[maskF], w=[maskF])
        S.op('pool', lambda e: e.affine_select(out=maskB[:], in_=maskB[:], compare_op=ALU.is_ge, fill=0.0,
                                               base=0, pattern=[[0, NCHT], [-1, CH]], channel_multiplier=1), r=[maskB], w=[maskB])

        S.op('pool', lambda e: e.iota(PIDXi[:], pattern=[[0, 1]], base=0, channel_multiplier=1), w=[PIDXi])
        cp('dve', PIDX[:], PIDXi[:], [PIDXi], [PIDX])
        memset('pool', Lst[:], 1.0, [Lst])
        S.op('pool', lambda e: e.affine_select(out=Lst[:], in_=Lst[:], compare_op=ALU.is_ge, fill=0.0,
                                               base=-1, pattern=[[1, 128]], channel_multiplier=-1), r=[Lst], w=[Lst])
        memset('pool', c128[:], float(cfg.BLK), [c128])
        S.op('dve', lambda e: e.tensor_tensor_scan(out=JR[:], data0=ones[:, :NB], data1=c128[:], initial=-float(cfg.BLK),
                                                   op0=ALU.mult, op1=ALU.add), r=[ones, c128], w=[JR])
        cp('dve', THR[:], JR[:, 0:18], [JR], [THR])

        dma('sp', vecs[:], vecs_d, [], [vecs])
        dma('sp', wr[:].rearrange("p l c n -> p (l c n)"), wr_d, [], [wr])
        dma('sp', br[:], br_d, [], [br])
        for (t0, n, sidx, gi) in cfg.groups:
            dma('sp', XT[:, :, t0:t0 + n], xT_v[:, :, t0:t0 + n], [], [XK(gi)])

        act(scT[:, :, 0], V(cfg.V_C, 8), AF.Silu, [vecs], [scT])
        act(scT[:, :, 1], V(cfg.V_CC, 8), AF.Silu, [vecs], [scT])
        with contextlib.ExitStack() as ph:
            nlb = 2 * DEPTH * 4
            E = sb(ph, "lbE", [128, nlb])
            sE = sb(ph, "lbS", [128, 8])
            rE = sb(ph, "lbR", [128, 8])
            act(E[:], V(cfg.V_LB, nlb), AF.Exp, [vecs], [E])
            E3 = E[:].rearrange("p (d l h) -> p d l h", d=2, l=DEPTH)
            sE2 = sE[:].rearrange("p (d h) -> p d h", d=2)
            cp('dve', sE2, E3[:, :, 0, :], [E], [sE])
            for l in range(1, DEPTH):
                tt('dve', sE2, sE2, E3[:, :, l, :], ALU.add, [sE, E], [sE])
            recip(rE[:], sE[:], [sE], [rE])
            rE2 = rE[:].rearrange("p (d h) -> p d h", d=2)
            lb3 = lbT[:].rearrange("p (d l h) -> p d l h", d=2, l=DEPTH)
            memset('dve', lbT[:], 0.0, [lbT])
            for l in range(1, DEPTH):
                tt('dve', E3[:, :, l, :], E3[:, :, l, :], rE2, ALU.mult, [E, rE], [E])
                tt('dve', lb3[:, :, l, :], lb3[:, :, l - 1, :], E3[:, :, l, :], ALU.add, [lbT, E], [lbT])
            tsc('dve', omlT[:], lbT[:], -1.0, 1.0, ALU.mult, ALU.add, [lbT], [omlT])

            wa = [sb(ph, "wada%d" % i, [128, DC, 512]) for i in range(4)]
            k = 0
            for l in cfg.layers:
                voff = cfg.V_L0 + l * cfg.V_LN
                for jb in range(12):
                    wt = wa[k % 4]
                    k += 1
                    dma('sp', wt[:], wada_d[l, :, jb * 512:(jb + 1) * 512].rearrange("(c p) n -> p c n", p=128), [], [wt])
                    pm = pb[jb % 2]
                    for jj in range(4):
                        for c in range(DC):
                            mm(pm[:, jj * 2:jj * 2 + 2], wt[:, c, jj * 128:(jj + 1) * 128], scT[:, c, :], c == 0, c == DC - 1,
                               [wt, scT], [pm])
                    for s in range(2):
                        tt('dve', modT[:, l, jb * 4:jb * 4 + 4, s], pm[:, 0:8].rearrange("p (j s) -> p j s", s=2)[:, :, s],
                           V(voff + 16 + jb * 4, 4), ALU.add, [pm, vecs], [modT])
                for s in range(2):
                    stt(A1[:, l, :, s], modT[:, l, 8:16, s], 1.0, V(voff + 0, 8), ALU.add, ALU.mult, [modT, vecs], [A1])
                    stt(A2[:, l, :, s], modT[:, l, 32:40, s], 1.0, V(voff + 8, 8), ALU.add, ALU.mult, [modT, vecs], [A2])
        S.barrier()

        def norm_modulate(l, which, router):
            A = A1 if which == 0 else A2
            sh0 = 0 if which == 0 else 24
            with contextlib.ExitStack() as ph:
                sq = [sb(ph, "nsq%d" % i, [128, 512]) for i in range(2)]
                rs = [sb(ph, "nrs%d" % i, [128, 512]) for i in range(2)]
                tmp = [sb(ph, "ntmp%d" % i, [128, 512]) for i in range(2)]
                if router:
                    h2f = [sb(ph, "h2f%d" % i, [128, DC, 512]) for i in range(2)]
                    rt = {nm: [sb(ph, "rt_%s%d" % (nm, i), shp) for i in range(2)] for nm, shp in
                          [("lg", [128, 36]), ("gm", [128, 1]), ("ngm", [128, 1]), ("gmask", [128, 4]), ("ge", [128, 4]),
                           ("gs", [128, 1]), ("pen", [128, 4]), ("el", [128, 32]), ("t8", [128, 8]), ("nm1", [128, 1]),
                           ("sel", [128, 32]), ("ex", [128, 32]), ("gx", [128, 32]), ("den", [128, 1]), ("pr", [128, 1]),
                           ("rp", [128, 1])]}
                kk = 0
                tl = 0
                cpar = [0]
                if router:
                    cums = [sb(ph, "cums%d" % i, [128, 32]) for i in range(2)]
                    memset('dve', cums[0][:], 0.0, [cums[0]])
                for (t0, n, sidx, gi) in cfg.groups:
                    pss = pb[gi % 2]
                    for c in range(DC):
                        q = sq[kk % 2]
                        kk += 1
                        act(q[:, :n], XT[:, c, t0:t0 + n], AF.Square, [XK(gi)], [q])
                        mm(pss[:, :n], ones[:], q[:, :n], c == 0, c == DC - 1, [ones, q], [pss])
                    r_ = rs[gi % 2]
                    act(r_[:, :n], pss[:, :n], AF.Sqrt, [pss], [r_], bias=cfg.EPS, scale=1.0 / 1024)
                    recip(r_[:, :n], r_[:, :n], [r_], [r_])
                    for c in range(DC):
                        tm = tmp[kk % 2]
                        kk += 1
                        tt('dve', tm[:, :n], XT[:, c, t0:t0 + n], r_[:, :n], ALU.mult, [XK(gi), r_], [tm])
                        if router:
                            hf = h2f[gi % 2]
                            act(hf[:, c, :n], tm[:, :n], AF.Identity, [tm, A, modT], [hf],
                                bias=modT[:, l, sh0 + c, sidx:sidx + 1], scale=A[:, l, c, sidx:sidx + 1])
                            cp('pool', hT[:, c, t0:t0 + n], hf[:, c, :n], [hf], [HK(gi)])
                        else:
                            act(hT[:, c, t0:t0 + n], tm[:, :n], AF.Identity, [tm, A, modT], [HK(gi)],
                                bias=modT[:, l, sh0 + c, sidx:sidx + 1], scale=A[:, l, c, sidx:sidx + 1])
                    if router:
                        hf = h2f[gi % 2]
                        for ti in range(n // 128):
                            tg = (t0 + ti * 128) // 128
                            R = {nm: v[tl % 2] for nm, v in rt.items()}
                            pl = pb[2 + tl % 2]
                            tl += 1
                            for c in range(DC):
                                mm(pl[:, 0:36], hf[:, c, ti * 128:(ti + 1) * 128], wr[:, l, c, :], c == 0, False, [hf, wr], [pl])
                            mm(pl[:, 0:36], ones1[:], br[:, l * 36:(l + 1) * 36], False, True, [ones1, br], [pl])
                            lg = R["lg"]
                            cp('dve', lg[:], pl[:, 0:36], [pl], [lg])
                            S.op('dve', lambda e, o=R["gm"], i=lg: e.tensor_reduce(out=o[:], in_=i[:, 0:4], axis=AX.X, op=ALU.max),
                                 r=[lg], w=[R["gm"]])
                            tsc('dve', R["ngm"][:], R["gm"][:], -1.0, None, ALU.mult, None, [R["gm"]], [R["ngm"]])
                            tsc('dve', R["gmask"][:], lg[:, 0:4], R["gm"][:], None, ALU.is_equal, None, [lg, R["gm"]], [R["gmask"]])
                            act(R["ge"][:], lg[:, 0:4], AF.Exp, [lg, R["ngm"]], [R["ge"], R["gs"]], bias=R["ngm"][:], scale=1.0,
                                accum=R["gs"][:])
                            tsc('dve', R["pen"][:], R["gmask"][:], 1e30, -1e30, ALU.mult, ALU.add, [R["gmask"]], [R["pen"]])
                            tt('dve', R["el"][:].rearrange("p (g e) -> p g e", g=4), lg[:, 4:36].rearrange("p (g e) -> p g e", g=4),
                               R["pen"][:].unsqueeze(2).to_broadcast([128, 4, 8]), ALU.add, [lg, R["pen"]], [R["el"]])
                            S.op('dve', lambda e, o=R["t8"], i=R["el"]: e.max(out=o[:], in_=i[:]), r=[R["el"]], w=[R["t8"]])
                            tsc('dve', R["nm1"][:], R["t8"][:, 0:1], -1.0, None, ALU.mult, None, [R["t8"]], [R["nm1"]])
                            tsc('dve', R["sel"][:], R["el"][:], R["t8"][:, 1:2], None, ALU.is_ge, None, [R["el"], R["t8"]], [R["sel"]])
                            act(R["ex"][:], R["el"][:], AF.Exp, [R["el"], R["nm1"]], [R["ex"]], bias=R["nm1"][:], scale=1.0)
                            tt('dve', R["gx"][:], R["sel"][:], R["ex"][:], ALU.mult, [R["sel"], R["ex"]], [R["gx"]])
                            S.op('dve', lambda e, o=R["den"], i=R["gx"]: e.tensor_reduce(out=o[:], in_=i[:], axis=AX.X, op=ALU.add),
                                 r=[R["gx"]], w=[R["den"]])
                            tt('dve', R["pr"][:], R["den"][:], R["gs"][:], ALU.mult, [R["den"], R["gs"]], [R["pr"]])
                            recip(R["rp"][:], R["pr"][:], [R["pr"]], [R["rp"]])
                            tsc('dve', G[:, tg, :], R["gx"][:], R["rp"][:], None, ALU.mult, None, [R["gx"], R["rp"]], [('G', gi)])
                            cp('dve', SEL[:, tg, :], R["sel"][:], [R["sel"]], [('SEL', tg)])
                            prk = pb[4 + tl % 2]
                            mm(prk[:, 0:32], Lst[:], R["sel"][:], True, False, [Lst, R["sel"]], [prk])
                            mm(prk[:, 0:32], ones[:], cums[cpar[0]][:], False, True, [ones, cums[cpar[0]]], [prk])
                            cp('dve', RK[:, tg, :], prk[:, 0:32], [prk], [('RK', tg)])
                            tt('dve', cums[1 - cpar[0]][:], cums[cpar[0]][:], R["sel"][:], ALU.add, [cums[cpar[0]], R["sel"]], [cums[1 - cpar[0]]])
                            cpar[0] = 1 - cpar[0]
                if router:
                    CNT = sb(ph, "CNT", [128, 32])
                    cmp18 = sb(ph, "cmp18", [128, 32, 18])
                    NBLK = sb(ph, "NBLK", [128, 32])
                    PADD = sb(ph, "PADD", [128, 32])
                    PEND = sb(ph, "PEND", [128, 32])
                    PST = sb(ph, "PST", [128, 32])
                    cmpB = sb(ph, "cmpB", [128, NB, 32])
                    BEf = sb(ph, "BEf", [128, NB])
                    pc = pb[6]
                    mm(pc[:, 0:32], ones[:], cums[cpar[0]][:], True, True, [ones, cums[cpar[0]]], [pc])
                    cp('dve', CNT[:], pc[:, 0:32], [pc], [CNT])
                    tt('dve', cmp18[:], CNT[:].unsqueeze(2).to_broadcast([128, 32, 18]), THR[:].unsqueeze(1).to_broadcast([128, 32, 18]),
                       ALU.is_gt, [CNT, THR], [cmp18])
                    S.op('dve', lambda e: e.tensor_reduce(out=NBLK[:], in_=cmp18[:], axis=AX.X, op=ALU.add), r=[cmp18], w=[NBLK])
                    tsc('dve', PADD[:], NBLK[:], float(cfg.BLK), None, ALU.mult, None, [NBLK], [PADD])
                    S.op('dve', lambda e: e.tensor_tensor_scan(out=PEND[:], data0=ones[:, 0:32], data1=PADD[:], initial=0.0,
                                                               op0=ALU.mult, op1=ALU.add), r=[ones, PADD], w=[PEND])
                    tt('dve', PST[:], PEND[:], PADD[:], ALU.subtract, [PEND, PADD], [PST])
                    tt('dve', cmpB[:], PEND[:].unsqueeze(1).to_broadcast([128, NB, 32]), JR[:].unsqueeze(2).to_broadcast([128, NB, 32]),
                       ALU.is_le, [PEND, JR], [cmpB])
                    S.op('dve', lambda e: e.tensor_reduce(out=BEf[:], in_=cmpB[:], axis=AX.X, op=ALU.add), r=[cmpB], w=[BEf])
                    tsc('dve', BEf[:], BEf[:], 31.0, float(32 * l), ALU.min, ALU.add, [BEf], [BEf])
                    UNU = sb(ph, "UNU", [128, NB])
                    tsc('dve', UNU[:], JR[:], PEND[:, 31:32], 8192.0, ALU.is_ge, ALU.mult, [JR, PEND], [UNU])
                    tt('dve', BEf[:], BEf[:], UNU[:], ALU.add, [BEf, UNU], [BEf])
                    tsc('dve', IDXW[:], BEf[:], 128.0, PIDX[:], ALU.mult, ALU.add, [BEf, PIDX], [IDXW])
                    pp = {nm: [sb(ph, "pp_%s%d" % (nm, i), shp) for i in range(2)] for nm, shp in
                          [("pos", [128, 32]), ("t8", [128, 8]), ("eq", [128, 32]), ("pr", [128, 32])]}
                    for tg in range(cfg.NT):
                        Q = {nm: v[tg % 2] for nm, v in pp.items()}
                        gi_t = tile_gi[tg][0]
                        tt('dve', Q["pos"][:], RK[:, tg, :], PST[:], ALU.add, [('RK', tg), PST], [Q["pos"]])
                        stt(Q["pos"][:], Q["pos"][:], 1.0, SEL[:, tg, :], ALU.add, ALU.mult, [Q["pos"], ('SEL', tg)], [Q["pos"]])
                        S.op('dve', lambda e, o=Q["t8"], i=Q["pos"]: e.max(out=o[:], in_=i[:]), r=[Q["pos"]], w=[Q["t8"]])
                        tsc('dve', IDX[:, tg, :], Q["t8"][:, 0:2], -1.0, None, ALU.add, None, [Q["t8"]], [('IDX', tg)])
                        for k in range(2):
                            tsc('dve', Q["eq"][:], Q["pos"][:], Q["t8"][:, k:k + 1], None, ALU.is_equal, None, [Q["pos"], Q["t8"]], [Q["eq"]])
                            tt('dve', Q["pr"][:], Q["eq"][:], G[:, tg, :], ALU.mult, [Q["eq"], ('G', gi_t)], [Q["pr"]])
                            S.op('dve', lambda e, o=GHL[:, tg, k:k + 1], i=Q["pr"]: e.tensor_reduce(out=o, in_=i[:], axis=AX.X, op=ALU.add),
                                 r=[Q["pr"]], w=[('GHL', tg)])
            S.barrier()

        def out_proj(l, Y, nk, k0, ph):
            Wo = sb(ph, "Wo", [128, nk, 1024], BF16)
            dma('pool', Wo[:], wout_d[l, k0 * 128:(k0 + nk) * 128, :].rearrange("(c p) n -> p c n", p=128), [], [Wo])
            kk = 0
            for (t0, n, sidx, gi) in cfg.groups:
                for j in range(DC):
                    po = pb[4 + kk % 4]
                    kk += 1
                    for c in range(nk):
                        rhs = Y[:, c, t0:t0 + n] if nk > 1 else Y[:, t0:t0 + n]
                        mm(po[:, :n], Wo[:, c, j * 128:(j + 1) * 128], rhs, c == 0, c == nk - 1, [Wo, Y], [po])
                    stt(XT[:, j, t0:t0 + n], po[:, :n], modT[:, l, 16 + j, sidx:sidx + 1], XT[:, j, t0:t0 + n],
                        ALU.mult, ALU.add, [po, modT, XK(gi)], [XK(gi)])

        def load_win(ph, name, l, col):
            W = sb(ph, name, [128, DC, 128], BF16)
            dma('pool', W[:], win_d[l, :, col:col + 128].rearrange("(c p) n -> p c n", p=128), [], [W])
            return W

        def proj(W, ps, t0, n, gi):
            for c in range(DC):
                mm(ps[:, :n], W[:, c, :], hT[:, c, t0:t0 + n], c == 0, c == DC - 1, [W, HK(gi)], [ps])

        def conv_phase(l):
            voff = cfg.V_L0 + l * cfg.V_LN
            R_ = TL // 64
            with contextlib.ExitStack() as ph:
                Z = sb(ph, "Z", [128, 4, T], BF16)
                SS = sb(ph, "SS", [128, T])
                u = sb(ph, "u", [128, T])
                Bs = sb(ph, "Bs", [128, T])
                y = sb(ph, "y", [128, T])
                hv = [sb(ph, "hv%d" % i, [128, 512]) for i in range(2)]
                zs = [sb(ph, "zs%d" % i, [128, 512]) for i in range(2)]
                for cc in range(4):
                    with contextlib.ExitStack() as ph2:
                        WB = load_win(ph2, "WB", l, cc * 128)
                        WC = load_win(ph2, "WC", l, 512 + cc * 128)
                        WH = load_win(ph2, "WH", l, 1024 + cc * 128)
                        for (t0, n, sidx, gi) in cfg.groups:
                            pB, pC, pH = pb[0 + 4 * (gi % 2)], pb[1 + 4 * (gi % 2)], pb[2 + 4 * (gi % 2)]
                            proj(WB, pB, t0, n, gi)
                            proj(WC, pC, t0, n, gi)
                            proj(WH, pH, t0, n, gi)
                            h_ = hv[gi % 2]
                            act(h_[:, :n], pH[:, :n], AF.Copy, [pH], [h_])
                            act(Bs[:, t0:t0 + n], pB[:, :n], AF.Copy, [pB], [Bs])
                            tt('dve', u[:, t0:t0 + n], pC[:, :n], h_[:, :n], ALU.mult, [pC, h_], [u])
                        w0, w1, w2 = V(voff + 64 + 0 * 4 + cc), V(voff + 64 + 1 * 4 + cc), V(voff + 64 + 2 * 4 + cc)
                        act(y[:], u[:], AF.Identity, [u, vecs], [y], scale=w1)
                        stt(y[:, 1:TC], u[:, 0:TC - 1], w0, y[:, 1:TC], ALU.mult, ALU.add, [u, vecs, y], [y])
                        stt(y[:, 0:TC - 1], u[:, 1:TC], w2, y[:, 0:TC - 1], ALU.mult, ALU.add, [u, vecs, y], [y])
                        if cc < 2:
                            ul = u[:, TC:T].rearrange("p (r w) -> p r w", w=64)
                            yl = y[:, TC:T].rearrange("p (r w) -> p r w", w=64)
                            stt(yl[:, :, 1:64], ul[:, :, 0:63], w0, yl[:, :, 1:64], ALU.mult, ALU.add, [u, vecs, y], [y])
                            stt(yl[:, :, 0:63], ul[:, :, 1:64], w2, yl[:, :, 0:63], ALU.mult, ALU.add, [u, vecs, y], [y])
                        else:
                            stt(y[:, TC + 64:T], u[:, TC:T - 64], w0, y[:, TC + 64:T], ALU.mult, ALU.add, [u, vecs, y], [y])
                            stt(y[:, TC:T - 64], u[:, TC + 64:T], w2, y[:, TC:T - 64], ALU.mult, ALU.add, [u, vecs, y], [y])
                        tt('dve', y[:], y[:], Bs[:], ALU.mult, [y, Bs], [y])
                        act(Z[:, cc, :], y[:], AF.Copy, [y], [Z])
                        for (t0, n, sidx, gi) in cfg.groups:
                            z_ = zs[gi % 2]
                            pz = pb[3 + 4 * (gi % 2)]
                            act(z_[:, :n], y[:, t0:t0 + n], AF.Square, [y], [z_])
                            mm(pz[:, :n], ones[:], z_[:, :n], True, True, [ones, z_], [pz])
                            if cc == 0:
                                cp('dve', SS[:, t0:t0 + n], pz[:, :n], [pz], [SS])
                            else:
                                tt('dve', SS[:, t0:t0 + n], pz[:, :n], SS[:, t0:t0 + n], ALU.add, [pz, SS], [SS])
                    S.barrier()
                act(SS[:], SS[:], AF.Sqrt, [SS], [SS], bias=cfg.EPS, scale=1.0 / 512)
                recip(SS[:], SS[:], [SS], [SS])
                for cc in range(4):
                    stt(Z[:, cc, :], Z[:, cc, :], V(voff + 76 + cc), SS[:], ALU.mult, ALU.mult, [Z, vecs, SS], [Z])
                out_proj(l, Z, 4, 0, ph)
            S.barrier()

        def heads_phase(l):
            voff = cfg.V_L0 + l * cfg.V_LN
            hnw = V(voff + 80)
            lat = cfg.groups[1:]
            order = [cfg.groups, [cfg.groups[0]] + lat[::-1]]
            for hh in range(4):
                with contextlib.ExitStack() as ph:
                    Wq = load_win(ph, "Wq", l, 1536 + hh * 128)
                    Wf = [load_win(ph, "Wzf", l, 2048 + hh * 128), load_win(ph, "Wzb", l, 2560 + hh * 128)]
                    Wi = load_win(ph, "Wi", l, 3072 + hh * 128)
                    Wg = load_win(ph, "Wg", l, 3584 + hh * 128)
                    Of = sb(ph, "Of", [128, T])
                    Yh = sb(ph, "Yh", [128, T], BF16)
                    NS = 4
                    St = [sb(ph, "St%d" % i, [128, 128]) for i in range(NS)]
                    names = ["qs", "sg", "kk", "vs", "b", "eb", "enb", "ko", "gs"]
                    tmps = [{nm: sb(ph, "g%s%d" % (nm, i), [128, 512]) for nm in names} for i in range(2)]
                    dch = [sb(ph, "dch%d" % i, [128, 16]) for i in range(2)]
                    totc = [sb(ph, "totc%d" % i, [128, 16]) for i in range(2)]
                    koT = [sb(ph, "koT%d" % i, [CH, NCHT, 128]) for i in range(2)]
                    vT = [sb(ph, "vT%d" % i, [CH, NCHT, 128]) for i in range(2)]
                    PT = [sb(ph, "PT%d" % i, [CH, NCHT, CH]) for i in range(2)]
                    midc = [sb(ph, "midc%d" % i, [128, 16]) for i in range(2)]
                    emid = [sb(ph, "emid%d" % i, [128, 16]) for i in range(2)]
                    etm = [sb(ph, "etm%d" % i, [128, 16]) for i in range(2)]
                    osq = sb(ph, "osq", [128, 512])
                    ors = sb(ph, "ors", [128, 512])
                    gpar = 0
                    tpar = 0
                    for dirn in range(2):
                        lbc = (dirn * DEPTH + l) * 4 + hh
                        lb_ap, oml_ap = lbT[:, lbc:lbc + 1], omlT[:, lbc:lbc + 1]
                        cur = 0
                        memset('dve', St[0][:], 0.0, [St[0]])
                        msk = maskF if dirn == 0 else maskB
                        for (t0, n, sidx, gi) in order[dirn]:
                            tp = tmps[gpar % 2]
                            dc_ = dch[gpar % 2]
                            gpar += 1
                            nch = n // CH
                            pq, pz_, pi_, pg = pb[0], pb[1], pb[2], pb[3]
                            proj(Wq, pq, t0, n, gi)
                            proj(Wf[dirn], pz_, t0, n, gi)
                            proj(Wi, pi_, t0, n, gi)
                            act(tp["qs"][:, :n], pq[:, :n], AF.Silu, [pq], [tp["qs"]])
                            act(tp["sg"][:, :n], pz_[:, :n], AF.Sigmoid, [pz_], [tp["sg"]])
                            act(tp["vs"][:, :n], pi_[:, :n], AF.Copy, [pi_], [tp["vs"]])
                            if dirn == 1:
                                proj(Wg, pg, t0, n, gi)
                                act(tp["gs"][:, :n], pg[:, :n], AF.Silu, [pg], [tp["gs"]])
                            tc_ = totc[(gpar - 1) % 2]
                            tsc('dve', tp["sg"][:, :n], tp["sg"][:, :n], oml_ap, lb_ap, ALU.mult, ALU.add, [tp["sg"], omlT, lbT], [tp["sg"]])
                            tsc('dve', tp["kk"][:, :n], tp["sg"][:, :n], -1.0, 1.0, ALU.mult, ALU.add, [tp["sg"]], [tp["kk"]])
                            act(tp["sg"][:, :n], tp["sg"][:, :n], AF.Ln, [tp["sg"]], [tp["sg"]])
                            S.op('dve', lambda e, o=tp["b"], m=rmask, d1=tp["sg"], n=n: e.tensor_tensor_scan(
                                out=o[:, :n], data0=m[:, :n], data1=d1[:, :n], initial=0.0, op0=ALU.mult, op1=ALU.add),
                                r=[rmask, tp["sg"]], w=[tp["b"]])
                            b3 = tp["b"][:, :n].rearrange("p (a c) -> p a c", c=CH)
                            cp('dve', tc_[:, :nch], b3[:, :, CH - 1], [tp["b"]], [tc_])
                            act(dc_[:, :nch], tc_[:, :nch], AF.Exp, [tc_], [dc_])
                            bb = tp["b"]
                            if dirn == 1:
                                tt('dve', b3, b3, tc_[:, :nch].unsqueeze(2).to_broadcast([128, nch, CH]), ALU.subtract, [tp["b"], tc_], [tp["b"]])
                                tt('dve', bb[:, :n], tp["sg"][:, :n], bb[:, :n], ALU.subtract, [tp["sg"], bb], [bb])
                            md_, em_, et_ = midc[(gpar - 1) % 2], emid[(gpar - 1) % 2], etm[(gpar - 1) % 2]
                            midcol = CH // 2 - 1 if dirn == 0 else CH // 2
                            cp('dve', md_[:, :nch], b3[:, :, midcol], [tp["b"]], [md_])
                            tt('dve', b3, b3, md_[:, :nch].unsqueeze(2).to_broadcast([128, nch, CH]), ALU.subtract, [tp["b"], md_], [tp["b"]])
                            act(tp["eb"][:, :n], bb[:, :n], AF.Exp, [bb], [tp["eb"]])
                            act(tp["enb"][:, :n], bb[:, :n], AF.Exp, [bb], [tp["enb"]], scale=-1.0)
                            act(em_[:, :nch], md_[:, :nch], AF.Exp, [md_], [em_])
                            tt('dve', et_[:, :nch], tc_[:, :nch], md_[:, :nch], ALU.subtract, [tc_, md_], [et_])
                            act(et_[:, :nch], et_[:, :nch], AF.Exp, [et_], [et_])
                            tt('dve', tp["qs"][:, :n], tp["qs"][:, :n], tp["eb"][:, :n], ALU.mult, [tp["qs"], tp["eb"]], [tp["qs"]])
                            tt('dve', tp["kk"][:, :n], tp["kk"][:, :n], tp["enb"][:, :n], ALU.mult, [tp["kk"], tp["enb"]], [tp["kk"]])
                            tt('dve', tp["eb"][:, :n].rearrange("p (a c) -> p a c", c=CH),
                               tp["qs"][:, :n].rearrange("p (a c) -> p a c", c=CH),
                               em_[:, :nch].unsqueeze(2).to_broadcast([128, nch, CH]), ALU.mult, [tp["qs"], em_], [tp["eb"]])
                            tt('dve', tp["ko"][:, :n].rearrange("p (a c) -> p a c", c=CH),
                               tp["kk"][:, :n].rearrange("p (a c) -> p a c", c=CH),
                               et_[:, :nch].unsqueeze(2).to_broadcast([128, nch, CH]), ALU.mult, [tp["kk"], et_], [tp["ko"]])
                            tiles = list(range(n // 128))
                            chunks = list(range(NCHT))
                            if dirn == 1:
                                tiles = tiles[::-1]
                                chunks = chunks[::-1]
                            for ti in tiles:
                                c0 = ti * 128
                                kT_, vT_, PT_ = koT[tpar % 2], vT[tpar % 2], PT[tpar % 2]
                                tpar += 1
                                pk, pv = pb[4], pb[5]
                                pk3 = pk[0:CH, 0:NCHT * 128].rearrange("p (j k) -> p j k", j=NCHT)
                                pv3 = pv[0:CH, 0:NCHT * 128].rearrange("p (j k) -> p j k", j=NCHT)
                                for j in range(NCHT):
                                    tr(pk3[:, j, :], tp["ko"][:, c0 + CH * j:c0 + CH * j + CH], [tp["ko"]], [pk])
                                for j in range(NCHT):
                                    tr(pv3[:, j, :], tp["vs"][:, c0 + CH * j:c0 + CH * j + CH], [tp["vs"]], [pv])
                                act(kT_[:], pk3, AF.Copy, [pk], [kT_])
                                cp('dve', vT_[:], pv3, [pv], [vT_])
                                par = tpar % 2
                                psc = pb[3][0:CH, 256:256 + NCHT * CH].rearrange("p (j k) -> p j k", j=NCHT)
                                for j in range(NCHT):
                                    cs = c0 + CH * j
                                    mm(psc[:, j, :], tp["kk"][:, cs:cs + CH], tp["qs"][:, cs:cs + CH], True, True,
                                       [tp["kk"], tp["qs"]], [pb[3]])
                                tsc('dve', PT_[:], psc, -1e30, 1e30, ALU.max, ALU.min, [pb[3]], [PT_])
                                tt('dve', PT_[:], PT_[:], msk[:], ALU.mult, [PT_, msk], [PT_])
                                po = pb[7][:, 256 * par:256 * par + 128]
                                for j in chunks:
                                    cs = c0 + CH * j
                                    jg = cs // CH
                                    mm(pb[6][:, 128 * j:128 * j + 128], kT_[:, j, :], vT_[:, j, :], True, True, [kT_, vT_], [('pb6', j)])
                                    mm(po[:, CH * j:CH * j + CH], St[cur][:], tp["eb"][:, cs:cs + CH], True, False, [St[cur], tp["eb"]], [('pb7', 'o', par)])
                                    mm(po[:, CH * j:CH * j + CH], vT_[:, j, :], PT_[:, j, :], False, True, [vT_, PT_], [('pb7', 'o', par)])
                                    nxt = (cur + 1) % NS
                                    stt(St[nxt][:], St[cur][:], dc_[:, jg:jg + 1], pb[6][:, 128 * j:128 * j + 128], ALU.mult, ALU.add,
                                        [St[cur], dc_, ('pb6', j)], [St[nxt]])
                                    cur = nxt
                                if dirn == 0:
                                    act(Of[:, t0 + c0:t0 + c0 + 128], po[:, 0:128], AF.Copy, [('pb7', 'o', par)], [Of])
                                else:
                                    tt('dve', Of[:, t0 + c0:t0 + c0 + 128], po[:, 0:128], Of[:, t0 + c0:t0 + c0 + 128], ALU.add, [('pb7', 'o', par), Of], [Of])
                            if dirn == 1:
                                pn = pb[3]
                                act(osq[:, :n], Of[:, t0:t0 + n], AF.Square, [Of], [osq])
                                mm(pn[:, :n], ones[:], osq[:, :n], True, True, [ones, osq], [pn])
                                act(ors[:, :n], pn[:, :n], AF.Sqrt, [pn], [ors], bias=cfg.EPS, scale=1.0 / 128)
                                recip(ors[:, :n], ors[:, :n], [ors], [ors])
                                stt(osq[:, :n], Of[:, t0:t0 + n], hnw, ors[:, :n], ALU.mult, ALU.mult, [Of, vecs, ors], [osq])
                                tt('dve', Yh[:, t0:t0 + n], osq[:, :n], tp["gs"][:, :n], ALU.mult, [osq, tp["gs"]], [Yh])
                    out_proj(l, Yh, 1, 4 + hh, ph)
                S.barrier()

        def moe_phase_dense(l):
            with contextlib.ExitStack() as ph:
                WA = [sb(ph, "WA%d" % i, [128, DC, 512], BF16) for i in range(2)]
                WU = [sb(ph, "WU%d" % i, [128, DC, 512], BF16) for i in range(2)]
                WD = [sb(ph, "WD%d" % i, [128, 4, 1024], BF16) for i in range(2)]
                gbc = [sb(ph, "gbc%d" % i, [128, 512], BF16) for i in range(2)]
                sa = [sb(ph, "sa%d" % i, [128, 512]) for i in range(2)]
                t1 = [sb(ph, "t1%d" % i, [128, 512], BF16) for i in range(2)]
                hm = [sb(ph, "hm%d" % i, [128, 4, 512], BF16) for i in range(2)]

                def load(e):
                    s = e % 2
                    dma('pool', WA[s][:], wgu_d[l, e, :, 0:512].rearrange("(c p) n -> p c n", p=128), [], [WA[s]])
                    dma('pool', WU[s][:], wgu_d[l, e, :, 512:1024].rearrange("(c p) n -> p c n", p=128), [], [WU[s]])
                    dma('pool', WD[s][:], wdn_d[l, e, :, :].rearrange("(c p) n -> p c n", p=128), [], [WD[s]])

                load(0)
                kk = 0
                k2 = 0
                for e in range(cfg.NE):
                    if e + 1 < cfg.NE:
                        load(e + 1)
                    s = e % 2
                    for (t0, n, sidx, gi) in cfg.groups:
                        pg = pb[0]
                        for ti in range(n // 128):
                            tg = (t0 + ti * 128) // 128
                            mm(pg[:, ti * 128:(ti + 1) * 128], G[:, tg, e:e + 1].to_broadcast([128, 128]), ident[:], True, True,
                               [('G', gi), ident], [pg])
                        gb = gbc[kk % 2]
                        hm_ = hm[kk % 2]
                        kk += 1
                        act(gb[:, :n], pg[:, :n], AF.Copy, [pg], [gb])
                        for hc in range(4):
                            pa, pu = pb[1 + 2 * (k2 % 2)], pb[2 + 2 * (k2 % 2)]
                            sa_, t1_ = sa[k2 % 2], t1[k2 % 2]
                            k2 += 1
                            for c in range(DC):
                                mm(pa[:, :n], WA[s][:, c, hc * 128:(hc + 1) * 128], hT[:, c, t0:t0 + n], c == 0, c == DC - 1,
                                   [WA[s], HK(gi)], [pa])
                            for c in range(DC):
                                mm(pu[:, :n], WU[s][:, c, hc * 128:(hc + 1) * 128], hT[:, c, t0:t0 + n], c == 0, c == DC - 1,
                                   [WU[s], HK(gi)], [pu])
                            act(sa_[:, :n], pa[:, :n], AF.Silu, [pa], [sa_])
                            tt('dve', t1_[:, :n], sa_[:, :n], pu[:, :n], ALU.mult, [sa_, pu], [t1_])
                            tt('pool', hm_[:, hc, :n], t1_[:, :n], gb[:, :n], ALU.mult, [t1_, gb], [hm_])
                        for j in range(DC):
                            py = pb[5 + j % 3]
                            for hc in range(4):
                                mm(py[:, :n], WD[s][:, hc, j * 128:(j + 1) * 128], hm_[:, hc, :n], hc == 0, hc == 3, [WD[s], hm_], [py])
                            stt(XT[:, j, t0:t0 + n], py[:, :n], modT[:, l, 40 + j, sidx:sidx + 1], XT[:, j, t0:t0 + n],
                                ALU.mult, ALU.add, [py, modT, XK(gi)], [XK(gi)])
            S.barrier()

        def moe_phase(l):
            IOA = bass.IndirectOffsetOnAxis
            with contextlib.ExitStack() as ph:
                WAU = [sb(ph, "WAU%d" % i, [128, DC, 1024], BF16) for i in range(2)]
                WD = [sb(ph, "WD%d" % i, [128, 4, 1024], BF16) for i in range(2)]
                h32 = sb(ph, "h32", [128, DC, 128])
                xb = [sb(ph, "xb%d" % i, [128, 1024]) for i in range(2)]
                yb = [sb(ph, "yb%d" % i, [128, 1024]) for i in range(2)]
                xbT = [sb(ph, "xbT%d" % i, [128, DC, 128], BF16) for i in range(2)]
                sa = [sb(ph, "sa0", [128, 512])] * 2
                hm = [sb(ph, "hm%d" % i, [128, 4, 128], BF16) for i in range(2)]
                pT = [pb[0], pb[1]]
                order = []
                for i_ in range(NB):
                    order.append(i_ // 2 if i_ % 2 == 0 else NB - 1 - i_ // 2)

                order_box.clear()
                order_box.append(order)


                wgu2 = wgu_d.rearrange("l e (p c) n -> (l e p) (c n)", c=8)
                wdn2 = wdn_d.rearrange("l e (p c) n -> (l e p) (c n)", c=4)

                def bcreg(e):
                    if 'r' not in bc_cache:
                        bc_cache['r'] = e.to_reg(DEPTH * 32 * 128 - 1)
                    return bc_cache['r']

                def load(bpos):
                    s_ = bpos % 2
                    j = order_of(bpos)
                    for q in range(4):
                        S.op('pool', lambda e, j=j, q=q, s_=s_: e.indirect_dma_start(
                            out=WAU[s_][:, 2 * q:2 * q + 2, :].rearrange("p a n -> p (a n)"), out_offset=None, in_=wgu2,
                            in_offset=IOA(ap=IDXW[:, j:j + 1], axis=0), element_offset=q * 2048, bounds_check=bcreg(e), oob_is_err=False),
                            r=[IDXW], w=[('WAU', s_, q)], dma=True)
                    for q in range(2):
                        S.op('pool', lambda e, j=j, q=q, s_=s_: e.indirect_dma_start(
                            out=WD[s_][:, 2 * q:2 * q + 2, :].rearrange("p a n -> p (a n)"), out_offset=None, in_=wdn2,
                            in_offset=IOA(ap=IDXW[:, j:j + 1], axis=0), element_offset=q * 2048, bounds_check=bcreg(e), oob_is_err=False),
                            r=[IDXW], w=[('WD', s_, q)], dma=True)

                load(0)
                scat = []
                for tg in range(cfg.NT):
                    gi, sidx = tile_gi[tg]
                    act(h32[:], hT[:, :, tg * 128:(tg + 1) * 128], AF.Copy, [HK(gi)], [h32])
                    for c in range(DC):
                        tr(pT[c // 4][:, (c % 4) * 128:(c % 4 + 1) * 128], h32[:, c, :], [h32], [pT[c // 4]])
                    x_ = xb[tg % 2]
                    act(x_[:, 0:512], pT[0][:, :], AF.Copy, [pT[0]], [x_])
                    cp('dve', x_[:, 512:1024], pT[1][:, :], [pT[1]], [x_])
                    for k in range(2):
                        scat.append(S.op('pool', lambda e, x_=x_, tg=tg, k=k: e.indirect_dma_start(
                            out=xs_d[:, :], out_offset=IOA(ap=IDX[:, tg, k:k + 1], axis=0), in_=x_[:, :], in_offset=None),
                            r=[x_, ('IDX', tg)], w=[('xs', tg, k)], dma=True))
                stores = []
                RT = cfg.BLK // 128
                def rows(pos):
                    return order[pos // RT] * RT + pos % RT

                def prefetch(rt):
                    s_ = rt % 2
                    ra = rows(rt)
                    S.op('sp', lambda e: e.dma_start(out=xb[s_][:], in_=xs_d[ra * 128:(ra + 1) * 128, :]), r=[], w=[xb[s_]],
                         dma=True, extra_deps=scat)

                def stage_a(rt):
                    w_, s_ = (rt // RT) % 2, rt % 2
                    for c in range(DC):
                        tr(pT[c // 4][:, (c % 4) * 128:(c % 4 + 1) * 128], xb[s_][:, :].rearrange("r (p c) -> r c p", c=8)[:, c, :], [xb[s_]], [pT[c // 4]])
                    act(xbT[s_][:, 0:4, :], pT[0][:, :].rearrange("p (c r) -> p c r", c=4), AF.Copy, [pT[0]], [xbT[s_]])
                    cp('dve', xbT[s_][:, 4:8, :], pT[1][:, :].rearrange("p (c r) -> p c r", c=4), [pT[1]], [xbT[s_]])
                    pa, pu = pb[2 + 2 * (rt % 2)], pb[3 + 2 * (rt % 2)]
                    for hc in range(4):
                        for c in range(DC):
                            mm(pa[:, hc * 128:(hc + 1) * 128], WAU[w_][:, c, 0:512].rearrange("p (m h) -> p h m", h=4)[:, hc, :],
                               xbT[s_][:, c, :], c == 0, c == DC - 1, [('WAU', w_, c // 2), xbT[s_]], [pa])
                    for hc in range(4):
                        for c in range(DC):
                            mm(pu[:, hc * 128:(hc + 1) * 128], WAU[w_][:, c, 512:1024].rearrange("p (m h) -> p h m", h=4)[:, hc, :],
                               xbT[s_][:, c, :], c == 0, c == DC - 1, [('WAU', w_, c // 2), xbT[s_]], [pu])
                    act(sa[s_][:], pa[:, :], AF.Silu, [pa], [sa[s_]])
                    tt('dve', hm[s_][:].rearrange("p c r -> p (c r)"), sa[s_][:], pu[:, :], ALU.mult, [sa[s_], pu], [hm[s_]])

                def stage_b(rt):
                    w_, s_ = (rt // RT) % 2, rt % 2
                    py = [pb[6], pb[7]]
                    for half in range(2):
                        for hc in range(4):
                            mm(py[half][:, :], hm[s_][:, hc, :], WD[w_][:, hc, half * 512:(half + 1) * 512], hc == 0, hc == 3,
                               [hm[s_], ('WD', w_, hc // 2)], [py[half]])
                    act(yb[s_][:, 0:512], py[0][:, :], AF.Copy, [py[0]], [yb[s_]])
                    cp('dve', yb[s_][:, 512:1024], py[1][:, :], [py[1]], [yb[s_]])
                    ra = rows(rt)
                    stores.append(S.op('act', lambda e: e.dma_start(out=ys_d[ra * 128:(ra + 1) * 128, :], in_=yb[s_][:]),
                                       r=[yb[s_]], w=[('yd', rt)], dma=True))

                prefetch(0)
                prefetch(1)
                for pos in range(NB * RT):
                    stage_a(pos)
                    if pos + 2 < NB * RT:
                        prefetch(pos + 2)
                    if pos > 0:
                        stage_b(pos - 1)
                    if pos % RT == 0 and pos // RT + 1 < NB:
                        load(pos // RT + 1)
                stage_b(NB * RT - 1)
                for tg in range(cfg.NT):
                    gi, sidx = tile_gi[tg]
                    yh_, yl_ = xb[tg % 2], yb[tg % 2]
                    S.op('pool', lambda e, yh_=yh_, tg=tg: e.indirect_dma_start(
                        out=yh_[:, :], out_offset=None, in_=ys_d[:, :], in_offset=IOA(ap=IDX[:, tg, 0:1], axis=0)),
                        r=[('IDX', tg)], w=[yh_], dma=True, extra_deps=stores)
                    S.op('pool', lambda e, yl_=yl_, tg=tg: e.indirect_dma_start(
                        out=yl_[:, :], out_offset=None, in_=ys_d[:, :], in_offset=IOA(ap=IDX[:, tg, 1:2], axis=0)),
                        r=[('IDX', tg)], w=[yl_], dma=True, extra_deps=stores)
                    tsc('dve', yh_[:], yh_[:], GHL[:, tg, 0:1], None, ALU.mult, None, [yh_, ('GHL', tg)], [yh_])
                    stt(yh_[:], yl_[:], GHL[:, tg, 1:2], yh_[:], ALU.mult, ALU.add, [yl_, ('GHL', tg), yh_], [yh_])
                    for c in range(DC):
                        tr(pT[c // 4][:, (c % 4) * 128:(c % 4 + 1) * 128], yh_[:, c * 128:(c + 1) * 128], [yh_], [pT[c // 4]])
                    for c in range(DC):
                        stt(XT[:, c, tg * 128:(tg + 1) * 128], pT[c // 4][:, (c % 4) * 128:(c % 4 + 1) * 128], modT[:, l, 40 + c, sidx:sidx + 1],
                            XT[:, c, tg * 128:(tg + 1) * 128], ALU.mult, ALU.add, [pT[c // 4], modT, XK(gi)], [XK(gi)])
            S.barrier()

        for l in cfg.layers:
            norm_modulate(l, 0, False)
            if 'conv' not in cfg.skip:
                conv_phase(l)
            if 'heads' not in cfg.skip:
                heads_phase(l)
            norm_modulate(l, 1, True)
            if 'moe' not in cfg.skip:
                moe_phase(l)

        fin = []
        with contextlib.ExitStack() as ph:
            sq = [sb(ph, "fsq%d" % i, [128, 512]) for i in range(2)]
            rs = [sb(ph, "frs%d" % i, [128, 512]) for i in range(2)]
            ob = [sb(ph, "fob%d" % i, [128, 512]) for i in range(4)]
            kk = 0
            for (t0, n, sidx, gi) in cfg.groups[1:]:
                pss = pb[gi % 2]
                for c in range(DC):
                    q = sq[kk % 2]
                    kk += 1
                    act(q[:, :n], XT[:, c, t0:t0 + n], AF.Square, [XK(gi)], [q])
                    mm(pss[:, :n], ones[:], q[:, :n], c == 0, c == DC - 1, [ones, q], [pss])
                r_ = rs[gi % 2]
                act(r_[:, :n], pss[:, :n], AF.Sqrt, [pss], [r_], bias=cfg.EPS, scale=1.0 / 1024)
                recip(r_[:, :n], r_[:, :n], [r_], [r_])
                for c in range(DC):
                    o_ = ob[kk % 4]
                    kk += 1
                    if cfg.final:
                        stt(o_[:, :n], XT[:, c, t0:t0 + n], V(cfg.V_FNW + c), r_[:, :n], ALU.mult, ALU.mult, [XK(gi), vecs, r_], [o_])
                    else:
                        cp('dve', o_[:, :n], XT[:, c, t0:t0 + n], [XK(gi)], [o_])
                    fin.append(dma('sp', outT_v[:, c, t0 - TC:t0 - TC + n], o_[:, :n], [o_], []))
        S.op('sp', lambda e: e.nop(), extra_deps=fin)
        S.run(nc)
        nc._sched_stats = S.stats
    return nc


def pack_inputs(cfg, b, x, c, ctx, c_ctx, norm_w, w_ada, b_ada, w_in, conv_w, conv_norm_w, hg_lb, hg_norm_w, w_out,
                w_rg, b_rg, w_re, b_re, w_e_gu, w_e_down, final_norm_w):
    d = cfg.DEPTH
    tok = np.concatenate([ctx[b], x[b]], axis=0)
    xT = np.ascontiguousarray(tok.T.reshape(cfg.DC, 128, cfg.T).transpose(1, 0, 2)).reshape(128, cfg.DC * cfg.T)

    def fm(v):
        return np.asarray(v).reshape(-1, 128).T

    vecs = np.zeros((128, cfg.NV), np.float32)
    vecs[:, cfg.V_C:cfg.V_C + 8] = fm(c[b])
    vecs[:, cfg.V_CC:cfg.V_CC + 8] = fm(c_ctx)
    vecs[:, cfg.V_FNW:cfg.V_FNW + 8] = fm(final_norm_w)
    for dirn in range(2):
        for l in range(d):
            o = cfg.V_LB + (dirn * d + l) * 4
            vecs[:, o:o + 4] = fm(hg_lb[dirn, l])
    for l in range(d):
        o = cfg.V_L0 + l * cfg.V_LN
        vecs[:, o:o + 8] = fm(norm_w[l, 0])
        vecs[:, o + 8:o + 16] = fm(norm_w[l, 1])
        vecs[:, o + 16:o + 64] = fm(b_ada[l])
        for tap in range(3):
            vecs[:, o + 64 + tap * 4:o + 64 + tap * 4 + 4] = fm(conv_w[l, tap])
        vecs[:, o + 76:o + 80] = fm(conv_norm_w[l])
        vecs[:, o + 80:o + 81] = fm(hg_norm_w[l])
    wr = np.concatenate([w_rg, w_re], axis=2)
    wr = np.ascontiguousarray(wr.reshape(d, cfg.DC, 128, 36).transpose(2, 0, 1, 3)).reshape(128, d * cfg.DC * 36)
    br = np.ascontiguousarray(np.concatenate([b_rg, b_re], axis=1)).reshape(1, d * 36)
    return {"xT": xT.astype(np.float32), "vecs": vecs, "wr": wr.astype(np.float32), "br": br.astype(np.float32)}


def run(cfg, inputs, n_cores):
    inputs = {k: np.asarray(v) for k, v in inputs.items()}
    nc = build_program(cfg)
    shared = {"w_ada": np.ascontiguousarray(inputs["w_ada"]), "w_in": np.ascontiguousarray(inputs["w_in"]),
              "w_out": np.ascontiguousarray(inputs["w_out"]), "w_gu": np.ascontiguousarray(inputs["w_e_gu"]),
              "w_dn": np.ascontiguousarray(inputs["w_e_down"])}
    in_maps = []
    for b in range(n_cores):
        m = pack_inputs(cfg, b, **inputs)
        m.update(shared)
        in_maps.append(m)
    res = run_bass_kernel_spmd(nc, in_maps, core_ids=list(range(n_cores)))
    outs = []
    for b in range(n_cores):
        oT = np.asarray(res.results[b]["outT"]).reshape(128, cfg.DC, cfg.TL)
        outs.append(oT.transpose(2, 1, 0).reshape(cfg.TL, cfg.D))
    return np.stack(outs, axis=0).astype(np.float32)


def kernel(**inputs):
    cfg = Cfg(depth=4, t_lat=2048)
    return run(cfg, inputs, 8)
```

```python
import contextlib
import numpy as np
import concourse.bass as bass
import concourse.mybir as mybir
from concourse.bass_utils import run_bass_kernel_spmd

F32 = mybir.dt.float32
BF16 = mybir.dt.bfloat16
AF = mybir.ActivationFunctionType
ALU = mybir.AluOpType
AX = mybir.AxisListType

ENGS = ['pe', 'act', 'dve', 'pool', 'sp']
DMA_RING = 8


class Op:
    __slots__ = ('eng', 'fn', 'deps', 'idx', 'signal', 'semkey', 'semval', 'dma', 'waits')

    def __init__(self, eng, fn, dma):
        self.eng = eng
        self.fn = fn
        self.dma = dma
        self.deps = []
        self.signal = False
        self.semkey = None
        self.semval = 0
        self.waits = []


class Sched:
    def __init__(self):
        self.ops = {e: [] for e in ENGS}
        self.bufs = {}
        self.ndma = {e: 0 for e in ENGS}
        self.dma_ops = {e: [] for e in ENGS}

    @staticmethod
    def _key(x):
        if isinstance(x, (tuple, str)):
            return x
        return x.name

    def op(self, eng, fn, r=(), w=(), dma=False, extra_deps=()):
        o = Op(eng, fn, dma)
        deps = list(extra_deps)
        rk = [self._key(x) for x in r]
        wk = [self._key(x) for x in w]
        for k in rk:
            st = self.bufs.get(k)
            if st is not None and st[0] is not None:
                deps.append(st[0])
        for k in wk:
            st = self.bufs.get(k)
            if st is not None:
                if st[0] is not None:
                    deps.append(st[0])
                deps.extend(st[1].values())
        if dma:
            i = self.ndma[eng]
            self.ndma[eng] += 1
            o.semkey = ('dma', eng, i % DMA_RING)
            o.semval = 16 * (i // DMA_RING + 1)
            if i >= DMA_RING:
                deps.append(self.dma_ops[eng][i - DMA_RING])
            self.dma_ops[eng].append(o)
        seen = set()
        for d in deps:
            if id(d) in seen or d is o:
                continue
            seen.add(id(d))
            o.deps.append(d)
        o.idx = len(self.ops[eng])
        self.ops[eng].append(o)
        for k in rk:
            st = self.bufs.setdefault(k, [None, {}])
            st[1][id(o) if dma else eng] = o
        for k in wk:
            self.bufs[k] = [o, {}]
        return o

    def barrier(self):
        last = []
        for e in ENGS:
            if self.ops[e]:
                last.append(self.ops[e][-1])
            last.extend(self.dma_ops[e][-DMA_RING:])
        for e in ENGS:
            self.op(e, lambda g: g.nop(), extra_deps=last)
        self.bufs = {}

    def finalize(self):
        for e in ENGS:
            for o in self.ops[e]:
                for d in o.deps:
                    if d.dma:
                        continue
                    if d.eng == 'pe' and o.eng == 'pe' and not o.dma:
                        continue
                    d.signal = True
        for e in ENGS:
            c = 0
            for o in self.ops[e]:
                if o.dma:
                    continue
                if o.signal:
                    c += 1
                    o.semkey = ('eng', e)
                    o.semval = c
        for e in ENGS:
            waited = {}
            for o in self.ops[e]:
                need = {}
                for d in o.deps:
                    if (not d.dma) and d.eng == 'pe' and o.eng == 'pe' and not o.dma:
                        continue
                    if d.semval > need.get(d.semkey, 0):
                        need[d.semkey] = d.semval
                for k, v in need.items():
                    if waited.get(k, 0) < v:
                        waited[k] = v
                        o.waits.append((k, v))

    def run(self, nc):
        self.finalize()
        keys = set()
        for e in ENGS:
            for o in self.ops[e]:
                if o.semkey is not None and (o.signal or o.dma):
                    keys.add(o.semkey)
        keys = sorted(keys, key=str)
        self.stats = {e: (len(self.ops[e]), max([o.semval for o in self.ops[e] if not o.dma] + [0])) for e in ENGS}
        with contextlib.ExitStack() as st:
            sems = {}
            for i, k in enumerate(keys):
                sems[k] = st.enter_context(nc.semaphore('s%d' % i))
            block = st.enter_context(nc.Block())

            def replay(eng_name):
                def body(e):
                    for o in self.ops[eng_name]:
                        for (k, v) in o.waits:
                            e.wait_ge(sems[k], v)
                        inst = o.fn(e)
                        if o.dma:
                            inst.then_inc(sems[o.semkey], 16)
                        elif o.signal:
                            inst.then_inc(sems[o.semkey], 1)
                return body

            block.tensor(replay('pe'))
            block.scalar(replay('act'))
            block.vector(replay('dve'))
            block.gpsimd(replay('pool'))
            block.sync(replay('sp'))


class Cfg:
    def __init__(self, depth=4, t_lat=2048, layers=None, first=True, final=True):
        self.D = 1024
        self.DC = 8
        self.TC = 256
        self.TL = t_lat
        self.T = self.TC + self.TL
        self.DEPTH = depth
        self.layers = list(range(depth)) if layers is None else layers
        self.first = first
        self.final = final
        self.NE = 32
        self.skip = set()
        self.EPS = 1e-6
        self.groups = [(0, 256, 1, 0)]
        for k in range(self.TL // 512):
            self.groups.append((256 + 512 * k, 512, 0, k + 1))
        self.NT = self.T // 128
        self.BLK = 256
        self.NB = -(-(2 * self.T + 32 * (self.BLK - 1)) // self.BLK)
        d = depth
        self.V_C = 0
        self.V_CC = 8
        self.V_FNW = 16
        self.V_LB = 24
        self.V_L0 = 24 + 2 * d * 4
        self.V_LN = 81
        self.NV = self.V_L0 + d * self.V_LN


def build_program(cfg):
    nc = bass.Bass("TRN2", target_bir_lowering=False)
    T, TC, TL, DC, DEPTH = cfg.T, cfg.TC, cfg.TL, cfg.DC, cfg.DEPTH
    xT_d = nc.dram_tensor("xT", [128, DC * T], F32, kind="ExternalInput").ap()
    vecs_d = nc.dram_tensor("vecs", [128, cfg.NV], F32, kind="ExternalInput").ap()
    wr_d = nc.dram_tensor("wr", [128, DEPTH * DC * 36], F32, kind="ExternalInput").ap()
    br_d = nc.dram_tensor("br", [1, DEPTH * 36], F32, kind="ExternalInput").ap()
    wada_d = nc.dram_tensor("w_ada", [DEPTH, 1024, 6144], F32, kind="ExternalInput").ap()
    win_d = nc.dram_tensor("w_in", [DEPTH, 1024, 4096], F32, kind="ExternalInput").ap()
    wout_d = nc.dram_tensor("w_out", [DEPTH, 1024, 1024], F32, kind="ExternalInput").ap()
    wgu_d = nc.dram_tensor("w_gu", [DEPTH, cfg.NE, 1024, 1024], F32, kind="ExternalInput").ap()
    wdn_d = nc.dram_tensor("w_dn", [DEPTH, cfg.NE, 512, 1024], F32, kind="ExternalInput").ap()
    outT_d = nc.dram_tensor("outT", [128, DC * TL], F32, kind="ExternalOutput").ap()
    xs_d = nc.dram_tensor("xs_scr", [cfg.NB * cfg.BLK, 1024], F32, kind="Internal").ap()
    ys_d = nc.dram_tensor("ys_scr", [cfg.NB * cfg.BLK, 1024], F32, kind="Internal").ap()
    xT_v = xT_d.rearrange("p (c t) -> p c t", c=DC)
    outT_v = outT_d.rearrange("p (c t) -> p c t", c=DC)

    S = Sched()
    uid = [0]

    with contextlib.ExitStack() as top:
        def sb(stack, name, shape, dt=F32):
            uid[0] += 1
            return stack.enter_context(nc.sbuf_tensor("%s_%d" % (name, uid[0]), shape, dt))

        XT = sb(top, "XT", [128, DC, T])
        hT = sb(top, "hT", [128, DC, T], BF16)
        G = sb(top, "G", [128, cfg.NT, 32])
        vecs = sb(top, "vecs", [128, cfg.NV])
        wr = sb(top, "wr", [128, DEPTH, DC, 36])
        br = sb(top, "br", [1, DEPTH * 36])
        ident = sb(top, "ident", [128, 128])
        ones = sb(top, "ones", [128, 128])
        ones1 = sb(top, "ones1", [1, 128])
        rmask = sb(top, "rmask", [128, 512], BF16)
        CH = 64
        NCHT = 128 // CH
        maskF = sb(top, "maskF", [CH, NCHT, CH])
        maskB = sb(top, "maskB", [CH, NCHT, CH])
        scT = sb(top, "scT", [128, DC, 2])
        modT = sb(top, "modT", [128, DEPTH, 48, 2])
        A1 = sb(top, "A1", [128, DEPTH, DC, 2])
        A2 = sb(top, "A2", [128, DEPTH, DC, 2])
        lbT = sb(top, "lbT", [128, 2 * DEPTH * 4])
        omlT = sb(top, "omlT", [128, 2 * DEPTH * 4])
        I32 = mybir.dt.int32
        NB = cfg.NB
        SEL = sb(top, "SEL", [128, cfg.NT, 32])
        RK = sb(top, "RK", [128, cfg.NT, 32])
        IDX = sb(top, "IDX", [128, cfg.NT, 2], I32)
        GHL = sb(top, "GHL", [128, cfg.NT, 2])
        IDXW = sb(top, "IDXW", [128, NB], I32)
        PIDXi = sb(top, "PIDXi", [128, 1], I32)
        PIDX = sb(top, "PIDX", [128, 1])
        Lst = sb(top, "Lst", [128, 128])
        THR = sb(top, "THR", [128, 18])
        JR = sb(top, "JR", [128, NB])
        c128 = sb(top, "c128", [128, NB])
        pb = [top.enter_context(nc.psum_tensor("pb%d" % i, [128, 512], F32)) for i in range(8)]

        tile_gi = {}
        for (t0_, n_, sidx_, gi_) in cfg.groups:
            for ti_ in range(n_ // 128):
                tile_gi[(t0_ + ti_ * 128) // 128] = (gi_, sidx_)
        tile_gi = {k_: v_ for k_, v_ in tile_gi.items()}

        bc_cache = {}
        order_box = []

        def order_of(bpos):
            return order_box[0][bpos]

        def XK(gi):
            return ('XT', gi)

        def HK(gi):
            return ('hT', gi)

        def mm(out, lhsT, rhs, start, stop, r, w):
            return S.op('pe', lambda e: e.matmul(out, lhsT, rhs, start=start, stop=stop), r=r, w=w)

        def tr(out, in_, r, w):
            return S.op('pe', lambda e: e.transpose(out, in_, ident[:in_.shape[0], :in_.shape[0]]), r=list(r) + [ident], w=w)

        def act(out, in_, func, r, w, bias=None, scale=None, accum=None):
            kw = {}
            if bias is not None:
                kw['bias'] = bias
            if scale is not None:
                kw['scale'] = scale
            if accum is not None:
                kw['accum_out'] = accum
            return S.op('act', lambda e: e.activation(out=out, in_=in_, func=func, **kw), r=r, w=w)

        def tt(eng, out, in0, in1, op, r, w):
            return S.op(eng, lambda e: e.tensor_tensor(out=out, in0=in0, in1=in1, op=op), r=r, w=w)

        def tsc(eng, out, in0, s1, s2, op0, op1, r, w):
            if op1 is None:
                return S.op(eng, lambda e: e.tensor_scalar(out=out, in0=in0, scalar1=s1, scalar2=None, op0=op0), r=r, w=w)
            return S.op(eng, lambda e: e.tensor_scalar(out=out, in0=in0, scalar1=s1, scalar2=s2, op0=op0, op1=op1), r=r, w=w)

        def stt(out, in0, scalar, in1, op0, op1, r, w):
            return S.op('dve', lambda e: e.scalar_tensor_tensor(out=out, in0=in0, scalar=scalar, in1=in1, op0=op0, op1=op1), r=r, w=w)

        def cp(eng, out, in_, r, w):
            return S.op(eng, lambda e: e.tensor_copy(out=out, in_=in_), r=r, w=w)

        def recip(out, in_, r, w):
            return S.op('dve', lambda e: e.reciprocal(out=out, in_=in_), r=r, w=w)

        def memset(eng, ap, val, w):
            return S.op(eng, lambda e: e.memset(ap, val), w=w)

        def dma(eng, out, in_, r, w):
            return S.op(eng, lambda e: e.dma_start(out=out, in_=in_), r=r, w=w, dma=True)

        def V(col, n=1):
            return vecs[:, col:col + n]

        memset('pool', ident[:], 0.0, [ident])
        S.op('pool', lambda e: e.affine_select(out=ident[:], in_=ident[:], compare_op=ALU.not_equal, fill=1.0,
                                               base=0, pattern=[[-1, 128]], channel_multiplier=1), r=[ident], w=[ident])
        memset('pool', ones[:], 1.0, [ones])
        memset('pool', ones1[:], 1.0, [ones1])
        memset('pool', rmask[:], 1.0, [rmask])
        S.op('pool', lambda e: e.memset(rmask[:].rearrange("p (a b) -> p a b", b=CH)[:, :, 0:1], 0.0), r=[rmask], w=[rmask])
        memset('pool', maskF[:], 1.0, [maskF])
        memset('pool', maskB[:], 1.0, [maskB])
        S.op('pool', lambda e: e.affine_select(out=maskF[:], in_=maskF[:], compare_op=ALU.is_ge, fill=0.0,
                                               base=0, pattern=[[0, NCHT], [1, CH]], channel_multiplier=-1), r=[maskF], w=[maskF])
        S.op('pool', lambda e: e.affine_select(out=maskB[:], in_=maskB[:], compare_op=ALU.is_ge, fill=0.0,
                                               base=0, pattern=[[0, NCHT], [-1, CH]], channel_multiplier=1), r=[maskB], w=[maskB])

        S.op('pool', lambda e: e.iota(PIDXi[:], pattern=[[0, 1]], base=0, channel_multiplier=1), w=[PIDXi])
        cp('dve', PIDX[:], PIDXi[:], [PIDXi], [PIDX])
        memset('pool', Lst[:], 1.0, [Lst])
        S.op('pool', lambda e: e.affine_select(out=Lst[:], in_=Lst[:], compare_op=ALU.is_ge, fill=0.0,
                                               base=-1, pattern=[[1, 128]], channel_multiplier=-1), r=[Lst], w=[Lst])
        memset('pool', c128[:], float(cfg.BLK), [c128])
        S.op('dve', lambda e: e.tensor_tensor_scan(out=JR[:], data0=ones[:, :NB], data1=c128[:], initial=-float(cfg.BLK),
                                                   op0=ALU.mult, op1=ALU.add), r=[ones, c128], w=[JR])
        cp('dve', THR[:], JR[:, 0:18], [JR], [THR])

        dma('sp', vecs[:], vecs_d, [], [vecs])
        dma('sp', wr[:].rearrange("p l c n -> p (l c n)"), wr_d, [], [wr])
        dma('sp', br[:], br_d, [], [br])
        for (t0, n, sidx, gi) in cfg.groups:
            dma('sp', XT[:, :, t0:t0 + n], xT_v[:, :, t0:t0 + n], [], [XK(gi)])

        act(scT[:, :, 0], V(cfg.V_C, 8), AF.Silu, [vecs], [scT])
        act(scT[:, :, 1], V(cfg.V_CC, 8), AF.Silu, [vecs], [scT])
        with contextlib.ExitStack() as ph:
            nlb = 2 * DEPTH * 4
            E = sb(ph, "lbE", [128, nlb])
            sE = sb(ph, "lbS", [128, 8])
            rE = sb(ph, "lbR", [128, 8])
            act(E[:], V(cfg.V_LB, nlb), AF.Exp, [vecs], [E])
            E3 = E[:].rearrange("p (d l h) -> p d l h", d=2, l=DEPTH)
            sE2 = sE[:].rearrange("p (d h) -> p d h", d=2)
            cp('dve', sE2, E3[:, :, 0, :], [E], [sE])
            for l in range(1, DEPTH):
                tt('dve', sE2, sE2, E3[:, :, l, :], ALU.add, [sE, E], [sE])
            recip(rE[:], sE[:], [sE], [rE])
            rE2 = rE[:].rearrange("p (d h) -> p d h", d=2)
            lb3 = lbT[:].rearrange("p (d l h) -> p d l h", d=2, l=DEPTH)
            memset('dve', lbT[:], 0.0, [lbT])
            for l in range(1, DEPTH):
                tt('dve', E3[:, :, l, :], E3[:, :, l, :], rE2, ALU.mult, [E, rE], [E])
                tt('dve', lb3[:, :, l, :], lb3[:, :, l - 1, :], E3[:, :, l, :], ALU.add, [lbT, E], [lbT])
            tsc('dve', omlT[:], lbT[:], -1.0, 1.0, ALU.mult, ALU.add, [lbT], [omlT])

            wa = [sb(ph, "wada%d" % i, [128, DC, 512]) for i in range(4)]
            k = 0
            for l in cfg.layers:
                voff = cfg.V_L0 + l * cfg.V_LN
                for jb in range(12):
                    wt = wa[k % 4]
                    k += 1
                    dma('sp', wt[:], wada_d[l, :, jb * 512:(jb + 1) * 512].rearrange("(c p) n -> p c n", p=128), [], [wt])
                    pm = pb[jb % 2]
                    for jj in range(4):
                        for c in range(DC):
                            mm(pm[:, jj * 2:jj * 2 + 2], wt[:, c, jj * 128:(jj + 1) * 128], scT[:, c, :], c == 0, c == DC - 1,
                               [wt, scT], [pm])
                    for s in range(2):
                        tt('dve', modT[:, l, jb * 4:jb * 4 + 4, s], pm[:, 0:8].rearrange("p (j s) -> p j s", s=2)[:, :, s],
                           V(voff + 16 + jb * 4, 4), ALU.add, [pm, vecs], [modT])
                for s in range(2):
                    stt(A1[:, l, :, s], modT[:, l, 8:16, s], 1.0, V(voff + 0, 8), ALU.add, ALU.mult, [modT, vecs], [A1])
                    stt(A2[:, l, :, s], modT[:, l, 32:40, s], 1.0, V(voff + 8, 8), ALU.add, ALU.mult, [modT, vecs], [A2])
        S.barrier()

        def norm_modulate(l, which, router):
            A = A1 if which == 0 else A2
            sh0 = 0 if which == 0 else 24
            with contextlib.ExitStack() as ph:
                sq = [sb(ph, "nsq%d" % i, [128, 512]) for i in range(2)]
                rs = [sb(ph, "nrs%d" % i, [128, 512]) for i in range(2)]
                tmp = [sb(ph, "ntmp%d" % i, [128, 512]) for i in range(2)]
                if router:
                    h2f = [sb(ph, "h2f%d" % i, [128, DC, 512]) for i in range(2)]
                    rt = {nm: [sb(ph, "rt_%s%d" % (nm, i), shp) for i in range(2)] for nm, shp in
                          [("lg", [128, 36]), ("gm", [128, 1]), ("ngm", [128, 1]), ("gmask", [128, 4]), ("ge", [128, 4]),
                           ("gs", [128, 1]), ("pen", [128, 4]), ("el", [128, 32]), ("t8", [128, 8]), ("nm1", [128, 1]),
                           ("sel", [128, 32]), ("ex", [128, 32]), ("gx", [128, 32]), ("den", [128, 1]), ("pr", [128, 1]),
                           ("rp", [128, 1])]}
                kk = 0
                tl = 0
                cpar = [0]
                if router:
                    cums = [sb(ph, "cums%d" % i, [128, 32]) for i in range(2)]
                    memset('dve', cums[0][:], 0.0, [cums[0]])
                for (t0, n, sidx, gi) in cfg.groups:
                    pss = pb[gi % 2]
                    for c in range(DC):
                        q = sq[kk % 2]
                        kk += 1
                        act(q[:, :n], XT[:, c, t0:t0 + n], AF.Square, [XK(gi)], [q])
                        mm(pss[:, :n], ones[:], q[:, :n], c == 0, c == DC - 1, [ones, q], [pss])
                    r_ = rs[gi % 2]
                    act(r_[:, :n], pss[:, :n], AF.Sqrt, [pss], [r_], bias=cfg.EPS, scale=1.0 / 1024)
                    recip(r_[:, :n], r_[:, :n], [r_], [r_])
                    for c in range(DC):
                        tm = tmp[kk % 2]
                        kk += 1
                        tt('dve', tm[:, :n], XT[:, c, t0:t0 + n], r_[:, :n], ALU.mult, [XK(gi), r_], [tm])
                        if router:
                            hf = h2f[gi % 2]
                            act(hf[:, c, :n], tm[:, :n], AF.Identity, [tm, A, modT], [hf],
                                bias=modT[:, l, sh0 + c, sidx:sidx + 1], scale=A[:, l, c, sidx:sidx + 1])
                            cp('pool', hT[:, c, t0:t0 + n], hf[:, c, :n], [hf], [HK(gi)])
                        else:
                            act(hT[:, c, t0:t0 + n], tm[:, :n], AF.Identity, [tm, A, modT], [HK(gi)],
                                bias=modT[:, l, sh0 + c, sidx:sidx + 1], scale=A[:, l, c, sidx:sidx + 1])
                    if router:
                        hf = h2f[gi % 2]
                        for ti in range(n // 128):
                            tg = (t0 + ti * 128) // 128
                            R = {nm: v[tl % 2] for nm, v in rt.items()}
                            pl = pb[2 + tl % 2]
                            tl += 1
                            for c in range(DC):
                                mm(pl[:, 0:36], hf[:, c, ti * 128:(ti + 1) * 128], wr[:, l, c, :], c == 0, False, [hf, wr], [pl])
                            mm(pl[:, 0:36], ones1[:], br[:, l * 36:(l + 1) * 36], False, True, [ones1, br], [pl])
                            lg = R["lg"]
                            cp('dve', lg[:], pl[:, 0:36], [pl], [lg])
                            S.op('dve', lambda e, o=R["gm"], i=lg: e.tensor_reduce(out=o[:], in_=i[:, 0:4], axis=AX.X, op=ALU.max),
                                 r=[lg], w=[R["gm"]])
                            tsc('dve', R["ngm"][:], R["gm"][:], -1.0, None, ALU.mult, None, [R["gm"]], [R["ngm"]])
                            tsc('dve', R["gmask"][:], lg[:, 0:4], R["gm"][:], None, ALU.is_equal, None, [lg, R["gm"]], [R["gmask"]])
                            act(R["ge"][:], lg[:, 0:4], AF.Exp, [lg, R["ngm"]], [R["ge"], R["gs"]], bias=R["ngm"][:], scale=1.0,
                                accum=R["gs"][:])
                            tsc('dve', R["pen"][:], R["gmask"][:], 1e30, -1e30, ALU.mult, ALU.add, [R["gmask"]], [R["pen"]])
                            tt('dve', R["el"][:].rearrange("p (g e) -> p g e", g=4), lg[:, 4:36].rearrange("p (g e) -> p g e", g=4),
                               R["pen"][:].unsqueeze(2).to_broadcast([128, 4, 8]), ALU.add, [lg, R["pen"]], [R["el"]])
                            S.op('dve', lambda e, o=R["t8"], i=R["el"]: e.max(out=o[:], in_=i[:]), r=[R["el"]], w=[R["t8"]])
                            tsc('dve', R["nm1"][:], R["t8"][:, 0:1], -1.0, None, ALU.mult, None, [R["t8"]], [R["nm1"]])
                            tsc('dve', R["sel"][:], R["el"][:], R["t8"][:, 1:2], None, ALU.is_ge, None, [R["el"], R["t8"]], [R["sel"]])
                            act(R["ex"][:], R["el"][:], AF.Exp, [R["el"], R["nm1"]], [R["ex"]], bias=R["nm1"][:], scale=1.0)
                            tt('dve', R["gx"][:], R["sel"][:], R["ex"][:], ALU.mult, [R["sel"], R["ex"]], [R["gx"]])
                            S.op('dve', lambda e, o=R["den"], i=R["gx"]: e.tensor_reduce(out=o[:], in_=i[:], axis=AX.X, op=ALU.add),
                                 r=[R["gx"]], w=[R["den"]])
                            tt('dve', R["pr"][:], R["den"][:], R["gs"][:], ALU.mult, [R["den"], R["gs"]], [R["pr"]])
                            recip(R["rp"][:], R["pr"][:], [R["pr"]], [R["rp"]])
                            tsc('dve', G[:, tg, :], R["gx"][:], R["rp"][:], None, ALU.mult, None, [R["gx"], R["rp"]], [('G', gi)])
                            cp('dve', SEL[:, tg, :], R["sel"][:], [R["sel"]], [('SEL', tg)])
                            prk = pb[4 + tl % 2]
                            mm(prk[:, 0:32], Lst[:], R["sel"][:], True, False, [Lst, R["sel"]], [prk])
                            mm(prk[:, 0:32], ones[:], cums[cpar[0]][:], False, True, [ones, cums[cpar[0]]], [prk])
                            cp('dve', RK[:, tg, :], prk[:, 0:32], [prk], [('RK', tg)])
                            tt('dve', cums[1 - cpar[0]][:], cums[cpar[0]][:], R["sel"][:], ALU.add, [cums[cpar[0]], R["sel"]], [cums[1 - cpar[0]]])
                            cpar[0] = 1 - cpar[0]
                if router:
                    CNT = sb(ph, "CNT", [128, 32])
                    cmp18 = sb(ph, "cmp18", [128, 32, 18])
                    NBLK = sb(ph, "NBLK", [128, 32])
                    PADD = sb(ph, "PADD", [128, 32])
                    PEND = sb(ph, "PEND", [128, 32])
                    PST = sb(ph, "PST", [128, 32])
                    cmpB = sb(ph, "cmpB", [128, NB, 32])
                    BEf = sb(ph, "BEf", [128, NB])
                    pc = pb[6]
                    mm(pc[:, 0:32], ones[:], cums[cpar[0]][:], True, True, [ones, cums[cpar[0]]], [pc])
                    cp('dve', CNT[:], pc[:, 0:32], [pc], [CNT])
                    tt('dve', cmp18[:], CNT[:].unsqueeze(2).to_broadcast([128, 32, 18]), THR[:].unsqueeze(1).to_broadcast([128, 32, 18]),
                       ALU.is_gt, [CNT, THR], [cmp18])
                    S.op('dve', lambda e: e.tensor_reduce(out=NBLK[:], in_=cmp18[:], axis=AX.X, op=ALU.add), r=[cmp18], w=[NBLK])
                    tsc('dve', PADD[:], NBLK[:], float(cfg.BLK), None, ALU.mult, None, [NBLK], [PADD])
                    S.op('dve', lambda e: e.tensor_tensor_scan(out=PEND[:], data0=ones[:, 0:32], data1=PADD[:], initial=0.0,
                                                               op0=ALU.mult, op1=ALU.add), r=[ones, PADD], w=[PEND])
                    tt('dve', PST[:], PEND[:], PADD[:], ALU.subtract, [PEND, PADD], [PST])
                    tt('dve', cmpB[:], PEND[:].unsqueeze(1).to_broadcast([128, NB, 32]), JR[:].unsqueeze(2).to_broadcast([128, NB, 32]),
                       ALU.is_le, [PEND, JR], [cmpB])
                    S.op('dve', lambda e: e.tensor_reduce(out=BEf[:], in_=cmpB[:], axis=AX.X, op=ALU.add), r=[cmpB], w=[BEf])
                    tsc('dve', BEf[:], BEf[:], 31.0, float(32 * l), ALU.min, ALU.add, [BEf], [BEf])
                    UNU = sb(ph, "UNU", [128, NB])
                    tsc('dve', UNU[:], JR[:], PEND[:, 31:32], 8192.0, ALU.is_ge, ALU.mult, [JR, PEND], [UNU])
                    tt('dve', BEf[:], BEf[:], UNU[:], ALU.add, [BEf, UNU], [BEf])
                    tsc('dve', IDXW[:], BEf[:], 128.0, PIDX[:], ALU.mult, ALU.add, [BEf, PIDX], [IDXW])
                    pp = {nm: [sb(ph, "pp_%s%d" % (nm, i), shp) for i in range(2)] for nm, shp in
                          [("pos", [128, 32]), ("t8", [128, 8]), ("eq", [128, 32]), ("pr", [128, 32])]}
                    for tg in range(cfg.NT):
                        Q = {nm: v[tg % 2] for nm, v in pp.items()}
                        gi_t = tile_gi[tg][0]
                        tt('dve', Q["pos"][:], RK[:, tg, :], PST[:], ALU.add, [('RK', tg), PST], [Q["pos"]])
                        stt(Q["pos"][:], Q["pos"][:], 1.0, SEL[:, tg, :], ALU.add, ALU.mult, [Q["pos"], ('SEL', tg)], [Q["pos"]])
                        S.op('dve', lambda e, o=Q["t8"], i=Q["pos"]: e.max(out=o[:], in_=i[:]), r=[Q["pos"]], w=[Q["t8"]])
                        tsc('dve', IDX[:, tg, :], Q["t8"][:, 0:2], -1.0, None, ALU.add, None, [Q["t8"]], [('IDX', tg)])
                        for k in range(2):
                            tsc('dve', Q["eq"][:], Q["pos"][:], Q["t8"][:, k:k + 1], None, ALU.is_equal, None, [Q["pos"], Q["t8"]], [Q["eq"]])
                            tt('dve', Q["pr"][:], Q["eq"][:], G[:, tg, :], ALU.mult, [Q["eq"], ('G', gi_t)], [Q["pr"]])
                            S.op('dve', lambda e, o=GHL[:, tg, k:k + 1], i=Q["pr"]: e.tensor_reduce(out=o, in_=i[:], axis=AX.X, op=ALU.add),
                                 r=[Q["pr"]], w=[('GHL', tg)])
            S.barrier()

        def out_proj(l, Y, nk, k0, ph):
            Wo = sb(ph, "Wo", [128, nk, 1024], BF16)
            dma('pool', Wo[:], wout_d[l, k0 * 128:(k0 + nk) * 128, :].rearrange("(c p) n -> p c n", p=128), [], [Wo])
            kk = 0
            for (t0, n, sidx, gi) in cfg.groups:
                for j in range(DC):
                    po = pb[4 + kk % 4]
                    kk += 1
                    for c in range(nk):
                        rhs = Y[:, c, t0:t0 + n] if nk > 1 else Y[:, t0:t0 + n]
                        mm(po[:, :n], Wo[:, c, j * 128:(j + 1) * 128], rhs, c == 0, c == nk - 1, [Wo, Y], [po])
                    stt(XT[:, j, t0:t0 + n], po[:, :n], modT[:, l, 16 + j, sidx:sidx + 1], XT[:, j, t0:t0 + n],
                        ALU.mult, ALU.add, [po, modT, XK(gi)], [XK(gi)])

        def load_win(ph, name, l, col):
            W = sb(ph, name, [128, DC, 128], BF16)
            dma('pool', W[:], win_d[l, :, col:col + 128].rearrange("(c p) n -> p c n", p=128), [], [W])
            return W

        def proj(W, ps, t0, n, gi):
            for c in range(DC):
                mm(ps[:, :n], W[:, c, :], hT[:, c, t0:t0 + n], c == 0, c == DC - 1, [W, HK(gi)], [ps])

        def conv_phase(l):
            voff = cfg.V_L0 + l * cfg.V_LN
            R_ = TL // 64
            with contextlib.ExitStack() as ph:
                Z = sb(ph, "Z", [128, 4, T], BF16)
                SS = sb(ph, "SS", [128, T])
                u = sb(ph, "u", [128, T])
                Bs = sb(ph, "Bs", [128, T])
                y = sb(ph, "y", [128, T])
                hv = [sb(ph, "hv%d" % i, [128, 512]) for i in range(2)]
                zs = [sb(ph, "zs%d" % i, [128, 512]) for i in range(2)]
                for cc in range(4):
                    with contextlib.ExitStack() as ph2:
                        WB = load_win(ph2, "WB", l, cc * 128)
                        WC = load_win(ph2, "WC", l, 512 + cc * 128)
                        WH = load_win(ph2, "WH", l, 1024 + cc * 128)
                        for (t0, n, sidx, gi) in cfg.groups:
                            pB, pC, pH = pb[0 + 4 * (gi % 2)], pb[1 + 4 * (gi % 2)], pb[2 + 4 * (gi % 2)]
                            proj(WB, pB, t0, n, gi)
                            proj(WC, pC, t0, n, gi)
                            proj(WH, pH, t0, n, gi)
                            h_ = hv[gi % 2]
                            act(h_[:, :n], pH[:, :n], AF.Copy, [pH], [h_])
                            act(Bs[:, t0:t0 + n], pB[:, :n], AF.Copy, [pB], [Bs])
                            tt('dve', u[:, t0:t0 + n], pC[:, :n], h_[:, :n], ALU.mult, [pC, h_], [u])
                        w0, w1, w2 = V(voff + 64 + 0 * 4 + cc), V(voff + 64 + 1 * 4 + cc), V(voff + 64 + 2 * 4 + cc)
                        act(y[:], u[:], AF.Identity, [u, vecs], [y], scale=w1)
                        stt(y[:, 1:TC], u[:, 0:TC - 1], w0, y[:, 1:TC], ALU.mult, ALU.add, [u, vecs, y], [y])
                        stt(y[:, 0:TC - 1], u[:, 1:TC], w2, y[:, 0:TC - 1], ALU.mult, ALU.add, [u, vecs, y], [y])
                        if cc < 2:
                            ul = u[:, TC:T].rearrange("p (r w) -> p r w", w=64)
                            yl = y[:, TC:T].rearrange("p (r w) -> p r w", w=64)
                            stt(yl[:, :, 1:64], ul[:, :, 0:63], w0, yl[:, :, 1:64], ALU.mult, ALU.add, [u, vecs, y], [y])
                            stt(yl[:, :, 0:63], ul[:, :, 1:64], w2, yl[:, :, 0:63], ALU.mult, ALU.add, [u, vecs, y], [y])
                        else:
                            stt(y[:, TC + 64:T], u[:, TC:T - 64], w0, y[:, TC + 64:T], ALU.mult, ALU.add, [u, vecs, y], [y])
                            stt(y[:, TC:T - 64], u[:, TC + 64:T], w2, y[:, TC:T - 64], ALU.mult, ALU.add, [u, vecs, y], [y])
                        tt('dve', y[:], y[:], Bs[:], ALU.mult, [y, Bs], [y])
                        act(Z[:, cc, :], y[:], AF.Copy, [y], [Z])
                        for (t0, n, sidx, gi) in cfg.groups:
                            z_ = zs[gi % 2]
                            pz = pb[3 + 4 * (gi % 2)]
                            act(z_[:, :n], y[:, t0:t0 + n], AF.Square, [y], [z_])
                            mm(pz[:, :n], ones[:], z_[:, :n], True, True, [ones, z_], [pz])
                            if cc == 0:
                                cp('dve', SS[:, t0:t0 + n], pz[:, :n], [pz], [SS])
                            else:
                                tt('dve', SS[:, t0:t0 + n], pz[:, :n], SS[:, t0:t0 + n], ALU.add, [pz, SS], [SS])
                    S.barrier()
                act(SS[:], SS[:], AF.Sqrt, [SS], [SS], bias=cfg.EPS, scale=1.0 / 512)
                recip(SS[:], SS[:], [SS], [SS])
                for cc in range(4):
                    stt(Z[:, cc, :], Z[:, cc, :], V(voff + 76 + cc), SS[:], ALU.mult, ALU.mult, [Z, vecs, SS], [Z])
                out_proj(l, Z, 4, 0, ph)
            S.barrier()

        def heads_phase(l):
            voff = cfg.V_L0 + l * cfg.V_LN
            hnw = V(voff + 80)
            lat = cfg.groups[1:]
            order = [cfg.groups, [cfg.groups[0]] + lat[::-1]]
            for hh in range(4):
                with contextlib.ExitStack() as ph:
                    Wq = load_win(ph, "Wq", l, 1536 + hh * 128)
                    Wf = [load_win(ph, "Wzf", l, 2048 + hh * 128), load_win(ph, "Wzb", l, 2560 + hh * 128)]
                    Wi = load_win(ph, "Wi", l, 3072 + hh * 128)
                    Wg = load_win(ph, "Wg", l, 3584 + hh * 128)
                    Of = sb(ph, "Of", [128, T])
                    Yh = sb(ph, "Yh", [128, T], BF16)
                    NS = 4
                    St = [sb(ph, "St%d" % i, [128, 128]) for i in range(NS)]
                    names = ["qs", "sg", "kk", "vs", "b", "eb", "enb", "ko", "gs"]
                    tmps = [{nm: sb(ph, "g%s%d" % (nm, i), [128, 512]) for nm in names} for i in range(2)]
                    dch = [sb(ph, "dch%d" % i, [128, 16]) for i in range(2)]
                    totc = [sb(ph, "totc%d" % i, [128, 16]) for i in range(2)]
                    koT = [sb(ph, "koT%d" % i, [CH, NCHT, 128]) for i in range(2)]
                    vT = [sb(ph, "vT%d" % i, [CH, NCHT, 128]) for i in range(2)]
                    PT = [sb(ph, "PT%d" % i, [CH, NCHT, CH]) for i in range(2)]
                    midc = [sb(ph, "midc%d" % i, [128, 16]) for i in range(2)]
                    emid = [sb(ph, "emid%d" % i, [128, 16]) for i in range(2)]
                    etm = [sb(ph, "etm%d" % i, [128, 16]) for i in range(2)]
                    osq = sb(ph, "osq", [128, 512])
                    ors = sb(ph, "ors", [128, 512])
                    gpar = 0
                    tpar = 0
                    for dirn in range(2):
                        lbc = (dirn * DEPTH + l) * 4 + hh
                        lb_ap, oml_ap = lbT[:, lbc:lbc + 1], omlT[:, lbc:lbc + 1]
                        cur = 0
                        memset('dve', St[0][:], 0.0, [St[0]])
                        msk = maskF if dirn == 0 else maskB
                        for (t0, n, sidx, gi) in order[dirn]:
                            tp = tmps[gpar % 2]
                            dc_ = dch[gpar % 2]
                            gpar += 1
                            nch = n // CH
                            pq, pz_, pi_, pg = pb[0], pb[1], pb[2], pb[3]
                            proj(Wq, pq, t0, n, gi)
                            proj(Wf[dirn], pz_, t0, n, gi)
                            proj(Wi, pi_, t0, n, gi)
                            act(tp["qs"][:, :n], pq[:, :n], AF.Silu, [pq], [tp["qs"]])
                            act(tp["sg"][:, :n], pz_[:, :n], AF.Sigmoid, [pz_], [tp["sg"]])
                            act(tp["vs"][:, :n], pi_[:, :n], AF.Copy, [pi_], [tp["vs"]])
                            if dirn == 1:
                                proj(Wg, pg, t0, n, gi)
                                act(tp["gs"][:, :n], pg[:, :n], AF.Silu, [pg], [tp["gs"]])
                            tc_ = totc[(gpar - 1) % 2]
                            tsc('dve', tp["sg"][:, :n], tp["sg"][:, :n], oml_ap, lb_ap, ALU.mult, ALU.add, [tp["sg"], omlT, lbT], [tp["sg"]])
                            tsc('dve', tp["kk"][:, :n], tp["sg"][:, :n], -1.0, 1.0, ALU.mult, ALU.add, [tp["sg"]], [tp["kk"]])
                            act(tp["sg"][:, :n], tp["sg"][:, :n], AF.Ln, [tp["sg"]], [tp["sg"]])
                            S.op('dve', lambda e, o=tp["b"], m=rmask, d1=tp["sg"], n=n: e.tensor_tensor_scan(
                                out=o[:, :n], data0=m[:, :n], data1=d1[:, :n], initial=0.0, op0=ALU.mult, op1=ALU.add),
                                r=[rmask, tp["sg"]], w=[tp["b"]])
                            b3 = tp["b"][:, :n].rearrange("p (a c) -> p a c", c=CH)
                            cp('dve', tc_[:, :nch], b3[:, :, CH - 1], [tp["b"]], [tc_])
                            act(dc_[:, :nch], tc_[:, :nch], AF.Exp, [tc_], [dc_])
                            bb = tp["b"]
                            if dirn == 1:
                                tt('dve', b3, b3, tc_[:, :nch].unsqueeze(2).to_broadcast([128, nch, CH]), ALU.subtract, [tp["b"], tc_], [tp["b"]])
                                tt('dve', bb[:, :n], tp["sg"][:, :n], bb[:, :n], ALU.subtract, [tp["sg"], bb], [bb])
                            md_, em_, et_ = midc[(gpar - 1) % 2], emid[(gpar - 1) % 2], etm[(gpar - 1) % 2]
                            midcol = CH // 2 - 1 if dirn == 0 else CH // 2
                            cp('dve', md_[:, :nch], b3[:, :, midcol], [tp["b"]], [md_])
                            tt('dve', b3, b3, md_[:, :nch].unsqueeze(2).to_broadcast([128, nch, CH]), ALU.subtract, [tp["b"], md_], [tp["b"]])
                            act(tp["eb"][:, :n], bb[:, :n], AF.Exp, [bb], [tp["eb"]])
                            act(tp["enb"][:, :n], bb[:, :n], AF.Exp, [bb], [tp["enb"]], scale=-1.0)
                            act(em_[:, :nch], md_[:, :nch], AF.Exp, [md_], [em_])
                            tt('dve', et_[:, :nch], tc_[:, :nch], md_[:, :nch], ALU.subtract, [tc_, md_], [et_])
                            act(et_[:, :nch], et_[:, :nch], AF.Exp, [et_], [et_])
                            tt('dve', tp["qs"][:, :n], tp["qs"][:, :n], tp["eb"][:, :n], ALU.mult, [tp["qs"], tp["eb"]], [tp["qs"]])
                            tt('dve', tp["kk"][:, :n], tp["kk"][:, :n], tp["enb"][:, :n], ALU.mult, [tp["kk"], tp["enb"]], [tp["kk"]])
                            tt('dve', tp["eb"][:, :n].rearrange("p (a c) -> p a c", c=CH),
                               tp["qs"][:, :n].rearrange("p (a c) -> p a c", c=CH),
                               em_[:, :nch].unsqueeze(2).to_broadcast([128, nch, CH]), ALU.mult, [tp["qs"], em_], [tp["eb"]])
                            tt('dve', tp["ko"][:, :n].rearrange("p (a c) -> p a c", c=CH),
                               tp["kk"][:, :n].rearrange("p (a c) -> p a c", c=CH),
                               et_[:, :nch].unsqueeze(2).to_broadcast([128, nch, CH]), ALU.mult, [tp["kk"], et_], [tp["ko"]])
                            tiles = list(range(n // 128))
                            chunks = list(range(NCHT))
                            if dirn == 1:
                                tiles = tiles[::-1]
                                chunks = chunks[::-1]
                            for ti in tiles:
                                c0 = ti * 128
                                kT_, vT_, PT_ = koT[tpar % 2], vT[tpar % 2], PT[tpar % 2]
                                tpar += 1
                                pk, pv = pb[4], pb[5]
                                pk3 = pk[0:CH, 0:NCHT * 128].rearrange("p (j k) -> p j k", j=NCHT)
                                pv3 = pv[0:CH, 0:NCHT * 128].rearrange("p (j k) -> p j k", j=NCHT)
                                for j in range(NCHT):
                                    tr(pk3[:, j, :], tp["ko"][:, c0 + CH * j:c0 + CH * j + CH], [tp["ko"]], [pk])
                                for j in range(NCHT):
                                    tr(pv3[:, j, :], tp["vs"][:, c0 + CH * j:c0 + CH * j + CH], [tp["vs"]], [pv])
                                act(kT_[:], pk3, AF.Copy, [pk], [kT_])
                                cp('dve', vT_[:], pv3, [pv], [vT_])
                                par = tpar % 2
                                psc = pb[3][0:CH, 256:256 + NCHT * CH].rearrange("p (j k) -> p j k", j=NCHT)
                                for j in range(NCHT):
                                    cs = c0 + CH * j
                                    mm(psc[:, j, :], tp["kk"][:, cs:cs + CH], tp["qs"][:, cs:cs + CH], True, True,
                                       [tp["kk"], tp["qs"]], [pb[3]])
                                tsc('dve', PT_[:], psc, -1e30, 1e30, ALU.max, ALU.min, [pb[3]], [PT_])
                                tt('dve', PT_[:], PT_[:], msk[:], ALU.mult, [PT_, msk], [PT_])
                                po = pb[7][:, 256 * par:256 * par + 128]
                                for j in chunks:
                                    cs = c0 + CH * j
                                    jg = cs // CH
                                    mm(pb[6][:, 128 * j:128 * j + 128], kT_[:, j, :], vT_[:, j, :], True, True, [kT_, vT_], [('pb6', j)])
                                    mm(po[:, CH * j:CH * j + CH], St[cur][:], tp["eb"][:, cs:cs + CH], True, False, [St[cur], tp["eb"]], [('pb7', 'o', par)])
                                    mm(po[:, CH * j:CH * j + CH], vT_[:, j, :], PT_[:, j, :], False, True, [vT_, PT_], [('pb7', 'o', par)])
                                    nxt = (cur + 1) % NS
                                    stt(St[nxt][:], St[cur][:], dc_[:, jg:jg + 1], pb[6][:, 128 * j:128 * j + 128], ALU.mult, ALU.add,
                                        [St[cur], dc_, ('pb6', j)], [St[nxt]])
                                    cur = nxt
                                if dirn == 0:
                                    act(Of[:, t0 + c0:t0 + c0 + 128], po[:, 0:128], AF.Copy, [('pb7', 'o', par)], [Of])
                                else:
                                    tt('dve', Of[:, t0 + c0:t0 + c0 + 128], po[:, 0:128], Of[:, t0 + c0:t0 + c0 + 128], ALU.add, [('pb7', 'o', par), Of], [Of])
                            if dirn == 1:
                                pn = pb[3]
                                act(osq[:, :n], Of[:, t0:t0 + n], AF.Square, [Of], [osq])
                                mm(pn[:, :n], ones[:], osq[:, :n], True, True, [ones, osq], [pn])
                                act(ors[:, :n], pn[:, :n], AF.Sqrt, [pn], [ors], bias=cfg.EPS, scale=1.0 / 128)
                                recip(ors[:, :n], ors[:, :n], [ors], [ors])
                                stt(osq[:, :n], Of[:, t0:t0 + n], hnw, ors[:, :n], ALU.mult, ALU.mult, [Of, vecs, ors], [osq])
                                tt('dve', Yh[:, t0:t0 + n], osq[:, :n], tp["gs"][:, :n], ALU.mult, [osq, tp["gs"]], [Yh])
                    out_proj(l, Yh, 1, 4 + hh, ph)
                S.barrier()

        def moe_phase_dense(l):
            with contextlib.ExitStack() as ph:
                WA = [sb(ph, "WA%d" % i, [128, DC, 512], BF16) for i in range(2)]
                WU = [sb(ph, "WU%d" % i, [128, DC, 512], BF16) for i in range(2)]
                WD = [sb(ph, "WD%d" % i, [128, 4, 1024], BF16) for i in range(2)]
                gbc = [sb(ph, "gbc%d" % i, [128, 512], BF16) for i in range(2)]
                sa = [sb(ph, "sa%d" % i, [128, 512]) for i in range(2)]
                t1 = [sb(ph, "t1%d" % i, [128, 512], BF16) for i in range(2)]
                hm = [sb(ph, "hm%d" % i, [128, 4, 512], BF16) for i in range(2)]

                def load(e):
                    s = e % 2
                    dma('pool', WA[s][:], wgu_d[l, e, :, 0:512].rearrange("(c p) n -> p c n", p=128), [], [WA[s]])
                    dma('pool', WU[s][:], wgu_d[l, e, :, 512:1024].rearrange("(c p) n -> p c n", p=128), [], [WU[s]])
                    dma('pool', WD[s][:], wdn_d[l, e, :, :].rearrange("(c p) n -> p c n", p=128), [], [WD[s]])

                load(0)
                kk = 0
                k2 = 0
                for e in range(cfg.NE):
                    if e + 1 < cfg.NE:
                        load(e + 1)
                    s = e % 2
                    for (t0, n, sidx, gi) in cfg.groups:
                        pg = pb[0]
                        for ti in range(n // 128):
                            tg = (t0 + ti * 128) // 128
                            mm(pg[:, ti * 128:(ti + 1) * 128], G[:, tg, e:e + 1].to_broadcast([128, 128]), ident[:], True, True,
                               [('G', gi), ident], [pg])
                        gb = gbc[kk % 2]
                        hm_ = hm[kk % 2]
                        kk += 1
                        act(gb[:, :n], pg[:, :n], AF.Copy, [pg], [gb])
                        for hc in range(4):
                            pa, pu = pb[1 + 2 * (k2 % 2)], pb[2 + 2 * (k2 % 2)]
                            sa_, t1_ = sa[k2 % 2], t1[k2 % 2]
                            k2 += 1
                            for c in range(DC):
                                mm(pa[:, :n], WA[s][:, c, hc * 128:(hc + 1) * 128], hT[:, c, t0:t0 + n], c == 0, c == DC - 1,
                                   [WA[s], HK(gi)], [pa])
                            for c in range(DC):
                                mm(pu[:, :n], WU[s][:, c, hc * 128:(hc + 1) * 128], hT[:, c, t0:t0 + n], c == 0, c == DC - 1,
                                   [WU[s], HK(gi)], [pu])
                            act(sa_[:, :n], pa[:, :n], AF.Silu, [pa], [sa_])
                            tt('dve', t1_[:, :n], sa_[:, :n], pu[:, :n], ALU.mult, [sa_, pu], [t1_])
                            tt('pool', hm_[:, hc, :n], t1_[:, :n], gb[:, :n], ALU.mult, [t1_, gb], [hm_])
                        for j in range(DC):
                            py = pb[5 + j % 3]
                            for hc in range(4):
                                mm(py[:, :n], WD[s][:, hc, j * 128:(j + 1) * 128], hm_[:, hc, :n], hc == 0, hc == 3, [WD[s], hm_], [py])
                            stt(XT[:, j, t0:t0 + n], py[:, :n], modT[:, l, 40 + j, sidx:sidx + 1], XT[:, j, t0:t0 + n],
                                ALU.mult, ALU.add, [py, modT, XK(gi)], [XK(gi)])
            S.barrier()

        def moe_phase(l):
            IOA = bass.IndirectOffsetOnAxis
            with contextlib.ExitStack() as ph:
                WAU = [sb(ph, "WAU%d" % i, [128, DC, 1024], BF16) for i in range(2)]
                WD = [sb(ph, "WD%d" % i, [128, 4, 1024], BF16) for i in range(2)]
                xb = [sb(ph, "xb%d" % i, [128, 1024]) for i in range(2)]
                yb = [sb(ph, "yb%d" % i, [128, 1024]) for i in range(2)]
                xbT2 = [sb(ph, "xbT2%d" % i, [128, DC, 256], BF16) for i in range(2)]
                h32 = yb[0][:, :].rearrange("p (c t) -> p c t", c=DC)
                sa2 = sb(ph, "sa2", [128, 2, 512], BF16)
                hm2 = [sb(ph, "hm2%d" % i, [128, 4, 256], BF16) for i in range(2)]
                pT = [pb[0], pb[1]]
                order = []
                for i_ in range(NB):
                    order.append(i_ // 2 if i_ % 2 == 0 else NB - 1 - i_ // 2)

                order_box.clear()
                order_box.append(order)


                wgu2 = wgu_d.rearrange("l e (p c) n -> (l e p) (c n)", c=8)
                wdn2 = wdn_d.rearrange("l e (p c) n -> (l e p) (c n)", c=4)

                def bcreg(e):
                    if 'r' not in bc_cache:
                        bc_cache['r'] = e.to_reg(DEPTH * 32 * 128 - 1)
                    return bc_cache['r']

                def load(bpos):
                    s_ = bpos % 2
                    j = order_of(bpos)
                    for q in range(4):
                        S.op('pool', lambda e, j=j, q=q, s_=s_: e.indirect_dma_start(
                            out=WAU[s_][:, 2 * q:2 * q + 2, :].rearrange("p a n -> p (a n)"), out_offset=None, in_=wgu2,
                            in_offset=IOA(ap=IDXW[:, j:j + 1], axis=0), element_offset=q * 2048, bounds_check=bcreg(e), oob_is_err=False),
                            r=[IDXW], w=[('WAU', s_, q)], dma=True)
                    for q in range(2):
                        S.op('pool', lambda e, j=j, q=q, s_=s_: e.indirect_dma_start(
                            out=WD[s_][:, 2 * q:2 * q + 2, :].rearrange("p a n -> p (a n)"), out_offset=None, in_=wdn2,
                            in_offset=IOA(ap=IDXW[:, j:j + 1], axis=0), element_offset=q * 2048, bounds_check=bcreg(e), oob_is_err=False),
                            r=[IDXW], w=[('WD', s_, q)], dma=True)

                load(0)
                scat = []
                for tg in range(cfg.NT):
                    gi, sidx = tile_gi[tg]
                    act(h32, hT[:, :, tg * 128:(tg + 1) * 128], AF.Copy, [HK(gi)], [yb[0]])
                    for c in range(DC):
                        tr(pT[c // 4][:, (c % 4) * 128:(c % 4 + 1) * 128], h32[:, c, :], [yb[0]], [pT[c // 4]])
                    x_ = xb[tg % 2]
                    act(x_[:, 0:512], pT[0][:, :], AF.Copy, [pT[0]], [x_])
                    cp('dve', x_[:, 512:1024], pT[1][:, :], [pT[1]], [x_])
                    for k in range(2):
                        scat.append(S.op('pool', lambda e, x_=x_, tg=tg, k=k: e.indirect_dma_start(
                            out=xs_d[:, :], out_offset=IOA(ap=IDX[:, tg, k:k + 1], axis=0), in_=x_[:, :], in_offset=None),
                            r=[x_, ('IDX', tg)], w=[('xs', tg, k)], dma=True))
                stores = []
                RT = cfg.BLK // 128
                def rows(pos):
                    return order[pos // RT] * RT + pos % RT

                def prefetch(rt):
                    s_ = rt % 2
                    ra = rows(rt)
                    S.op('sp', lambda e: e.dma_start(out=xb[s_][:], in_=xs_d[ra * 128:(ra + 1) * 128, :]), r=[], w=[xb[s_]],
                         dma=True, extra_deps=scat)

                PA, PU = [pb[2], pb[3]], [pb[4], pb[5]]

                def stage_a(bpos):
                    w_ = bpos % 2
                    for r_ in range(RT):
                        pos = bpos * RT + r_
                        s_ = pos % 2
                        for c in range(DC):
                            tr(pT[c // 4][:, (c % 4) * 128:(c % 4 + 1) * 128], xb[s_][:, :].rearrange("r (p c) -> r c p", c=8)[:, c, :], [xb[s_]], [pT[c // 4]])
                        act(xbT2[w_][:, 0:4, r_ * 128:(r_ + 1) * 128], pT[0][:, :].rearrange("p (c r) -> p c r", c=4), AF.Copy, [pT[0]], [xbT2[w_]])
                        cp('dve', xbT2[w_][:, 4:8, r_ * 128:(r_ + 1) * 128], pT[1][:, :].rearrange("p (c r) -> p c r", c=4), [pT[1]], [xbT2[w_]])
                        if pos + 2 < NB * RT:
                            prefetch(pos + 2)
                    NW = RT * 128
                    for hc in range(4):
                        for c in range(DC):
                            mm(PA[hc // 2][:, (hc % 2) * NW:(hc % 2 + 1) * NW], WAU[w_][:, c, 0:512].rearrange("p (m h) -> p h m", h=4)[:, hc, :],
                               xbT2[w_][:, c, :], c == 0, c == DC - 1, [('WAU', w_, c // 2), xbT2[w_]], [PA[hc // 2]])
                    for hc in range(4):
                        for c in range(DC):
                            mm(PU[hc // 2][:, (hc % 2) * NW:(hc % 2 + 1) * NW], WAU[w_][:, c, 512:1024].rearrange("p (m h) -> p h m", h=4)[:, hc, :],
                               xbT2[w_][:, c, :], c == 0, c == DC - 1, [('WAU', w_, c // 2), xbT2[w_]], [PU[hc // 2]])
                    for h2 in range(2):
                        act(sa2[:, h2, :], PA[h2][:, 0:2 * NW], AF.Silu, [PA[h2]], [('sa2', h2)])
                        tt('dve', hm2[w_][:, 2 * h2:2 * h2 + 2, :].rearrange("p c r -> p (c r)"), sa2[:, h2, :], PU[h2][:, 0:2 * NW], ALU.mult,
                           [('sa2', h2), PU[h2]], [('hm2', w_, h2)])

                def stage_b(bpos):
                    w_ = bpos % 2
                    py = [pb[6], pb[7]]
                    for r_ in range(RT):
                        pos = bpos * RT + r_
                        s_ = pos % 2
                        for half in range(2):
                            for hc in range(4):
                                mm(py[half][:, :], hm2[w_][:, hc, r_ * 128:(r_ + 1) * 128], WD[w_][:, hc, half * 512:(half + 1) * 512], hc == 0, hc == 3,
                                   [('hm2', w_, hc // 2), ('WD', w_, hc // 2)], [py[half]])
                        act(yb[s_][:, 0:512], py[0][:, :], AF.Copy, [py[0]], [yb[s_]])
                        cp('dve', yb[s_][:, 512:1024], py[1][:, :], [py[1]], [yb[s_]])
                        ra = rows(pos)
                        stores.append(S.op('act', lambda e, ra=ra, s_=s_: e.dma_start(out=ys_d[ra * 128:(ra + 1) * 128, :], in_=yb[s_][:]),
                                           r=[yb[s_]], w=[('yd', pos)], dma=True))

                prefetch(0)
                prefetch(1)
                for bpos in range(NB):
                    stage_a(bpos)
                    if bpos > 0:
                        stage_b(bpos - 1)
                    if bpos + 1 < NB:
                        load(bpos + 1)
                stage_b(NB - 1)
                for tg in range(cfg.NT):
                    gi, sidx = tile_gi[tg]
                    yh_, yl_ = xb[tg % 2], yb[tg % 2]
                    S.op('pool', lambda e, yh_=yh_, tg=tg: e.indirect_dma_start(
                        out=yh_[:, :], out_offset=None, in_=ys_d[:, :], in_offset=IOA(ap=IDX[:, tg, 0:1], axis=0)),
                        r=[('IDX', tg)], w=[yh_], dma=True, extra_deps=stores)
                    S.op('pool', lambda e, yl_=yl_, tg=tg: e.indirect_dma_start(
                        out=yl_[:, :], out_offset=None, in_=ys_d[:, :], in_offset=IOA(ap=IDX[:, tg, 1:2], axis=0)),
                        r=[('IDX', tg)], w=[yl_], dma=True, extra_deps=stores)
                    tsc('dve', yh_[:], yh_[:], GHL[:, tg, 0:1], None, ALU.mult, None, [yh_, ('GHL', tg)], [yh_])
                    stt(yh_[:], yl_[:], GHL[:, tg, 1:2], yh_[:], ALU.mult, ALU.add, [yl_, ('GHL', tg), yh_], [yh_])
                    for c in range(DC):
                        tr(pT[c // 4][:, (c % 4) * 128:(c % 4 + 1) * 128], yh_[:, c * 128:(c + 1) * 128], [yh_], [pT[c // 4]])
                    for c in range(DC):
                        stt(XT[:, c, tg * 128:(tg + 1) * 128], pT[c // 4][:, (c % 4) * 128:(c % 4 + 1) * 128], modT[:, l, 40 + c, sidx:sidx + 1],
                            XT[:, c, tg * 128:(tg + 1) * 128], ALU.mult, ALU.add, [pT[c // 4], modT, XK(gi)], [XK(gi)])
            S.barrier()

        for l in cfg.layers:
            norm_modulate(l, 0, False)
            if 'conv' not in cfg.skip:
                conv_phase(l)
            if 'heads' not in cfg.skip:
                heads_phase(l)
            norm_modulate(l, 1, True)
            if 'moe' not in cfg.skip:
                moe_phase(l)

        fin = []
        with contextlib.ExitStack() as ph:
            sq = [sb(ph, "fsq%d" % i, [128, 512]) for i in range(2)]
            rs = [sb(ph, "frs%d" % i, [128, 512]) for i in range(2)]
            ob = [sb(ph, "fob%d" % i, [128, 512]) for i in range(4)]
            kk = 0
            for (t0, n, sidx, gi) in cfg.groups[1:]:
                pss = pb[gi % 2]
                for c in range(DC):
                    q = sq[kk % 2]
                    kk += 1
                    act(q[:, :n], XT[:, c, t0:t0 + n], AF.Square, [XK(gi)], [q])
                    mm(pss[:, :n], ones[:], q[:, :n], c == 0, c == DC - 1, [ones, q], [pss])
                r_ = rs[gi % 2]
                act(r_[:, :n], pss[:, :n], AF.Sqrt, [pss], [r_], bias=cfg.EPS, scale=1.0 / 1024)
                recip(r_[:, :n], r_[:, :n], [r_], [r_])
                for c in range(DC):
                    o_ = ob[kk % 4]
                    kk += 1
                    if cfg.final:
                        stt(o_[:, :n], XT[:, c, t0:t0 + n], V(cfg.V_FNW + c), r_[:, :n], ALU.mult, ALU.mult, [XK(gi), vecs, r_], [o_])
                    else:
                        cp('dve', o_[:, :n], XT[:, c, t0:t0 + n], [XK(gi)], [o_])
                    fin.append(dma('sp', outT_v[:, c, t0 - TC:t0 - TC + n], o_[:, :n], [o_], []))
        S.op('sp', lambda e: e.nop(), extra_deps=fin)
        S.run(nc)
        nc._sched_stats = S.stats
    return nc


def pack_inputs(cfg, b, x, c, ctx, c_ctx, norm_w, w_ada, b_ada, w_in, conv_w, conv_norm_w, hg_lb, hg_norm_w, w_out,
                w_rg, b_rg, w_re, b_re, w_e_gu, w_e_down, final_norm_w):
    d = cfg.DEPTH
    tok = np.concatenate([ctx[b], x[b]], axis=0)
    xT = np.ascontiguousarray(tok.T.reshape(cfg.DC, 128, cfg.T).transpose(1, 0, 2)).reshape(128, cfg.DC * cfg.T)

    def fm(v):
        return np.asarray(v).reshape(-1, 128).T

    vecs = np.zeros((128, cfg.NV), np.float32)
    vecs[:, cfg.V_C:cfg.V_C + 8] = fm(c[b])
    vecs[:, cfg.V_CC:cfg.V_CC + 8] = fm(c_ctx)
    vecs[:, cfg.V_FNW:cfg.V_FNW + 8] = fm(final_norm_w)
    for dirn in range(2):
        for l in range(d):
            o = cfg.V_LB + (dirn * d + l) * 4
            vecs[:, o:o + 4] = fm(hg_lb[dirn, l])
    for l in range(d):
        o = cfg.V_L0 + l * cfg.V_LN
        vecs[:, o:o + 8] = fm(norm_w[l, 0])
        vecs[:, o + 8:o + 16] = fm(norm_w[l, 1])
        vecs[:, o + 16:o + 64] = fm(b_ada[l])
        for tap in range(3):
            vecs[:, o + 64 + tap * 4:o + 64 + tap * 4 + 4] = fm(conv_w[l, tap])
        vecs[:, o + 76:o + 80] = fm(conv_norm_w[l])
        vecs[:, o + 80:o + 81] = fm(hg_norm_w[l])
    wr = np.concatenate([w_rg, w_re], axis=2)
    wr = np.ascontiguousarray(wr.reshape(d, cfg.DC, 128, 36).transpose(2, 0, 1, 3)).reshape(128, d * cfg.DC * 36)
    br = np.ascontiguousarray(np.concatenate([b_rg, b_re], axis=1)).reshape(1, d * 36)
    return {"xT": xT.astype(np.float32), "vecs": vecs, "wr": wr.astype(np.float32), "br": br.astype(np.float32)}


def run(cfg, inputs, n_cores):
    inputs = {k: np.asarray(v) for k, v in inputs.items()}
    nc = build_program(cfg)
    shared = {"w_ada": np.ascontiguousarray(inputs["w_ada"]), "w_in": np.ascontiguousarray(inputs["w_in"]),
              "w_out": np.ascontiguousarray(inputs["w_out"]), "w_gu": np.ascontiguousarray(inputs["w_e_gu"]),
              "w_dn": np.ascontiguousarray(inputs["w_e_down"])}
    in_maps = []
    for b in range(n_cores):
        m = pack_inputs(cfg, b, **inputs)
        m.update(shared)
        in_maps.append(m)
    res = run_bass_kernel_spmd(nc, in_maps, core_ids=list(range(n_cores)))
    outs = []
    for b in range(n_cores):
        oT = np.asarray(res.results[b]["outT"]).reshape(128, cfg.DC, cfg.TL)
        outs.append(oT.transpose(2, 1, 0).reshape(cfg.TL, cfg.D))
    return np.stack(outs, axis=0).astype(np.float32)


def kernel(**inputs):
    cfg = Cfg(depth=4, t_lat=2048)
    return run(cfg, inputs, 8)
```

```python
import contextlib
import numpy as np
import concourse.bass as bass
import concourse.mybir as mybir
from concourse.bass_utils import run_bass_kernel_spmd

F32 = mybir.dt.float32
BF16 = mybir.dt.bfloat16
AF = mybir.ActivationFunctionType
ALU = mybir.AluOpType
AX = mybir.AxisListType

ENGS = ['pe', 'act', 'dve', 'pool', 'sp']
DMA_RING = 8


class Op:
    __slots__ = ('eng', 'fn', 'deps', 'idx', 'signal', 'semkey', 'semval', 'dma', 'waits')

    def __init__(self, eng, fn, dma):
        self.eng = eng
        self.fn = fn
        self.dma = dma
        self.deps = []
        self.signal = False
        self.semkey = None
        self.semval = 0
        self.waits = []


class Sched:
    def __init__(self):
        self.ops = {e: [] for e in ENGS}
        self.bufs = {}
        self.ndma = {e: 0 for e in ENGS}
        self.dma_ops = {e: [] for e in ENGS}

    @staticmethod
    def _key(x):
        if isinstance(x, (tuple, str)):
            return x
        return x.name

    def op(self, eng, fn, r=(), w=(), dma=False, extra_deps=()):
        o = Op(eng, fn, dma)
        deps = list(extra_deps)
        rk = [self._key(x) for x in r]
        wk = [self._key(x) for x in w]
        for k in rk:
            st = self.bufs.get(k)
            if st is not None and st[0] is not None:
                deps.append(st[0])
        for k in wk:
            st = self.bufs.get(k)
            if st is not None:
                if st[0] is not None:
                    deps.append(st[0])
                deps.extend(st[1].values())
        if dma:
            i = self.ndma[eng]
            self.ndma[eng] += 1
            o.semkey = ('dma', eng, i % DMA_RING)
            o.semval = 16 * (i // DMA_RING + 1)
            if i >= DMA_RING:
                deps.append(self.dma_ops[eng][i - DMA_RING])
            self.dma_ops[eng].append(o)
        seen = set()
        for d in deps:
            if id(d) in seen or d is o:
                continue
            seen.add(id(d))
            o.deps.append(d)
        o.idx = len(self.ops[eng])
        self.ops[eng].append(o)
        for k in rk:
            st = self.bufs.setdefault(k, [None, {}])
            st[1][id(o) if dma else eng] = o
        for k in wk:
            self.bufs[k] = [o, {}]
        return o

    def barrier(self):
        last = []
        for e in ENGS:
            if self.ops[e]:
                last.append(self.ops[e][-1])
            last.extend(self.dma_ops[e][-DMA_RING:])
        for e in ENGS:
            self.op(e, lambda g: g.nop(), extra_deps=last)
        self.bufs = {}

    def finalize(self):
        for e in ENGS:
            for o in self.ops[e]:
                for d in o.deps:
                    if d.dma:
                        continue
                    if d.eng == 'pe' and o.eng == 'pe' and not o.dma:
                        continue
                    d.signal = True
        for e in ENGS:
            c = 0
            for o in self.ops[e]:
                if o.dma:
                    continue
                if o.signal:
                    c += 1
                    o.semkey = ('eng', e)
                    o.semval = c
        for e in ENGS:
            waited = {}
            for o in self.ops[e]:
                need = {}
                for d in o.deps:
                    if (not d.dma) and d.eng == 'pe' and o.eng == 'pe' and not o.dma:
                        continue
                    if d.semval > need.get(d.semkey, 0):
                        need[d.semkey] = d.semval
                for k, v in need.items():
                    if waited.get(k, 0) < v:
                        waited[k] = v
                        o.waits.append((k, v))

    def run(self, nc):
        self.finalize()
        keys = set()
        for e in ENGS:
            for o in self.ops[e]:
                if o.semkey is not None and (o.signal or o.dma):
                    keys.add(o.semkey)
        keys = sorted(keys, key=str)
        self.stats = {e: (len(self.ops[e]), max([o.semval for o in self.ops[e] if not o.dma] + [0])) for e in ENGS}
        with contextlib.ExitStack() as st:
            sems = {}
            for i, k in enumerate(keys):
                sems[k] = st.enter_context(nc.semaphore('s%d' % i))
            block = st.enter_context(nc.Block())

            def replay(eng_name):
                def body(e):
                    for o in self.ops[eng_name]:
                        for (k, v) in o.waits:
                            e.wait_ge(sems[k], v)
                        inst = o.fn(e)
                        if o.dma:
                            inst.then_inc(sems[o.semkey], 16)
                        elif o.signal:
                            inst.then_inc(sems[o.semkey], 1)
                return body

            block.tensor(replay('pe'))
            block.scalar(replay('act'))
            block.vector(replay('dve'))
            block.gpsimd(replay('pool'))
            block.sync(replay('sp'))


class Cfg:
    def __init__(self, depth=4, t_lat=2048, layers=None, first=True, final=True):
        self.D = 1024
        self.DC = 8
        self.TC = 256
        self.TL = t_lat
        self.T = self.TC + self.TL
        self.DEPTH = depth
        self.layers = list(range(depth)) if layers is None else layers
        self.first = first
        self.final = final
        self.NE = 32
        self.skip = set()
        self.EPS = 1e-6
        self.groups = [(0, 256, 1, 0)]
        for k in range(self.TL // 512):
            self.groups.append((256 + 512 * k, 512, 0, k + 1))
        self.NT = self.T // 128
        self.BLK = 256
        self.NB = -(-(2 * self.T + 32 * (self.BLK - 1)) // self.BLK)
        d = depth
        self.V_C = 0
        self.V_CC = 8
        self.V_FNW = 16
        self.V_LB = 24
        self.V_L0 = 24 + 2 * d * 4
        self.V_LN = 81
        self.NV = self.V_L0 + d * self.V_LN


def build_program(cfg):
    nc = bass.Bass("TRN2", target_bir_lowering=False)
    T, TC, TL, DC, DEPTH = cfg.T, cfg.TC, cfg.TL, cfg.DC, cfg.DEPTH
    xT_d = nc.dram_tensor("xT", [128, DC * T], F32, kind="ExternalInput").ap()
    vecs_d = nc.dram_tensor("vecs", [128, cfg.NV], F32, kind="ExternalInput").ap()
    wr_d = nc.dram_tensor("wr", [128, DEPTH * DC * 36], F32, kind="ExternalInput").ap()
    br_d = nc.dram_tensor("br", [1, DEPTH * 36], F32, kind="ExternalInput").ap()
    wada_d = nc.dram_tensor("w_ada", [DEPTH, 1024, 6144], F32, kind="ExternalInput").ap()
    win_d = nc.dram_tensor("w_in", [DEPTH, 1024, 4096], F32, kind="ExternalInput").ap()
    wout_d = nc.dram_tensor("w_out", [DEPTH, 1024, 1024], F32, kind="ExternalInput").ap()
    wgu_d = nc.dram_tensor("w_gu", [DEPTH, cfg.NE, 1024, 1024], F32, kind="ExternalInput").ap()
    wdn_d = nc.dram_tensor("w_dn", [DEPTH, cfg.NE, 512, 1024], F32, kind="ExternalInput").ap()
    outT_d = nc.dram_tensor("outT", [128, DC * TL], F32, kind="ExternalOutput").ap()
    xs_d = nc.dram_tensor("xs_scr", [cfg.NB * cfg.BLK, 1024], F32, kind="Internal").ap()
    ys_d = nc.dram_tensor("ys_scr", [cfg.NB * cfg.BLK, 1024], F32, kind="Internal").ap()
    xT_v = xT_d.rearrange("p (c t) -> p c t", c=DC)
    outT_v = outT_d.rearrange("p (c t) -> p c t", c=DC)

    S = Sched()
    uid = [0]

    with contextlib.ExitStack() as top:
        def sb(stack, name, shape, dt=F32):
            uid[0] += 1
            return stack.enter_context(nc.sbuf_tensor("%s_%d" % (name, uid[0]), shape, dt))

        XT = sb(top, "XT", [128, DC, T])
        hT = sb(top, "hT", [128, DC, T], BF16)
        G = sb(top, "G", [128, cfg.NT, 32])
        vecs = sb(top, "vecs", [128, cfg.NV])
        wr = sb(top, "wr", [128, DEPTH, DC, 36])
        br = sb(top, "br", [1, DEPTH * 36])
        ident = sb(top, "ident", [128, 128])
        ones = sb(top, "ones", [128, 128])
        ones1 = sb(top, "ones1", [1, 128])
        rmask = sb(top, "rmask", [128, 512], BF16)
        CH = 64
        NCHT = 128 // CH
        maskF = sb(top, "maskF", [CH, NCHT, CH])
        maskB = sb(top, "maskB", [CH, NCHT, CH])
        scT = sb(top, "scT", [128, DC, 2])
        modT = sb(top, "modT", [128, DEPTH, 48, 2])
        A1 = sb(top, "A1", [128, DEPTH, DC, 2])
        A2 = sb(top, "A2", [128, DEPTH, DC, 2])
        lbT = sb(top, "lbT", [128, 2 * DEPTH * 4])
        omlT = sb(top, "omlT", [128, 2 * DEPTH * 4])
        I32 = mybir.dt.int32
        NB = cfg.NB
        SEL = sb(top, "SEL", [128, cfg.NT, 32])
        RK = sb(top, "RK", [128, cfg.NT, 32])
        IDX = sb(top, "IDX", [128, cfg.NT, 2], I32)
        GHL = sb(top, "GHL", [128, cfg.NT, 2])
        IDXW = sb(top, "IDXW", [128, NB], I32)
        PIDXi = sb(top, "PIDXi", [128, 1], I32)
        PIDX = sb(top, "PIDX", [128, 1])
        Lst = sb(top, "Lst", [128, 128])
        THR = sb(top, "THR", [128, 18])
        JR = sb(top, "JR", [128, NB])
        c128 = sb(top, "c128", [128, NB])
        pb = [top.enter_context(nc.psum_tensor("pb%d" % i, [128, 512], F32)) for i in range(8)]

        tile_gi = {}
        for (t0_, n_, sidx_, gi_) in cfg.groups:
            for ti_ in range(n_ // 128):
                tile_gi[(t0_ + ti_ * 128) // 128] = (gi_, sidx_)
        tile_gi = {k_: v_ for k_, v_ in tile_gi.items()}

        bc_cache = {}
        order_box = []

        def order_of(bpos):
            return order_box[0][bpos]

        def XK(gi):
            return ('XT', gi)

        def HK(gi):
            return ('hT', gi)

        def mm(out, lhsT, rhs, start, stop, r, w):
            return S.op('pe', lambda e: e.matmul(out, lhsT, rhs, start=start, stop=stop), r=r, w=w)

        def tr(out, in_, r, w):
            return S.op('pe', lambda e: e.transpose(out, in_, ident[:in_.shape[0], :in_.shape[0]]), r=list(r) + [ident], w=w)

        def act(out, in_, func, r, w, bias=None, scale=None, accum=None):
            kw = {}
            if bias is not None:
                kw['bias'] = bias
            if scale is not None:
                kw['scale'] = scale
            if accum is not None:
                kw['accum_out'] = accum
            return S.op('act', lambda e: e.activation(out=out, in_=in_, func=func, **kw), r=r, w=w)

        def tt(eng, out, in0, in1, op, r, w):
            return S.op(eng, lambda e: e.tensor_tensor(out=out, in0=in0, in1=in1, op=op), r=r, w=w)

        def tsc(eng, out, in0, s1, s2, op0, op1, r, w):
            if op1 is None:
                return S.op(eng, lambda e: e.tensor_scalar(out=out, in0=in0, scalar1=s1, scalar2=None, op0=op0), r=r, w=w)
            return S.op(eng, lambda e: e.tensor_scalar(out=out, in0=in0, scalar1=s1, scalar2=s2, op0=op0, op1=op1), r=r, w=w)

        def stt(out, in0, scalar, in1, op0, op1, r, w):
            return S.op('dve', lambda e: e.scalar_tensor_tensor(out=out, in0=in0, scalar=scalar, in1=in1, op0=op0, op1=op1), r=r, w=w)

        def cp(eng, out, in_, r, w):
            return S.op(eng, lambda e: e.tensor_copy(out=out, in_=in_), r=r, w=w)

        def recip(out, in_, r, w):
            return S.op('dve', lambda e: e.reciprocal(out=out, in_=in_), r=r, w=w)

        def memset(eng, ap, val, w):
            return S.op(eng, lambda e: e.memset(ap, val), w=w)

        def dma(eng, out, in_, r, w):
            return S.op(eng, lambda e: e.dma_start(out=out, in_=in_), r=r, w=w, dma=True)

        def V(col, n=1):
            return vecs[:, col:col + n]

        memset('pool', ident[:], 0.0, [ident])
        S.op('pool', lambda e: e.affine_select(out=ident[:], in_=ident[:], compare_op=ALU.not_equal, fill=1.0,
                                               base=0, pattern=[[-1, 128]], channel_multiplier=1), r=[ident], w=[ident])
        memset('pool', ones[:], 1.0, [ones])
        memset('pool', ones1[:], 1.0, [ones1])
        memset('pool', rmask[:], 1.0, [rmask])
        S.op('pool', lambda e: e.memset(rmask[:].rearrange("p (a b) -> p a b", b=CH)[:, :, 0:1], 0.0), r=[rmask], w=[rmask])
        memset('pool', maskF[:], 1.0, [maskF])
        memset('pool', maskB[:], 1.0, [maskB])
        S.op('pool', lambda e: e.affine_select(out=maskF[:], in_=maskF[:], compare_op=ALU.is_ge, fill=0.0,
                                               base=0, pattern=[[0, NCHT], [1, CH]], channel_multiplier=-1), r=[maskF], w=[maskF])
        S.op('pool', lambda e: e.affine_select(out=maskB[:], in_=maskB[:], compare_op=ALU.is_ge, fill=0.0,
                                               base=0, pattern=[[0, NCHT], [-1, CH]], channel_multiplier=1), r=[maskB], w=[maskB])

        S.op('pool', lambda e: e.iota(PIDXi[:], pattern=[[0, 1]], base=0, channel_multiplier=1), w=[PIDXi])
        cp('dve', PIDX[:], PIDXi[:], [PIDXi], [PIDX])
        memset('pool', Lst[:], 1.0, [Lst])
        S.op('pool', lambda e: e.affine_select(out=Lst[:], in_=Lst[:], compare_op=ALU.is_ge, fill=0.0,
                                               base=-1, pattern=[[1, 128]], channel_multiplier=-1), r=[Lst], w=[Lst])
        memset('pool', c128[:], float(cfg.BLK), [c128])
        S.op('dve', lambda e: e.tensor_tensor_scan(out=JR[:], data0=ones[:, :NB], data1=c128[:], initial=-float(cfg.BLK),
                                                   op0=ALU.mult, op1=ALU.add), r=[ones, c128], w=[JR])
        cp('dve', THR[:], JR[:, 0:18], [JR], [THR])

        dma('sp', vecs[:], vecs_d, [], [vecs])
        dma('sp', wr[:].rearrange("p l c n -> p (l c n)"), wr_d, [], [wr])
        dma('sp', br[:], br_d, [], [br])
        for (t0, n, sidx, gi) in cfg.groups:
            dma('sp', XT[:, :, t0:t0 + n], xT_v[:, :, t0:t0 + n], [], [XK(gi)])

        act(scT[:, :, 0], V(cfg.V_C, 8), AF.Silu, [vecs], [scT])
        act(scT[:, :, 1], V(cfg.V_CC, 8), AF.Silu, [vecs], [scT])
        with contextlib.ExitStack() as ph:
            nlb = 2 * DEPTH * 4
            E = sb(ph, "lbE", [128, nlb])
            sE = sb(ph, "lbS", [128, 8])
            rE = sb(ph, "lbR", [128, 8])
            act(E[:], V(cfg.V_LB, nlb), AF.Exp, [vecs], [E])
            E3 = E[:].rearrange("p (d l h) -> p d l h", d=2, l=DEPTH)
            sE2 = sE[:].rearrange("p (d h) -> p d h", d=2)
            cp('dve', sE2, E3[:, :, 0, :], [E], [sE])
            for l in range(1, DEPTH):
                tt('dve', sE2, sE2, E3[:, :, l, :], ALU.add, [sE, E], [sE])
            recip(rE[:], sE[:], [sE], [rE])
            rE2 = rE[:].rearrange("p (d h) -> p d h", d=2)
            lb3 = lbT[:].rearrange("p (d l h) -> p d l h", d=2, l=DEPTH)
            memset('dve', lbT[:], 0.0, [lbT])
            for l in range(1, DEPTH):
                tt('dve', E3[:, :, l, :], E3[:, :, l, :], rE2, ALU.mult, [E, rE], [E])
                tt('dve', lb3[:, :, l, :], lb3[:, :, l - 1, :], E3[:, :, l, :], ALU.add, [lbT, E], [lbT])
            tsc('dve', omlT[:], lbT[:], -1.0, 1.0, ALU.mult, ALU.add, [lbT], [omlT])

            wa = [sb(ph, "wada%d" % i, [128, DC, 512]) for i in range(4)]
            k = 0
            for l in cfg.layers:
                voff = cfg.V_L0 + l * cfg.V_LN
                for jb in range(12):
                    wt = wa[k % 4]
                    k += 1
                    dma('sp', wt[:], wada_d[l, :, jb * 512:(jb + 1) * 512].rearrange("(c p) n -> p c n", p=128), [], [wt])
                    pm = pb[jb % 2]
                    for jj in range(4):
                        for c in range(DC):
                            mm(pm[:, jj * 2:jj * 2 + 2], wt[:, c, jj * 128:(jj + 1) * 128], scT[:, c, :], c == 0, c == DC - 1,
                               [wt, scT], [pm])
                    for s in range(2):
                        tt('dve', modT[:, l, jb * 4:jb * 4 + 4, s], pm[:, 0:8].rearrange("p (j s) -> p j s", s=2)[:, :, s],
                           V(voff + 16 + jb * 4, 4), ALU.add, [pm, vecs], [modT])
                for s in range(2):
                    stt(A1[:, l, :, s], modT[:, l, 8:16, s], 1.0, V(voff + 0, 8), ALU.add, ALU.mult, [modT, vecs], [A1])
                    stt(A2[:, l, :, s], modT[:, l, 32:40, s], 1.0, V(voff + 8, 8), ALU.add, ALU.mult, [modT, vecs], [A2])
        S.barrier()

        def norm_modulate(l, which, router):
            A = A1 if which == 0 else A2
            sh0 = 0 if which == 0 else 24
            with contextlib.ExitStack() as ph:
                sq = [sb(ph, "nsq%d" % i, [128, 512]) for i in range(2)]
                rs = [sb(ph, "nrs%d" % i, [128, 512]) for i in range(2)]
                tmp = [sb(ph, "ntmp%d" % i, [128, 512]) for i in range(2)]
                if router:
                    h2f = [sb(ph, "h2f%d" % i, [128, DC, 512]) for i in range(2)]
                    rt = {nm: [sb(ph, "rt_%s%d" % (nm, i), shp) for i in range(2)] for nm, shp in
                          [("lg", [128, 36]), ("gm", [128, 1]), ("ngm", [128, 1]), ("gmask", [128, 4]), ("ge", [128, 4]),
                           ("gs", [128, 1]), ("pen", [128, 4]), ("el", [128, 32]), ("t8", [128, 8]), ("nm1", [128, 1]),
                           ("sel", [128, 32]), ("ex", [128, 32]), ("gx", [128, 32]), ("den", [128, 1]), ("pr", [128, 1]),
                           ("rp", [128, 1])]}
                kk = 0
                tl = 0
                cpar = [0]
                if router:
                    cums = [sb(ph, "cums%d" % i, [128, 32]) for i in range(2)]
                    memset('dve', cums[0][:], 0.0, [cums[0]])
                for (t0, n, sidx, gi) in cfg.groups:
                    pss = pb[gi % 2]
                    for c in range(DC):
                        q = sq[kk % 2]
                        kk += 1
                        act(q[:, :n], XT[:, c, t0:t0 + n], AF.Square, [XK(gi)], [q])
                        mm(pss[:, :n], ones[:], q[:, :n], c == 0, c == DC - 1, [ones, q], [pss])
                    r_ = rs[gi % 2]
                    act(r_[:, :n], pss[:, :n], AF.Sqrt, [pss], [r_], bias=cfg.EPS, scale=1.0 / 1024)
                    recip(r_[:, :n], r_[:, :n], [r_], [r_])
                    for c in range(DC):
                        tm = tmp[kk % 2]
                        kk += 1
                        tt('dve', tm[:, :n], XT[:, c, t0:t0 + n], r_[:, :n], ALU.mult, [XK(gi), r_], [tm])
                        if router:
                            hf = h2f[gi % 2]
                            act(hf[:, c, :n], tm[:, :n], AF.Identity, [tm, A, modT], [hf],
                                bias=modT[:, l, sh0 + c, sidx:sidx + 1], scale=A[:, l, c, sidx:sidx + 1])
                            cp('pool', hT[:, c, t0:t0 + n], hf[:, c, :n], [hf], [HK(gi)])
                        else:
                            act(hT[:, c, t0:t0 + n], tm[:, :n], AF.Identity, [tm, A, modT], [HK(gi)],
                                bias=modT[:, l, sh0 + c, sidx:sidx + 1], scale=A[:, l, c, sidx:sidx + 1])
                    if router:
                        hf = h2f[gi % 2]
                        for ti in range(n // 128):
                            tg = (t0 + ti * 128) // 128
                            R = {nm: v[tl % 2] for nm, v in rt.items()}
                            pl = pb[2 + tl % 2]
                            tl += 1
                            for c in range(DC):
                                mm(pl[:, 0:36], hf[:, c, ti * 128:(ti + 1) * 128], wr[:, l, c, :], c == 0, False, [hf, wr], [pl])
                            mm(pl[:, 0:36], ones1[:], br[:, l * 36:(l + 1) * 36], False, True, [ones1, br], [pl])
                            lg = R["lg"]
                            cp('dve', lg[:], pl[:, 0:36], [pl], [lg])
                            S.op('dve', lambda e, o=R["gm"], i=lg: e.tensor_reduce(out=o[:], in_=i[:, 0:4], axis=AX.X, op=ALU.max),
                                 r=[lg], w=[R["gm"]])
                            tsc('dve', R["ngm"][:], R["gm"][:], -1.0, None, ALU.mult, None, [R["gm"]], [R["ngm"]])
                            tsc('dve', R["gmask"][:], lg[:, 0:4], R["gm"][:], None, ALU.is_equal, None, [lg, R["gm"]], [R["gmask"]])
                            act(R["ge"][:], lg[:, 0:4], AF.Exp, [lg, R["ngm"]], [R["ge"], R["gs"]], bias=R["ngm"][:], scale=1.0,
                                accum=R["gs"][:])
                            tsc('dve', R["pen"][:], R["gmask"][:], 1e30, -1e30, ALU.mult, ALU.add, [R["gmask"]], [R["pen"]])
                            tt('dve', R["el"][:].rearrange("p (g e) -> p g e", g=4), lg[:, 4:36].rearrange("p (g e) -> p g e", g=4),
                               R["pen"][:].unsqueeze(2).to_broadcast([128, 4, 8]), ALU.add, [lg, R["pen"]], [R["el"]])
                            S.op('dve', lambda e, o=R["t8"], i=R["el"]: e.max(out=o[:], in_=i[:]), r=[R["el"]], w=[R["t8"]])
                            tsc('dve', R["nm1"][:], R["t8"][:, 0:1], -1.0, None, ALU.mult, None, [R["t8"]], [R["nm1"]])
                            tsc('dve', R["sel"][:], R["el"][:], R["t8"][:, 1:2], None, ALU.is_ge, None, [R["el"], R["t8"]], [R["sel"]])
                            act(R["ex"][:], R["el"][:], AF.Exp, [R["el"], R["nm1"]], [R["ex"]], bias=R["nm1"][:], scale=1.0)
                            tt('dve', R["gx"][:], R["sel"][:], R["ex"][:], ALU.mult, [R["sel"], R["ex"]], [R["gx"]])
                            S.op('dve', lambda e, o=R["den"], i=R["gx"]: e.tensor_reduce(out=o[:], in_=i[:], axis=AX.X, op=ALU.add),
                                 r=[R["gx"]], w=[R["den"]])
                            tt('dve', R["pr"][:], R["den"][:], R["gs"][:], ALU.mult, [R["den"], R["gs"]], [R["pr"]])
                            recip(R["rp"][:], R["pr"][:], [R["pr"]], [R["rp"]])
                            tsc('dve', G[:, tg, :], R["gx"][:], R["rp"][:], None, ALU.mult, None, [R["gx"], R["rp"]], [('G', gi)])
                            cp('dve', SEL[:, tg, :], R["sel"][:], [R["sel"]], [('SEL', tg)])
                            prk = pb[4 + tl % 2]
                            mm(prk[:, 0:32], Lst[:], R["sel"][:], True, False, [Lst, R["sel"]], [prk])
                            mm(prk[:, 0:32], ones[:], cums[cpar[0]][:], False, True, [ones, cums[cpar[0]]], [prk])
                            cp('dve', RK[:, tg, :], prk[:, 0:32], [prk], [('RK', tg)])
                            tt('dve', cums[1 - cpar[0]][:], cums[cpar[0]][:], R["sel"][:], ALU.add, [cums[cpar[0]], R["sel"]], [cums[1 - cpar[0]]])
                            cpar[0] = 1 - cpar[0]
                if router:
                    CNT = sb(ph, "CNT", [128, 32])
                    cmp18 = sb(ph, "cmp18", [128, 32, 18])
                    NBLK = sb(ph, "NBLK", [128, 32])
                    PADD = sb(ph, "PADD", [128, 32])
                    PEND = sb(ph, "PEND", [128, 32])
                    PST = sb(ph, "PST", [128, 32])
                    cmpB = sb(ph, "cmpB", [128, NB, 32])
                    BEf = sb(ph, "BEf", [128, NB])
                    pc = pb[6]
                    mm(pc[:, 0:32], ones[:], cums[cpar[0]][:], True, True, [ones, cums[cpar[0]]], [pc])
                    cp('dve', CNT[:], pc[:, 0:32], [pc], [CNT])
                    tt('dve', cmp18[:], CNT[:].unsqueeze(2).to_broadcast([128, 32, 18]), THR[:].unsqueeze(1).to_broadcast([128, 32, 18]),
                       ALU.is_gt, [CNT, THR], [cmp18])
                    S.op('dve', lambda e: e.tensor_reduce(out=NBLK[:], in_=cmp18[:], axis=AX.X, op=ALU.add), r=[cmp18], w=[NBLK])
                    tsc('dve', PADD[:], NBLK[:], float(cfg.BLK), None, ALU.mult, None, [NBLK], [PADD])
                    S.op('dve', lambda e: e.tensor_tensor_scan(out=PEND[:], data0=ones[:, 0:32], data1=PADD[:], initial=0.0,
                                                               op0=ALU.mult, op1=ALU.add), r=[ones, PADD], w=[PEND])
                    tt('dve', PST[:], PEND[:], PADD[:], ALU.subtract, [PEND, PADD], [PST])
                    tt('dve', cmpB[:], PEND[:].unsqueeze(1).to_broadcast([128, NB, 32]), JR[:].unsqueeze(2).to_broadcast([128, NB, 32]),
                       ALU.is_le, [PEND, JR], [cmpB])
                    S.op('dve', lambda e: e.tensor_reduce(out=BEf[:], in_=cmpB[:], axis=AX.X, op=ALU.add), r=[cmpB], w=[BEf])
                    tsc('dve', BEf[:], BEf[:], 31.0, float(32 * l), ALU.min, ALU.add, [BEf], [BEf])
                    UNU = sb(ph, "UNU", [128, NB])
                    tsc('dve', UNU[:], JR[:], PEND[:, 31:32], 8192.0, ALU.is_ge, ALU.mult, [JR, PEND], [UNU])
                    tt('dve', BEf[:], BEf[:], UNU[:], ALU.add, [BEf, UNU], [BEf])
                    tsc('dve', IDXW[:], BEf[:], 128.0, PIDX[:], ALU.mult, ALU.add, [BEf, PIDX], [IDXW])
                    pp = {nm: [sb(ph, "pp_%s%d" % (nm, i), shp) for i in range(2)] for nm, shp in
                          [("pos", [128, 32]), ("t8", [128, 8]), ("eq", [128, 32]), ("pr", [128, 32])]}
                    for tg in range(cfg.NT):
                        Q = {nm: v[tg % 2] for nm, v in pp.items()}
                        gi_t = tile_gi[tg][0]
                        tt('dve', Q["pos"][:], RK[:, tg, :], PST[:], ALU.add, [('RK', tg), PST], [Q["pos"]])
                        stt(Q["pos"][:], Q["pos"][:], 1.0, SEL[:, tg, :], ALU.add, ALU.mult, [Q["pos"], ('SEL', tg)], [Q["pos"]])
                        S.op('dve', lambda e, o=Q["t8"], i=Q["pos"]: e.max(out=o[:], in_=i[:]), r=[Q["pos"]], w=[Q["t8"]])
                        tsc('dve', IDX[:, tg, :], Q["t8"][:, 0:2], -1.0, None, ALU.add, None, [Q["t8"]], [('IDX', tg)])
                        for k in range(2):
                            tsc('dve', Q["eq"][:], Q["pos"][:], Q["t8"][:, k:k + 1], None, ALU.is_equal, None, [Q["pos"], Q["t8"]], [Q["eq"]])
                            tt('dve', Q["pr"][:], Q["eq"][:], G[:, tg, :], ALU.mult, [Q["eq"], ('G', gi_t)], [Q["pr"]])
                            S.op('dve', lambda e, o=GHL[:, tg, k:k + 1], i=Q["pr"]: e.tensor_reduce(out=o, in_=i[:], axis=AX.X, op=ALU.add),
                                 r=[Q["pr"]], w=[('GHL', tg)])
            S.barrier()

        def out_proj(l, Y, nk, k0, ph, wo_buf=None, banks=None):
            if wo_buf is None:
                Wo = sb(ph, "Wo", [128, nk, 1024], BF16)
            else:
                Wo = wo_buf
            dma('pool', Wo[:], wout_d[l, k0 * 128:(k0 + nk) * 128, :].rearrange("(c p) n -> p c n", p=128), [], [Wo])
            kk = 0
            for (t0, n, sidx, gi) in cfg.groups:
                for j in range(DC):
                    po = pb[4 + kk % 4] if banks is None else banks[kk % len(banks)]
                    kk += 1
                    for c in range(nk):
                        rhs = Y[:, c, t0:t0 + n] if nk > 1 else Y[:, t0:t0 + n]
                        mm(po[:, :n], Wo[:, c, j * 128:(j + 1) * 128], rhs, c == 0, c == nk - 1, [Wo, Y], [po])
                    stt(XT[:, j, t0:t0 + n], po[:, :n], modT[:, l, 16 + j, sidx:sidx + 1], XT[:, j, t0:t0 + n],
                        ALU.mult, ALU.add, [po, modT, XK(gi)], [XK(gi)])

        def load_win(ph, name, l, col, W=None):
            if W is None:
                W = sb(ph, name, [128, DC, 128], BF16)
            dma('pool', W[:], win_d[l, :, col:col + 128].rearrange("(c p) n -> p c n", p=128), [], [W])
            return W

        def proj(W, ps, t0, n, gi):
            for c in range(DC):
                mm(ps[:, :n], W[:, c, :], hT[:, c, t0:t0 + n], c == 0, c == DC - 1, [W, HK(gi)], [ps])

        def conv_phase(l):
            voff = cfg.V_L0 + l * cfg.V_LN
            R_ = TL // 64
            with contextlib.ExitStack() as ph:
                Z = sb(ph, "Z", [128, 4, T], BF16)
                SS = sb(ph, "SS", [128, T])
                u = sb(ph, "u", [128, T])
                Bs = sb(ph, "Bs", [128, T])
                y = sb(ph, "y", [128, T])
                hv = [sb(ph, "hv%d" % i, [128, 512]) for i in range(2)]
                zs = [sb(ph, "zs%d" % i, [128, 512]) for i in range(2)]
                wsets = [[sb(ph, "cW%d_%d" % (i, k_), [128, DC, 128], BF16) for k_ in range(3)] for i in range(1)]
                for cc in range(4):
                    with contextlib.ExitStack() as ph2:
                        WB = load_win(ph2, "WB", l, cc * 128, wsets[0][0])
                        WC = load_win(ph2, "WC", l, 512 + cc * 128, wsets[0][1])
                        WH = load_win(ph2, "WH", l, 1024 + cc * 128, wsets[0][2])
                        for (t0, n, sidx, gi) in cfg.groups:
                            pB, pC, pH = pb[0 + 4 * (gi % 2)], pb[1 + 4 * (gi % 2)], pb[2 + 4 * (gi % 2)]
                            proj(WB, pB, t0, n, gi)
                            proj(WC, pC, t0, n, gi)
                            proj(WH, pH, t0, n, gi)
                            h_ = hv[gi % 2]
                            act(h_[:, :n], pH[:, :n], AF.Copy, [pH], [h_])
                            act(Bs[:, t0:t0 + n], pB[:, :n], AF.Copy, [pB], [Bs])
                            tt('dve', u[:, t0:t0 + n], pC[:, :n], h_[:, :n], ALU.mult, [pC, h_], [u])
                        w0, w1, w2 = V(voff + 64 + 0 * 4 + cc), V(voff + 64 + 1 * 4 + cc), V(voff + 64 + 2 * 4 + cc)
                        act(y[:], u[:], AF.Identity, [u, vecs], [y], scale=w1)
                        stt(y[:, 1:TC], u[:, 0:TC - 1], w0, y[:, 1:TC], ALU.mult, ALU.add, [u, vecs, y], [y])
                        stt(y[:, 0:TC - 1], u[:, 1:TC], w2, y[:, 0:TC - 1], ALU.mult, ALU.add, [u, vecs, y], [y])
                        if cc < 2:
                            ul = u[:, TC:T].rearrange("p (r w) -> p r w", w=64)
                            yl = y[:, TC:T].rearrange("p (r w) -> p r w", w=64)
                            stt(yl[:, :, 1:64], ul[:, :, 0:63], w0, yl[:, :, 1:64], ALU.mult, ALU.add, [u, vecs, y], [y])
                            stt(yl[:, :, 0:63], ul[:, :, 1:64], w2, yl[:, :, 0:63], ALU.mult, ALU.add, [u, vecs, y], [y])
                        else:
                            stt(y[:, TC + 64:T], u[:, TC:T - 64], w0, y[:, TC + 64:T], ALU.mult, ALU.add, [u, vecs, y], [y])
                            stt(y[:, TC:T - 64], u[:, TC + 64:T], w2, y[:, TC:T - 64], ALU.mult, ALU.add, [u, vecs, y], [y])
                        tt('dve', y[:], y[:], Bs[:], ALU.mult, [y, Bs], [y])
                        act(Z[:, cc, :], y[:], AF.Copy, [y], [Z])
                        for (t0, n, sidx, gi) in cfg.groups:
                            z_ = zs[gi % 2]
                            pz = pb[3 + 4 * (gi % 2)]
                            act(z_[:, :n], y[:, t0:t0 + n], AF.Square, [y], [z_])
                            mm(pz[:, :n], ones[:], z_[:, :n], True, True, [ones, z_], [pz])
                            if cc == 0:
                                cp('dve', SS[:, t0:t0 + n], pz[:, :n], [pz], [SS])
                            else:
                                tt('dve', SS[:, t0:t0 + n], pz[:, :n], SS[:, t0:t0 + n], ALU.add, [pz, SS], [SS])
                act(SS[:], SS[:], AF.Sqrt, [SS], [SS], bias=cfg.EPS, scale=1.0 / 512)
                recip(SS[:], SS[:], [SS], [SS])
                for cc in range(4):
                    stt(Z[:, cc, :], Z[:, cc, :], V(voff + 76 + cc), SS[:], ALU.mult, ALU.mult, [Z, vecs, SS], [Z])
                out_proj(l, Z, 4, 0, ph)
            S.barrier()

        def heads_phase(l):
            voff = cfg.V_L0 + l * cfg.V_LN
            hnw = V(voff + 80)
            lat = cfg.groups[1:]
            order = [cfg.groups, [cfg.groups[0]] + lat[::-1]]
            with contextlib.ExitStack() as ph:
                hW = [sb(ph, "hW%d" % k_, [128, DC, 128], BF16) for k_ in range(5)]
                WoH = sb(ph, "WoH", [128, 1, 1024], BF16)
                Of = sb(ph, "Of", [128, T])
                Yh = sb(ph, "Yh", [128, T], BF16)
                NS = 4
                St = [sb(ph, "St%d" % i, [128, 128]) for i in range(NS)]
                names = ["qs", "sg", "kk", "vs", "b", "eb", "enb", "ko", "gs"]
                tmps = [{nm: sb(ph, "g%s%d" % (nm, i), [128, 512]) for nm in names} for i in range(2)]
                dch = [sb(ph, "dch%d" % i, [128, 16]) for i in range(2)]
                totc = [sb(ph, "totc%d" % i, [128, 16]) for i in range(2)]
                koT = [sb(ph, "koT%d" % i, [CH, NCHT, 128]) for i in range(2)]
                vT = [sb(ph, "vT%d" % i, [CH, NCHT, 128]) for i in range(2)]
                PT = [sb(ph, "PT%d" % i, [CH, NCHT, CH]) for i in range(2)]
                midc = [sb(ph, "midc%d" % i, [128, 16]) for i in range(2)]
                emid = [sb(ph, "emid%d" % i, [128, 16]) for i in range(2)]
                etm = [sb(ph, "etm%d" % i, [128, 16]) for i in range(2)]
                osq = sb(ph, "osq", [128, 512])
                ors = sb(ph, "ors", [128, 512])
                for hh in range(4):
                    Wq = load_win(ph, "Wq", l, 1536 + hh * 128, hW[0])
                    Wf = [load_win(ph, "Wzf", l, 2048 + hh * 128, hW[1]), load_win(ph, "Wzb", l, 2560 + hh * 128, hW[2])]
                    Wi = load_win(ph, "Wi", l, 3072 + hh * 128, hW[3])
                    Wg = load_win(ph, "Wg", l, 3584 + hh * 128, hW[4])
                    gpar = 0
                    tpar = 0
                    for dirn in range(2):
                        lbc = (dirn * DEPTH + l) * 4 + hh
                        lb_ap, oml_ap = lbT[:, lbc:lbc + 1], omlT[:, lbc:lbc + 1]
                        cur = 0
                        memset('dve', St[0][:], 0.0, [St[0]])
                        msk = maskF if dirn == 0 else maskB
                        for (t0, n, sidx, gi) in order[dirn]:
                            tp = tmps[gpar % 2]
                            dc_ = dch[gpar % 2]
                            gpar += 1
                            nch = n // CH
                            pq, pz_, pi_, pg = pb[0], pb[1], pb[2], pb[3]
                            proj(Wq, pq, t0, n, gi)
                            proj(Wf[dirn], pz_, t0, n, gi)
                            proj(Wi, pi_, t0, n, gi)
                            act(tp["qs"][:, :n], pq[:, :n], AF.Silu, [pq], [tp["qs"]])
                            act(tp["sg"][:, :n], pz_[:, :n], AF.Sigmoid, [pz_], [tp["sg"]])
                            act(tp["vs"][:, :n], pi_[:, :n], AF.Copy, [pi_], [tp["vs"]])
                            if dirn == 1:
                                proj(Wg, pg, t0, n, gi)
                                act(tp["gs"][:, :n], pg[:, :n], AF.Silu, [pg], [tp["gs"]])
                            tc_ = totc[(gpar - 1) % 2]
                            tsc('dve', tp["sg"][:, :n], tp["sg"][:, :n], oml_ap, lb_ap, ALU.mult, ALU.add, [tp["sg"], omlT, lbT], [tp["sg"]])
                            tsc('dve', tp["kk"][:, :n], tp["sg"][:, :n], -1.0, 1.0, ALU.mult, ALU.add, [tp["sg"]], [tp["kk"]])
                            act(tp["sg"][:, :n], tp["sg"][:, :n], AF.Ln, [tp["sg"]], [tp["sg"]])
                            S.op('dve', lambda e, o=tp["b"], m=rmask, d1=tp["sg"], n=n: e.tensor_tensor_scan(
                                out=o[:, :n], data0=m[:, :n], data1=d1[:, :n], initial=0.0, op0=ALU.mult, op1=ALU.add),
                                r=[rmask, tp["sg"]], w=[tp["b"]])
                            b3 = tp["b"][:, :n].rearrange("p (a c) -> p a c", c=CH)
                            cp('dve', tc_[:, :nch], b3[:, :, CH - 1], [tp["b"]], [tc_])
                            act(dc_[:, :nch], tc_[:, :nch], AF.Exp, [tc_], [dc_])
                            bb = tp["b"]
                            if dirn == 1:
                                tt('dve', b3, b3, tc_[:, :nch].unsqueeze(2).to_broadcast([128, nch, CH]), ALU.subtract, [tp["b"], tc_], [tp["b"]])
                                tt('dve', bb[:, :n], tp["sg"][:, :n], bb[:, :n], ALU.subtract, [tp["sg"], bb], [bb])
                            md_, em_, et_ = midc[(gpar - 1) % 2], emid[(gpar - 1) % 2], etm[(gpar - 1) % 2]
                            midcol = CH // 2 - 1 if dirn == 0 else CH // 2
                            cp('dve', md_[:, :nch], b3[:, :, midcol], [tp["b"]], [md_])
                            tt('dve', b3, b3, md_[:, :nch].unsqueeze(2).to_broadcast([128, nch, CH]), ALU.subtract, [tp["b"], md_], [tp["b"]])
                            act(tp["eb"][:, :n], bb[:, :n], AF.Exp, [bb], [tp["eb"]])
                            act(tp["enb"][:, :n], bb[:, :n], AF.Exp, [bb], [tp["enb"]], scale=-1.0)
                            act(em_[:, :nch], md_[:, :nch], AF.Exp, [md_], [em_])
                            tt('dve', et_[:, :nch], tc_[:, :nch], md_[:, :nch], ALU.subtract, [tc_, md_], [et_])
                            act(et_[:, :nch], et_[:, :nch], AF.Exp, [et_], [et_])
                            tt('dve', tp["qs"][:, :n], tp["qs"][:, :n], tp["eb"][:, :n], ALU.mult, [tp["qs"], tp["eb"]], [tp["qs"]])
                            tt('dve', tp["kk"][:, :n], tp["kk"][:, :n], tp["enb"][:, :n], ALU.mult, [tp["kk"], tp["enb"]], [tp["kk"]])
                            tt('dve', tp["eb"][:, :n].rearrange("p (a c) -> p a c", c=CH),
                               tp["qs"][:, :n].rearrange("p (a c) -> p a c", c=CH),
                               em_[:, :nch].unsqueeze(2).to_broadcast([128, nch, CH]), ALU.mult, [tp["qs"], em_], [tp["eb"]])
                            tt('dve', tp["ko"][:, :n].rearrange("p (a c) -> p a c", c=CH),
                               tp["kk"][:, :n].rearrange("p (a c) -> p a c", c=CH),
                               et_[:, :nch].unsqueeze(2).to_broadcast([128, nch, CH]), ALU.mult, [tp["kk"], et_], [tp["ko"]])
                            tiles = list(range(n // 128))
                            chunks = list(range(NCHT))
                            if dirn == 1:
                                tiles = tiles[::-1]
                                chunks = chunks[::-1]
                            for ti in tiles:
                                c0 = ti * 128
                                kT_, vT_, PT_ = koT[tpar % 2], vT[tpar % 2], PT[tpar % 2]
                                tpar += 1
                                pk, pv = pb[4], pb[5]
                                pk3 = pk[0:CH, 0:NCHT * 128].rearrange("p (j k) -> p j k", j=NCHT)
                                pv3 = pv[0:CH, 0:NCHT * 128].rearrange("p (j k) -> p j k", j=NCHT)
                                for j in range(NCHT):
                                    tr(pk3[:, j, :], tp["ko"][:, c0 + CH * j:c0 + CH * j + CH], [tp["ko"]], [pk])
                                for j in range(NCHT):
                                    tr(pv3[:, j, :], tp["vs"][:, c0 + CH * j:c0 + CH * j + CH], [tp["vs"]], [pv])
                                act(kT_[:], pk3, AF.Copy, [pk], [kT_])
                                cp('dve', vT_[:], pv3, [pv], [vT_])
                                par = tpar % 2
                                psc = pb[3][0:CH, 256:256 + NCHT * CH].rearrange("p (j k) -> p j k", j=NCHT)
                                for j in range(NCHT):
                                    cs = c0 + CH * j
                                    mm(psc[:, j, :], tp["kk"][:, cs:cs + CH], tp["qs"][:, cs:cs + CH], True, True,
                                       [tp["kk"], tp["qs"]], [pb[3]])
                                tsc('dve', PT_[:], psc, -1e30, 1e30, ALU.max, ALU.min, [pb[3]], [PT_])
                                tt('dve', PT_[:], PT_[:], msk[:], ALU.mult, [PT_, msk], [PT_])
                                po = pb[7][:, 256 * par:256 * par + 128]
                                for j in chunks:
                                    cs = c0 + CH * j
                                    jg = cs // CH
                                    mm(pb[6][:, 128 * j:128 * j + 128], kT_[:, j, :], vT_[:, j, :], True, True, [kT_, vT_], [('pb6', j)])
                                    mm(po[:, CH * j:CH * j + CH], St[cur][:], tp["eb"][:, cs:cs + CH], True, False, [St[cur], tp["eb"]], [('pb7', 'o', par)])
                                    mm(po[:, CH * j:CH * j + CH], vT_[:, j, :], PT_[:, j, :], False, True, [vT_, PT_], [('pb7', 'o', par)])
                                    nxt = (cur + 1) % NS
                                    stt(St[nxt][:], St[cur][:], dc_[:, jg:jg + 1], pb[6][:, 128 * j:128 * j + 128], ALU.mult, ALU.add,
                                        [St[cur], dc_, ('pb6', j)], [St[nxt]])
                                    cur = nxt
                                if dirn == 0:
                                    act(Of[:, t0 + c0:t0 + c0 + 128], po[:, 0:128], AF.Copy, [('pb7', 'o', par)], [Of])
                                else:
                                    tt('dve', Of[:, t0 + c0:t0 + c0 + 128], po[:, 0:128], Of[:, t0 + c0:t0 + c0 + 128], ALU.add, [('pb7', 'o', par), Of], [Of])
                            if dirn == 1:
                                pn = pb[3]
                                act(osq[:, :n], Of[:, t0:t0 + n], AF.Square, [Of], [osq])
                                mm(pn[:, :n], ones[:], osq[:, :n], True, True, [ones, osq], [pn])
                                act(ors[:, :n], pn[:, :n], AF.Sqrt, [pn], [ors], bias=cfg.EPS, scale=1.0 / 128)
                                recip(ors[:, :n], ors[:, :n], [ors], [ors])
                                stt(osq[:, :n], Of[:, t0:t0 + n], hnw, ors[:, :n], ALU.mult, ALU.mult, [Of, vecs, ors], [osq])
                                tt('dve', Yh[:, t0:t0 + n], osq[:, :n], tp["gs"][:, :n], ALU.mult, [osq, tp["gs"]], [Yh])
                    out_proj(l, Yh, 1, 4 + hh, ph, WoH, [pb[0], pb[1], pb[2], pb[3]])
            S.barrier()

        def moe_phase_dense(l):
            with contextlib.ExitStack() as ph:
                WA = [sb(ph, "WA%d" % i, [128, DC, 512], BF16) for i in range(2)]
                WU = [sb(ph, "WU%d" % i, [128, DC, 512], BF16) for i in range(2)]
                WD = [sb(ph, "WD%d" % i, [128, 4, 1024], BF16) for i in range(2)]
                gbc = [sb(ph, "gbc%d" % i, [128, 512], BF16) for i in range(2)]
                sa = [sb(ph, "sa%d" % i, [128, 512]) for i in range(2)]
                t1 = [sb(ph, "t1%d" % i, [128, 512], BF16) for i in range(2)]
                hm = [sb(ph, "hm%d" % i, [128, 4, 512], BF16) for i in range(2)]

                def load(e):
                    s = e % 2
                    dma('pool', WA[s][:], wgu_d[l, e, :, 0:512].rearrange("(c p) n -> p c n", p=128), [], [WA[s]])
                    dma('pool', WU[s][:], wgu_d[l, e, :, 512:1024].rearrange("(c p) n -> p c n", p=128), [], [WU[s]])
                    dma('pool', WD[s][:], wdn_d[l, e, :, :].rearrange("(c p) n -> p c n", p=128), [], [WD[s]])

                load(0)
                kk = 0
                k2 = 0
                for e in range(cfg.NE):
                    if e + 1 < cfg.NE:
                        load(e + 1)
                    s = e % 2
                    for (t0, n, sidx, gi) in cfg.groups:
                        pg = pb[0]
                        for ti in range(n // 128):
                            tg = (t0 + ti * 128) // 128
                            mm(pg[:, ti * 128:(ti + 1) * 128], G[:, tg, e:e + 1].to_broadcast([128, 128]), ident[:], True, True,
                               [('G', gi), ident], [pg])
                        gb = gbc[kk % 2]
                        hm_ = hm[kk % 2]
                        kk += 1
                        act(gb[:, :n], pg[:, :n], AF.Copy, [pg], [gb])
                        for hc in range(4):
                            pa, pu = pb[1 + 2 * (k2 % 2)], pb[2 + 2 * (k2 % 2)]
                            sa_, t1_ = sa[k2 % 2], t1[k2 % 2]
                            k2 += 1
                            for c in range(DC):
                                mm(pa[:, :n], WA[s][:, c, hc * 128:(hc + 1) * 128], hT[:, c, t0:t0 + n], c == 0, c == DC - 1,
                                   [WA[s], HK(gi)], [pa])
                            for c in range(DC):
                                mm(pu[:, :n], WU[s][:, c, hc * 128:(hc + 1) * 128], hT[:, c, t0:t0 + n], c == 0, c == DC - 1,
                                   [WU[s], HK(gi)], [pu])
                            act(sa_[:, :n], pa[:, :n], AF.Silu, [pa], [sa_])
                            tt('dve', t1_[:, :n], sa_[:, :n], pu[:, :n], ALU.mult, [sa_, pu], [t1_])
                            tt('pool', hm_[:, hc, :n], t1_[:, :n], gb[:, :n], ALU.mult, [t1_, gb], [hm_])
                        for j in range(DC):
                            py = pb[5 + j % 3]
                            for hc in range(4):
                                mm(py[:, :n], WD[s][:, hc, j * 128:(j + 1) * 128], hm_[:, hc, :n], hc == 0, hc == 3, [WD[s], hm_], [py])
                            stt(XT[:, j, t0:t0 + n], py[:, :n], modT[:, l, 40 + j, sidx:sidx + 1], XT[:, j, t0:t0 + n],
                                ALU.mult, ALU.add, [py, modT, XK(gi)], [XK(gi)])
            S.barrier()

        def moe_phase(l):
            IOA = bass.IndirectOffsetOnAxis
            with contextlib.ExitStack() as ph:
                WAU = [sb(ph, "WAU%d" % i, [128, DC, 1024], BF16) for i in range(2)]
                WD = [sb(ph, "WD%d" % i, [128, 4, 1024], BF16) for i in range(2)]
                xb = [sb(ph, "xb%d" % i, [128, 1024]) for i in range(2)]
                yb = [sb(ph, "yb%d" % i, [128, 1024]) for i in range(2)]
                xbT2 = [sb(ph, "xbT2%d" % i, [128, DC, 256], BF16) for i in range(2)]
                h32 = yb[0][:, :].rearrange("p (c t) -> p c t", c=DC)
                sa2 = sb(ph, "sa2", [128, 2, 512], BF16)
                hm2 = [sb(ph, "hm2%d" % i, [128, 4, 256], BF16) for i in range(2)]
                pT = [pb[0], pb[1]]
                order = []
                for i_ in range(NB):
                    order.append(i_ // 2 if i_ % 2 == 0 else NB - 1 - i_ // 2)

                order_box.clear()
                order_box.append(order)


                wgu2 = wgu_d.rearrange("l e (p c) n -> (l e p) (c n)", c=8)
                wdn2 = wdn_d.rearrange("l e (p c) n -> (l e p) (c n)", c=4)

                def bcreg(e):
                    if 'r' not in bc_cache:
                        bc_cache['r'] = e.to_reg(DEPTH * 32 * 128 - 1)
                    return bc_cache['r']

                def load(bpos):
                    s_ = bpos % 2
                    j = order_of(bpos)
                    for q in range(4):
                        S.op('pool', lambda e, j=j, q=q, s_=s_: e.indirect_dma_start(
                            out=WAU[s_][:, 2 * q:2 * q + 2, :].rearrange("p a n -> p (a n)"), out_offset=None, in_=wgu2,
                            in_offset=IOA(ap=IDXW[:, j:j + 1], axis=0), element_offset=q * 2048, bounds_check=bcreg(e), oob_is_err=False),
                            r=[IDXW], w=[('WAU', s_, q)], dma=True)
                    for q in range(2):
                        S.op('pool', lambda e, j=j, q=q, s_=s_: e.indirect_dma_start(
                            out=WD[s_][:, 2 * q:2 * q + 2, :].rearrange("p a n -> p (a n)"), out_offset=None, in_=wdn2,
                            in_offset=IOA(ap=IDXW[:, j:j + 1], axis=0), element_offset=q * 2048, bounds_check=bcreg(e), oob_is_err=False),
                            r=[IDXW], w=[('WD', s_, q)], dma=True)

                load(0)
                scat = []
                for tg in range(cfg.NT):
                    gi, sidx = tile_gi[tg]
                    act(h32, hT[:, :, tg * 128:(tg + 1) * 128], AF.Copy, [HK(gi)], [yb[0]])
                    for c in range(DC):
                        tr(pT[c // 4][:, (c % 4) * 128:(c % 4 + 1) * 128], h32[:, c, :], [yb[0]], [pT[c // 4]])
                    x_ = xb[tg % 2]
                    act(x_[:, 0:512], pT[0][:, :], AF.Copy, [pT[0]], [x_])
                    cp('dve', x_[:, 512:1024], pT[1][:, :], [pT[1]], [x_])
                    for k in range(2):
                        scat.append(S.op('pool', lambda e, x_=x_, tg=tg, k=k: e.indirect_dma_start(
                            out=xs_d[:, :], out_offset=IOA(ap=IDX[:, tg, k:k + 1], axis=0), in_=x_[:, :], in_offset=None),
                            r=[x_, ('IDX', tg)], w=[('xs', tg, k)], dma=True))
                stores = []
                RT = cfg.BLK // 128
                def rows(pos):
                    return order[pos // RT] * RT + pos % RT

                def prefetch(rt):
                    s_ = rt % 2
                    ra = rows(rt)
                    S.op('sp', lambda e: e.dma_start(out=xb[s_][:], in_=xs_d[ra * 128:(ra + 1) * 128, :]), r=[], w=[xb[s_]],
                         dma=True, extra_deps=scat)

                PA, PU = [pb[2], pb[3]], [pb[4], pb[5]]

                def stage_a(bpos):
                    w_ = bpos % 2
                    for r_ in range(RT):
                        pos = bpos * RT + r_
                        s_ = pos % 2
                        for c in range(DC):
                            tr(pT[c // 4][:, (c % 4) * 128:(c % 4 + 1) * 128], xb[s_][:, :].rearrange("r (p c) -> r c p", c=8)[:, c, :], [xb[s_]], [pT[c // 4]])
                        act(xbT2[w_][:, 0:4, r_ * 128:(r_ + 1) * 128], pT[0][:, :].rearrange("p (c r) -> p c r", c=4), AF.Copy, [pT[0]], [xbT2[w_]])
                        cp('dve', xbT2[w_][:, 4:8, r_ * 128:(r_ + 1) * 128], pT[1][:, :].rearrange("p (c r) -> p c r", c=4), [pT[1]], [xbT2[w_]])
                        if pos + 2 < NB * RT:
                            prefetch(pos + 2)
                    NW = RT * 128
                    for hc in range(4):
                        for c in range(DC):
                            mm(PA[hc // 2][:, (hc % 2) * NW:(hc % 2 + 1) * NW], WAU[w_][:, c, 0:512].rearrange("p (m h) -> p h m", h=4)[:, hc, :],
                               xbT2[w_][:, c, :], c == 0, c == DC - 1, [('WAU', w_, c // 2), xbT2[w_]], [PA[hc // 2]])
                    for hc in range(4):
                        for c in range(DC):
                            mm(PU[hc // 2][:, (hc % 2) * NW:(hc % 2 + 1) * NW], WAU[w_][:, c, 512:1024].rearrange("p (m h) -> p h m", h=4)[:, hc, :],
                               xbT2[w_][:, c, :], c == 0, c == DC - 1, [('WAU', w_, c // 2), xbT2[w_]], [PU[hc // 2]])
                    for h2 in range(2):
                        act(sa2[:, h2, :], PA[h2][:, 0:2 * NW], AF.Silu, [PA[h2]], [('sa2', h2)])
                        tt('dve', hm2[w_][:, 2 * h2:2 * h2 + 2, :].rearrange("p c r -> p (c r)"), sa2[:, h2, :], PU[h2][:, 0:2 * NW], ALU.mult,
                           [('sa2', h2), PU[h2]], [('hm2', w_, h2)])

                def stage_b(bpos):
                    w_ = bpos % 2
                    py = [pb[6], pb[7]]
                    for r_ in range(RT):
                        pos = bpos * RT + r_
                        s_ = pos % 2
                        for half in range(2):
                            for hc in range(4):
                                mm(py[half][:, :], hm2[w_][:, hc, r_ * 128:(r_ + 1) * 128], WD[w_][:, hc, half * 512:(half + 1) * 512], hc == 0, hc == 3,
                                   [('hm2', w_, hc // 2), ('WD', w_, hc // 2)], [py[half]])
                        act(yb[s_][:, 0:512], py[0][:, :], AF.Copy, [py[0]], [yb[s_]])
                        cp('dve', yb[s_][:, 512:1024], py[1][:, :], [py[1]], [yb[s_]])
                        ra = rows(pos)
                        stores.append(S.op('act', lambda e, ra=ra, s_=s_: e.dma_start(out=ys_d[ra * 128:(ra + 1) * 128, :], in_=yb[s_][:]),
                                           r=[yb[s_]], w=[('yd', pos)], dma=True))

                prefetch(0)
                prefetch(1)
                for bpos in range(NB):
                    stage_a(bpos)
                    if bpos > 0:
                        stage_b(bpos - 1)
                    if bpos + 1 < NB:
                        load(bpos + 1)
                stage_b(NB - 1)
                for tg in range(cfg.NT):
                    gi, sidx = tile_gi[tg]
                    yh_, yl_ = xb[tg % 2], yb[tg % 2]
                    S.op('pool', lambda e, yh_=yh_, tg=tg: e.indirect_dma_start(
                        out=yh_[:, :], out_offset=None, in_=ys_d[:, :], in_offset=IOA(ap=IDX[:, tg, 0:1], axis=0)),
                        r=[('IDX', tg)], w=[yh_], dma=True, extra_deps=stores)
                    S.op('pool', lambda e, yl_=yl_, tg=tg: e.indirect_dma_start(
                        out=yl_[:, :], out_offset=None, in_=ys_d[:, :], in_offset=IOA(ap=IDX[:, tg, 1:2], axis=0)),
                        r=[('IDX', tg)], w=[yl_], dma=True, extra_deps=stores)
                    tsc('dve', yh_[:], yh_[:], GHL[:, tg, 0:1], None, ALU.mult, None, [yh_, ('GHL', tg)], [yh_])
                    stt(yh_[:], yl_[:], GHL[:, tg, 1:2], yh_[:], ALU.mult, ALU.add, [yl_, ('GHL', tg), yh_], [yh_])
                    for c in range(DC):
                        tr(pT[c // 4][:, (c % 4) * 128:(c % 4 + 1) * 128], yh_[:, c * 128:(c + 1) * 128], [yh_], [pT[c // 4]])
                    for c in range(DC):
                        stt(XT[:, c, tg * 128:(tg + 1) * 128], pT[c // 4][:, (c % 4) * 128:(c % 4 + 1) * 128], modT[:, l, 40 + c, sidx:sidx + 1],
                            XT[:, c, tg * 128:(tg + 1) * 128], ALU.mult, ALU.add, [pT[c // 4], modT, XK(gi)], [XK(gi)])
            S.barrier()

        for l in cfg.layers:
            norm_modulate(l, 0, False)
            if 'conv' not in cfg.skip:
                conv_phase(l)
            if 'heads' not in cfg.skip:
                heads_phase(l)
            norm_modulate(l, 1, True)
            if 'moe' not in cfg.skip:
                moe_phase(l)

        fin = []
        with contextlib.ExitStack() as ph:
            sq = [sb(ph, "fsq%d" % i, [128, 512]) for i in range(2)]
            rs = [sb(ph, "frs%d" % i, [128, 512]) for i in range(2)]
            ob = [sb(ph, "fob%d" % i, [128, 512]) for i in range(4)]
            kk = 0
            for (t0, n, sidx, gi) in cfg.groups[1:]:
                pss = pb[gi % 2]
                for c in range(DC):
                    q = sq[kk % 2]
                    kk += 1
                    act(q[:, :n], XT[:, c, t0:t0 + n], AF.Square, [XK(gi)], [q])
                    mm(pss[:, :n], ones[:], q[:, :n], c == 0, c == DC - 1, [ones, q], [pss])
                r_ = rs[gi % 2]
                act(r_[:, :n], pss[:, :n], AF.Sqrt, [pss], [r_], bias=cfg.EPS, scale=1.0 / 1024)
                recip(r_[:, :n], r_[:, :n], [r_], [r_])
                for c in range(DC):
                    o_ = ob[kk % 4]
                    kk += 1
                    if cfg.final:
                        stt(o_[:, :n], XT[:, c, t0:t0 + n], V(cfg.V_FNW + c), r_[:, :n], ALU.mult, ALU.mult, [XK(gi), vecs, r_], [o_])
                    else:
                        cp('dve', o_[:, :n], XT[:, c, t0:t0 + n], [XK(gi)], [o_])
                    fin.append(dma('sp', outT_v[:, c, t0 - TC:t0 - TC + n], o_[:, :n], [o_], []))
        S.op('sp', lambda e: e.nop(), extra_deps=fin)
        S.run(nc)
        nc._sched_stats = S.stats
    return nc


def pack_inputs(cfg, b, x, c, ctx, c_ctx, norm_w, w_ada, b_ada, w_in, conv_w, conv_norm_w, hg_lb, hg_norm_w, w_out,
                w_rg, b_rg, w_re, b_re, w_e_gu, w_e_down, final_norm_w):
    d = cfg.DEPTH
    tok = np.concatenate([ctx[b], x[b]], axis=0)
    xT = np.ascontiguousarray(tok.T.reshape(cfg.DC, 128, cfg.T).transpose(1, 0, 2)).reshape(128, cfg.DC * cfg.T)

    def fm(v):
        return np.asarray(v).reshape(-1, 128).T

    vecs = np.zeros((128, cfg.NV), np.float32)
    vecs[:, cfg.V_C:cfg.V_C + 8] = fm(c[b])
    vecs[:, cfg.V_CC:cfg.V_CC + 8] = fm(c_ctx)
    vecs[:, cfg.V_FNW:cfg.V_FNW + 8] = fm(final_norm_w)
    for dirn in range(2):
        for l in range(d):
            o = cfg.V_LB + (dirn * d + l) * 4
            vecs[:, o:o + 4] = fm(hg_lb[dirn, l])
    for l in range(d):
        o = cfg.V_L0 + l * cfg.V_LN
        vecs[:, o:o + 8] = fm(norm_w[l, 0])
        vecs[:, o + 8:o + 16] = fm(norm_w[l, 1])
        vecs[:, o + 16:o + 64] = fm(b_ada[l])
        for tap in range(3):
            vecs[:, o + 64 + tap * 4:o + 64 + tap * 4 + 4] = fm(conv_w[l, tap])
        vecs[:, o + 76:o + 80] = fm(conv_norm_w[l])
        vecs[:, o + 80:o + 81] = fm(hg_norm_w[l])
    wr = np.concatenate([w_rg, w_re], axis=2)
    wr = np.ascontiguousarray(wr.reshape(d, cfg.DC, 128, 36).transpose(2, 0, 1, 3)).reshape(128, d * cfg.DC * 36)
    br = np.ascontiguousarray(np.concatenate([b_rg, b_re], axis=1)).reshape(1, d * 36)
    return {"xT": xT.astype(np.float32), "vecs": vecs, "wr": wr.astype(np.float32), "br": br.astype(np.float32)}


def run(cfg, inputs, n_cores):
    inputs = {k: np.asarray(v) for k, v in inputs.items()}
    nc = build_program(cfg)
    shared = {"w_ada": np.ascontiguousarray(inputs["w_ada"]), "w_in": np.ascontiguousarray(inputs["w_in"]),
              "w_out": np.ascontiguousarray(inputs["w_out"]), "w_gu": np.ascontiguousarray(inputs["w_e_gu"]),
              "w_dn": np.ascontiguousarray(inputs["w_e_down"])}
    in_maps = []
    for b in range(n_cores):
        m = pack_inputs(cfg, b, **inputs)
        m.update(shared)
        in_maps.append(m)
    res = run_bass_kernel_spmd(nc, in_maps, core_ids=list(range(n_cores)))
    outs = []
    for b in range(n_cores):
        oT = np.asarray(res.results[b]["outT"]).reshape(128, cfg.DC, cfg.TL)
        outs.append(oT.transpose(2, 1, 0).reshape(cfg.TL, cfg.D))
    return np.stack(outs, axis=0).astype(np.float32)


def kernel(**inputs):
    cfg = Cfg(depth=4, t_lat=2048)
    return run(cfg, inputs, 8)
```

```python
import contextlib
import numpy as np
import concourse.bass as bass
import concourse.mybir as mybir
from concourse.bass_utils import run_bass_kernel_spmd

F32 = mybir.dt.float32
BF16 = mybir.dt.bfloat16
AF = mybir.ActivationFunctionType
ALU = mybir.AluOpType
AX = mybir.AxisListType

ENGS = ['pe', 'act', 'dve', 'pool', 'sp']
DMA_RING = 8


class Op:
    __slots__ = ('eng', 'fn', 'deps', 'idx', 'signal', 'semkey', 'semval', 'dma', 'waits')

    def __init__(self, eng, fn, dma):
        self.eng = eng
        self.fn = fn
        self.dma = dma
        self.deps = []
        self.signal = False
        self.semkey = None
        self.semval = 0
        self.waits = []


class Sched:
    def __init__(self):
        self.ops = {e: [] for e in ENGS}
        self.bufs = {}
        self.ndma = {e: 0 for e in ENGS}
        self.dma_ops = {e: [] for e in ENGS}

    @staticmethod
    def _key(x):
        if isinstance(x, (tuple, str)):
            return x
        return x.name

    def op(self, eng, fn, r=(), w=(), dma=False, extra_deps=()):
        o = Op(eng, fn, dma)
        deps = list(extra_deps)
        rk = [self._key(x) for x in r]
        wk = [self._key(x) for x in w]
        for k in rk:
            st = self.bufs.get(k)
            if st is not None and st[0] is not None:
                deps.append(st[0])
        for k in wk:
            st = self.bufs.get(k)
            if st is not None:
                if st[0] is not None:
                    deps.append(st[0])
                deps.extend(st[1].values())
        if dma:
            i = self.ndma[eng]
            self.ndma[eng] += 1
            o.semkey = ('dma', eng, i % DMA_RING)
            o.semval = 16 * (i // DMA_RING + 1)
            if i >= DMA_RING:
                deps.append(self.dma_ops[eng][i - DMA_RING])
            self.dma_ops[eng].append(o)
        seen = set()
        for d in deps:
            if id(d) in seen or d is o:
                continue
            seen.add(id(d))
            o.deps.append(d)
        o.idx = len(self.ops[eng])
        self.ops[eng].append(o)
        for k in rk:
            st = self.bufs.setdefault(k, [None, {}])
            st[1][id(o) if dma else eng] = o
        for k in wk:
            self.bufs[k] = [o, {}]
        return o

    def barrier(self):
        last = []
        for e in ENGS:
            if self.ops[e]:
                last.append(self.ops[e][-1])
            last.extend(self.dma_ops[e][-DMA_RING:])
        for e in ENGS:
            self.op(e, lambda g: g.nop(), extra_deps=last)
        self.bufs = {}

    def finalize(self):
        for e in ENGS:
            for o in self.ops[e]:
                for d in o.deps:
                    if d.dma:
                        continue
                    if d.eng == 'pe' and o.eng == 'pe' and not o.dma:
                        continue
                    d.signal = True
        for e in ENGS:
            c = 0
            for o in self.ops[e]:
                if o.dma:
                    continue
                if o.signal:
                    c += 1
                    o.semkey = ('eng', e)
                    o.semval = c
        for e in ENGS:
            waited = {}
            for o in self.ops[e]:
                need = {}
                for d in o.deps:
                    if (not d.dma) and d.eng == 'pe' and o.eng == 'pe' and not o.dma:
                        continue
                    if d.semval > need.get(d.semkey, 0):
                        need[d.semkey] = d.semval
                for k, v in need.items():
                    if waited.get(k, 0) < v:
                        waited[k] = v
                        o.waits.append((k, v))

    def run(self, nc):
        self.finalize()
        keys = set()
        for e in ENGS:
            for o in self.ops[e]:
                if o.semkey is not None and (o.signal or o.dma):
                    keys.add(o.semkey)
        keys = sorted(keys, key=str)
        self.stats = {e: (len(self.ops[e]), max([o.semval for o in self.ops[e] if not o.dma] + [0])) for e in ENGS}
        with contextlib.ExitStack() as st:
            sems = {}
            for i, k in enumerate(keys):
                sems[k] = st.enter_context(nc.semaphore('s%d' % i))
            block = st.enter_context(nc.Block())

            def replay(eng_name):
                def body(e):
                    for o in self.ops[eng_name]:
                        for (k, v) in o.waits:
                            e.wait_ge(sems[k], v)
                        inst = o.fn(e)
                        if o.dma:
                            inst.then_inc(sems[o.semkey], 16)
                        elif o.signal:
                            inst.then_inc(sems[o.semkey], 1)
                return body

            block.tensor(replay('pe'))
            block.scalar(replay('act'))
            block.vector(replay('dve'))
            block.gpsimd(replay('pool'))
            block.sync(replay('sp'))


class Cfg:
    def __init__(self, depth=4, t_lat=2048, layers=None, first=True, final=True):
        self.D = 1024
        self.DC = 8
        self.TC = 256
        self.TL = t_lat
        self.T = self.TC + self.TL
        self.DEPTH = depth
        self.layers = list(range(depth)) if layers is None else layers
        self.first = first
        self.final = final
        self.NE = 32
        self.skip = set()
        self.EPS = 1e-6
        self.groups = [(0, 256, 1, 0)]
        for k in range(self.TL // 512):
            self.groups.append((256 + 512 * k, 512, 0, k + 1))
        self.NT = self.T // 128
        self.BLK = 256
        self.NB = -(-(2 * self.T + 32 * (self.BLK - 1)) // self.BLK)
        d = depth
        self.V_C = 0
        self.V_CC = 8
        self.V_FNW = 16
        self.V_LB = 24
        self.V_L0 = 24 + 2 * d * 4
        self.V_LN = 81
        self.NV = self.V_L0 + d * self.V_LN


def build_program(cfg):
    nc = bass.Bass("TRN2", target_bir_lowering=False)
    T, TC, TL, DC, DEPTH = cfg.T, cfg.TC, cfg.TL, cfg.DC, cfg.DEPTH
    xT_d = nc.dram_tensor("xT", [128, DC * T], F32, kind="ExternalInput").ap()
    vecs_d = nc.dram_tensor("vecs", [128, cfg.NV], F32, kind="ExternalInput").ap()
    wr_d = nc.dram_tensor("wr", [128, DEPTH * DC * 36], F32, kind="ExternalInput").ap()
    br_d = nc.dram_tensor("br", [1, DEPTH * 36], F32, kind="ExternalInput").ap()
    wada_d = nc.dram_tensor("w_ada", [DEPTH, 1024, 6144], F32, kind="ExternalInput").ap()
    win_d = nc.dram_tensor("w_in", [DEPTH, 1024, 4096], F32, kind="ExternalInput").ap()
    wout_d = nc.dram_tensor("w_out", [DEPTH, 1024, 1024], F32, kind="ExternalInput").ap()
    wgu_d = nc.dram_tensor("w_gu", [DEPTH, cfg.NE, 1024, 1024], F32, kind="ExternalInput").ap()
    wdn_d = nc.dram_tensor("w_dn", [DEPTH, cfg.NE, 512, 1024], F32, kind="ExternalInput").ap()
    outT_d = nc.dram_tensor("outT", [128, DC * TL], F32, kind="ExternalOutput").ap()
    xs_d = nc.dram_tensor("xs_scr", [cfg.NB * cfg.BLK, 1024], F32, kind="Internal").ap()
    ys_d = nc.dram_tensor("ys_scr", [cfg.NB * cfg.BLK, 1024], F32, kind="Internal").ap()
    xT_v = xT_d.rearrange("p (c t) -> p c t", c=DC)
    outT_v = outT_d.rearrange("p (c t) -> p c t", c=DC)

    S = Sched()
    uid = [0]

    with contextlib.ExitStack() as top:
        def sb(stack, name, shape, dt=F32):
            uid[0] += 1
            return stack.enter_context(nc.sbuf_tensor("%s_%d" % (name, uid[0]), shape, dt))

        XT = sb(top, "XT", [128, DC, T])
        hT = sb(top, "hT", [128, DC, T], BF16)
        G = sb(top, "G", [128, cfg.NT, 32])
        vecs = sb(top, "vecs", [128, cfg.NV])
        wr = sb(top, "wr", [128, DEPTH, DC, 36])
        br = sb(top, "br", [1, DEPTH * 36])
        ident = sb(top, "ident", [128, 128])
        ones = sb(top, "ones", [128, 128])
        ones1 = sb(top, "ones1", [1, 128])
        rmask = sb(top, "rmask", [128, 512], BF16)
        CH = 64
        NCHT = 128 // CH
        maskF = sb(top, "maskF", [CH, NCHT, CH])
        maskB = sb(top, "maskB", [CH, NCHT, CH])
        scT = sb(top, "scT", [128, DC, 2])
        modT = sb(top, "modT", [128, DEPTH, 48, 2])
        A1 = sb(top, "A1", [128, DEPTH, DC, 2])
        A2 = sb(top, "A2", [128, DEPTH, DC, 2])
        lbT = sb(top, "lbT", [128, 2 * DEPTH * 4])
        omlT = sb(top, "omlT", [128, 2 * DEPTH * 4])
        I32 = mybir.dt.int32
        NB = cfg.NB
        SEL = sb(top, "SEL", [128, cfg.NT, 32])
        RK = sb(top, "RK", [128, cfg.NT, 32])
        IDX = sb(top, "IDX", [128, cfg.NT, 2], I32)
        GHL = sb(top, "GHL", [128, cfg.NT, 2])
        IDXW = sb(top, "IDXW", [128, NB], I32)
        PIDXi = sb(top, "PIDXi", [128, 1], I32)
        PIDX = sb(top, "PIDX", [128, 1])
        Lst = sb(top, "Lst", [128, 128])
        THR = sb(top, "THR", [128, 18])
        JR = sb(top, "JR", [128, NB])
        c128 = sb(top, "c128", [128, NB])
        pb = [top.enter_context(nc.psum_tensor("pb%d" % i, [128, 512], F32)) for i in range(8)]

        tile_gi = {}
        for (t0_, n_, sidx_, gi_) in cfg.groups:
            for ti_ in range(n_ // 128):
                tile_gi[(t0_ + ti_ * 128) // 128] = (gi_, sidx_)
        tile_gi = {k_: v_ for k_, v_ in tile_gi.items()}

        bc_cache = {}
        order_box = []

        def order_of(bpos):
            return order_box[0][bpos]

        def XK(gi):
            return ('XT', gi)

        def HK(gi):
            return ('hT', gi)

        def mm(out, lhsT, rhs, start, stop, r, w):
            return S.op('pe', lambda e: e.matmul(out, lhsT, rhs, start=start, stop=stop), r=r, w=w)

        def tr(out, in_, r, w):
            return S.op('pe', lambda e: e.transpose(out, in_, ident[:in_.shape[0], :in_.shape[0]]), r=list(r) + [ident], w=w)

        def act(out, in_, func, r, w, bias=None, scale=None, accum=None):
            kw = {}
            if bias is not None:
                kw['bias'] = bias
            if scale is not None:
                kw['scale'] = scale
            if accum is not None:
                kw['accum_out'] = accum
            return S.op('act', lambda e: e.activation(out=out, in_=in_, func=func, **kw), r=r, w=w)

        def tt(eng, out, in0, in1, op, r, w):
            return S.op(eng, lambda e: e.tensor_tensor(out=out, in0=in0, in1=in1, op=op), r=r, w=w)

        def tsc(eng, out, in0, s1, s2, op0, op1, r, w):
            if op1 is None:
                return S.op(eng, lambda e: e.tensor_scalar(out=out, in0=in0, scalar1=s1, scalar2=None, op0=op0), r=r, w=w)
            return S.op(eng, lambda e: e.tensor_scalar(out=out, in0=in0, scalar1=s1, scalar2=s2, op0=op0, op1=op1), r=r, w=w)

        def stt(out, in0, scalar, in1, op0, op1, r, w):
            return S.op('dve', lambda e: e.scalar_tensor_tensor(out=out, in0=in0, scalar=scalar, in1=in1, op0=op0, op1=op1), r=r, w=w)

        def cp(eng, out, in_, r, w):
            return S.op(eng, lambda e: e.tensor_copy(out=out, in_=in_), r=r, w=w)

        def recip(out, in_, r, w):
            return S.op('dve', lambda e: e.reciprocal(out=out, in_=in_), r=r, w=w)

        def memset(eng, ap, val, w):
            return S.op(eng, lambda e: e.memset(ap, val), w=w)

        def dma(eng, out, in_, r, w):
            return S.op(eng, lambda e: e.dma_start(out=out, in_=in_), r=r, w=w, dma=True)

        def V(col, n=1):
            return vecs[:, col:col + n]

        memset('pool', ident[:], 0.0, [ident])
        S.op('pool', lambda e: e.affine_select(out=ident[:], in_=ident[:], compare_op=ALU.not_equal, fill=1.0,
                                               base=0, pattern=[[-1, 128]], channel_multiplier=1), r=[ident], w=[ident])
        memset('pool', ones[:], 1.0, [ones])
        memset('pool', ones1[:], 1.0, [ones1])
        memset('pool', rmask[:], 1.0, [rmask])
        S.op('pool', lambda e: e.memset(rmask[:].rearrange("p (a b) -> p a b", b=CH)[:, :, 0:1], 0.0), r=[rmask], w=[rmask])
        memset('pool', maskF[:], 1.0, [maskF])
        memset('pool', maskB[:], 1.0, [maskB])
        S.op('pool', lambda e: e.affine_select(out=maskF[:], in_=maskF[:], compare_op=ALU.is_ge, fill=0.0,
                                               base=0, pattern=[[0, NCHT], [1, CH]], channel_multiplier=-1), r=[maskF], w=[maskF])
        S.op('pool', lambda e: e.affine_select(out=maskB[:], in_=maskB[:], compare_op=ALU.is_ge, fill=0.0,
                                               base=0, pattern=[[0, NCHT], [-1, CH]], channel_multiplier=1), r=[maskB], w=[maskB])

        S.op('pool', lambda e: e.iota(PIDXi[:], pattern=[[0, 1]], base=0, channel_multiplier=1), w=[PIDXi])
        cp('dve', PIDX[:], PIDXi[:], [PIDXi], [PIDX])
        memset('pool', Lst[:], 1.0, [Lst])
        S.op('pool', lambda e: e.affine_select(out=Lst[:], in_=Lst[:], compare_op=ALU.is_ge, fill=0.0,
                                               base=-1, pattern=[[1, 128]], channel_multiplier=-1), r=[Lst], w=[Lst])
        memset('pool', c128[:], float(cfg.BLK), [c128])
        S.op('dve', lambda e: e.tensor_tensor_scan(out=JR[:], data0=ones[:, :NB], data1=c128[:], initial=-float(cfg.BLK),
                                                   op0=ALU.mult, op1=ALU.add), r=[ones, c128], w=[JR])
        cp('dve', THR[:], JR[:, 0:18], [JR], [THR])

        dma('sp', vecs[:], vecs_d, [], [vecs])
        dma('sp', wr[:].rearrange("p l c n -> p (l c n)"), wr_d, [], [wr])
        dma('sp', br[:], br_d, [], [br])
        for (t0, n, sidx, gi) in cfg.groups:
            dma('sp', XT[:, :, t0:t0 + n], xT_v[:, :, t0:t0 + n], [], [XK(gi)])

        act(scT[:, :, 0], V(cfg.V_C, 8), AF.Silu, [vecs], [scT])
        act(scT[:, :, 1], V(cfg.V_CC, 8), AF.Silu, [vecs], [scT])
        with contextlib.ExitStack() as ph:
            nlb = 2 * DEPTH * 4
            E = sb(ph, "lbE", [128, nlb])
            sE = sb(ph, "lbS", [128, 8])
            rE = sb(ph, "lbR", [128, 8])
            act(E[:], V(cfg.V_LB, nlb), AF.Exp, [vecs], [E])
            E3 = E[:].rearrange("p (d l h) -> p d l h", d=2, l=DEPTH)
            sE2 = sE[:].rearrange("p (d h) -> p d h", d=2)
            cp('dve', sE2, E3[:, :, 0, :], [E], [sE])
            for l in range(1, DEPTH):
                tt('dve', sE2, sE2, E3[:, :, l, :], ALU.add, [sE, E], [sE])
            recip(rE[:], sE[:], [sE], [rE])
            rE2 = rE[:].rearrange("p (d h) -> p d h", d=2)
            lb3 = lbT[:].rearrange("p (d l h) -> p d l h", d=2, l=DEPTH)
            memset('dve', lbT[:], 0.0, [lbT])
            for l in range(1, DEPTH):
                tt('dve', E3[:, :, l, :], E3[:, :, l, :], rE2, ALU.mult, [E, rE], [E])
                tt('dve', lb3[:, :, l, :], lb3[:, :, l - 1, :], E3[:, :, l, :], ALU.add, [lbT, E], [lbT])
            tsc('dve', omlT[:], lbT[:], -1.0, 1.0, ALU.mult, ALU.add, [lbT], [omlT])

            wa = [sb(ph, "wada%d" % i, [128, DC, 512]) for i in range(4)]
            row2 = [sb(ph, "row2%d" % i, [2, 512]) for i in range(2)]
            k = 0
            for l in cfg.layers:
                voff = cfg.V_L0 + l * cfg.V_LN
                for jb in range(12):
                    wt = wa[k % 4]
                    k += 1
                    dma('sp', wt[:], wada_d[l, :, jb * 512:(jb + 1) * 512].rearrange("(c p) n -> p c n", p=128), [], [wt])
                    pm = pb[jb % 2]
                    for c in range(DC):
                        mm(pm[0:2, :], scT[:, c, :], wt[:, c, :], c == 0, c == DC - 1, [wt, scT], [pm])
                    r2 = row2[jb % 2]
                    act(r2[:], pm[0:2, :], AF.Copy, [pm], [r2])
                    pt_ = pb[2 + jb % 2]
                    for jj in range(4):
                        tr(pt_[:, jj * 2:jj * 2 + 2], r2[:, jj * 128:(jj + 1) * 128], [r2], [pt_])
                    for s in range(2):
                        tt('dve', modT[:, l, jb * 4:jb * 4 + 4, s], pt_[:, 0:8].rearrange("p (j s) -> p j s", s=2)[:, :, s],
                           V(voff + 16 + jb * 4, 4), ALU.add, [pt_, vecs], [modT])
                for s in range(2):
                    stt(A1[:, l, :, s], modT[:, l, 8:16, s], 1.0, V(voff + 0, 8), ALU.add, ALU.mult, [modT, vecs], [A1])
                    stt(A2[:, l, :, s], modT[:, l, 32:40, s], 1.0, V(voff + 8, 8), ALU.add, ALU.mult, [modT, vecs], [A2])
        S.barrier()

        def norm_modulate(l, which, router):
            A = A1 if which == 0 else A2
            sh0 = 0 if which == 0 else 24
            with contextlib.ExitStack() as ph:
                sq = [sb(ph, "nsq%d" % i, [128, 512]) for i in range(2)]
                rs = [sb(ph, "nrs%d" % i, [128, 512]) for i in range(2)]
                tmp = [sb(ph, "ntmp%d" % i, [128, 512]) for i in range(2)]
                if router:
                    h2f = [sb(ph, "h2f%d" % i, [128, DC, 512]) for i in range(2)]
                    rt = {nm: [sb(ph, "rt_%s%d" % (nm, i), shp) for i in range(2)] for nm, shp in
                          [("lg", [128, 36]), ("gm", [128, 1]), ("ngm", [128, 1]), ("gmask", [128, 4]), ("ge", [128, 4]),
                           ("gs", [128, 1]), ("pen", [128, 4]), ("el", [128, 32]), ("t8", [128, 8]), ("nm1", [128, 1]),
                           ("sel", [128, 32]), ("ex", [128, 32]), ("gx", [128, 32]), ("den", [128, 1]), ("pr", [128, 1]),
                           ("rp", [128, 1])]}
                kk = 0
                tl = 0
                cpar = [0]
                if router:
                    cums = [sb(ph, "cums%d" % i, [128, 32]) for i in range(2)]
                    memset('dve', cums[0][:], 0.0, [cums[0]])
                for (t0, n, sidx, gi) in cfg.groups:
                    pss = pb[gi % 2]
                    for c in range(DC):
                        q = sq[kk % 2]
                        kk += 1
                        act(q[:, :n], XT[:, c, t0:t0 + n], AF.Square, [XK(gi)], [q])
                        mm(pss[:, :n], ones[:], q[:, :n], c == 0, c == DC - 1, [ones, q], [pss])
                    r_ = rs[gi % 2]
                    act(r_[:, :n], pss[:, :n], AF.Sqrt, [pss], [r_], bias=cfg.EPS, scale=1.0 / 1024)
                    recip(r_[:, :n], r_[:, :n], [r_], [r_])
                    for c in range(DC):
                        tm = tmp[kk % 2]
                        kk += 1
                        tt('dve', tm[:, :n], XT[:, c, t0:t0 + n], r_[:, :n], ALU.mult, [XK(gi), r_], [tm])
                        if router:
                            hf = h2f[gi % 2]
                            act(hf[:, c, :n], tm[:, :n], AF.Identity, [tm, A, modT], [hf],
                                bias=modT[:, l, sh0 + c, sidx:sidx + 1], scale=A[:, l, c, sidx:sidx + 1])
                            cp('pool', hT[:, c, t0:t0 + n], hf[:, c, :n], [hf], [HK(gi)])
                        else:
                            act(hT[:, c, t0:t0 + n], tm[:, :n], AF.Identity, [tm, A, modT], [HK(gi)],
                                bias=modT[:, l, sh0 + c, sidx:sidx + 1], scale=A[:, l, c, sidx:sidx + 1])
                    if router:
                        hf = h2f[gi % 2]
                        for ti in range(n // 128):
                            tg = (t0 + ti * 128) // 128
                            R = {nm: v[tl % 2] for nm, v in rt.items()}
                            pl = pb[2 + tl % 2]
                            tl += 1
                            for c in range(DC):
                                mm(pl[:, 0:36], hf[:, c, ti * 128:(ti + 1) * 128], wr[:, l, c, :], c == 0, False, [hf, wr], [pl])
                            mm(pl[:, 0:36], ones1[:], br[:, l * 36:(l + 1) * 36], False, True, [ones1, br], [pl])
                            lg = R["lg"]
                            cp('dve', lg[:], pl[:, 0:36], [pl], [lg])
                            S.op('dve', lambda e, o=R["gm"], i=lg: e.tensor_reduce(out=o[:], in_=i[:, 0:4], axis=AX.X, op=ALU.max),
                                 r=[lg], w=[R["gm"]])
                            tsc('dve', R["ngm"][:], R["gm"][:], -1.0, None, ALU.mult, None, [R["gm"]], [R["ngm"]])
                            tsc('dve', R["gmask"][:], lg[:, 0:4], R["gm"][:], None, ALU.is_equal, None, [lg, R["gm"]], [R["gmask"]])
                            act(R["ge"][:], lg[:, 0:4], AF.Exp, [lg, R["ngm"]], [R["ge"], R["gs"]], bias=R["ngm"][:], scale=1.0,
                                accum=R["gs"][:])
                            tsc('dve', R["pen"][:], R["gmask"][:], 1e30, -1e30, ALU.mult, ALU.add, [R["gmask"]], [R["pen"]])
                            tt('dve', R["el"][:].rearrange("p (g e) -> p g e", g=4), lg[:, 4:36].rearrange("p (g e) -> p g e", g=4),
                               R["pen"][:].unsqueeze(2).to_broadcast([128, 4, 8]), ALU.add, [lg, R["pen"]], [R["el"]])
                            S.op('dve', lambda e, o=R["t8"], i=R["el"]: e.max(out=o[:], in_=i[:]), r=[R["el"]], w=[R["t8"]])
                            tsc('dve', R["nm1"][:], R["t8"][:, 0:1], -1.0, None, ALU.mult, None, [R["t8"]], [R["nm1"]])
                            tsc('dve', R["sel"][:], R["el"][:], R["t8"][:, 1:2], None, ALU.is_ge, None, [R["el"], R["t8"]], [R["sel"]])
                            act(R["ex"][:], R["el"][:], AF.Exp, [R["el"], R["nm1"]], [R["ex"]], bias=R["nm1"][:], scale=1.0)
                            tt('dve', R["gx"][:], R["sel"][:], R["ex"][:], ALU.mult, [R["sel"], R["ex"]], [R["gx"]])
                            S.op('dve', lambda e, o=R["den"], i=R["gx"]: e.tensor_reduce(out=o[:], in_=i[:], axis=AX.X, op=ALU.add),
                                 r=[R["gx"]], w=[R["den"]])
                            tt('dve', R["pr"][:], R["den"][:], R["gs"][:], ALU.mult, [R["den"], R["gs"]], [R["pr"]])
                            recip(R["rp"][:], R["pr"][:], [R["pr"]], [R["rp"]])
                            tsc('dve', G[:, tg, :], R["gx"][:], R["rp"][:], None, ALU.mult, None, [R["gx"], R["rp"]], [('G', gi)])
                            cp('dve', SEL[:, tg, :], R["sel"][:], [R["sel"]], [('SEL', tg)])
                            prk = pb[4 + tl % 2]
                            mm(prk[:, 0:32], Lst[:], R["sel"][:], True, False, [Lst, R["sel"]], [prk])
                            mm(prk[:, 0:32], ones[:], cums[cpar[0]][:], False, True, [ones, cums[cpar[0]]], [prk])
                            cp('dve', RK[:, tg, :], prk[:, 0:32], [prk], [('RK', tg)])
                            tt('dve', cums[1 - cpar[0]][:], cums[cpar[0]][:], R["sel"][:], ALU.add, [cums[cpar[0]], R["sel"]], [cums[1 - cpar[0]]])
                            cpar[0] = 1 - cpar[0]
                if router:
                    CNT = sb(ph, "CNT", [128, 32])
                    cmp18 = sb(ph, "cmp18", [128, 32, 18])
                    NBLK = sb(ph, "NBLK", [128, 32])
                    PADD = sb(ph, "PADD", [128, 32])
                    PEND = sb(ph, "PEND", [128, 32])
                    PST = sb(ph, "PST", [128, 32])
                    cmpB = sb(ph, "cmpB", [128, NB, 32])
                    BEf = sb(ph, "BEf", [128, NB])
                    pc = pb[6]
                    mm(pc[:, 0:32], ones[:], cums[cpar[0]][:], True, True, [ones, cums[cpar[0]]], [pc])
                    cp('dve', CNT[:], pc[:, 0:32], [pc], [CNT])
                    tt('dve', cmp18[:], CNT[:].unsqueeze(2).to_broadcast([128, 32, 18]), THR[:].unsqueeze(1).to_broadcast([128, 32, 18]),
                       ALU.is_gt, [CNT, THR], [cmp18])
                    S.op('dve', lambda e: e.tensor_reduce(out=NBLK[:], in_=cmp18[:], axis=AX.X, op=ALU.add), r=[cmp18], w=[NBLK])
                    tsc('dve', PADD[:], NBLK[:], float(cfg.BLK), None, ALU.mult, None, [NBLK], [PADD])
                    S.op('dve', lambda e: e.tensor_tensor_scan(out=PEND[:], data0=ones[:, 0:32], data1=PADD[:], initial=0.0,
                                                               op0=ALU.mult, op1=ALU.add), r=[ones, PADD], w=[PEND])
                    tt('dve', PST[:], PEND[:], PADD[:], ALU.subtract, [PEND, PADD], [PST])
                    tt('dve', cmpB[:], PEND[:].unsqueeze(1).to_broadcast([128, NB, 32]), JR[:].unsqueeze(2).to_broadcast([128, NB, 32]),
                       ALU.is_le, [PEND, JR], [cmpB])
                    S.op('dve', lambda e: e.tensor_reduce(out=BEf[:], in_=cmpB[:], axis=AX.X, op=ALU.add), r=[cmpB], w=[BEf])
                    tsc('dve', BEf[:], BEf[:], 31.0, float(32 * l), ALU.min, ALU.add, [BEf], [BEf])
                    UNU = sb(ph, "UNU", [128, NB])
                    tsc('dve', UNU[:], JR[:], PEND[:, 31:32], 8192.0, ALU.is_ge, ALU.mult, [JR, PEND], [UNU])
                    tt('dve', BEf[:], BEf[:], UNU[:], ALU.add, [BEf, UNU], [BEf])
                    tsc('dve', IDXW[:], BEf[:], 128.0, PIDX[:], ALU.mult, ALU.add, [BEf, PIDX], [IDXW])
                    pp = {nm: [sb(ph, "pp_%s%d" % (nm, i), shp) for i in range(2)] for nm, shp in
                          [("pos", [128, 32]), ("t8", [128, 8]), ("eq", [128, 32]), ("pr", [128, 32])]}
                    for tg in range(cfg.NT):
                        Q = {nm: v[tg % 2] for nm, v in pp.items()}
                        gi_t = tile_gi[tg][0]
                        tt('dve', Q["pos"][:], RK[:, tg, :], PST[:], ALU.add, [('RK', tg), PST], [Q["pos"]])
                        stt(Q["pos"][:], Q["pos"][:], 1.0, SEL[:, tg, :], ALU.add, ALU.mult, [Q["pos"], ('SEL', tg)], [Q["pos"]])
                        S.op('dve', lambda e, o=Q["t8"], i=Q["pos"]: e.max(out=o[:], in_=i[:]), r=[Q["pos"]], w=[Q["t8"]])
                        tsc('dve', IDX[:, tg, :], Q["t8"][:, 0:2], -1.0, None, ALU.add, None, [Q["t8"]], [('IDX', tg)])
                        for k in range(2):
                            tsc('dve', Q["eq"][:], Q["pos"][:], Q["t8"][:, k:k + 1], None, ALU.is_equal, None, [Q["pos"], Q["t8"]], [Q["eq"]])
                            tt('dve', Q["pr"][:], Q["eq"][:], G[:, tg, :], ALU.mult, [Q["eq"], ('G', gi_t)], [Q["pr"]])
                            S.op('dve', lambda e, o=GHL[:, tg, k:k + 1], i=Q["pr"]: e.tensor_reduce(out=o, in_=i[:], axis=AX.X, op=ALU.add),
                                 r=[Q["pr"]], w=[('GHL', tg)])
            S.barrier()

        def out_proj(l, Y, nk, k0, ph, wo_buf=None, banks=None):
            if wo_buf is None:
                Wo = sb(ph, "Wo", [128, nk, 1024], BF16)
            else:
                Wo = wo_buf
            dma('pool', Wo[:], wout_d[l, k0 * 128:(k0 + nk) * 128, :].rearrange("(c p) n -> p c n", p=128), [], [Wo])
            kk = 0
            for (t0, n, sidx, gi) in cfg.groups:
                for j in range(DC):
                    po = pb[4 + kk % 4] if banks is None else banks[kk % len(banks)]
                    kk += 1
                    for c in range(nk):
                        rhs = Y[:, c, t0:t0 + n] if nk > 1 else Y[:, t0:t0 + n]
                        mm(po[:, :n], Wo[:, c, j * 128:(j + 1) * 128], rhs, c == 0, c == nk - 1, [Wo, Y], [po])
                    stt(XT[:, j, t0:t0 + n], po[:, :n], modT[:, l, 16 + j, sidx:sidx + 1], XT[:, j, t0:t0 + n],
                        ALU.mult, ALU.add, [po, modT, XK(gi)], [XK(gi)])

        def load_win(ph, name, l, col, W=None):
            if W is None:
                W = sb(ph, name, [128, DC, 128], BF16)
            dma('pool', W[:], win_d[l, :, col:col + 128].rearrange("(c p) n -> p c n", p=128), [], [W])
            return W

        def proj(W, ps, t0, n, gi):
            for c in range(DC):
                mm(ps[:, :n], W[:, c, :], hT[:, c, t0:t0 + n], c == 0, c == DC - 1, [W, HK(gi)], [ps])

        def conv_phase(l):
            voff = cfg.V_L0 + l * cfg.V_LN
            R_ = TL // 64
            with contextlib.ExitStack() as ph:
                Z = sb(ph, "Z", [128, 4, T], BF16)
                SS = sb(ph, "SS", [128, T])
                u = sb(ph, "u", [128, T])
                Bs = sb(ph, "Bs", [128, T])
                y = sb(ph, "y", [128, T])
                hv = [sb(ph, "hv%d" % i, [128, 512]) for i in range(2)]
                zs = [sb(ph, "zs%d" % i, [128, 512]) for i in range(2)]
                wsets = [[sb(ph, "cW%d_%d" % (i, k_), [128, DC, 128], BF16) for k_ in range(3)] for i in range(1)]
                for cc in range(4):
                    with contextlib.ExitStack() as ph2:
                        WB = load_win(ph2, "WB", l, cc * 128, wsets[0][0])
                        WC = load_win(ph2, "WC", l, 512 + cc * 128, wsets[0][1])
                        WH = load_win(ph2, "WH", l, 1024 + cc * 128, wsets[0][2])
                        for (t0, n, sidx, gi) in cfg.groups:
                            pB, pC, pH = pb[0 + 4 * (gi % 2)], pb[1 + 4 * (gi % 2)], pb[2 + 4 * (gi % 2)]
                            proj(WB, pB, t0, n, gi)
                            proj(WC, pC, t0, n, gi)
                            proj(WH, pH, t0, n, gi)
                            h_ = hv[gi % 2]
                            act(h_[:, :n], pH[:, :n], AF.Copy, [pH], [h_])
                            act(Bs[:, t0:t0 + n], pB[:, :n], AF.Copy, [pB], [Bs])
                            tt('dve', u[:, t0:t0 + n], pC[:, :n], h_[:, :n], ALU.mult, [pC, h_], [u])
                        w0, w1, w2 = V(voff + 64 + 0 * 4 + cc), V(voff + 64 + 1 * 4 + cc), V(voff + 64 + 2 * 4 + cc)
                        act(y[:], u[:], AF.Identity, [u, vecs], [y], scale=w1)
                        stt(y[:, 1:TC], u[:, 0:TC - 1], w0, y[:, 1:TC], ALU.mult, ALU.add, [u, vecs, y], [y])
                        stt(y[:, 0:TC - 1], u[:, 1:TC], w2, y[:, 0:TC - 1], ALU.mult, ALU.add, [u, vecs, y], [y])
                        if cc < 2:
                            ul = u[:, TC:T].rearrange("p (r w) -> p r w", w=64)
                            yl = y[:, TC:T].rearrange("p (r w) -> p r w", w=64)
                            stt(yl[:, :, 1:64], ul[:, :, 0:63], w0, yl[:, :, 1:64], ALU.mult, ALU.add, [u, vecs, y], [y])
                            stt(yl[:, :, 0:63], ul[:, :, 1:64], w2, yl[:, :, 0:63], ALU.mult, ALU.add, [u, vecs, y], [y])
                        else:
                            stt(y[:, TC + 64:T], u[:, TC:T - 64], w0, y[:, TC + 64:T], ALU.mult, ALU.add, [u, vecs, y], [y])
                            stt(y[:, TC:T - 64], u[:, TC + 64:T], w2, y[:, TC:T - 64], ALU.mult, ALU.add, [u, vecs, y], [y])
                        tt('dve', y[:], y[:], Bs[:], ALU.mult, [y, Bs], [y])
                        act(Z[:, cc, :], y[:], AF.Copy, [y], [Z])
                        for (t0, n, sidx, gi) in cfg.groups:
                            z_ = zs[gi % 2]
                            pz = pb[3 + 4 * (gi % 2)]
                            act(z_[:, :n], y[:, t0:t0 + n], AF.Square, [y], [z_])
                            mm(pz[:, :n], ones[:], z_[:, :n], True, True, [ones, z_], [pz])
                            if cc == 0:
                                cp('dve', SS[:, t0:t0 + n], pz[:, :n], [pz], [SS])
                            else:
                                tt('dve', SS[:, t0:t0 + n], pz[:, :n], SS[:, t0:t0 + n], ALU.add, [pz, SS], [SS])
                act(SS[:], SS[:], AF.Sqrt, [SS], [SS], bias=cfg.EPS, scale=1.0 / 512)
                recip(SS[:], SS[:], [SS], [SS])
                for cc in range(4):
                    stt(Z[:, cc, :], Z[:, cc, :], V(voff + 76 + cc), SS[:], ALU.mult, ALU.mult, [Z, vecs, SS], [Z])
                out_proj(l, Z, 4, 0, ph)
            S.barrier()

        def heads_phase(l):
            voff = cfg.V_L0 + l * cfg.V_LN
            hnw = V(voff + 80)
            lat = cfg.groups[1:]
            order = [cfg.groups, [cfg.groups[0]] + lat[::-1]]
            with contextlib.ExitStack() as ph:
                hW = [sb(ph, "hW%d" % k_, [128, DC, 128], BF16) for k_ in range(5)]
                WoH = sb(ph, "WoH", [128, 1, 1024], BF16)
                Of = sb(ph, "Of", [128, T])
                Yh = sb(ph, "Yh", [128, T], BF16)
                NS = 4
                St = [sb(ph, "St%d" % i, [128, 128]) for i in range(NS)]
                names = ["qs", "sg", "kk", "vs", "b", "eb", "enb", "ko", "gs"]
                tmps = [{nm: sb(ph, "g%s%d" % (nm, i), [128, 512]) for nm in names} for i in range(2)]
                dch = [sb(ph, "dch%d" % i, [128, 16]) for i in range(2)]
                totc = [sb(ph, "totc%d" % i, [128, 16]) for i in range(2)]
                koT = [sb(ph, "koT%d" % i, [CH, NCHT, 128]) for i in range(2)]
                vT = [sb(ph, "vT%d" % i, [CH, NCHT, 128]) for i in range(2)]
                PT = [sb(ph, "PT%d" % i, [CH, NCHT, CH]) for i in range(2)]
                midc = [sb(ph, "midc%d" % i, [128, 16]) for i in range(2)]
                emid = [sb(ph, "emid%d" % i, [128, 16]) for i in range(2)]
                etm = [sb(ph, "etm%d" % i, [128, 16]) for i in range(2)]
                osq = sb(ph, "osq", [128, 512])
                ors = sb(ph, "ors", [128, 512])
                for hh in range(4):
                    Wq = load_win(ph, "Wq", l, 1536 + hh * 128, hW[0])
                    Wf = [load_win(ph, "Wzf", l, 2048 + hh * 128, hW[1]), load_win(ph, "Wzb", l, 2560 + hh * 128, hW[2])]
                    Wi = load_win(ph, "Wi", l, 3072 + hh * 128, hW[3])
                    Wg = load_win(ph, "Wg", l, 3584 + hh * 128, hW[4])
                    gpar = 0
                    tpar = 0
                    for dirn in range(2):
                        lbc = (dirn * DEPTH + l) * 4 + hh
                        lb_ap, oml_ap = lbT[:, lbc:lbc + 1], omlT[:, lbc:lbc + 1]
                        cur = 0
                        memset('dve', St[0][:], 0.0, [St[0]])
                        msk = maskF if dirn == 0 else maskB
                        for (t0, n, sidx, gi) in order[dirn]:
                            tp = tmps[gpar % 2]
                            dc_ = dch[gpar % 2]
                            gpar += 1
                            nch = n // CH
                            pq, pz_, pi_, pg = pb[0], pb[1], pb[2], pb[3]
                            proj(Wq, pq, t0, n, gi)
                            proj(Wf[dirn], pz_, t0, n, gi)
                            proj(Wi, pi_, t0, n, gi)
                            act(tp["qs"][:, :n], pq[:, :n], AF.Silu, [pq], [tp["qs"]])
                            act(tp["sg"][:, :n], pz_[:, :n], AF.Sigmoid, [pz_], [tp["sg"]])
                            act(tp["vs"][:, :n], pi_[:, :n], AF.Copy, [pi_], [tp["vs"]])
                            if dirn == 1:
                                proj(Wg, pg, t0, n, gi)
                                act(tp["gs"][:, :n], pg[:, :n], AF.Silu, [pg], [tp["gs"]])
                            tc_ = totc[(gpar - 1) % 2]
                            tsc('dve', tp["sg"][:, :n], tp["sg"][:, :n], oml_ap, lb_ap, ALU.mult, ALU.add, [tp["sg"], omlT, lbT], [tp["sg"]])
                            tsc('dve', tp["kk"][:, :n], tp["sg"][:, :n], -1.0, 1.0, ALU.mult, ALU.add, [tp["sg"]], [tp["kk"]])
                            act(tp["sg"][:, :n], tp["sg"][:, :n], AF.Ln, [tp["sg"]], [tp["sg"]])
                            S.op('dve', lambda e, o=tp["b"], m=rmask, d1=tp["sg"], n=n: e.tensor_tensor_scan(
                                out=o[:, :n], data0=m[:, :n], data1=d1[:, :n], initial=0.0, op0=ALU.mult, op1=ALU.add),
                                r=[rmask, tp["sg"]], w=[tp["b"]])
                            b3 = tp["b"][:, :n].rearrange("p (a c) -> p a c", c=CH)
                            cp('dve', tc_[:, :nch], b3[:, :, CH - 1], [tp["b"]], [tc_])
                            act(dc_[:, :nch], tc_[:, :nch], AF.Exp, [tc_], [dc_])
                            bb = tp["b"]
                            if dirn == 1:
                                tt('dve', b3, b3, tc_[:, :nch].unsqueeze(2).to_broadcast([128, nch, CH]), ALU.subtract, [tp["b"], tc_], [tp["b"]])
                                tt('dve', bb[:, :n], tp["sg"][:, :n], bb[:, :n], ALU.subtract, [tp["sg"], bb], [bb])
                            md_, em_, et_ = midc[(gpar - 1) % 2], emid[(gpar - 1) % 2], etm[(gpar - 1) % 2]
                            midcol = CH // 2 - 1 if dirn == 0 else CH // 2
                            cp('dve', md_[:, :nch], b3[:, :, midcol], [tp["b"]], [md_])
                            tt('dve', b3, b3, md_[:, :nch].unsqueeze(2).to_broadcast([128, nch, CH]), ALU.subtract, [tp["b"], md_], [tp["b"]])
                            act(tp["eb"][:, :n], bb[:, :n], AF.Exp, [bb], [tp["eb"]])
                            act(tp["enb"][:, :n], bb[:, :n], AF.Exp, [bb], [tp["enb"]], scale=-1.0)
                            act(em_[:, :nch], md_[:, :nch], AF.Exp, [md_], [em_])
                            tt('dve', et_[:, :nch], tc_[:, :nch], md_[:, :nch], ALU.subtract, [tc_, md_], [et_])
                            act(et_[:, :nch], et_[:, :nch], AF.Exp, [et_], [et_])
                            tt('dve', tp["qs"][:, :n], tp["qs"][:, :n], tp["eb"][:, :n], ALU.mult, [tp["qs"], tp["eb"]], [tp["qs"]])
                            tt('dve', tp["kk"][:, :n], tp["kk"][:, :n], tp["enb"][:, :n], ALU.mult, [tp["kk"], tp["enb"]], [tp["kk"]])
                            tt('dve', tp["eb"][:, :n].rearrange("p (a c) -> p a c", c=CH),
                               tp["qs"][:, :n].rearrange("p (a c) -> p a c", c=CH),
                               em_[:, :nch].unsqueeze(2).to_broadcast([128, nch, CH]), ALU.mult, [tp["qs"], em_], [tp["eb"]])
                            tt('dve', tp["ko"][:, :n].rearrange("p (a c) -> p a c", c=CH),
                               tp["kk"][:, :n].rearrange("p (a c) -> p a c", c=CH),
                               et_[:, :nch].unsqueeze(2).to_broadcast([128, nch, CH]), ALU.mult, [tp["kk"], et_], [tp["ko"]])
                            tiles = list(range(n // 128))
                            chunks = list(range(NCHT))
                            if dirn == 1:
                                tiles = tiles[::-1]
                                chunks = chunks[::-1]
                            for ti in tiles:
                                c0 = ti * 128
                                kT_, vT_, PT_ = koT[tpar % 2], vT[tpar % 2], PT[tpar % 2]
                                tpar += 1
                                pk, pv = pb[4], pb[5]
                                pk3 = pk[0:CH, 0:NCHT * 128].rearrange("p (j k) -> p j k", j=NCHT)
                                pv3 = pv[0:CH, 0:NCHT * 128].rearrange("p (j k) -> p j k", j=NCHT)
                                for j in range(NCHT):
                                    tr(pk3[:, j, :], tp["ko"][:, c0 + CH * j:c0 + CH * j + CH], [tp["ko"]], [pk])
                                for j in range(NCHT):
                                    tr(pv3[:, j, :], tp["vs"][:, c0 + CH * j:c0 + CH * j + CH], [tp["vs"]], [pv])
                                act(kT_[:], pk3, AF.Copy, [pk], [kT_])
                                cp('dve', vT_[:], pv3, [pv], [vT_])
                                par = tpar % 2
                                psc = pb[3][0:CH, 256:256 + NCHT * CH].rearrange("p (j k) -> p j k", j=NCHT)
                                for j in range(NCHT):
                                    cs = c0 + CH * j
                                    mm(psc[:, j, :], tp["kk"][:, cs:cs + CH], tp["qs"][:, cs:cs + CH], True, True,
                                       [tp["kk"], tp["qs"]], [pb[3]])
                                tsc('dve', PT_[:], psc, -1e30, 1e30, ALU.max, ALU.min, [pb[3]], [PT_])
                                tt('dve', PT_[:], PT_[:], msk[:], ALU.mult, [PT_, msk], [PT_])
                                po = pb[7][:, 0:128]
                                for j in chunks:
                                    cs = c0 + CH * j
                                    jg = cs // CH
                                    mm(pb[6][:, 128 * j:128 * j + 128], kT_[:, j, :], vT_[:, j, :], True, True, [kT_, vT_], [pb[6]])
                                    mm(po[:, CH * j:CH * j + CH], St[cur][:], tp["eb"][:, cs:cs + CH], True, False, [St[cur], tp["eb"]], [pb[7]])
                                    mm(po[:, CH * j:CH * j + CH], vT_[:, j, :], PT_[:, j, :], False, True, [vT_, PT_], [pb[7]])
                                    nxt = (cur + 1) % NS
                                    stt(St[nxt][:], St[cur][:], dc_[:, jg:jg + 1], pb[6][:, 128 * j:128 * j + 128], ALU.mult, ALU.add,
                                        [St[cur], dc_, pb[6]], [St[nxt]])
                                    cur = nxt
                                if dirn == 0:
                                    act(Of[:, t0 + c0:t0 + c0 + 128], po[:, 0:128], AF.Copy, [pb[7]], [Of])
                                else:
                                    tt('dve', Of[:, t0 + c0:t0 + c0 + 128], po[:, 0:128], Of[:, t0 + c0:t0 + c0 + 128], ALU.add, [pb[7], Of], [Of])
                            if dirn == 1:
                                pn = pb[3]
                                act(osq[:, :n], Of[:, t0:t0 + n], AF.Square, [Of], [osq])
                                mm(pn[:, :n], ones[:], osq[:, :n], True, True, [ones, osq], [pn])
                                act(ors[:, :n], pn[:, :n], AF.Sqrt, [pn], [ors], bias=cfg.EPS, scale=1.0 / 128)
                                recip(ors[:, :n], ors[:, :n], [ors], [ors])
                                stt(osq[:, :n], Of[:, t0:t0 + n], hnw, ors[:, :n], ALU.mult, ALU.mult, [Of, vecs, ors], [osq])
                                tt('dve', Yh[:, t0:t0 + n], osq[:, :n], tp["gs"][:, :n], ALU.mult, [osq, tp["gs"]], [Yh])
                    out_proj(l, Yh, 1, 4 + hh, ph, WoH, [pb[0], pb[1], pb[2], pb[3]])
            S.barrier()

        def moe_phase_dense(l):
            with contextlib.ExitStack() as ph:
                WA = [sb(ph, "WA%d" % i, [128, DC, 512], BF16) for i in range(2)]
                WU = [sb(ph, "WU%d" % i, [128, DC, 512], BF16) for i in range(2)]
                WD = [sb(ph, "WD%d" % i, [128, 4, 1024], BF16) for i in range(2)]
                gbc = [sb(ph, "gbc%d" % i, [128, 512], BF16) for i in range(2)]
                sa = [sb(ph, "sa%d" % i, [128, 512]) for i in range(2)]
                t1 = [sb(ph, "t1%d" % i, [128, 512], BF16) for i in range(2)]
                hm = [sb(ph, "hm%d" % i, [128, 4, 512], BF16) for i in range(2)]

                def load(e):
                    s = e % 2
                    dma('pool', WA[s][:], wgu_d[l, e, :, 0:512].rearrange("(c p) n -> p c n", p=128), [], [WA[s]])
                    dma('pool', WU[s][:], wgu_d[l, e, :, 512:1024].rearrange("(c p) n -> p c n", p=128), [], [WU[s]])
                    dma('pool', WD[s][:], wdn_d[l, e, :, :].rearrange("(c p) n -> p c n", p=128), [], [WD[s]])

                load(0)
                kk = 0
                k2 = 0
                for e in range(cfg.NE):
                    if e + 1 < cfg.NE:
                        load(e + 1)
                    s = e % 2
                    for (t0, n, sidx, gi) in cfg.groups:
                        pg = pb[0]
                        for ti in range(n // 128):
                            tg = (t0 + ti * 128) // 128
                            mm(pg[:, ti * 128:(ti + 1) * 128], G[:, tg, e:e + 1].to_broadcast([128, 128]), ident[:], True, True,
                               [('G', gi), ident], [pg])
                        gb = gbc[kk % 2]
                        hm_ = hm[kk % 2]
                        kk += 1
                        act(gb[:, :n], pg[:, :n], AF.Copy, [pg], [gb])
                        for hc in range(4):
                            pa, pu = pb[1 + 2 * (k2 % 2)], pb[2 + 2 * (k2 % 2)]
                            sa_, t1_ = sa[k2 % 2], t1[k2 % 2]
                            k2 += 1
                            for c in range(DC):
                                mm(pa[:, :n], WA[s][:, c, hc * 128:(hc + 1) * 128], hT[:, c, t0:t0 + n], c == 0, c == DC - 1,
                                   [WA[s], HK(gi)], [pa])
                            for c in range(DC):
                                mm(pu[:, :n], WU[s][:, c, hc * 128:(hc + 1) * 128], hT[:, c, t0:t0 + n], c == 0, c == DC - 1,
                                   [WU[s], HK(gi)], [pu])
                            act(sa_[:, :n], pa[:, :n], AF.Silu, [pa], [sa_])
                            tt('dve', t1_[:, :n], sa_[:, :n], pu[:, :n], ALU.mult, [sa_, pu], [t1_])
                            tt('pool', hm_[:, hc, :n], t1_[:, :n], gb[:, :n], ALU.mult, [t1_, gb], [hm_])
                        for j in range(DC):
                            py = pb[5 + j % 3]
                            for hc in range(4):
                                mm(py[:, :n], WD[s][:, hc, j * 128:(j + 1) * 128], hm_[:, hc, :n], hc == 0, hc == 3, [WD[s], hm_], [py])
                            stt(XT[:, j, t0:t0 + n], py[:, :n], modT[:, l, 40 + j, sidx:sidx + 1], XT[:, j, t0:t0 + n],
                                ALU.mult, ALU.add, [py, modT, XK(gi)], [XK(gi)])
            S.barrier()

        def moe_phase(l):
            IOA = bass.IndirectOffsetOnAxis
            with contextlib.ExitStack() as ph:
                WAU = [sb(ph, "WAU%d" % i, [128, DC, 1024], BF16) for i in range(2)]
                WD = [sb(ph, "WD%d" % i, [128, 4, 1024], BF16) for i in range(2)]
                xb = [sb(ph, "xb%d" % i, [128, 1024]) for i in range(2)]
                yb = [sb(ph, "yb%d" % i, [128, 1024]) for i in range(2)]
                xbT2 = [sb(ph, "xbT2%d" % i, [128, DC, 256], BF16) for i in range(2)]
                h32 = yb[0][:, :].rearrange("p (c t) -> p c t", c=DC)
                sa2 = sb(ph, "sa2", [128, 2, 512], BF16)
                hm2 = [sb(ph, "hm2%d" % i, [128, 4, 256], BF16) for i in range(2)]
                pT = [pb[0], pb[1]]
                order = []
                for i_ in range(NB):
                    order.append(i_ // 2 if i_ % 2 == 0 else NB - 1 - i_ // 2)

                order_box.clear()
                order_box.append(order)


                wgu2 = wgu_d.rearrange("l e (p c) n -> (l e p) (c n)", c=8)
                wdn2 = wdn_d.rearrange("l e (p c) n -> (l e p) (c n)", c=4)

                def bcreg(e):
                    if 'r' not in bc_cache:
                        bc_cache['r'] = e.to_reg(DEPTH * 32 * 128 - 1)
                    return bc_cache['r']

                def load(bpos):
                    s_ = bpos % 2
                    j = order_of(bpos)
                    for q in range(4):
                        S.op('pool', lambda e, j=j, q=q, s_=s_: e.indirect_dma_start(
                            out=WAU[s_][:, 2 * q:2 * q + 2, :].rearrange("p a n -> p (a n)"), out_offset=None, in_=wgu2,
                            in_offset=IOA(ap=IDXW[:, j:j + 1], axis=0), element_offset=q * 2048, bounds_check=bcreg(e), oob_is_err=False),
                            r=[IDXW], w=[('WAU', s_, q)], dma=True)
                    for q in range(2):
                        S.op('pool', lambda e, j=j, q=q, s_=s_: e.indirect_dma_start(
                            out=WD[s_][:, 2 * q:2 * q + 2, :].rearrange("p a n -> p (a n)"), out_offset=None, in_=wdn2,
                            in_offset=IOA(ap=IDXW[:, j:j + 1], axis=0), element_offset=q * 2048, bounds_check=bcreg(e), oob_is_err=False),
                            r=[IDXW], w=[('WD', s_, q)], dma=True)

                load(0)
                scat = []
                for tg in range(cfg.NT):
                    gi, sidx = tile_gi[tg]
                    act(h32, hT[:, :, tg * 128:(tg + 1) * 128], AF.Copy, [HK(gi)], [yb[0]])
                    for c in range(DC):
                        tr(pT[c // 4][:, (c % 4) * 128:(c % 4 + 1) * 128], h32[:, c, :], [yb[0]], [pT[c // 4]])
                    x_ = xb[tg % 2]
                    act(x_[:, 0:512], pT[0][:, :], AF.Copy, [pT[0]], [x_])
                    cp('dve', x_[:, 512:1024], pT[1][:, :], [pT[1]], [x_])
                    for k in range(2):
                        scat.append(S.op('pool', lambda e, x_=x_, tg=tg, k=k: e.indirect_dma_start(
                            out=xs_d[:, :], out_offset=IOA(ap=IDX[:, tg, k:k + 1], axis=0), in_=x_[:, :], in_offset=None),
                            r=[x_, ('IDX', tg)], w=[('xs', tg, k)], dma=True))
                stores = []
                RT = cfg.BLK // 128
                def rows(pos):
                    return order[pos // RT] * RT + pos % RT

                def prefetch(rt):
                    s_ = rt % 2
                    ra = rows(rt)
                    S.op('sp', lambda e: e.dma_start(out=xb[s_][:], in_=xs_d[ra * 128:(ra + 1) * 128, :]), r=[], w=[xb[s_]],
                         dma=True, extra_deps=scat)

                PA, PU = [pb[2], pb[3]], [pb[4], pb[5]]

                def stage_a(bpos):
                    w_ = bpos % 2
                    for r_ in range(RT):
                        pos = bpos * RT + r_
                        s_ = pos % 2
                        for c in range(DC):
                            tr(pT[c // 4][:, (c % 4) * 128:(c % 4 + 1) * 128], xb[s_][:, :].rearrange("r (p c) -> r c p", c=8)[:, c, :], [xb[s_]], [pT[c // 4]])
                        act(xbT2[w_][:, 0:4, r_ * 128:(r_ + 1) * 128], pT[0][:, :].rearrange("p (c r) -> p c r", c=4), AF.Copy, [pT[0]], [xbT2[w_]])
                        cp('dve', xbT2[w_][:, 4:8, r_ * 128:(r_ + 1) * 128], pT[1][:, :].rearrange("p (c r) -> p c r", c=4), [pT[1]], [xbT2[w_]])
                        if pos + 2 < NB * RT:
                            prefetch(pos + 2)
                    NW = RT * 128
                    for hc in range(4):
                        for c in range(DC):
                            mm(PA[hc // 2][:, (hc % 2) * NW:(hc % 2 + 1) * NW], WAU[w_][:, c, 0:512].rearrange("p (m h) -> p h m", h=4)[:, hc, :],
                               xbT2[w_][:, c, :], c == 0, c == DC - 1, [('WAU', w_, c // 2), xbT2[w_]], [PA[hc // 2]])
                    for hc in range(4):
                        for c in range(DC):
                            mm(PU[hc // 2][:, (hc % 2) * NW:(hc % 2 + 1) * NW], WAU[w_][:, c, 512:1024].rearrange("p (m h) -> p h m", h=4)[:, hc, :],
                               xbT2[w_][:, c, :], c == 0, c == DC - 1, [('WAU', w_, c // 2), xbT2[w_]], [PU[hc // 2]])
                    for h2 in range(2):
                        act(sa2[:, h2, :], PA[h2][:, 0:2 * NW], AF.Silu, [PA[h2]], [('sa2', h2)])
                        tt('dve', hm2[w_][:, 2 * h2:2 * h2 + 2, :].rearrange("p c r -> p (c r)"), sa2[:, h2, :], PU[h2][:, 0:2 * NW], ALU.mult,
                           [('sa2', h2), PU[h2]], [('hm2', w_, h2)])

                def stage_b(bpos):
                    w_ = bpos % 2
                    py = [pb[6], pb[7]]
                    for r_ in range(RT):
                        pos = bpos * RT + r_
                        s_ = pos % 2
                        for half in range(2):
                            for hc in range(4):
                                mm(py[half][:, :], hm2[w_][:, hc, r_ * 128:(r_ + 1) * 128], WD[w_][:, hc, half * 512:(half + 1) * 512], hc == 0, hc == 3,
                                   [('hm2', w_, hc // 2), ('WD', w_, hc // 2)], [py[half]])
                        act(yb[s_][:, 0:512], py[0][:, :], AF.Copy, [py[0]], [yb[s_]])
                        cp('dve', yb[s_][:, 512:1024], py[1][:, :], [py[1]], [yb[s_]])
                        ra = rows(pos)
                        stores.append(S.op('act', lambda e, ra=ra, s_=s_: e.dma_start(out=ys_d[ra * 128:(ra + 1) * 128, :], in_=yb[s_][:]),
                                           r=[yb[s_]], w=[('yd', pos)], dma=True))

                prefetch(0)
                prefetch(1)
                for bpos in range(NB):
                    stage_a(bpos)
                    if bpos > 0:
                        stage_b(bpos - 1)
                    if bpos + 1 < NB:
                        load(bpos + 1)
                stage_b(NB - 1)
                for tg in range(cfg.NT):
                    gi, sidx = tile_gi[tg]
                    yh_, yl_ = xb[tg % 2], yb[tg % 2]
                    S.op('pool', lambda e, yh_=yh_, tg=tg: e.indirect_dma_start(
                        out=yh_[:, :], out_offset=None, in_=ys_d[:, :], in_offset=IOA(ap=IDX[:, tg, 0:1], axis=0)),
                        r=[('IDX', tg)], w=[yh_], dma=True, extra_deps=stores)
                    S.op('pool', lambda e, yl_=yl_, tg=tg: e.indirect_dma_start(
                        out=yl_[:, :], out_offset=None, in_=ys_d[:, :], in_offset=IOA(ap=IDX[:, tg, 1:2], axis=0)),
                        r=[('IDX', tg)], w=[yl_], dma=True, extra_deps=stores)
                    tsc('dve', yh_[:], yh_[:], GHL[:, tg, 0:1], None, ALU.mult, None, [yh_, ('GHL', tg)], [yh_])
                    stt(yh_[:], yl_[:], GHL[:, tg, 1:2], yh_[:], ALU.mult, ALU.add, [yl_, ('GHL', tg), yh_], [yh_])
                    for c in range(DC):
                        tr(pT[c // 4][:, (c % 4) * 128:(c % 4 + 1) * 128], yh_[:, c * 128:(c + 1) * 128], [yh_], [pT[c // 4]])
                    for c in range(DC):
                        stt(XT[:, c, tg * 128:(tg + 1) * 128], pT[c // 4][:, (c % 4) * 128:(c % 4 + 1) * 128], modT[:, l, 40 + c, sidx:sidx + 1],
                            XT[:, c, tg * 128:(tg + 1) * 128], ALU.mult, ALU.add, [pT[c // 4], modT, XK(gi)], [XK(gi)])
            S.barrier()

        for l in cfg.layers:
            norm_modulate(l, 0, False)
            if 'conv' not in cfg.skip:
                conv_phase(l)
            if 'heads' not in cfg.skip:
                heads_phase(l)
            norm_modulate(l, 1, True)
            if 'moe' not in cfg.skip:
                moe_phase(l)

        fin = []
        with contextlib.ExitStack() as ph:
            sq = [sb(ph, "fsq%d" % i, [128, 512]) for i in range(2)]
            rs = [sb(ph, "frs%d" % i, [128, 512]) for i in range(2)]
            ob = [sb(ph, "fob%d" % i, [128, 512]) for i in range(4)]
            kk = 0
            for (t0, n, sidx, gi) in cfg.groups[1:]:
                pss = pb[gi % 2]
                for c in range(DC):
                    q = sq[kk % 2]
                    kk += 1
                    act(q[:, :n], XT[:, c, t0:t0 + n], AF.Square, [XK(gi)], [q])
                    mm(pss[:, :n], ones[:], q[:, :n], c == 0, c == DC - 1, [ones, q], [pss])
                r_ = rs[gi % 2]
                act(r_[:, :n], pss[:, :n], AF.Sqrt, [pss], [r_], bias=cfg.EPS, scale=1.0 / 1024)
                recip(r_[:, :n], r_[:, :n], [r_], [r_])
                for c in range(DC):
                    o_ = ob[kk % 4]
                    kk += 1
                    if cfg.final:
                        stt(o_[:, :n], XT[:, c, t0:t0 + n], V(cfg.V_FNW + c), r_[:, :n], ALU.mult, ALU.mult, [XK(gi), vecs, r_], [o_])
                    else:
                        cp('dve', o_[:, :n], XT[:, c, t0:t0 + n], [XK(gi)], [o_])
                    fin.append(dma('sp', outT_v[:, c, t0 - TC:t0 - TC + n], o_[:, :n], [o_], []))
        S.op('sp', lambda e: e.nop(), extra_deps=fin)
        S.run(nc)
        nc._sched_stats = S.stats
    return nc


def pack_inputs(cfg, b, x, c, ctx, c_ctx, norm_w, w_ada, b_ada, w_in, conv_w, conv_norm_w, hg_lb, hg_norm_w, w_out,
                w_rg, b_rg, w_re, b_re, w_e_gu, w_e_down, final_norm_w):
    d = cfg.DEPTH
    tok = np.concatenate([ctx[b], x[b]], axis=0)
    xT = np.ascontiguousarray(tok.T.reshape(cfg.DC, 128, cfg.T).transpose(1, 0, 2)).reshape(128, cfg.DC * cfg.T)

    def fm(v):
        return np.asarray(v).reshape(-1, 128).T

    vecs = np.zeros((128, cfg.NV), np.float32)
    vecs[:, cfg.V_C:cfg.V_C + 8] = fm(c[b])
    vecs[:, cfg.V_CC:cfg.V_CC + 8] = fm(c_ctx)
    vecs[:, cfg.V_FNW:cfg.V_FNW + 8] = fm(final_norm_w)
    for dirn in range(2):
        for l in range(d):
            o = cfg.V_LB + (dirn * d + l) * 4
            vecs[:, o:o + 4] = fm(hg_lb[dirn, l])
    for l in range(d):
        o = cfg.V_L0 + l * cfg.V_LN
        vecs[:, o:o + 8] = fm(norm_w[l, 0])
        vecs[:, o + 8:o + 16] = fm(norm_w[l, 1])
        vecs[:, o + 16:o + 64] = fm(b_ada[l])
        for tap in range(3):
            vecs[:, o + 64 + tap * 4:o + 64 + tap * 4 + 4] = fm(conv_w[l, tap])
        vecs[:, o + 76:o + 80] = fm(conv_norm_w[l])
        vecs[:, o + 80:o + 81] = fm(hg_norm_w[l])
    wr = np.concatenate([w_rg, w_re], axis=2)
    wr = np.ascontiguousarray(wr.reshape(d, cfg.DC, 128, 36).transpose(2, 0, 1, 3)).reshape(128, d * cfg.DC * 36)
    br = np.ascontiguousarray(np.concatenate([b_rg, b_re], axis=1)).reshape(1, d * 36)
    return {"xT": xT.astype(np.float32), "vecs": vecs, "wr": wr.astype(np.float32), "br": br.astype(np.float32)}


def run(cfg, inputs, n_cores):
    inputs = {k: np.asarray(v) for k, v in inputs.items()}
    nc = build_program(cfg)
    shared = {"w_ada": np.ascontiguousarray(inputs["w_ada"]), "w_in": np.ascontiguousarray(inputs["w_in"]),
              "w_out": np.ascontiguousarray(inputs["w_out"]), "w_gu": np.ascontiguousarray(inputs["w_e_gu"]),
              "w_dn": np.ascontiguousarray(inputs["w_e_down"])}
    in_maps = []
    for b in range(n_cores):
        m = pack_inputs(cfg, b, **inputs)
        m.update(shared)
        in_maps.append(m)
    res = run_bass_kernel_spmd(nc, in_maps, core_ids=list(range(n_cores)))
    outs = []
    for b in range(n_cores):
        oT = np.asarray(res.results[b]["outT"]).reshape(128, cfg.DC, cfg.TL)
        outs.append(oT.transpose(2, 1, 0).reshape(cfg.TL, cfg.D))
    return np.stack(outs, axis=0).astype(np.float32)


def kernel(**inputs):
    cfg = Cfg(depth=4, t_lat=2048)
    return run(cfg, inputs, 8)
```
